# Optimizing a Trainium2 kernel written in Bass

```python
import math
import jax, jax.numpy as jnp
from jax import lax
import numpy as np

D_MODEL = 1024
BATCH = 8
SEQ = 4096
DEPTH = 4

N_MEM = 256
N_MIXERS = 4
HEAD_DIM = 64
GROUP_WIDTH = D_MODEL // N_MIXERS
HEADS_PER_GROUP = GROUP_WIDTH // HEAD_DIM
ROPE_THETA = 500000.0
Q_BLOCK = 128
MAX_POS_OFFSET = 1024

MLA_Q_LORA = 192
MLA_KV_LORA = 128
MLA_NOPE = HEAD_DIM
MLA_ROPE = HEAD_DIM // 2
MLA_V = HEAD_DIM

MOBA_BLOCK = 256
MOBA_TOPK = 3
MOBA_Q_CHUNK = 32
MOBA_ROT = HEAD_DIM // 4

DIFF_HALF = HEAD_DIM // 2
DIFF_ROT = DIFF_HALF // 4

XATTN_HEADS = 4
XATTN_HEAD_DIM = D_MODEL // XATTN_HEADS

N_GROUPS = 8
EXPERTS_PER_GROUP = 4
N_EXPERTS = N_GROUPS * EXPERTS_PER_GROUP
TOP_K_IN_GROUP = 2
EXPERT_FF = D_MODEL // 4

DEEPNORM_ALPHA = (2 * DEPTH) ** 0.25
DEEPNORM_BETA = (8 * DEPTH) ** -0.25
LN_EPS = 1e-5
RMS_EPS = 1e-6

IN_SIZES = (MLA_Q_LORA, MLA_KV_LORA, MLA_ROPE, 3 * GROUP_WIDTH, 3 * GROUP_WIDTH, 3 * GROUP_WIDTH)
IN_COLS = sum(IN_SIZES)
IN_SPLIT_POINTS = [int(c) for c in np.cumsum(IN_SIZES)[:-1]]

kernel_name = 'hybrid_mla_sb_moba_diff_hmoe_deepnorm'


def layer_norm(x, g, b):
    xf = x.astype(jnp.float32)
    mu = jnp.mean(xf, axis=-1, keepdims=True)
    var = jnp.mean(jnp.square(xf - mu), axis=-1, keepdims=True)
    return ((xf - mu) * lax.rsqrt(var + LN_EPS) * g + b).astype(x.dtype)


def rms_norm(x, g):
    xf = x.astype(jnp.float32)
    return (xf * lax.rsqrt(jnp.mean(jnp.square(xf), axis=-1, keepdims=True) + RMS_EPS) * g).astype(x.dtype)


def to_heads(t, n_heads):
    b, s, _ = t.shape
    return t.reshape(b, s, n_heads, -1).transpose(0, 2, 1, 3)


def from_heads(o):
    b, h, s, d = o.shape
    return o.transpose(0, 2, 1, 3).reshape(b, s, h * d)


def rope_tables(positions, rot_dim):
    inv = ROPE_THETA ** (-jnp.arange(0, rot_dim, 2, dtype=jnp.float32) / rot_dim)
    ang = positions.astype(jnp.float32)[..., None] * inv
    return jnp.cos(ang), jnp.sin(ang)


def apply_rope(t, cos, sin):
    c = cos[:, None].astype(t.dtype)
    s = sin[:, None].astype(t.dtype)
    t1, t2 = jnp.split(t, 2, axis=-1)
    return jnp.concatenate([t1 * c - t2 * s, t1 * s + t2 * c], axis=-1)


def partial_rope(t, cos, sin):
    rot = 2 * cos.shape[-1]
    return jnp.concatenate([apply_rope(t[..., :rot], cos, sin), t[..., rot:]], axis=-1)


def query_blocks(t, qb):
    b, h, s, d = t.shape
    return t.reshape(b, h, s // qb, qb, d).transpose(2, 0, 1, 3, 4)


def merge_blocks(o):
    n, b, h, qb, d = o.shape
    return o.transpose(1, 2, 0, 3, 4).reshape(b, h, n * qb, d)


def causal_softmax_attention(q, k, v, scale):
    s = q.shape[2]
    key_pos = jnp.arange(s)

    def block(args):
        qb, i = args
        q_pos = i * Q_BLOCK + jnp.arange(Q_BLOCK)
        logits = jnp.einsum('bhqd,bhkd->bhqk', qb, k).astype(jnp.float32) * scale
        logits = jnp.where(key_pos[None, :] <= q_pos[:, None], logits, -jnp.inf)
        p = jax.nn.softmax(logits, axis=-1).astype(v.dtype)
        return jnp.einsum('bhqk,bhkd->bhqd', p, v)

    return merge_blocks(lax.map(block, (query_blocks(q, Q_BLOCK), jnp.arange(s // Q_BLOCK))))


def mla_attention(q_lat, kv_lat, k_rope, q_norm_g, w_uq, kv_norm_g, w_ukv, rope):
    cos, sin = rope
    q = to_heads(rms_norm(q_lat, q_norm_g) @ w_uq, HEADS_PER_GROUP)
    q = jnp.concatenate([q[..., :MLA_NOPE], apply_rope(q[..., MLA_NOPE:], cos, sin)], axis=-1)
    kv = to_heads(rms_norm(kv_lat, kv_norm_g) @ w_ukv, HEADS_PER_GROUP)
    k_nope, v = kv[..., :MLA_NOPE], kv[..., MLA_NOPE:]
    k_pe = apply_rope(k_rope[:, None], cos, sin)
    k = jnp.concatenate([k_nope, jnp.broadcast_to(k_pe, k_nope.shape[:3] + (MLA_ROPE,))], axis=-1)
    return causal_softmax_attention(q, k, v, (MLA_NOPE + MLA_ROPE) ** -0.5)


def stick_breaking_attention(q, k, v):
    s, d = q.shape[2], q.shape[3]
    key_pos = jnp.arange(s)

    def block(args):
        qb, i = args
        q_pos = i * Q_BLOCK + jnp.arange(Q_BLOCK)
        z = jnp.einsum('bhqd,bhkd->bhqk', qb, k).astype(jnp.float32) * (d ** -0.5)
        past = key_pos[None, :] < q_pos[:, None]
        log_one_minus = jnp.where(past, jax.nn.log_sigmoid(-z), 0.0)
        after = lax.cumsum(log_one_minus, axis=log_one_minus.ndim - 1, reverse=True) - log_one_minus
        a = jnp.where(past, jnp.exp(jax.nn.log_sigmoid(z) + after), 0.0).astype(v.dtype)
        return jnp.einsum('bhqk,bhkd->bhqd', a, v)

    return merge_blocks(lax.map(block, (query_blocks(q, Q_BLOCK), jnp.arange(s // Q_BLOCK))))


def moba_attention(q, k, v):
    b, h, s, d = q.shape
    nb = -(-s // MOBA_BLOCK)
    pad = nb * MOBA_BLOCK - s
    k_blk = jnp.pad(k, ((0, 0), (0, 0), (0, pad), (0, 0))).reshape(b, h, nb, MOBA_BLOCK, d)
    v_blk = jnp.pad(v, ((0, 0), (0, 0), (0, pad), (0, 0))).reshape(b, h, nb, MOBA_BLOCK, d)
    k_mean = jnp.mean(k_blk.astype(jnp.float32), axis=3).astype(k.dtype)
    n_sel = max(1, min(MOBA_TOPK, nb - 1))
    scale = d ** -0.5
    bi = jnp.arange(b)[:, None, None, None]
    hi = jnp.arange(h)[None, :, None, None]
    blk_ids = jnp.arange(nb)
    offs = jnp.arange(MOBA_BLOCK)

    def chunk(args):
        qc, i = args
        q_pos = i * MOBA_Q_CHUNK + jnp.arange(MOBA_Q_CHUNK)
        own = (i * MOBA_Q_CHUNK) // MOBA_BLOCK
        gate = jnp.einsum('bhqd,bhnd->bhqn', qc, k_mean).astype(jnp.float32)
        gate = jnp.where(blk_ids < own, gate, -jnp.inf)
        g_val, g_idx = lax.top_k(gate, n_sel)
        valid = g_val > -jnp.inf
        k_sel = k_blk[bi, hi, g_idx]
        v_sel = v_blk[bi, hi, g_idx]
        l_past = jnp.einsum('bhqd,bhqnkd->bhqnk', qc, k_sel).astype(jnp.float32) * scale
        l_past = jnp.where(valid[..., None], l_past, -jnp.inf).reshape(b, h, MOBA_Q_CHUNK, n_sel * MOBA_BLOCK)
        k_own = lax.dynamic_index_in_dim(k_blk, own, axis=2, keepdims=False)
        v_own = lax.dynamic_index_in_dim(v_blk, own, axis=2, keepdims=False)
        l_own = jnp.einsum('bhqd,bhkd->bhqk', qc, k_own).astype(jnp.float32) * scale
        l_own = jnp.where((own * MOBA_BLOCK + offs)[None, :] <= q_pos[:, None], l_own, -jnp.inf)
        p = jax.nn.softmax(jnp.concatenate([l_past, l_own], axis=-1), axis=-1).astype(v.dtype)
        p_past = p[..., :n_sel * MOBA_BLOCK].reshape(b, h, MOBA_Q_CHUNK, n_sel, MOBA_BLOCK)
        p_own = p[..., n_sel * MOBA_BLOCK:]
        return (jnp.einsum('bhqnk,bhqnkd->bhqd', p_past, v_sel)
                + jnp.einsum('bhqk,bhkd->bhqd', p_own, v_own))

    return merge_blocks(lax.map(chunk, (query_blocks(q, MOBA_Q_CHUNK), jnp.arange(s // MOBA_Q_CHUNK))))


def diff_attention(q, k, v, lam_params, subln_g, rope, lambda_init):
    cos, sin = rope
    s = q.shape[2]
    q1, q2 = partial_rope(q[..., :DIFF_HALF], cos, sin), partial_rope(q[..., DIFF_HALF:], cos, sin)
    k1, k2 = partial_rope(k[..., :DIFF_HALF], cos, sin), partial_rope(k[..., DIFF_HALF:], cos, sin)
    lp = lam_params.astype(jnp.float32)
    lam = jnp.exp(jnp.sum(lp[0] * lp[1])) - jnp.exp(jnp.sum(lp[2] * lp[3])) + lambda_init
    scale = DIFF_HALF ** -0.5
    key_pos = jnp.arange(s)

    def block(args):
        q1b, q2b, i = args
        q_pos = i * Q_BLOCK + jnp.arange(Q_BLOCK)
        mask = key_pos[None, :] <= q_pos[:, None]
        l1 = jnp.where(mask, jnp.einsum('bhqd,bhkd->bhqk', q1b, k1).astype(jnp.float32) * scale, -jnp.inf)
        l2 = jnp.where(mask, jnp.einsum('bhqd,bhkd->bhqk', q2b, k2).astype(jnp.float32) * scale, -jnp.inf)
        p = (jax.nn.softmax(l1, axis=-1) - lam * jax.nn.softmax(l2, axis=-1)).astype(v.dtype)
        return jnp.einsum('bhqk,bhkd->bhqd', p, v)

    o = merge_blocks(lax.map(block, (query_blocks(q1, Q_BLOCK), query_blocks(q2, Q_BLOCK), jnp.arange(s // Q_BLOCK))))
    return rms_norm(o, subln_g) * (1.0 - lambda_init)


def hybrid_mixer(x, w_in, mla_q_norm, w_uq, mla_kv_norm, w_ukv, diff_lambda, diff_subln, w_out,
                 rope_mla, rope_moba, rope_diff, lambda_init):
    h = x @ w_in
    q_lat, kv_lat, k_rope, sb_qkv, moba_qkv, diff_qkv = jnp.split(h, IN_SPLIT_POINTS, axis=-1)
    o_a = mla_attention(q_lat, kv_lat, k_rope, mla_q_norm, w_uq, mla_kv_norm, w_ukv, rope_mla)
    qb, kb, vb = [to_heads(t, HEADS_PER_GROUP) for t in jnp.split(sb_qkv, 3, axis=-1)]
    o_b = stick_breaking_attention(qb, kb, vb)
    qc, kc, vc = [to_heads(t, HEADS_PER_GROUP) for t in jnp.split(moba_qkv, 3, axis=-1)]
    o_c = moba_attention(partial_rope(qc, *rope_moba), partial_rope(kc, *rope_moba), vc)
    qd, kd, vd = [to_heads(t, HEADS_PER_GROUP) for t in jnp.split(diff_qkv, 3, axis=-1)]
    o_d = diff_attention(qd, kd, vd, diff_lambda, diff_subln, rope_diff, lambda_init)
    o = jnp.concatenate([from_heads(o_a), from_heads(o_b), from_heads(o_c), from_heads(o_d)], axis=-1)
    return o @ w_out


def memory_cross_attention(x, mem, wq, wk, wv, wo):
    q = to_heads(x @ wq, XATTN_HEADS)
    k = to_heads(mem @ wk, XATTN_HEADS)
    v = to_heads(mem @ wv, XATTN_HEADS)
    logits = jnp.einsum('bhqd,bhmd->bhqm', q, k).astype(jnp.float32) * (XATTN_HEAD_DIM ** -0.5)
    p = jax.nn.softmax(logits, axis=-1).astype(v.dtype)
    return from_heads(jnp.einsum('bhqm,bhmd->bhqd', p, v)) @ wo


def hierarchical_moe(x, w_group, b_group, w_expert, b_expert, w_gate, w_up, w_down):
    def one_sequence(xs):
        s = xs.shape[0]
        g_logits = (xs @ w_group + b_group).astype(jnp.float32)
        g_idx = jnp.argmax(g_logits, axis=-1)
        g_weight = jnp.take_along_axis(jax.nn.softmax(g_logits, axis=-1), g_idx[:, None], axis=-1)
        e_logits = (xs @ w_expert + b_expert).astype(jnp.float32).reshape(s, N_GROUPS, EXPERTS_PER_GROUP)
        e_logits = e_logits[jnp.arange(s), g_idx]
        top_val, top_idx = lax.top_k(e_logits, TOP_K_IN_GROUP)
        top_w = jax.nn.softmax(top_val, axis=-1) * g_weight
        expert_id = g_idx[:, None] * EXPERTS_PER_GROUP + top_idx
        gates = jnp.einsum('ske,sk->se', jax.nn.one_hot(expert_id, N_EXPERTS, dtype=jnp.float32), top_w).astype(xs.dtype)
        hid = jax.nn.silu(jnp.einsum('sd,edf->sef', xs, w_gate)) * jnp.einsum('sd,edf->sef', xs, w_up)
        return jnp.einsum('sef,efd->sd', hid * gates[:, :, None], w_down)

    return lax.map(one_sequence, x)


def setup_inputs(seed: int = 0) -> dict:
    key = jax.random.key(seed)
    ks = jax.random.split(key, 40)
    L, D = DEPTH, D_MODEL
    f32 = jnp.float32

    def normal(k, shape, std):
        return jax.random.normal(k, shape, f32) * std

    def gain(k, shape):
        return 1.0 + 0.02 * jax.random.normal(k, shape, f32)

    offset = jax.random.randint(ks[2], (BATCH, 1), 0, MAX_POS_OFFSET, dtype=jnp.int32)
    return {
        'x': normal(ks[0], (BATCH, SEQ, D), 1.0),
        'mem': normal(ks[1], (BATCH, N_MEM, D), 1.0),
        'positions': offset + jnp.arange(SEQ, dtype=jnp.int32)[None, :],
        'w_in': normal(ks[3], (L, D, IN_COLS), D ** -0.5),
        'mla_q_norm': gain(ks[4], (L, MLA_Q_LORA)),
        'w_uq': normal(ks[5], (L, MLA_Q_LORA, HEADS_PER_GROUP * (MLA_NOPE + MLA_ROPE)), MLA_Q_LORA ** -0.5),
        'mla_kv_norm': gain(ks[6], (L, MLA_KV_LORA)),
        'w_ukv': normal(ks[7], (L, MLA_KV_LORA, HEADS_PER_GROUP * (MLA_NOPE + MLA_V)), MLA_KV_LORA ** -0.5),
        'diff_lambda': normal(ks[8], (L, 4, DIFF_HALF), 0.1),
        'diff_subln': gain(ks[9], (L, HEAD_DIM)),
        'w_out': normal(ks[10], (L, D, D), DEEPNORM_BETA * D ** -0.5),
        'ln_mix_g': gain(ks[11], (L, D)),
        'ln_mix_b': normal(ks[12], (L, D), 0.02),
        'xattn_wq': normal(ks[13], (L, D, D), D ** -0.5),
        'xattn_wk': normal(ks[14], (L, D, D), D ** -0.5),
        'xattn_wv': normal(ks[15], (L, D, D), D ** -0.5),
        'xattn_wo': normal(ks[16], (L, D, D), DEEPNORM_BETA * D ** -0.5),
        'ln_mem_g': gain(ks[17], (L, D)),
        'ln_mem_b': normal(ks[18], (L, D), 0.02),
        'router_group_w': normal(ks[19], (L, D, N_GROUPS), D ** -0.5),
        'router_group_b': normal(ks[20], (L, N_GROUPS), 0.01),
        'router_expert_w': normal(ks[21], (L, D, N_EXPERTS), D ** -0.5),
        'router_expert_b': normal(ks[22], (L, N_EXPERTS), 0.01),
        'expert_w_gate': normal(ks[23], (L, N_EXPERTS, D, EXPERT_FF), D ** -0.5),
        'expert_w_up': normal(ks[24], (L, N_EXPERTS, D, EXPERT_FF), D ** -0.5),
        'expert_w_down': normal(ks[25], (L, N_EXPERTS, EXPERT_FF, D), DEEPNORM_BETA * EXPERT_FF ** -0.5),
        'ln_ffn_g': gain(ks[26], (L, D)),
        'ln_ffn_b': normal(ks[27], (L, D), 0.02),
    }


def reference(x, mem, positions, w_in, mla_q_norm, w_uq, mla_kv_norm, w_ukv, diff_lambda, diff_subln,
              w_out, ln_mix_g, ln_mix_b, xattn_wq, xattn_wk, xattn_wv, xattn_wo, ln_mem_g, ln_mem_b,
              router_group_w, router_group_b, router_expert_w, router_expert_b,
              expert_w_gate, expert_w_up, expert_w_down, ln_ffn_g, ln_ffn_b):
    rope_mla = rope_tables(positions, MLA_ROPE)
    rope_moba = rope_tables(positions, MOBA_ROT)
    rope_diff = rope_tables(positions, DIFF_ROT)
    for l in range(DEPTH):
        lambda_init = 0.8 - 0.6 * math.exp(-0.3 * l)
        mix = hybrid_mixer(x, w_in[l], mla_q_norm[l], w_uq[l], mla_kv_norm[l], w_ukv[l],
                           diff_lambda[l], diff_subln[l], w_out[l], rope_mla, rope_moba, rope_diff, lambda_init)
        x = layer_norm(DEEPNORM_ALPHA * x + mix, ln_mix_g[l], ln_mix_b[l])
        xa = memory_cross_attention(x, mem, xattn_wq[l], xattn_wk[l], xattn_wv[l], xattn_wo[l])
        x = layer_norm(DEEPNORM_ALPHA * x + xa, ln_mem_g[l], ln_mem_b[l])
        ff = hierarchical_moe(x, router_group_w[l], router_group_b[l], router_expert_w[l], router_expert_b[l],
                              expert_w_gate[l], expert_w_up[l], expert_w_down[l])
        x = layer_norm(DEEPNORM_ALPHA * x + ff, ln_ffn_g[l], ln_ffn_b[l])
    return x
```

```python
import math
import os
LV = int(os.environ.get('P1LV', '9'))
SUB = int(os.environ.get('P1SUB', '9'))
from contextlib import ExitStack

import numpy as np
import ml_dtypes
import concourse.bass as bass
import concourse.mybir as mybir
from concourse.bass_utils import run_bass_kernel_spmd

F32 = mybir.dt.float32
BF16 = mybir.dt.bfloat16
I32 = mybir.dt.int32
AF = mybir.ActivationFunctionType
ALU = mybir.AluOpType
AX = mybir.AxisListType

D = 1024
S = 4096
NT = S // 128
NB = S // 512
DEPTH = 4
N_MEM = 256
IN_COLS = 2656
ALPHA = (2 * DEPTH) ** 0.25
LN_EPS = 1e-5
RMS_EPS = 1e-6
ROPE_THETA = 500000.0
BIGV = 30000.0
TWO_PI = 2.0 * math.pi

O_SB = 352
O_MB = 352 + 768
O_DF = 352 + 1536
COL_PERM = np.concatenate([
    np.arange(0, 352),
    np.arange(O_SB, O_SB + 512),
    np.arange(O_SB + 512, O_SB + 768), np.arange(O_MB + 512, O_MB + 768),
    np.arange(O_MB, O_MB + 512),
    np.arange(O_DF, O_DF + 512),
    np.arange(O_DF + 512, O_DF + 768),
])
CH = [(0, 352), (352, 512), (864, 512), (1376, 512), (1888, 512), (2400, 256)]


class Ctx:
    def __init__(self, nc, stack, n_dma_sems=8):
        self.nc = nc
        self.eng = {"pe": nc.tensor, "act": nc.scalar, "dve": nc.vector, "pool": nc.gpsimd, "sp": nc.sync}
        self.sem = {}
        self.cnt = {}
        for e in self.eng:
            self.sem[e] = stack.enter_context(nc.semaphore("s_" + e))
            self.cnt[e] = 0
        self.dring = {}
        for q in ("sp", "pool"):
            sems = [stack.enter_context(nc.semaphore("d_%s_%d" % (q, i))) for i in range(n_dma_sems)]
            self.dring[q] = {"sems": sems, "cnt": [0] * n_dma_sems, "next": 0}
        self.waited = {e: {} for e in self.eng}
        self.last_w = {}
        self.readers = {}
        self.prog = {e: [] for e in self.eng}
        self.n_ins = 0

    def _wait(self, e, tok):
        sem, val, src = tok
        if src == e and e == "pe":
            return
        w = self.waited[e]
        if w.get(sem.name, 0) >= val:
            return
        w[sem.name] = val
        self.prog[e].append(("w", sem, val))

    def _deps(self, e, reads, writes):
        for k in reads:
            t = self.last_w.get(k)
            if t is not None:
                self._wait(e, t)
            if k.startswith("ps"):
                for t in self.readers.get(k, ()):
                    if t[2] != e:
                        self._wait(e, t)
        for k in writes:
            t = self.last_w.get(k)
            if t is not None:
                self._wait(e, t)
            for t in self.readers.get(k, ()):
                self._wait(e, t)

    def _commit(self, tok, reads, writes):
        for k in reads:
            lst = self.readers.setdefault(k, [])
            lst.append(tok)
            if len(lst) > 16:
                best = {}
                for t in lst:
                    if t[0].name not in best or best[t[0].name][1] < t[1]:
                        best[t[0].name] = t
                self.readers[k] = list(best.values())
        for k in writes:
            self.last_w[k] = tok
            self.readers[k] = []

    def op(self, e, fn, reads=(), writes=()):
        self._deps(e, reads, writes)
        self.cnt[e] += 1
        self.n_ins += 1
        self.prog[e].append(("i", fn, self.sem[e], 1))
        tok = (self.sem[e], self.cnt[e], e)
        self._commit(tok, reads, writes)
        return tok

    def dma(self, q, out, in_, reads=(), writes=(), **kw):
        ring = self.dring[q]
        i = ring["next"]
        ring["next"] = (i + 1) % len(ring["sems"])
        sem = ring["sems"][i]
        if ring["cnt"][i] > 0:
            self._wait(q, (sem, ring["cnt"][i], None))
        self._deps(q, reads, writes)
        self.prog[q].append(("d", out, in_, kw, sem))
        self.n_ins += 1
        ring["cnt"][i] += 16
        tok = (sem, ring["cnt"][i], None)
        self._commit(tok, reads, writes)
        return tok

    def wait_all(self, e):
        for x in self.eng:
            if self.cnt[x] > 0 and x != e:
                self._wait(e, (self.sem[x], self.cnt[x], x))
        for q, ring in self.dring.items():
            for sem, c in zip(ring["sems"], ring["cnt"]):
                if c > 0:
                    self._wait(e, (sem, c, None))

    def emit(self):
        nc = self.nc
        self.wait_all("sp")
        prog = self.prog
        self.prog = {e: [] for e in self.eng}

        def run(e, engobj):
            for it in prog[e]:
                if it[0] == "w":
                    engobj.wait_ge(it[1], it[2])
                elif it[0] == "i":
                    it[1]().then_inc(it[2], it[3])
                else:
                    engobj.dma_start(out=it[1], in_=it[2], **it[3]).then_inc(it[4], 16)

        with nc.Block() as block:
            @block.sync
            def _(eng):
                run("sp", eng)

            @block.scalar
            def _(eng):
                run("act", eng)

            @block.tensor
            def _(eng):
                run("pe", eng)

            @block.vector
            def _(eng):
                run("dve", eng)

            @block.gpsimd
            def _(eng):
                run("pool", eng)


class Builder:
    def __init__(self, n_layers=DEPTH, debug=False, stop_after=None):
        self.n_layers = n_layers
        self.debug = debug
        self.stop_after = stop_after
        self.nc = bass.Bass("TRN2", target_bir_lowering=False)
        self.dbg_names = []

    def mm(self, out, lhsT, rhs, start, stop, r, w):
        nc = self.nc
        self.c.op("pe", lambda: nc.tensor.matmul(out, lhsT=lhsT, rhs=rhs, start=start, stop=stop), r, w)

    def tr(self, out, in_, r, w, f32=False):
        nc = self.nc
        n = in_.shape[0]
        ident = (self.ident_f if f32 else self.ident_b)[0:n, 0:n]
        self.c.op("pe", lambda: nc.tensor.transpose(out, in_, ident), list(r) + ["const"], w)

    def act(self, out, in_, func, r, w, scale=1.0, bias=0.0, accum_out=None):
        nc = self.nc
        kw = {}
        if accum_out is not None:
            kw["accum_out"] = accum_out
        self.c.op("act", lambda: nc.scalar.activation(out=out, in_=in_, func=func, scale=scale, bias=bias, **kw), r, w)

    def tt(self, e, out, in0, in1, op, r, w):
        eng = self.c.eng[e]
        self.c.op(e, lambda: eng.tensor_tensor(out=out, in0=in0, in1=in1, op=op), r, w)

    def ts(self, e, out, in0, s1, op0, r, w, s2=None, op1=ALU.bypass, accum_out=None):
        eng = self.c.eng[e]
        kw = {}
        if accum_out is not None:
            kw["accum_out"] = accum_out
        self.c.op(e, lambda: eng.tensor_scalar(out=out, in0=in0, scalar1=s1, scalar2=s2, op0=op0, op1=op1, **kw), r, w)

    def stt(self, out, in0, scalar, in1, op0, op1, r, w):
        nc = self.nc
        self.c.op("dve", lambda: nc.vector.scalar_tensor_tensor(out=out, in0=in0, scalar=scalar, in1=in1, op0=op0, op1=op1), r, w)

    def cp(self, e, out, in_, r, w):
        if e == "act":
            nc = self.nc
            self.c.op("act", lambda: nc.scalar.copy(out=out, in_=in_), r, w)
        else:
            eng = self.c.eng[e]
            self.c.op(e, lambda: eng.tensor_copy(out=out, in_=in_), r, w)

    def ms(self, e, ap, val, w):
        eng = self.c.eng[e]
        self.c.op(e, lambda: eng.memset(ap, val), (), w)

    def recip(self, out, in_, r, w):
        nc = self.nc
        self.c.op("dve", lambda: nc.vector.reciprocal(out=out, in_=in_), r, w)

    def red(self, out, in_, op, r, w, axis=AX.X):
        nc = self.nc
        self.c.op("dve", lambda: nc.vector.tensor_reduce(out=out, in_=in_, axis=axis, op=op), r, w)

    def sb(self, name, shape, dt, st=None):
        self.uid = getattr(self, "uid", 0) + 1
        return (st or self.st).enter_context(self.nc.sbuf_tensor("%s_u%d" % (name, self.uid), list(shape), dt))

    def scr(self, name, shape, dt):
        kind = "ExternalOutput" if self.debug else "Internal"
        if self.debug:
            self.dbg_names.append(name)
        return self.nc.dram_tensor(name, list(shape), dt, kind=kind).ap()

    def build(self):
        nc = self.nc
        L = DEPTH
        inp = lambda n, s, dt=F32: nc.dram_tensor(n, list(s), dt, kind="ExternalInput").ap()
        self.x_in = inp("x", [S, D])
        self.mem_in = inp("mem", [N_MEM, D])
        self.pos_in = inp("pos", [128, NT], I32)
        self.w_in = inp("w_in", [L, D, IN_COLS])
        self.mla_q_norm = inp("mla_q_norm", [L, 192])
        self.w_uq = inp("w_uq", [L, 192, 384])
        self.mla_kv_norm = inp("mla_kv_norm", [L, 128])
        self.w_ukv = inp("w_ukv", [L, 128, 512])
        self.diff_lambda = inp("diff_lambda", [L, 128])
        self.diff_subln = inp("diff_subln", [L, 64])
        self.w_out = inp("w_out", [L, D, D])
        self.ln_g = [inp(n, [L, D]) for n in ("ln_mix_g", "ln_mem_g", "ln_ffn_g")]
        self.ln_b = [inp(n, [L, D]) for n in ("ln_mix_b", "ln_mem_b", "ln_ffn_b")]
        self.wq = inp("xattn_wq", [L, D, D])
        self.wk = inp("xattn_wk", [L, D, D])
        self.wv = inp("xattn_wv", [L, D, D])
        self.wo = inp("xattn_wo", [L, D, D])
        self.w_router = inp("w_router", [L, D, 40])
        self.b_router = inp("b_router", [L, 40])
        self.e_gate = inp("expert_w_gate", [L, 32, D, 256])
        self.e_up = inp("expert_w_up", [L, 32, D, 256])
        self.e_down = inp("expert_w_down", [L, 32, 256, D])
        self.c_ident_b = inp("c_ident_b", [128, 128], BF16)
        self.c_ident_f = inp("c_ident_f", [128, 128], F32)
        self.c_mask_le = inp("c_mask_le", [128, 128], BF16)
        self.c_mask_lt = inp("c_mask_lt", [128, 128], BF16)
        self.c_tri = inp("c_tri", [128, 128], BF16)
        self.c_invf = inp("c_invf", [128, 28], F32)
        self.c_blk = inp("c_blk", [16, S], BF16)
        self.out = nc.dram_tensor("out", [S, D], F32, kind="ExternalOutput").ap()
        self.xT_s = self.scr("xT_s", [128, 8, S], BF16)
        self.xres_s = self.scr("xres_s", [S, D], F32)
        self.qk_s = self.scr("qk_s", [4, 4, 2, 96, S], BF16)
        self.v_s = self.scr("v_s", [4, S, 4 * 65], BF16)
        self.o_s = self.scr("o_s", [S, D], BF16)
        self.od_s = self.scr("od_s", [2, S, 256], F32)
        self.gT_s = self.scr("gT_s", [32, S], F32)

        with ExitStack() as st:
            self.st = st
            self.c = Ctx(nc, st)
            self.ps = [st.enter_context(nc.psum_tensor("ps%d" % i, [128, 512], F32)) for i in range(8)]
            self.ident_b = self.sb("ident_b", [128, 128], BF16)
            self.ident_f = self.sb("ident_f", [128, 128], F32)
            self.mask_le = self.sb("mask_le", [128, 128], BF16)
            self.mask_lt = self.sb("mask_lt", [128, 128], BF16)
            self.tri = self.sb("tri", [128, 128], BF16)
            self.ones_b = self.sb("ones_b", [128, 128], BF16)
            self.cs = self.sb("rope_cos", [128, NT, 28], F32)
            self.sn = self.sb("rope_sin", [128, NT, 28], F32)
            self.memT = self.sb("memT", [128, 8, N_MEM], BF16)
            self.phase0()
            for l in range(self.n_layers if self.stop_after != (0, 0) else 0):
                last = (l == DEPTH - 1)
                self.phase1(l)
                if self.stop_after == (l, 1):
                    break
                self.phase2(l)
                if self.stop_after == (l, 2):
                    break
                self.phase34(l)
                if self.stop_after == (l, 3):
                    break
                self.phase5(l, last)
                if self.stop_after == (l, 5):
                    break
        return nc

    def phase0(self):
        nc, c = self.nc, self.c
        with ExitStack() as st:
            for dst, src, k in ((self.ident_b, self.c_ident_b, "const"), (self.ident_f, self.c_ident_f, "const"),
                                (self.mask_le, self.c_mask_le, "const"), (self.mask_lt, self.c_mask_lt, "const"),
                                (self.tri, self.c_tri, "const")):
                c.dma("sp", dst[:, :], src, writes=[k])
            self.ms("pool", self.ones_b[:, :], 1.0, ["const"])
            pos_i = self.sb("pos_i", [128, NT], I32, st)
            pos_f = self.sb("pos_f", [128, NT], F32, st)
            invf = self.sb("invf", [128, 28], F32, st)
            ang = self.sb("ang", [128, NT, 28], F32, st)
            t1 = self.sb("rp_t1", [128, NT, 28], F32, st)
            ki = self.sb("rp_ki", [128, NT, 28], I32, st)
            kf = self.sb("rp_kf", [128, NT, 28], F32, st)
            r = self.sb("rp_r", [128, NT, 28], F32, st)
            m = self.sb("rp_m", [128, NT, 28], F32, st)
            rc = self.sb("rp_rc", [128, NT, 28], F32, st)
            c.dma("sp", pos_i[:, :], self.pos_in, writes=["pos_i"])
            c.dma("sp", invf[:, :], self.c_invf, writes=["invf"])
            self.cp("dve", pos_f[:, :], pos_i[:, :], ["pos_i"], ["pos_f"])
            self.tt("dve", ang[:, :, :], pos_f[:, :].unsqueeze(2).to_broadcast([128, NT, 28]),
                    invf[:, :].unsqueeze(1).to_broadcast([128, NT, 28]), ALU.mult, ["pos_f", "invf"], ["ang"])
            self.ts("dve", t1[:, :, :], ang[:, :, :], 1.0 / TWO_PI, ALU.mult, ["ang"], ["rp_t1"])
            self.cp("dve", ki[:, :, :], t1[:, :, :], ["rp_t1"], ["rp_ki"])
            self.cp("dve", kf[:, :, :], ki[:, :, :], ["rp_ki"], ["rp_kf"])
            HI = 6.28125
            LO = TWO_PI - HI
            self.stt(r[:, :, :], kf[:, :, :], -HI, ang[:, :, :], ALU.mult, ALU.add, ["rp_kf", "ang"], ["rp_r"])
            self.stt(r[:, :, :], kf[:, :, :], -LO, r[:, :, :], ALU.mult, ALU.add, ["rp_kf", "rp_r"], ["rp_r"])

            def wrap(buf, key):
                self.ts("dve", m[:, :, :], buf[:, :, :], math.pi, ALU.is_gt, [key], ["rp_m"])
                self.stt(buf[:, :, :], m[:, :, :], -TWO_PI, buf[:, :, :], ALU.mult, ALU.add, ["rp_m", key], [key])
                self.ts("dve", m[:, :, :], buf[:, :, :], -math.pi, ALU.is_lt, [key], ["rp_m"])
                self.stt(buf[:, :, :], m[:, :, :], TWO_PI, buf[:, :, :], ALU.mult, ALU.add, ["rp_m", key], [key])
                self.ts("dve", buf[:, :, :], buf[:, :, :], math.pi, ALU.min, [key], [key], s2=-math.pi, op1=ALU.max)

            wrap(r, "rp_r")
            self.ts("dve", rc[:, :, :], r[:, :, :], math.pi / 2, ALU.add, ["rp_r"], ["rp_rc"])
            wrap(rc, "rp_rc")
            self.act(self.sn[:, :, :], r[:, :, :], AF.Sin, ["rp_r"], ["rope"])
            self.act(self.cs[:, :, :], rc[:, :, :], AF.Sin, ["rp_rc"], ["rope"])
            memb = self.sb("memb", [128, 2, D], BF16, st)
            c.dma("pool", memb[:, :, :], self.mem_in.rearrange("(t p) d -> p t d", p=128), writes=["memb"])
            for t in range(2):
                pst = self.ps[t][:, :].bitcast(BF16)
                for k in range(8):
                    self.tr(pst[:, k * 128:(k + 1) * 128], memb[:, t, k * 128:(k + 1) * 128], ["memb"], ["ps%d" % t])
                self.cp("dve", self.memT[:, :, t * 128:(t + 1) * 128],
                        pst.rearrange("p (k m) -> p k m", k=8), ["ps%d" % t], ["memT"])
            xb = [self.sb("p0_xb%d" % i, [128, D], BF16, st) for i in range(2)]
            xt = [self.sb("p0_xt%d" % i, [128, 8, 128], BF16, st) for i in range(2)]
            for t in range(NT):
                i = t % 2
                c.dma("pool", xb[i][:, :], self.x_in[t * 128:(t + 1) * 128, :], writes=["p0_xb%d" % i])
                pst = self.ps[2 + i][:, :].bitcast(BF16)
                for k in range(8):
                    self.tr(pst[:, k * 128:(k + 1) * 128], xb[i][:, k * 128:(k + 1) * 128], ["p0_xb%d" % i], ["ps%d" % (2 + i)])
                self.cp("dve" if i == 0 else "act", xt[i][:, :, :], pst.rearrange("p (k m) -> p k m", k=8),
                        ["ps%d" % (2 + i)], ["p0_xt%d" % i])
                c.dma("sp", self.xT_s[:, :, t * 128:(t + 1) * 128], xt[i][:, :, :], reads=["p0_xt%d" % i], writes=["xT_s"])
            c.emit()

    def rope(self, e, out1, out2, a, b, cos, sin, tmp1, tmp2, rk, wk_):
        k1, k2 = "rtmp1" + e, "rtmp2" + e
        self.tt(e, tmp1, a, cos, ALU.mult, rk + ["rope"], [k1])
        self.tt(e, tmp2, b, sin, ALU.mult, rk + ["rope"], [k2])
        self.tt(e, out1, tmp1, tmp2, ALU.subtract, [k1, k2], wk_)
        self.tt(e, tmp1, a, sin, ALU.mult, rk + ["rope"], [k1])
        self.tt(e, tmp2, b, cos, ALU.mult, rk + ["rope"], [k2])
        self.tt(e, out2, tmp1, tmp2, ALU.add, [k1, k2], wk_)

    def phase1(self, l):
        nc, c = self.nc, self.c
        with ExitStack() as st:
            win = self.sb("p1_win", [128, 8, IN_COLS], BF16, st)
            wuq = self.sb("p1_wuq", [128, 2, 384], BF16, st)
            wukv = self.sb("p1_wukv", [128, 512], BF16, st)
            gq = self.sb("p1_gq", [128, 2], F32, st)
            gkv = self.sb("p1_gkv", [128, 1], F32, st)
            for k in range(8):
                c.dma("pool", win[:, k, :], self.w_in[l, k * 128:(k + 1) * 128, :], writes=["p1_win%d" % k])
            wuq_f = self.sb("p1_wuq_f", [128, 2, 384], F32, st)
            wukv_f = self.sb("p1_wukv_f", [128, 512], F32, st)
            c.dma("sp", wuq_f[:, 0, :], self.w_uq[l, 0:128, :], writes=["p1_wuq_f"])
            c.dma("sp", wuq_f[0:64, 1, :], self.w_uq[l, 128:192, :], writes=["p1_wuq_f"])
            c.dma("sp", wukv_f[:, :], self.w_ukv[l, :, :], writes=["p1_wukv_f"])
            c.dma("sp", gq[:, 0:1], self.mla_q_norm[l, 0:128].rearrange("(p o) -> p o", o=1), writes=["p1_gq"])
            c.dma("sp", gq[0:64, 1:2], self.mla_q_norm[l, 128:192].rearrange("(p o) -> p o", o=1), writes=["p1_gq"])
            c.dma("sp", gkv[:, 0:1], self.mla_kv_norm[l, :].rearrange("(p o) -> p o", o=1), writes=["p1_gkv"])
            self.ts("dve", wuq[:, 0, :], wuq_f[:, 0, :], gq[:, 0:1], ALU.mult, ["p1_wuq_f", "p1_gq"], ["p1_wuq"])
            self.ts("dve", wuq[0:64, 1, :], wuq_f[0:64, 1, :], gq[0:64, 1:2], ALU.mult, ["p1_wuq_f", "p1_gq"], ["p1_wuq"])
            self.ts("dve", wukv[:, :], wukv_f[:, :], gkv[:, 0:1], ALU.mult, ["p1_wukv_f", "p1_gkv"], ["p1_wukv"])

            xT = [self.sb("p1_xT%d" % i, [128, 8, 512], BF16, st) for i in range(2)]
            c0 = [self.sb("p1_c0_%d" % i, [128, 352], BF16, st) for i in range(2)]
            ssq = [self.sb("p1_ssq%d" % i, [128, 4], F32, st) for i in range(2)]
            junk = self.sb("p1_junk", [128, 512], F32, st)
            qkS = [self.sb("p1_qkS%d" % i, [128, 512], BF16, st) for i in range(2)]
            qkM = [self.sb("p1_qkM%d" % i, [128, 512], BF16, st) for i in range(2)]
            qkD = [self.sb("p1_qkD%d" % i, [128, 512], BF16, st) for i in range(2)]
            qA = [self.sb("p1_qA%d" % i, [128, 4, 96], BF16, st) for i in range(2)]
            kA = [self.sb("p1_kA%d" % i, [128, 4, 96], BF16, st) for i in range(2)]
            vst = [[self.sb("p1_v%d_%d" % (m, i), [128, 4, 65], BF16, st) for i in range(2)] for m in range(4)]
            latT = [self.sb("p1_latT%d" % i, [128, 3, 128], BF16, st) for i in range(2)]
            rt1 = self.sb("p1_rt1", [128, 128], F32, st)
            rt2 = self.sb("p1_rt2", [128, 128], F32, st)
            rt3 = self.sb("p1_rt3", [128, 128], F32, st)
            rt4 = self.sb("p1_rt4", [128, 128], F32, st)
            hf = [self.sb("p1_hf%d" % i, [128, 512], F32, st) for i in range(2)]
            kpe = self.sb("p1_kpe", [128, 32], F32, st)
            stT = {}
            for nm, rows in (("A", 96), ("S", 128), ("M", 128), ("D", 128)):
                for role in range(2):
                    n = 4 if nm == "A" else 2
                    stT[(nm, role)] = [self.sb("p1_T%s%d_%d" % (nm, role, j), [rows, 512], BF16, st) for j in range(n)]
            mbT = self.sb("p1_mbT", [64, 512], BF16, st)
            kmT = self.sb("p1_kmT", [128, 2, 16], BF16, st)
            gate = self.sb("p1_gate", [128, 4, 16], F32, st)
            top8 = self.sb("p1_top8", [128, 4, 8], F32, st)
            mb = [self.sb("p1_mb%d" % i, [128, 4, 16], BF16, st) for i in range(2)]
            for m in range(4):
                for i in range(2):
                    self.ms("pool", vst[m][i][:, :, 64:65], 1.0, ["p1_v%d_%d" % (m, i)])
            self.ms("pool", kmT[:, :, :], 0.0, ["p1_kmT"])

            def load_x(b):
                c.dma("sp", xT[b % 2][:, :, :], self.xT_s[:, :, b * 512:(b + 1) * 512], reads=["xT_s"], writes=["p1_xT%d" % (b % 2)])

            load_x(0)
            psi = [0]

            def next_ps():
                i = psi[0] % 4
                psi[0] += 1
                return self.ps[i], "ps%d" % i

            for b in range(NB if LV >= 2 else 0):
                if b + 1 < NB:
                    load_x(b + 1)
                xTb = xT[b % 2]
                xk = "p1_xT%d" % (b % 2)
                for tl in range(4):
                    t = b * 4 + tl
                    i = t % 2
                    cosA, sinA = self.cs[:, t, 0:16], self.sn[:, t, 0:16]
                    cosM, sinM = self.cs[:, t, 16:24], self.sn[:, t, 16:24]
                    cosD, sinD = self.cs[:, t, 24:28], self.sn[:, t, 24:28]
                    for ci, (c0_, cw) in enumerate(CH):
                        ps, pk = next_ps()
                        for k in range(8):
                            self.mm(ps[:, 0:cw], xTb[:, k, tl * 128:(tl + 1) * 128], win[:, k, c0_:c0_ + cw],
                                    k == 0, k == 7, [xk, "p1_win%d" % k], [pk])
                        if (ci == 0 and LV < 3) or (ci in (3, 4) and LV < 4):
                            continue
                        if ci == 0:
                            ck = "p1_c0_%d" % i
                            self.cp("act", c0[i][:, 0:352], ps[:, 0:352], [pk], [ck])
                            self.act(junk[:, 0:192], ps[:, 0:192], AF.Square, [pk], ["p1_junk", "p1_ssq%d" % i], accum_out=ssq[i][:, 0:1])
                            self.act(junk[:, 192:320], ps[:, 192:320], AF.Square, [pk], ["p1_junk", "p1_ssq%d" % i], accum_out=ssq[i][:, 1:2])
                            if SUB < 2:
                                continue
                            self.cp("dve", hf[i][:, 0:32], c0[i][:, 320:352], [ck], ["p1_hf%d" % i])
                            if os.environ.get("P1X") != "1":
                                self.rope("dve", kpe[:, 0:16], kpe[:, 16:32], hf[i][:, 0:16], hf[i][:, 16:32], cosA, sinA,
                                          rt1[:, 0:16], rt2[:, 0:16], ["p1_hf%d" % i], ["p1_kpe"])
                            if SUB < 3:
                                continue
                            self.ts("dve", ssq[i][:, 2:3], ssq[i][:, 0:1], 1.0 / 192, ALU.mult, ["p1_ssq%d" % i], ["p1_ssq%d" % i], s2=RMS_EPS, op1=ALU.add)
                            self.ts("dve", ssq[i][:, 3:4], ssq[i][:, 1:2], 1.0 / 128, ALU.mult, ["p1_ssq%d" % i], ["p1_ssq%d" % i], s2=RMS_EPS, op1=ALU.add)
                            self.act(ssq[i][:, 2:4], ssq[i][:, 2:4], AF.Ln, ["p1_ssq%d" % i], ["p1_ssq%d" % i])
                            self.act(ssq[i][:, 2:4], ssq[i][:, 2:4], AF.Exp, ["p1_ssq%d" % i], ["p1_ssq%d" % i], scale=-0.5)
                            if SUB < 4:
                                continue
                            pt, ptk = self.ps[4], "ps4"
                            ptb = pt[:, :].bitcast(BF16)
                            self.tr(ptb[:, 0:128], c0[i][:, 0:128], [ck], [ptk])
                            self.tr(ptb[0:64, 128:256], c0[i][:, 128:192], [ck], [ptk])
                            self.tr(ptb[:, 256:384], c0[i][:, 192:320], [ck], [ptk])
                            lk = "p1_latT%d" % i
                            self.cp("dve", latT[i][:, 0:3:2, :], ptb[:, 0:384].rearrange("p (a m) -> p a m", a=3)[:, 0:3:2, :], [ptk], [lk])
                            self.cp("dve", latT[i][0:64, 1, :], ptb[0:64, 128:256], [ptk], [lk])
                            if SUB < 5:
                                continue
                            pq, pqk = self.ps[5], "ps5"
                            self.mm(pq[:, 0:384], latT[i][:, 0, :], wuq[:, 0, :], True, False, [lk, "p1_wuq"], [pqk])
                            self.mm(pq[:, 0:384], latT[i][0:64, 1, :], wuq[0:64, 1, :], False, True, [lk, "p1_wuq"], [pqk])
                            pq3 = pq[:, 0:384].rearrange("p (h d) -> p h d", h=4)
                            qk_ = "p1_qA%d" % i
                            self.ts("dve", qA[i][:, :, 0:64], pq3[:, :, 0:64], ssq[i][:, 2:3], ALU.mult, [pqk, "p1_ssq%d" % i], [qk_])
                            hk = "p1_hf%d" % i
                            self.ts("dve", hf[i][:, 64:192].rearrange("p (h d) -> p h d", h=4), pq3[:, :, 64:96], ssq[i][:, 2:3], ALU.mult,
                                    [pqk, "p1_ssq%d" % i], [hk])
                            h3 = hf[i][:, 64:192].rearrange("p (h d) -> p h d", h=4)
                            cb = cosA.unsqueeze(1).to_broadcast([128, 4, 16])
                            sb_ = sinA.unsqueeze(1).to_broadcast([128, 4, 16])
                            self.rope("dve", qA[i][:, :, 64:80], qA[i][:, :, 80:96], h3[:, :, 0:16], h3[:, :, 16:32], cb, sb_,
                                      rt1[:, 0:64].rearrange("p (h d) -> p h d", h=4), rt2[:, 0:64].rearrange("p (h d) -> p h d", h=4),
                                      [hk], [qk_])
                            if SUB < 6:
                                continue
                            pkv, pkvk = self.ps[6], "ps6"
                            self.mm(pkv[:, :], latT[i][:, 2, :], wukv[:, :], True, True, [lk, "p1_wukv"], [pkvk])
                            pkv3 = pkv[:, :].rearrange("p (h d) -> p h d", h=4)
                            kk_ = "p1_kA%d" % i
                            self.ts("dve", kA[i][:, :, 0:64], pkv3[:, :, 0:64], ssq[i][:, 3:4], ALU.mult, [pkvk, "p1_ssq%d" % i], [kk_])
                            self.cp("pool", kA[i][:, :, 64:96], kpe[:, :].unsqueeze(1).to_broadcast([128, 4, 32]), ["p1_kpe"], [kk_])
                            self.ts("dve", vst[0][i][:, :, 0:64], pkv3[:, :, 64:128], ssq[i][:, 3:4], ALU.mult, [pkvk, "p1_ssq%d" % i], ["p1_v0_%d" % i])
                        elif ci == 1:
                            self.cp("act", qkS[i][:, :], ps[:, :], [pk], ["p1_qkS%d" % i])
                        elif ci == 2:
                            self.cp("act", vst[1][i][:, :, 0:64], ps[:, 0:256].rearrange("p (h d) -> p h d", h=4), [pk], ["p1_v1_%d" % i])
                            self.cp("act", vst[2][i][:, :, 0:64], ps[:, 256:512].rearrange("p (h d) -> p h d", h=4), [pk], ["p1_v2_%d" % i])
                        elif ci in (3, 4):
                            dst = (qkM if ci == 3 else qkD)[i]
                            dk = ("p1_qkM%d" if ci == 3 else "p1_qkD%d") % i
                            hk = "p1_hf%d" % i
                            self.cp("dve", dst[:, :], ps[:, :], [pk], [dk])
                            if ci == 3:
                                g, dd, nr = 8, 64, 8
                                cb, sb_ = cosM, sinM
                            else:
                                g, dd, nr = 16, 32, 4
                                cb, sb_ = cosD, sinD
                            p3 = ps[:, :].rearrange("p (g d) -> p g d", g=g)
                            d3 = dst[:, :].rearrange("p (g d) -> p g d", g=g)
                            cbb = cb.unsqueeze(1).to_broadcast([128, g, nr])
                            sbb = sb_.unsqueeze(1).to_broadcast([128, g, nr])
                            h3 = hf[i][:, 0:g * 2 * nr].rearrange("p (g d) -> p g d", g=g)
                            self.cp("dve", h3, p3[:, :, 0:2 * nr], [pk], [hk])
                            self.rope("pool", d3[:, :, 0:nr], d3[:, :, nr:2 * nr], h3[:, :, 0:nr], h3[:, :, nr:2 * nr], cbb, sbb,
                                      rt3[:, 0:g * nr].rearrange("p (g d) -> p g d", g=g), rt4[:, 0:g * nr].rearrange("p (g d) -> p g d", g=g),
                                      [hk], [dk])
                        else:
                            self.cp("act", vst[3][i][:, :, 0:64], ps[:, 0:256].rearrange("p (h d) -> p h d", h=4), [pk], ["p1_v3_%d" % i])
                    if LV < 5:
                        continue
                    for m in range(4):
                        c.dma("sp", self.v_s[m, t * 128:(t + 1) * 128, :], vst[m][i][:, :, :].rearrange("p h d -> p (h d)"),
                              reads=["p1_v%d_%d" % (m, i)], writes=["v_s"])
                    pt, ptk = self.ps[7], "ps7"
                    ptb = pt[:, :].bitcast(BF16)
                    cols = slice(tl * 128, (tl + 1) * 128)
                    for role, src, sk in ((0, qA[i], "p1_qA%d" % i), (1, kA[i], "p1_kA%d" % i)):
                        for h in range(4):
                            self.tr(ptb[0:96, h * 128:(h + 1) * 128], src[:, h, :], [sk], [ptk])
                        for h in range(4):
                            self.cp("dve" if role == 0 else "act", stT[("A", role)][h][:, cols], ptb[0:96, h * 128:(h + 1) * 128], [ptk],
                                    ["p1_TA%d_%d" % (role, h)])
                    for nm, src, sk in (("S", qkS[i], "p1_qkS%d" % i), ("M", qkM[i], "p1_qkM%d" % i), ("D", qkD[i], "p1_qkD%d" % i)):
                        for j in range(4):
                            self.tr(ptb[:, j * 128:(j + 1) * 128], src[:, j * 128:(j + 1) * 128], [sk], [ptk])
                        for j in range(4):
                            role, pr = j // 2, j % 2
                            self.cp("act" if nm == "M" else "dve", stT[(nm, role)][pr][:, cols], ptb[:, j * 128:(j + 1) * 128], [ptk],
                                    ["p1_T%s%d_%d" % (nm, role, pr)])
                    if LV < 6:
                        continue
                    blk = t // 2
                    if t % 2 == 1:
                        pg, pgk = self.ps[6], "ps6"
                        for pr in range(2):
                            for u in range(2):
                                src = qkM[(t - 1 + u) % 2]
                                self.mm(pg[:, pr:pr + 1], src[:, 256 + pr * 128:256 + (pr + 1) * 128], self.ones_b[:, 0:1], u == 0, u == 1,
                                        ["p1_qkM0", "p1_qkM1", "const"], [pgk])
                        self.ts("dve", kmT[:, :, blk], pg[:, 0:2], 1.0 / 256, ALU.mult, [pgk], ["p1_kmT"])
                    own = blk
                    mk = "p1_mb%d" % i
                    if own >= 4:
                        g3 = gate[:, :, :].rearrange("p (a b) j -> p a b j", b=2)
                        self.ms("pool", gate[:, :, :], -BIGV, ["p1_gate"])
                        for hh, (pg, pgk) in enumerate(((self.ps[5], "ps5"), (self.ps[6], "ps6"))):
                            p0 = hh * 64
                            for pr in range(2):
                                qT = stT[("M", 0)][pr][p0:p0 + 64, cols]
                                self.mm(pg[:, pr * 16:(pr + 1) * 16], qT, kmT[p0:p0 + 64, pr, :],
                                        True, True, ["p1_TM0_%d" % pr, "p1_kmT"], [pgk])
                            self.cp("dve", g3[:, :, hh, 0:own], pg[:, 0:32].rearrange("p (a j) -> p a j", a=2)[:, :, 0:own], [pgk], ["p1_gate"])
                        for h in range(4):
                            nc_ = self.nc
                            gh = gate[:, h, :]
                            th = top8[:, h, :]
                            c.op("dve", (lambda gh=gh, th=th: nc_.vector.max(out=th, in_=gh)), ["p1_gate"], ["p1_top8"])
                        for h in range(4):
                            self.ts("dve", mb[i][:, h, :], gate[:, h, :], top8[:, h, 2:3], ALU.is_ge, ["p1_gate", "p1_top8"], [mk],
                                    s2=-1.0, op1=ALU.add)
                        self.ms("pool", mb[i][:, :, own:own + 1], 0.0, [mk])
                    else:
                        self.ms("pool", mb[i][:, :, :], 0.0, [mk])
                    pm, pmk = self.ps[4], "ps4"
                    pmb = pm[:, :].bitcast(BF16)
                    self.tr(pmb[0:64, 0:128], mb[i][:, :, :].rearrange("p h j -> p (h j)"), [mk], [pmk])
                    self.cp("dve", mbT[:, cols], pmb[0:64, 0:128], [pmk], ["p1_mbT"])
                if LV < 7:
                    continue
                bc = slice(b * 512, (b + 1) * 512)
                for role in range(2):
                    for h in range(4):
                        c.dma("sp", self.qk_s[0, h, role, 0:96, bc], stT[("A", role)][h][:, :], reads=["p1_TA%d_%d" % (role, h)], writes=["qk_s"])
                    for mi, nm in ((1, "S"), (2, "M"), (3, "D")):
                        for pr in range(2):
                            for hh in range(2):
                                h = pr * 2 + hh
                                c.dma("sp", self.qk_s[mi, h, role, 0:64, bc], stT[(nm, role)][pr][hh * 64:(hh + 1) * 64, :],
                                      reads=["p1_T%s%d_%d" % (nm, role, pr)], writes=["qk_s"])
                for h in range(4):
                    c.dma("sp", self.qk_s[2, h, 0, 64:80, bc], mbT[h * 16:(h + 1) * 16, :], reads=["p1_mbT"], writes=["qk_s"])
            for h in range(4 if LV >= 8 else 0):
                c.dma("sp", self.qk_s[2, h, 1, 64:80, :], self.c_blk, writes=["qk_s"])
            c.emit()

    def phase2(self, l):
        nc, c = self.nc, self.c
        lambda_init = 0.8 - 0.6 * math.exp(-0.3 * l)
        with ExitStack() as st:
            KT = [self.sb("p2_KT%d" % i, [96, S], BF16, st) for i in range(2)]
            QT = [self.sb("p2_QT%d" % i, [96, S], BF16, st) for i in range(2)]
            Vm = [self.sb("p2_Vm%d" % i, [128, NT, 260], BF16, st) for i in range(2)]
            om = [self.sb("p2_om%d" % i, [128, NT, 256], BF16, st) for i in range(2)]
            od = [self.sb("p2_od%d" % i, [128, NT, 64], F32, st) for i in range(2)]
            PT = [self.sb("p2_PT%d" % i, [128, 512], BF16, st) for i in range(3)]
            E32 = [self.sb("p2_E%d" % i, [128, 512], F32, st) for i in range(3)]
            Psp = [self.sb("p2_P%d" % i, [128, 512], BF16, st) for i in range(3)]
            T = [self.sb("p2_T%d" % i, [128, 512], F32, st) for i in range(3)]
            Aex = [self.sb("p2_A%d" % i, [128, 512], BF16, st) for i in range(3)]
            Run = self.sb("p2_Run", [128, 512], BF16, st)
            rc = self.sb("p2_rc", [128, 4], F32, st)
            hcount = [0]
            vcount = [0]

            def load_head(m, h, rows, r0=0):
                j = hcount[0] % 2
                hcount[0] += 1
                c.dma("sp", KT[j][0:rows, :], self.qk_s[m, h, 1, r0:r0 + rows, :], reads=["qk_s"], writes=["p2_KT%d" % j])
                c.dma("sp", QT[j][0:rows, :], self.qk_s[m, h, 0, r0:r0 + rows, :], reads=["qk_s"], writes=["p2_QT%d" % j])
                return KT[j], QT[j], ["p2_KT%d" % j, "p2_QT%d" % j]

            def load_v(m):
                j = vcount[0] % 2
                vcount[0] += 1
                c.dma("sp", Vm[j][:, :, :], self.v_s[m].rearrange("(t p) c -> p t c", p=128), reads=["v_s"], writes=["p2_Vm%d" % j])
                return Vm[j], "p2_Vm%d" % j

            sslot = [0]

            def softmax_head(kt, qt, kqk, R, scale, vm, vkey, h, out_fn):
                for b in range(NB):
                    n = 4 * b + 4

                    def S_stage(i):
                        j = i - 4 * b
                        c0 = max(0, j) * 128
                        slot = sslot[0]
                        sslot[0] = (slot + 1) % 3
                        ps, pk = self.ps[slot], "ps%d" % slot
                        self.mm(ps[:, c0:512], kt[0:R, i * 128:(i + 1) * 128], qt[0:R, b * 512 + c0:(b + 1) * 512], True, True, kqk, [pk])
                        pt, ptk = PT[slot], "p2_PT%d" % slot
                        self.act(pt[:, c0:512], ps[:, c0:512], AF.Exp, [pk], [ptk], scale=scale)
                        if j >= 0:
                            self.tt("pool", pt[:, c0:c0 + 128], pt[:, c0:c0 + 128], self.mask_le[:, :], ALU.mult, [ptk, "const"], [ptk])
                        return slot

                    def PV_stage(i, slot):
                        for s_ in range(4):
                            if 4 * b + s_ >= i:
                                self.mm(self.ps[3 + s_][:, 0:65], PT[slot][:, s_ * 128:(s_ + 1) * 128], vm[:, i, h * 65:(h + 1) * 65],
                                        i == 0, i == 4 * b + s_, ["p2_PT%d" % slot, vkey], ["ps%d" % (3 + s_)])

                    prev = None
                    for i in range(n):
                        cur = S_stage(i)
                        if prev is not None:
                            PV_stage(i - 1, prev)
                        prev = cur
                    PV_stage(n - 1, prev)
                    for s_ in range(4):
                        out_fn(b, s_, self.ps[3 + s_], "ps%d" % (3 + s_))

            def sb_head(kt, qt, kqk, vm, vkey, h, omt, omk):
                scale = 0.125
                for b in range(NB):
                    order = list(range(4 * b + 3, -1, -1))
                    n = len(order)
                    self.ms("pool", Run[:, :], 0.0, ["p2_Run"])

                    def geo(idx):
                        i = order[idx]
                        j = i - 4 * b
                        return i, j, max(0, j) * 128

                    def A_stage(idx):
                        i, j, c0 = geo(idx)
                        z, q3 = idx % 2, idx % 3
                        ps, pk = self.ps[z], "ps%d" % z
                        self.mm(ps[:, c0:512], kt[0:64, i * 128:(i + 1) * 128], qt[0:64, b * 512 + c0:(b + 1) * 512], True, True, kqk, [pk])
                        e, ek = E32[q3], "p2_E%d" % q3
                        self.act(e[:, c0:512], ps[:, c0:512], AF.Exp, [pk], [ek], scale=scale)
                        if j >= 0:
                            self.tt("pool", e[:, c0:c0 + 128], e[:, c0:c0 + 128], self.mask_lt[:, :], ALU.mult, [ek, "const"], [ek])
                        self.act(Psp[q3][:, c0:512], e[:, c0:512], AF.Ln, [ek], ["p2_P%d" % q3], bias=1.0)

                    def B_stage(idx):
                        i, j, c0 = geo(idx)
                        z, q3 = idx % 2, idx % 3
                        af, afk = (self.ps[2], "ps2") if z == 0 else (self.ps[7], "ps7")
                        pk2 = "p2_P%d" % q3
                        first = idx == 0
                        self.mm(af[:, c0:512], self.tri[:, :], Psp[q3][:, c0:512], True, first, ["const", pk2], [afk])
                        if not first:
                            self.mm(af[:, c0:512], self.ones_b[:, :], Run[:, c0:512], False, True, ["const", "p2_Run"], [afk])
                        self.tt("pool", Run[:, c0:512], Run[:, c0:512], Psp[q3][:, c0:512], ALU.add, ["p2_Run", pk2], ["p2_Run"])
                        t, tk = T[q3], "p2_T%d" % q3
                        self.stt(t[:, c0:512], self.ps[z][:, c0:512], scale, Psp[q3][:, c0:512], ALU.mult, ALU.subtract, ["ps%d" % z, pk2], [tk])
                        self.tt("dve", t[:, c0:512], t[:, c0:512], af[:, c0:512], ALU.subtract, [tk, afk], [tk])
                        a, ak = Aex[q3], "p2_A%d" % q3
                        self.act(a[:, c0:512], t[:, c0:512], AF.Exp, [tk], [ak])
                        if j >= 0:
                            self.tt("pool", a[:, c0:c0 + 128], a[:, c0:c0 + 128], self.mask_lt[:, :], ALU.mult, [ak, "const"], [ak])

                    def C_stage(idx):
                        i, j, c0 = geo(idx)
                        q3 = idx % 3
                        for s_ in range(4):
                            if 4 * b + s_ >= i:
                                self.mm(self.ps[3 + s_][:, 0:64], Aex[q3][:, s_ * 128:(s_ + 1) * 128], vm[:, i, h * 65:h * 65 + 64],
                                        i == 4 * b + s_, i == 0, ["p2_A%d" % q3, vkey], ["ps%d" % (3 + s_)])

                    for step in range(n + 2):
                        if step < n:
                            A_stage(step)
                        if 0 <= step - 1 < n:
                            B_stage(step - 1)
                        if 0 <= step - 2 < n:
                            C_stage(step - 2)
                    for s_ in range(4):
                        self.cp("dve", omt[:, 4 * b + s_, h * 64:(h + 1) * 64], self.ps[3 + s_][:, 0:64], ["ps%d" % (3 + s_)], [omk])

            def norm_out(dst, dkey, col0, f32=False):
                def fn(b, s_, O, ok):
                    self.recip(rc[:, s_:s_ + 1], O[:, 64:65], [ok], ["p2_rc"])
                    self.ts("dve", dst[:, 4 * b + s_, col0:col0 + 64], O[:, 0:64], rc[:, s_:s_ + 1], ALU.mult, [ok, "p2_rc"], [dkey])
                return fn

            def store_om(m, omt, omk):
                c.dma("sp", self.o_s[:, m * 256:(m + 1) * 256].rearrange("(t p) c -> p t c", p=128), omt[:, :, :], reads=[omk], writes=["o_s"])

            oi = 0
            for m, R, scale in ((0, 96, 96 ** -0.5), (2, 80, 0.125)):
                vm, vkey = load_v(m)
                omt, omk = om[oi % 2], "p2_om%d" % (oi % 2)
                oi += 1
                for h in range(4):
                    kt, qt, kqk = load_head(m, h, R)
                    softmax_head(kt, qt, kqk, R, scale, vm, vkey, h, norm_out(omt, omk, h * 64))
                store_om(m, omt, omk)
            vm, vkey = load_v(1)
            omt, omk = om[oi % 2], "p2_om%d" % (oi % 2)
            oi += 1
            for h in range(4):
                kt, qt, kqk = load_head(1, h, 64)
                sb_head(kt, qt, kqk, vm, vkey, h, omt, omk)
            store_om(1, omt, omk)
            vm, vkey = load_v(3)
            dcount = 0
            for comp in range(2):
                for h in range(4):
                    kt, qt, kqk = load_head(3, h, 32, comp * 32)
                    odt, odk = od[dcount % 2], "p2_od%d" % (dcount % 2)
                    dcount += 1
                    softmax_head(kt, qt, kqk, 32, 32 ** -0.5, vm, vkey, h, norm_out(odt, odk, 0))
                    c.dma("sp", self.od_s[comp, :, h * 64:(h + 1) * 64].rearrange("(t p) c -> p t c", p=128), odt[:, :, :],
                          reads=[odk], writes=["od_s"])
            dl = self.sb("p2_dl", [128, 128], F32, st)
            lam = self.sb("p2_lam", [128, 4], F32, st)
            gsub = self.sb("p2_gsub", [128, 64], F32, st)
            junk = self.sb("p2_junk", [128, 64], F32, st)
            c.dma("sp", dl[:, :], self.diff_lambda[l, :].partition_broadcast(128), writes=["p2_dl"])
            c.dma("sp", gsub[:, :], self.diff_subln[l, :].partition_broadcast(128), writes=["p2_gsub"])
            self.tt("dve", junk[:, 0:32], dl[:, 0:32], dl[:, 32:64], ALU.mult, ["p2_dl"], ["p2_junk"])
            self.red(lam[:, 0:1], junk[:, 0:32], ALU.add, ["p2_junk"], ["p2_lam"])
            self.tt("dve", junk[:, 32:64], dl[:, 64:96], dl[:, 96:128], ALU.mult, ["p2_dl"], ["p2_junk"])
            self.red(lam[:, 1:2], junk[:, 32:64], ALU.add, ["p2_junk"], ["p2_lam"])
            self.act(lam[:, 0:2], lam[:, 0:2], AF.Exp, ["p2_lam"], ["p2_lam"])
            self.tt("dve", lam[:, 2:3], lam[:, 1:2], lam[:, 0:1], ALU.subtract, ["p2_lam"], ["p2_lam"])
            self.ts("dve", lam[:, 2:3], lam[:, 2:3], -lambda_init, ALU.add, ["p2_lam"], ["p2_lam"])
            self.ts("dve", gsub[:, :], gsub[:, :], 1.0 - lambda_init, ALU.mult, ["p2_gsub"], ["p2_gsub"])
            omt, omk = om[oi % 2], "p2_om%d" % (oi % 2)
            oi += 1
            d1 = [self.sb("p2_d1_%d" % i, [128, 256], F32, st) for i in range(2)]
            d2 = [self.sb("p2_d2_%d" % i, [128, 256], F32, st) for i in range(2)]
            sq = self.sb("p2_sq", [128, 256], F32, st)
            ss = self.sb("p2_ss", [128, 4], F32, st)
            for t in range(NT):
                i = t % 2
                c.dma("sp", d1[i][:, :], self.od_s[0, t * 128:(t + 1) * 128, :], reads=["od_s"], writes=["p2_d1_%d" % i])
                c.dma("sp", d2[i][:, :], self.od_s[1, t * 128:(t + 1) * 128, :], reads=["od_s"], writes=["p2_d2_%d" % i])
                k1 = "p2_d1_%d" % i
                self.stt(d1[i][:, :], d2[i][:, :], lam[:, 2:3], d1[i][:, :], ALU.mult, ALU.add, ["p2_d2_%d" % i, k1, "p2_lam"], [k1])
                self.tt("dve", sq[:, :], d1[i][:, :], d1[i][:, :], ALU.mult, [k1], ["p2_sq"])
                self.red(ss[:, :], sq[:, :].rearrange("p (h d) -> p h d", h=4), ALU.add, ["p2_sq"], ["p2_ss"])
                self.ts("dve", ss[:, :], ss[:, :], 1.0 / 64, ALU.mult, ["p2_ss"], ["p2_ss"], s2=RMS_EPS, op1=ALU.add)
                self.act(ss[:, :], ss[:, :], AF.Ln, ["p2_ss"], ["p2_ss"])
                self.act(ss[:, :], ss[:, :], AF.Exp, ["p2_ss"], ["p2_ss"], scale=-0.5)
                d3 = d1[i][:, :].rearrange("p (h d) -> p h d", h=4)
                self.tt("dve", d3, d3, ss[:, :].unsqueeze(2).to_broadcast([128, 4, 64]), ALU.mult, [k1, "p2_ss"], [k1])
                self.tt("dve", omt[:, t, :].rearrange("p (h d) -> p h d", h=4), d3, gsub[:, :].unsqueeze(1).to_broadcast([128, 4, 64]),
                        ALU.mult, [k1, "p2_gsub"], [omk])
            store_om(3, omt, omk)
            c.emit()

    def ln_tile(self, y, yk, res, resk, pa, pak, pb, pbk, g_t, b_t, gbk, out, outk, tmp):
        nc = self.nc
        st6, mv = tmp["st6"], tmp["mv"]
        self.stt(y[:, 0:512], res[:, 0:512], ALPHA, pa[:, 0:512], ALU.mult, ALU.add, [resk, pak], [yk])
        self.stt(y[:, 512:1024], res[:, 512:1024], ALPHA, pb[:, 0:512], ALU.mult, ALU.add, [resk, pbk], [yk])
        self.c.op("dve", lambda: nc.vector.bn_stats(out=st6[:, 0:6], in_=y[:, 0:512]), [yk], ["ln_st6"])
        self.c.op("dve", lambda: nc.vector.bn_stats(out=st6[:, 6:12], in_=y[:, 512:1024]), [yk], ["ln_st6"])
        self.c.op("dve", lambda: nc.vector.bn_aggr(out=mv[:, 0:2], in_=st6[:, 0:12]), ["ln_st6"], ["ln_mv"])
        self.ts("dve", mv[:, 2:3], mv[:, 1:2], LN_EPS, ALU.add, ["ln_mv"], ["ln_mv"])
        self.act(mv[:, 2:3], mv[:, 2:3], AF.Ln, ["ln_mv"], ["ln_mv"])
        self.act(mv[:, 2:3], mv[:, 2:3], AF.Exp, ["ln_mv"], ["ln_mv"], scale=-0.5)
        self.ts("dve", y[:, :], y[:, :], mv[:, 0:1], ALU.subtract, [yk, "ln_mv"], [yk], s2=mv[:, 2:3], op1=ALU.mult)
        self.tt("pool", y[:, :], y[:, :], g_t[:, :], ALU.mult, [yk, gbk], [yk])
        self.tt("dve", out, y[:, :], b_t[:, :], ALU.add, [yk, gbk], [outk])

    def to_xT(self, src, srck, xb, xbk, xts, xtsk, cols, eng):
        self.cp("act", xb[:, :], src, [srck], [xbk])
        ptb = self.ps[7][:, :].bitcast(BF16)
        for k in range(8):
            self.tr(ptb[:, k * 128:(k + 1) * 128], xb[:, k * 128:(k + 1) * 128], [xbk], ["ps7"])
        self.cp(eng, xts[:, :, cols], ptb.rearrange("p (k m) -> p k m", k=8), ["ps7"], [xtsk])

    def phase34(self, l):
        nc, c = self.nc, self.c
        xres_src = self.x_in if l == 0 else self.xres_s
        with ExitStack() as st:
            KmT = self.sb("p3_KmT", [128, 8, N_MEM], BF16, st)
            Vmem = self.sb("p3_Vmem", [128, 2, 4 * 257], BF16, st)
            with ExitStack() as st2:
                wk = self.sb("p3_wk", [128, 8, D], BF16, st2)
                wv = self.sb("p3_wv", [128, 8, D], BF16, st2)
                for k in range(8):
                    c.dma("pool", wk[:, k, :], self.wk[l, k * 128:(k + 1) * 128, :], writes=["p3_wk%d" % k])
                    c.dma("pool", wv[:, k, :], self.wv[l, k * 128:(k + 1) * 128, :], writes=["p3_wv%d" % k])
                self.ms("pool", Vmem[:, :, :], 1.0, ["p3_Vmem"])
                for j in range(8):
                    ps, pk = self.ps[j % 2], "ps%d" % (j % 2)
                    for k in range(8):
                        self.mm(ps[:, 0:N_MEM], wk[:, k, j * 128:(j + 1) * 128], self.memT[:, k, :], k == 0, k == 7, ["p3_wk%d" % k, "memT"], [pk])
                    self.cp("act" if j % 2 == 0 else "dve", KmT[:, j, :], ps[:, 0:N_MEM], [pk], ["p3_KmT"])
                for mt in range(2):
                    for nh in range(2):
                        ps, pk = self.ps[2 + nh], "ps%d" % (2 + nh)
                        for k in range(8):
                            self.mm(ps[:, :], self.memT[:, k, mt * 128:(mt + 1) * 128], wv[:, k, nh * 512:(nh + 1) * 512], k == 0, k == 7,
                                    ["p3_wv%d" % k, "memT"], [pk])
                        dst = Vmem[:, mt, nh * 514:(nh + 1) * 514].rearrange("p (h d) -> p h d", h=2)[:, :, 0:256]
                        self.cp("act" if nh == 0 else "dve", dst, ps[:, :].rearrange("p (h d) -> p h d", h=2), [pk], ["p3_Vmem"])
                c.emit()
            wout = self.sb("p3_wout", [128, 8, D], BF16, st)
            wq = self.sb("p3_wq", [128, 8, D], BF16, st)
            wo = self.sb("p3_wo", [128, 8, D], BF16, st)
            for k in range(8):
                c.dma("pool", wout[:, k, :], self.w_out[l, k * 128:(k + 1) * 128, :], writes=["p3_wout%d" % k])
            for k in range(8):
                c.dma("pool", wq[:, k, :], self.wq[l, k * 128:(k + 1) * 128, :], writes=["p3_wq%d" % k])
            for k in range(8):
                c.dma("pool", wo[:, k, :], self.wo[l, k * 128:(k + 1) * 128, :], writes=["p3_wo%d" % k])
            gb = {}
            for nm, src in (("g1", self.ln_g[0]), ("b1", self.ln_b[0]), ("g2", self.ln_g[1]), ("b2", self.ln_b[1])):
                gb[nm] = self.sb("p3_" + nm, [128, D], F32, st)
                c.dma("sp", gb[nm][:, :], src[l, :].partition_broadcast(128), writes=["p3_gb"])
            tmp = {"st6": self.sb("p3_st6", [128, 12], F32, st), "mv": self.sb("p3_mv", [128, 4], F32, st)}
            ob = [self.sb("p3_ob%d" % i, [128, 4, D], BF16, st) for i in range(2)]
            oT = self.sb("p3_oT", [128, 8, 512], BF16, st)
            xr = [self.sb("p3_xr%d" % i, [128, D], F32, st) for i in range(2)]
            y = self.sb("p3_y", [128, D], F32, st)
            x1 = self.sb("p3_x1", [128, 4, D], F32, st)
            xb = self.sb("p3_xb", [128, D], BF16, st)
            x1T = self.sb("p3_x1T", [128, 8, 512], BF16, st)
            qT = self.sb("p3_qT", [128, 8, 512], BF16, st)
            PT2 = [self.sb("p3_PT%d" % i, [128, 512], BF16, st) for i in range(4)]
            xo = self.sb("p3_xo", [128, 4, D], BF16, st)
            xoT = self.sb("p3_xoT", [128, 8, 512], BF16, st)
            x2 = [self.sb("p3_x2_%d" % i, [128, D], F32, st) for i in range(2)]
            x2T = [self.sb("p3_x2T%d" % i, [128, 8, 512], BF16, st) for i in range(2)]
            rc = self.sb("p3_rc", [128, 2], F32, st)
            wk_all = lambda nm: ["p3_%s%d" % (nm, k) for k in range(8)]

            def load_ob(b):
                c.dma("sp", ob[b % 2][:, :, :], self.o_s[b * 512:(b + 1) * 512, :].rearrange("(t p) c -> p t c", p=128),
                      reads=["o_s"], writes=["p3_ob%d" % (b % 2)])

            load_ob(0)
            xcount = 0
            pcount = 0
            for b in range(NB):
                if b + 1 < NB:
                    load_ob(b + 1)
                obk = "p3_ob%d" % (b % 2)
                for tl in range(4):
                    ptb = self.ps[7][:, :].bitcast(BF16)
                    for k in range(8):
                        self.tr(ptb[:, k * 128:(k + 1) * 128], ob[b % 2][:, tl, k * 128:(k + 1) * 128], [obk], ["ps7"])
                    self.cp("act" if tl % 2 == 0 else "dve", oT[:, :, tl * 128:(tl + 1) * 128], ptb.rearrange("p (k m) -> p k m", k=8), ["ps7"], ["p3_oT"])
                for tl in range(4):
                    t = b * 4 + tl
                    xi = xcount % 2
                    xcount += 1
                    c.dma("sp", xr[xi][:, :], xres_src[t * 128:(t + 1) * 128, :], reads=["xres_s%d" % t], writes=["p3_xr%d" % xi])
                    for nh in range(2):
                        for k in range(8):
                            self.mm(self.ps[nh][:, :], oT[:, k, tl * 128:(tl + 1) * 128], wout[:, k, nh * 512:(nh + 1) * 512], k == 0, k == 7,
                                    ["p3_oT", "p3_wout%d" % k], ["ps%d" % nh])
                    self.ln_tile(y, "p3_y", xr[xi], "p3_xr%d" % xi, self.ps[0], "ps0", self.ps[1], "ps1", gb["g1"], gb["b1"], "p3_gb",
                                 x1[:, tl, :], "p3_x1", tmp)
                    self.to_xT(x1[:, tl, :], "p3_x1", xb, "p3_xb", x1T, "p3_x1T", slice(tl * 128, (tl + 1) * 128), "dve")
                for j in range(8):
                    ps, pk = self.ps[2 + j % 2], "ps%d" % (2 + j % 2)
                    for k in range(8):
                        self.mm(ps[:, :], wq[:, k, j * 128:(j + 1) * 128], x1T[:, k, :], k == 0, k == 7, ["p3_wq%d" % k, "p3_x1T"], [pk])
                    self.cp("act" if j % 2 == 0 else "dve", qT[:, j, :], ps[:, :], [pk], ["p3_qT"])
                for h in range(4):
                    pts = []
                    for mt in range(2):
                        ps, pk = self.ps[4 + mt], "ps%d" % (4 + mt)
                        for cc in range(2):
                            self.mm(ps[:, :], KmT[:, h * 2 + cc, mt * 128:(mt + 1) * 128], qT[:, h * 2 + cc, :], cc == 0, cc == 1,
                                    ["p3_KmT", "p3_qT"], [pk])
                        pi = pcount % 4
                        pcount += 1
                        self.act(PT2[pi][:, :], ps[:, :], AF.Exp, [pk], ["p3_PT%d" % pi], scale=256 ** -0.5)
                        pts.append(pi)
                    for tl in range(4):
                        ps, pk = (self.ps[6], "ps6") if tl % 2 == 0 else (self.ps[7], "ps7")
                        for mt in range(2):
                            self.mm(ps[:, 0:257], PT2[pts[mt]][:, tl * 128:(tl + 1) * 128], Vmem[:, mt, h * 257:(h + 1) * 257], mt == 0, mt == 1,
                                    ["p3_PT%d" % pts[mt], "p3_Vmem"], [pk])
                        self.recip(rc[:, tl % 2:tl % 2 + 1], ps[:, 256:257], [pk], ["p3_rc"])
                        self.ts("dve", xo[:, tl, h * 256:(h + 1) * 256], ps[:, 0:256], rc[:, tl % 2:tl % 2 + 1], ALU.mult, [pk, "p3_rc"], ["p3_xo"])
                for tl in range(4):
                    ptb = self.ps[7][:, :].bitcast(BF16)
                    for k in range(8):
                        self.tr(ptb[:, k * 128:(k + 1) * 128], xo[:, tl, k * 128:(k + 1) * 128], ["p3_xo"], ["ps7"])
                    self.cp("act" if tl % 2 == 0 else "dve", xoT[:, :, tl * 128:(tl + 1) * 128], ptb.rearrange("p (k m) -> p k m", k=8), ["ps7"], ["p3_xoT"])
                x2Tb, x2Tk = x2T[b % 2], "p3_x2T%d" % (b % 2)
                for tl in range(4):
                    t = b * 4 + tl
                    for nh in range(2):
                        for k in range(8):
                            self.mm(self.ps[nh][:, :], xoT[:, k, tl * 128:(tl + 1) * 128], wo[:, k, nh * 512:(nh + 1) * 512], k == 0, k == 7,
                                    ["p3_xoT", "p3_wo%d" % k], ["ps%d" % nh])
                    x2t, x2k = x2[t % 2], "p3_x2_%d" % (t % 2)
                    self.ln_tile(y, "p3_y", x1[:, tl, :], "p3_x1", self.ps[0], "ps0", self.ps[1], "ps1", gb["g2"], gb["b2"], "p3_gb",
                                 x2t[:, :], x2k, tmp)
                    c.dma("sp", self.xres_s[t * 128:(t + 1) * 128, :], x2t[:, :], reads=[x2k], writes=["xres_s%d" % t])
                    self.to_xT(x2t[:, :], x2k, xb, "p3_xb", x2Tb, x2Tk, slice(tl * 128, (tl + 1) * 128), "dve")
                c.dma("sp", self.xT_s[:, :, b * 512:(b + 1) * 512], x2Tb[:, :, :], reads=[x2Tk], writes=["xT_s"])
            c.emit()

    def phase5(self, l, last):
        nc, c = self.nc, self.c
        with ExitStack() as st:
            wr = self.sb("p5_wr", [128, 8, 40], BF16, st)
            br = self.sb("p5_br", [128, 40], F32, st)
            c.dma("pool", wr[:, :, :], self.w_router[l].rearrange("(k p) n -> p k n", p=128), writes=["p5_wr"])
            c.dma("sp", br[:, :], self.b_router[l, :].partition_broadcast(128), writes=["p5_br"])
            xT = [self.sb("p5_xT%d" % i, [128, 8, 512], BF16, st) for i in range(2)]
            lg = self.sb("p5_lg", [128, 40], F32, st)
            sm = self.sb("p5_sm", [128, 16], F32, st)
            ohg = self.sb("p5_ohg", [128, 8], F32, st)
            j8 = self.sb("p5_j8", [128, 8], F32, st)
            t32 = self.sb("p5_t32", [128, 32], F32, st)
            el = self.sb("p5_el", [128, 4], F32, st)
            el2 = self.sb("p5_el2", [128, 4], F32, st)
            mk1 = self.sb("p5_mk1", [128, 4], F32, st)
            mk2 = self.sb("p5_mk2", [128, 4], F32, st)
            g4 = self.sb("p5_g4", [128, 4], F32, st)
            g32 = self.sb("p5_g32", [128, 32], F32, st)
            gTs = [self.sb("p5_gTs%d" % i, [32, 512], F32, st) for i in range(2)]

            def load_x(b):
                c.dma("sp", xT[b % 2][:, :, :], self.xT_s[:, :, b * 512:(b + 1) * 512], reads=["xT_s"], writes=["p5_xT%d" % (b % 2)])

            load_x(0)
            K = "p5_r"
            for b in range(NB):
                if b + 1 < NB:
                    load_x(b + 1)
                gT, gTk = gTs[b % 2], "p5_gTs%d" % (b % 2)
                for tl in range(4):
                    ps, pk = self.ps[tl % 2], "ps%d" % (tl % 2)
                    for k in range(8):
                        self.mm(ps[:, 0:40], xT[b % 2][:, k, tl * 128:(tl + 1) * 128], wr[:, k, :], k == 0, k == 7, ["p5_xT%d" % (b % 2), "p5_wr"], [pk])
                    self.tt("dve", lg[:, :], ps[:, 0:40], br[:, :], ALU.add, [pk, "p5_br"], [K])
                    self.red(sm[:, 0:1], lg[:, 0:8], ALU.max, [K], [K])
                    self.ts("dve", ohg[:, :], lg[:, 0:8], sm[:, 0:1], ALU.is_equal, [K], [K])
                    self.ts("dve", sm[:, 1:2], sm[:, 0:1], -1.0, ALU.mult, [K], [K])
                    self.act(j8[:, :], lg[:, 0:8], AF.Exp, [K], [K], bias=sm[:, 1:2], accum_out=sm[:, 2:3])
                    self.recip(sm[:, 3:4], sm[:, 2:3], [K], [K])
                    lge = lg[:, 8:40].rearrange("p (g e) -> p g e", g=8)
                    self.tt("dve", t32[:, :].rearrange("p (g e) -> p g e", g=8), lge, ohg[:, :].unsqueeze(2).to_broadcast([128, 8, 4]),
                            ALU.mult, [K], [K])
                    self.red(el[:, :], t32[:, :].rearrange("p (g e) -> p e g", g=8), ALU.add, [K], [K])
                    self.red(sm[:, 4:5], el[:, :], ALU.max, [K], [K])
                    self.ts("dve", mk1[:, :], el[:, :], sm[:, 4:5], ALU.is_equal, [K], [K])
                    self.stt(el2[:, :], mk1[:, :], -1e30, el[:, :], ALU.mult, ALU.add, [K], [K])
                    self.red(sm[:, 5:6], el2[:, :], ALU.max, [K], [K])
                    self.ts("dve", mk2[:, :], el2[:, :], sm[:, 5:6], ALU.is_equal, [K], [K])
                    self.tt("dve", sm[:, 6:7], sm[:, 5:6], sm[:, 4:5], ALU.subtract, [K], [K])
                    self.act(sm[:, 6:7], sm[:, 6:7], AF.Exp, [K], [K])
                    self.ts("dve", sm[:, 7:8], sm[:, 6:7], 1.0, ALU.add, [K], [K])
                    self.recip(sm[:, 7:8], sm[:, 7:8], [K], [K])
                    self.tt("dve", sm[:, 8:9], sm[:, 7:8], sm[:, 6:7], ALU.mult, [K], [K])
                    self.tt("dve", sm[:, 7:8], sm[:, 7:8], sm[:, 3:4], ALU.mult, [K], [K])
                    self.tt("dve", sm[:, 8:9], sm[:, 8:9], sm[:, 3:4], ALU.mult, [K], [K])
                    self.ts("dve", g4[:, :], mk1[:, :], sm[:, 7:8], ALU.mult, [K], [K])
                    self.stt(g4[:, :], mk2[:, :], sm[:, 8:9], g4[:, :], ALU.mult, ALU.add, [K], [K])
                    self.tt("dve", g32[:, :].rearrange("p (g e) -> p g e", g=8), ohg[:, :].unsqueeze(2).to_broadcast([128, 8, 4]),
                            g4[:, :].unsqueeze(1).to_broadcast([128, 8, 4]), ALU.mult, [K], [K])
                    pt, ptk = self.ps[2 + tl % 2], "ps%d" % (2 + tl % 2)
                    self.tr(pt[0:32, 0:128], g32[:, :], [K], [ptk], f32=True)
                    self.cp("dve", gT[:, tl * 128:(tl + 1) * 128], pt[0:32, 0:128], [ptk], [gTk])
                c.dma("sp", self.gT_s[:, b * 512:(b + 1) * 512], gT[:, :], reads=[gTk], writes=["gT_s"])
            c.emit()
        SBT = 2048
        NSB = S // SBT
        TPS = SBT // 128
        with ExitStack() as st:
            acc = self.sb("p5_acc", [128, TPS, D], F32, st)
            for sbi in range(NSB):
                t0 = sbi * TPS
                with ExitStack() as st2:
                    xTs = self.sb("p5_xTs", [128, 8, SBT], BF16, st2)
                    Wg = [self.sb("p5_Wg%d" % i, [128, 8, 256], BF16, st2) for i in range(3)]
                    Wu = [self.sb("p5_Wu%d" % i, [128, 8, 256], BF16, st2) for i in range(3)]
                    Wd = [self.sb("p5_Wd%d" % i, [128, 2, D], BF16, st2) for i in range(3)]
                    gbt = [self.sb("p5_gbt%d" % i, [128, 512], F32, st2) for i in range(3)]
                    sg = [self.sb("p5_sg%d" % i, [128, 512], F32, st2) for i in range(2)]
                    tu = [self.sb("p5_tu%d" % i, [128, 512], F32, st2) for i in range(2)]
                    hid = [self.sb("p5_hid%d" % i, [128, 2, 512], BF16, st2) for i in range(2)]
                    for q in range(SBT // 512):
                        c.dma("sp", xTs[:, :, q * 512:(q + 1) * 512], self.xT_s[:, :, sbi * SBT + q * 512: sbi * SBT + (q + 1) * 512],
                              reads=["xT_s"], writes=["p5_xTs%d" % q])

                    def load_w(e):
                        i = e % 3
                        c.dma("pool", Wg[i][:, :, :], self.e_gate[l, e].rearrange("(k p) f -> p k f", p=128), writes=["p5_Wg%d" % i])
                        c.dma("pool", Wu[i][:, :, :], self.e_up[l, e].rearrange("(k p) f -> p k f", p=128), writes=["p5_Wu%d" % i])
                        c.dma("pool", Wd[i][:, :, :], self.e_down[l, e].rearrange("(k p) d -> p k d", p=128), writes=["p5_Wd%d" % i])

                    load_w(0)
                    load_w(1)
                    cnt = 0
                    ocnt = 0
                    for e in range(32):
                        if e + 2 < 32:
                            load_w(e + 2)
                        wi = e % 3
                        for tb in range(SBT // 512):
                            gi = cnt % 3
                            hi = cnt % 2
                            cnt += 1
                            tok0 = sbi * SBT + tb * 512
                            c.dma("sp", gbt[gi][:, :], self.gT_s[e, tok0:tok0 + 512].partition_broadcast(128), reads=["gT_s"], writes=["p5_gbt%d" % gi])
                            for fc in range(2):
                                pg, pgk = self.ps[fc], "ps%d" % fc
                                pu, puk = self.ps[2 + fc], "ps%d" % (2 + fc)
                                for k in range(8):
                                    self.mm(pg[:, :], Wg[wi][:, k, fc * 128:(fc + 1) * 128], xTs[:, k, tb * 512:(tb + 1) * 512], k == 0, k == 7,
                                            ["p5_Wg%d" % wi, "p5_xTs%d" % tb], [pgk])
                                for k in range(8):
                                    self.mm(pu[:, :], Wu[wi][:, k, fc * 128:(fc + 1) * 128], xTs[:, k, tb * 512:(tb + 1) * 512], k == 0, k == 7,
                                            ["p5_Wu%d" % wi, "p5_xTs%d" % tb], [puk])
                                self.act(sg[fc][:, :], pg[:, :], AF.Silu, [pgk], ["p5_sg%d" % fc])
                                self.tt("dve", tu[fc][:, :], sg[fc][:, :], pu[:, :], ALU.mult, ["p5_sg%d" % fc, puk], ["p5_tu%d" % fc])
                                self.tt("pool", hid[hi][:, fc, :], tu[fc][:, :], gbt[gi][:, :], ALU.mult, ["p5_tu%d" % fc, "p5_gbt%d" % gi], ["p5_hid%d" % hi])
                            for tl in range(4):
                                tile = tb * 4 + tl
                                for dh in range(2):
                                    po, pok = self.ps[4 + ocnt % 4], "ps%d" % (4 + ocnt % 4)
                                    ocnt += 1
                                    for fc in range(2):
                                        self.mm(po[:, :], hid[hi][:, fc, tl * 128:(tl + 1) * 128], Wd[wi][:, fc, dh * 512:(dh + 1) * 512], fc == 0, fc == 1,
                                                ["p5_hid%d" % hi, "p5_Wd%d" % wi], [pok])
                                    a = acc[:, tile, dh * 512:(dh + 1) * 512]
                                    ak = "p5_acc%d" % tile
                                    if e == 0:
                                        self.cp("dve", a, po[:, :], [pok], [ak])
                                    else:
                                        self.tt("dve", a, a, po[:, :], ALU.add, [pok, ak], [ak])
                    c.emit()
                with ExitStack() as st2:
                    g_t = self.sb("p5_g", [128, D], F32, st2)
                    b_t = self.sb("p5_b", [128, D], F32, st2)
                    c.dma("sp", g_t[:, :], self.ln_g[2][l, :].partition_broadcast(128), writes=["p5_gb"])
                    c.dma("sp", b_t[:, :], self.ln_b[2][l, :].partition_broadcast(128), writes=["p5_gb"])
                    tmp = {"st6": self.sb("p5_st6", [128, 12], F32, st2), "mv": self.sb("p5_mv", [128, 4], F32, st2)}
                    xr = [self.sb("p5_xr%d" % i, [128, D], F32, st2) for i in range(2)]
                    y = self.sb("p5_y", [128, D], F32, st2)
                    x3 = [self.sb("p5_x3_%d" % i, [128, D], F32, st2) for i in range(2)]
                    xb = self.sb("p5_xb", [128, D], BF16, st2)
                    x3T = [self.sb("p5_x3T%d" % i, [128, 8, 512], BF16, st2) for i in range(2)]
                    for tile in range(TPS):
                        t = t0 + tile
                        i = tile % 2
                        c.dma("sp", xr[i][:, :], self.xres_s[t * 128:(t + 1) * 128, :], reads=["xres_s%d" % t], writes=["p5_xr%d" % i])
                        ak = "p5_acc%d" % tile
                        self.ln_tile(y, "p5_y", xr[i], "p5_xr%d" % i, acc[:, tile, 0:512], ak, acc[:, tile, 512:1024], ak, g_t, b_t, "p5_gb",
                                     x3[i][:, :], "p5_x3_%d" % i, tmp)
                        dst = self.out if last else self.xres_s
                        c.dma("sp", dst[t * 128:(t + 1) * 128, :], x3[i][:, :], reads=["p5_x3_%d" % i], writes=["xres_s%d" % t])
                        if not last:
                            bq = tile // 4
                            self.to_xT(x3[i][:, :], "p5_x3_%d" % i, xb, "p5_xb", x3T[bq % 2], "p5_x3T%d" % (bq % 2),
                                       slice((tile % 4) * 128, (tile % 4 + 1) * 128), "dve")
                            if tile % 4 == 3:
                                tok0 = (t0 + bq * 4) * 128
                                c.dma("sp", self.xT_s[:, :, tok0:tok0 + 512], x3T[bq % 2][:, :, :], reads=["p5_x3T%d" % (bq % 2)], writes=["xT_s"])
                    c.emit()


_CACHE = {}


def _consts():
    ident = np.eye(128, dtype=np.float32)
    p = np.arange(128)[:, None]
    f = np.arange(128)[None, :]
    invs = []
    for rot in (32, 16, 8):
        invs.append((np.float32(ROPE_THETA) ** (-np.arange(0, rot, 2, dtype=np.float32) / np.float32(rot))).astype(np.float32))
    invf = np.tile(np.concatenate(invs)[None, :], (128, 1)).astype(np.float32)
    blk = np.zeros((16, S), dtype=np.float32)
    for j in range(16):
        blk[j, j * 256:(j + 1) * 256] = BIGV
    bf = ml_dtypes.bfloat16
    return {
        "c_ident_b": ident.astype(bf), "c_ident_f": ident,
        "c_mask_le": (p <= f).astype(np.float32).astype(bf),
        "c_mask_lt": (p < f).astype(np.float32).astype(bf),
        "c_tri": (p > f).astype(np.float32).astype(bf),
        "c_invf": invf, "c_blk": blk.astype(bf),
    }


def make_in_maps(inputs, n_cores=8):
    f = lambda a: np.ascontiguousarray(np.asarray(a))
    shared = {
        "w_in": f(np.asarray(inputs["w_in"])[:, :, COL_PERM]),
        "mla_q_norm": f(inputs["mla_q_norm"]), "w_uq": f(inputs["w_uq"]),
        "mla_kv_norm": f(inputs["mla_kv_norm"]), "w_ukv": f(inputs["w_ukv"]),
        "diff_lambda": f(np.asarray(inputs["diff_lambda"]).reshape(DEPTH, 128)),
        "diff_subln": f(inputs["diff_subln"]), "w_out": f(inputs["w_out"]),
        "ln_mix_g": f(inputs["ln_mix_g"]), "ln_mix_b": f(inputs["ln_mix_b"]),
        "ln_mem_g": f(inputs["ln_mem_g"]), "ln_mem_b": f(inputs["ln_mem_b"]),
        "ln_ffn_g": f(inputs["ln_ffn_g"]), "ln_ffn_b": f(inputs["ln_ffn_b"]),
        "xattn_wq": f(inputs["xattn_wq"]), "xattn_wk": f(inputs["xattn_wk"]),
        "xattn_wv": f(inputs["xattn_wv"]), "xattn_wo": f(inputs["xattn_wo"]),
        "w_router": f(np.concatenate([np.asarray(inputs["router_group_w"]), np.asarray(inputs["router_expert_w"])], axis=2)),
        "b_router": f(np.concatenate([np.asarray(inputs["router_group_b"]), np.asarray(inputs["router_expert_b"])], axis=1)),
        "expert_w_gate": f(inputs["expert_w_gate"]), "expert_w_up": f(inputs["expert_w_up"]),
        "expert_w_down": f(inputs["expert_w_down"]),
    }
    shared.update(_consts())
    maps = []
    x = np.asarray(inputs["x"])
    mem = np.asarray(inputs["mem"])
    pos = np.asarray(inputs["positions"])
    for b in range(n_cores):
        m = dict(shared)
        m["x"] = f(x[b])
        m["mem"] = f(mem[b])
        m["pos"] = f(pos[b].reshape(NT, 128).T.astype(np.int32))
        maps.append(m)
    return maps


def kernel(**inputs):
    if "nc" not in _CACHE:
        _CACHE["nc"] = Builder().build()
    nc = _CACHE["nc"]
    maps = make_in_maps(inputs)
    res = run_bass_kernel_spmd(nc, maps, core_ids=list(range(8)))
    return np.stack([r["out"] for r in res.results], axis=0).astype(np.float32)
```

```python
import math
import os
LV = int(os.environ.get('P1LV', '9'))
SUB = int(os.environ.get('P1SUB', '9'))
from contextlib import ExitStack

import numpy as np
import ml_dtypes
import concourse.bass as bass
import concourse.mybir as mybir
from concourse.bass_utils import run_bass_kernel_spmd

F32 = mybir.dt.float32
BF16 = mybir.dt.bfloat16
I32 = mybir.dt.int32
AF = mybir.ActivationFunctionType
ALU = mybir.AluOpType
AX = mybir.AxisListType

D = 1024
S = 4096
NT = S // 128
NB = S // 512
DEPTH = 4
N_MEM = 256
IN_COLS = 2656
ALPHA = (2 * DEPTH) ** 0.25
LN_EPS = 1e-5
RMS_EPS = 1e-6
ROPE_THETA = 500000.0
BIGV = 30000.0
TWO_PI = 2.0 * math.pi

O_SB = 352
O_MB = 352 + 768
O_DF = 352 + 1536
COL_PERM = np.concatenate([
    np.arange(0, 352),
    np.arange(O_SB, O_SB + 512),
    np.arange(O_SB + 512, O_SB + 768), np.arange(O_MB + 512, O_MB + 768),
    np.arange(O_MB, O_MB + 512),
    np.arange(O_DF, O_DF + 512),
    np.arange(O_DF + 512, O_DF + 768),
])
CH = [(0, 352), (352, 512), (864, 512), (1376, 512), (1888, 512), (2400, 256)]


class Ctx:
    def __init__(self, nc, stack, n_dma_sems=24):
        self.nc = nc
        self.eng = {"pe": nc.tensor, "act": nc.scalar, "dve": nc.vector, "pool": nc.gpsimd, "sp": nc.sync}
        self.sem = {}
        self.cnt = {}
        for e in self.eng:
            self.sem[e] = stack.enter_context(nc.semaphore("s_" + e))
            self.cnt[e] = 0
        self.dring = {}
        for q in ("sp", "pool"):
            sems = [stack.enter_context(nc.semaphore("d_%s_%d" % (q, i))) for i in range(n_dma_sems)]
            self.dring[q] = {"sems": sems, "cnt": [0] * n_dma_sems, "next": 0}
        self.waited = {e: {} for e in self.eng}
        self.last_w = {}
        self.readers = {}
        self.prog = {e: [] for e in self.eng}
        self.n_ins = 0

    def _wait(self, e, tok):
        sem, val, src = tok
        if src == e and e == "pe":
            return
        w = self.waited[e]
        if w.get(sem.name, 0) >= val:
            return
        w[sem.name] = val
        self.prog[e].append(("w", sem, val))

    def _deps(self, e, reads, writes):
        for k in reads:
            t = self.last_w.get(k)
            if t is not None:
                self._wait(e, t)
            if k.startswith("ps"):
                for t in self.readers.get(k, ()):
                    if t[2] != e:
                        self._wait(e, t)
        for k in writes:
            t = self.last_w.get(k)
            if t is not None:
                self._wait(e, t)
            for t in self.readers.get(k, ()):
                self._wait(e, t)

    def _commit(self, tok, reads, writes):
        for k in reads:
            lst = self.readers.setdefault(k, [])
            lst.append(tok)
            if len(lst) > 16:
                best = {}
                for t in lst:
                    if t[0].name not in best or best[t[0].name][1] < t[1]:
                        best[t[0].name] = t
                self.readers[k] = list(best.values())
        for k in writes:
            self.last_w[k] = tok
            self.readers[k] = []

    def op(self, e, fn, reads=(), writes=()):
        self._deps(e, reads, writes)
        self.cnt[e] += 1
        self.n_ins += 1
        self.prog[e].append(("i", fn, self.sem[e], 1))
        tok = (self.sem[e], self.cnt[e], e)
        self._commit(tok, reads, writes)
        return tok

    def dma(self, q, out, in_, reads=(), writes=(), **kw):
        ring = self.dring[q]
        i = ring["next"]
        ring["next"] = (i + 1) % len(ring["sems"])
        sem = ring["sems"][i]
        if ring["cnt"][i] > 0:
            self._wait(q, (sem, ring["cnt"][i], None))
        self._deps(q, reads, writes)
        self.prog[q].append(("d", out, in_, kw, sem))
        self.n_ins += 1
        ring["cnt"][i] += 16
        tok = (sem, ring["cnt"][i], None)
        self._commit(tok, reads, writes)
        return tok

    def wait_all(self, e):
        for x in self.eng:
            if self.cnt[x] > 0 and x != e:
                self._wait(e, (self.sem[x], self.cnt[x], x))
        for q, ring in self.dring.items():
            for sem, c in zip(ring["sems"], ring["cnt"]):
                if c > 0:
                    self._wait(e, (sem, c, None))

    def emit(self):
        nc = self.nc
        self.wait_all("sp")
        prog = self.prog
        self.prog = {e: [] for e in self.eng}

        def run(e, engobj):
            for it in prog[e]:
                if it[0] == "w":
                    engobj.wait_ge(it[1], it[2])
                elif it[0] == "i":
                    it[1]().then_inc(it[2], it[3])
                else:
                    engobj.dma_start(out=it[1], in_=it[2], **it[3]).then_inc(it[4], 16)

        with nc.Block() as block:
            @block.sync
            def _(eng):
                run("sp", eng)

            @block.scalar
            def _(eng):
                run("act", eng)

            @block.tensor
            def _(eng):
                run("pe", eng)

            @block.vector
            def _(eng):
                run("dve", eng)

            @block.gpsimd
            def _(eng):
                run("pool", eng)


class Builder:
    def __init__(self, n_layers=DEPTH, debug=False, stop_after=None):
        self.n_layers = n_layers
        self.debug = debug
        self.stop_after = stop_after
        self.nc = bass.Bass("TRN2", target_bir_lowering=False)
        self.dbg_names = []

    def mm(self, out, lhsT, rhs, start, stop, r, w):
        nc = self.nc
        self.c.op("pe", lambda: nc.tensor.matmul(out, lhsT=lhsT, rhs=rhs, start=start, stop=stop), r, w)

    def tr(self, out, in_, r, w, f32=False):
        nc = self.nc
        n = in_.shape[0]
        ident = (self.ident_f if f32 else self.ident_b)[0:n, 0:n]
        self.c.op("pe", lambda: nc.tensor.transpose(out, in_, ident), list(r) + ["const"], w)

    def act(self, out, in_, func, r, w, scale=1.0, bias=0.0, accum_out=None):
        nc = self.nc
        kw = {}
        if accum_out is not None:
            kw["accum_out"] = accum_out
        self.c.op("act", lambda: nc.scalar.activation(out=out, in_=in_, func=func, scale=scale, bias=bias, **kw), r, w)

    def tt(self, e, out, in0, in1, op, r, w):
        eng = self.c.eng[e]
        self.c.op(e, lambda: eng.tensor_tensor(out=out, in0=in0, in1=in1, op=op), r, w)

    def ts(self, e, out, in0, s1, op0, r, w, s2=None, op1=ALU.bypass, accum_out=None):
        eng = self.c.eng[e]
        kw = {}
        if accum_out is not None:
            kw["accum_out"] = accum_out
        self.c.op(e, lambda: eng.tensor_scalar(out=out, in0=in0, scalar1=s1, scalar2=s2, op0=op0, op1=op1, **kw), r, w)

    def stt(self, out, in0, scalar, in1, op0, op1, r, w):
        nc = self.nc
        self.c.op("dve", lambda: nc.vector.scalar_tensor_tensor(out=out, in0=in0, scalar=scalar, in1=in1, op0=op0, op1=op1), r, w)

    def cp(self, e, out, in_, r, w):
        if e == "act":
            nc = self.nc
            self.c.op("act", lambda: nc.scalar.copy(out=out, in_=in_), r, w)
        else:
            eng = self.c.eng[e]
            self.c.op(e, lambda: eng.tensor_copy(out=out, in_=in_), r, w)

    def ms(self, e, ap, val, w):
        eng = self.c.eng[e]
        self.c.op(e, lambda: eng.memset(ap, val), (), w)

    def recip(self, out, in_, r, w):
        nc = self.nc
        self.c.op("dve", lambda: nc.vector.reciprocal(out=out, in_=in_), r, w)

    def red(self, out, in_, op, r, w, axis=AX.X):
        nc = self.nc
        self.c.op("dve", lambda: nc.vector.tensor_reduce(out=out, in_=in_, axis=axis, op=op), r, w)

    def sb(self, name, shape, dt, st=None):
        self.uid = getattr(self, "uid", 0) + 1
        return (st or self.st).enter_context(self.nc.sbuf_tensor("%s_u%d" % (name, self.uid), list(shape), dt))

    def scr(self, name, shape, dt):
        kind = "ExternalOutput" if self.debug else "Internal"
        if self.debug:
            self.dbg_names.append(name)
        return self.nc.dram_tensor(name, list(shape), dt, kind=kind).ap()

    def build(self):
        nc = self.nc
        L = DEPTH
        inp = lambda n, s, dt=F32: nc.dram_tensor(n, list(s), dt, kind="ExternalInput").ap()
        self.x_in = inp("x", [S, D])
        self.mem_in = inp("mem", [N_MEM, D])
        self.pos_in = inp("pos", [128, NT], I32)
        self.w_in = inp("w_in", [L, D, IN_COLS])
        self.mla_q_norm = inp("mla_q_norm", [L, 192])
        self.w_uq = inp("w_uq", [L, 192, 384])
        self.mla_kv_norm = inp("mla_kv_norm", [L, 128])
        self.w_ukv = inp("w_ukv", [L, 128, 512])
        self.diff_lambda = inp("diff_lambda", [L, 128])
        self.diff_subln = inp("diff_subln", [L, 64])
        self.w_out = inp("w_out", [L, D, D])
        self.ln_g = [inp(n, [L, D]) for n in ("ln_mix_g", "ln_mem_g", "ln_ffn_g")]
        self.ln_b = [inp(n, [L, D]) for n in ("ln_mix_b", "ln_mem_b", "ln_ffn_b")]
        self.wq = inp("xattn_wq", [L, D, D])
        self.wk = inp("xattn_wk", [L, D, D])
        self.wv = inp("xattn_wv", [L, D, D])
        self.wo = inp("xattn_wo", [L, D, D])
        self.w_router = inp("w_router", [L, D, 40])
        self.b_router = inp("b_router", [L, 40])
        self.e_gate = inp("expert_w_gate", [L, 32, D, 256])
        self.e_up = inp("expert_w_up", [L, 32, D, 256])
        self.e_down = inp("expert_w_down", [L, 32, 256, D])
        self.c_ident_b = inp("c_ident_b", [128, 128], BF16)
        self.c_ident_f = inp("c_ident_f", [128, 128], F32)
        self.c_mask_le = inp("c_mask_le", [128, 128], BF16)
        self.c_mask_lt = inp("c_mask_lt", [128, 128], BF16)
        self.c_tri = inp("c_tri", [128, 128], BF16)
        self.c_invf = inp("c_invf", [128, 28], F32)
        self.c_blk = inp("c_blk", [16, S], BF16)
        self.out = nc.dram_tensor("out", [S, D], F32, kind="ExternalOutput").ap()
        self.xT_s = self.scr("xT_s", [128, 8, S], BF16)
        self.xres_s = self.scr("xres_s", [S, D], F32)
        self.qk_s = self.scr("qk_s", [4, 4, 2, 96, S], BF16)
        self.v_s = self.scr("v_s", [S, 4 * 260], BF16)
        self.qkp_s = self.scr("qkp_s", [3, 2, 2, 128, S], BF16)
        self.mb_s = self.scr("mb_s", [64, S], BF16)
        self.o_s = self.scr("o_s", [S, D], BF16)
        self.od_s = self.scr("od_s", [2, S, 256], F32)
        self.gT_s = self.scr("gT_s", [32, S], F32)
        self.x1_s = self.scr("x1_s", [S, D], F32)

        with ExitStack() as st:
            self.st = st
            self.c = Ctx(nc, st)
            self.ps = [st.enter_context(nc.psum_tensor("ps%d" % i, [128, 512], F32)) for i in range(8)]
            self.ident_b = self.sb("ident_b", [128, 128], BF16)
            self.ident_f = self.sb("ident_f", [128, 128], F32)
            self.mask_le = self.sb("mask_le", [128, 128], BF16)
            self.mask_lt = self.sb("mask_lt", [128, 128], BF16)
            self.tri = self.sb("tri", [128, 128], BF16)
            self.ones_b = self.sb("ones_b", [128, 128], BF16)
            self.cs = self.sb("rope_cos", [128, NT, 28], F32)
            self.sn = self.sb("rope_sin", [128, NT, 28], F32)
            self.memT = self.sb("memT", [128, 8, N_MEM], BF16)
            self.phase0()
            for l in range(self.n_layers if self.stop_after != (0, 0) else 0):
                last = (l == DEPTH - 1)
                self.phase1(l)
                if self.stop_after == (l, 1):
                    break
                self.phase2(l)
                if self.stop_after == (l, 2):
                    break
                self.phase34(l)
                if self.stop_after == (l, 3):
                    break
                self.phase5(l, last)
                if self.stop_after == (l, 5):
                    break
        return nc

    def phase0(self):
        nc, c = self.nc, self.c
        with ExitStack() as st:
            for dst, src, k in ((self.ident_b, self.c_ident_b, "const"), (self.ident_f, self.c_ident_f, "const"),
                                (self.mask_le, self.c_mask_le, "const"), (self.mask_lt, self.c_mask_lt, "const"),
                                (self.tri, self.c_tri, "const")):
                c.dma("sp", dst[:, :], src, writes=[k])
            self.ms("pool", self.ones_b[:, :], 1.0, ["const"])
            pos_i = self.sb("pos_i", [128, NT], I32, st)
            pos_f = self.sb("pos_f", [128, NT], F32, st)
            invf = self.sb("invf", [128, 28], F32, st)
            ang = self.sb("ang", [128, NT, 28], F32, st)
            t1 = self.sb("rp_t1", [128, NT, 28], F32, st)
            ki = self.sb("rp_ki", [128, NT, 28], I32, st)
            kf = self.sb("rp_kf", [128, NT, 28], F32, st)
            r = self.sb("rp_r", [128, NT, 28], F32, st)
            m = self.sb("rp_m", [128, NT, 28], F32, st)
            rc = self.sb("rp_rc", [128, NT, 28], F32, st)
            c.dma("sp", pos_i[:, :], self.pos_in, writes=["pos_i"])
            c.dma("sp", invf[:, :], self.c_invf, writes=["invf"])
            self.cp("dve", pos_f[:, :], pos_i[:, :], ["pos_i"], ["pos_f"])
            self.tt("dve", ang[:, :, :], pos_f[:, :].unsqueeze(2).to_broadcast([128, NT, 28]),
                    invf[:, :].unsqueeze(1).to_broadcast([128, NT, 28]), ALU.mult, ["pos_f", "invf"], ["ang"])
            self.ts("dve", t1[:, :, :], ang[:, :, :], 1.0 / TWO_PI, ALU.mult, ["ang"], ["rp_t1"])
            self.cp("dve", ki[:, :, :], t1[:, :, :], ["rp_t1"], ["rp_ki"])
            self.cp("dve", kf[:, :, :], ki[:, :, :], ["rp_ki"], ["rp_kf"])
            HI = 6.28125
            LO = TWO_PI - HI
            self.stt(r[:, :, :], kf[:, :, :], -HI, ang[:, :, :], ALU.mult, ALU.add, ["rp_kf", "ang"], ["rp_r"])
            self.stt(r[:, :, :], kf[:, :, :], -LO, r[:, :, :], ALU.mult, ALU.add, ["rp_kf", "rp_r"], ["rp_r"])

            def wrap(buf, key):
                self.ts("dve", m[:, :, :], buf[:, :, :], math.pi, ALU.is_gt, [key], ["rp_m"])
                self.stt(buf[:, :, :], m[:, :, :], -TWO_PI, buf[:, :, :], ALU.mult, ALU.add, ["rp_m", key], [key])
                self.ts("dve", m[:, :, :], buf[:, :, :], -math.pi, ALU.is_lt, [key], ["rp_m"])
                self.stt(buf[:, :, :], m[:, :, :], TWO_PI, buf[:, :, :], ALU.mult, ALU.add, ["rp_m", key], [key])
                self.ts("dve", buf[:, :, :], buf[:, :, :], math.pi, ALU.min, [key], [key], s2=-math.pi, op1=ALU.max)

            wrap(r, "rp_r")
            self.ts("dve", rc[:, :, :], r[:, :, :], math.pi / 2, ALU.add, ["rp_r"], ["rp_rc"])
            wrap(rc, "rp_rc")
            self.act(self.sn[:, :, :], r[:, :, :], AF.Sin, ["rp_r"], ["rope"])
            self.act(self.cs[:, :, :], rc[:, :, :], AF.Sin, ["rp_rc"], ["rope"])
            memb = self.sb("memb", [128, 2, D], BF16, st)
            c.dma("pool", memb[:, :, :], self.mem_in.rearrange("(t p) d -> p t d", p=128), writes=["memb"])
            for t in range(2):
                pst = self.ps[t][:, :].bitcast(BF16)
                for k in range(8):
                    self.tr(pst[:, k * 128:(k + 1) * 128], memb[:, t, k * 128:(k + 1) * 128], ["memb"], ["ps%d" % t])
                self.cp("dve", self.memT[:, :, t * 128:(t + 1) * 128],
                        pst.rearrange("p (k m) -> p k m", k=8), ["ps%d" % t], ["memT"])
            xb = [self.sb("p0_xb%d" % i, [128, D], BF16, st) for i in range(2)]
            xt = [self.sb("p0_xt%d" % i, [128, 8, 128], BF16, st) for i in range(2)]
            for t in range(NT):
                i = t % 2
                c.dma("pool", xb[i][:, :], self.x_in[t * 128:(t + 1) * 128, :], writes=["p0_xb%d" % i])
                pst = self.ps[2 + i][:, :].bitcast(BF16)
                for k in range(8):
                    self.tr(pst[:, k * 128:(k + 1) * 128], xb[i][:, k * 128:(k + 1) * 128], ["p0_xb%d" % i], ["ps%d" % (2 + i)])
                self.cp("dve" if i == 0 else "act", xt[i][:, :, :], pst.rearrange("p (k m) -> p k m", k=8),
                        ["ps%d" % (2 + i)], ["p0_xt%d" % i])
                c.dma("sp", self.xT_s[:, :, t * 128:(t + 1) * 128], xt[i][:, :, :], reads=["p0_xt%d" % i], writes=["xT_s"])
            c.emit()

    def rope(self, e, out1, out2, a, b, cos, sin, tmp1, tmp2, rk, wk_):
        k1, k2 = "rtmp1" + e, "rtmp2" + e
        self.tt(e, tmp1, a, cos, ALU.mult, rk + ["rope"], [k1])
        self.tt(e, tmp2, b, sin, ALU.mult, rk + ["rope"], [k2])
        self.tt(e, out1, tmp1, tmp2, ALU.subtract, [k1, k2], wk_)
        self.tt(e, tmp1, a, sin, ALU.mult, rk + ["rope"], [k1])
        self.tt(e, tmp2, b, cos, ALU.mult, rk + ["rope"], [k2])
        self.tt(e, out2, tmp1, tmp2, ALU.add, [k1, k2], wk_)

    def phase1(self, l):
        nc, c = self.nc, self.c
        with ExitStack() as st:
            win = self.sb("p1_win", [128, 8, IN_COLS], BF16, st)
            wuq = self.sb("p1_wuq", [128, 2, 384], BF16, st)
            wukv = self.sb("p1_wukv", [128, 512], BF16, st)
            gq = self.sb("p1_gq", [128, 2], F32, st)
            gkv = self.sb("p1_gkv", [128, 1], F32, st)
            for k in range(8):
                c.dma("pool", win[:, k, :], self.w_in[l, k * 128:(k + 1) * 128, :], writes=["p1_win%d" % k])
            wuq_f = self.sb("p1_wuq_f", [128, 2, 384], F32, st)
            wukv_f = self.sb("p1_wukv_f", [128, 512], F32, st)
            c.dma("sp", wuq_f[:, 0, :], self.w_uq[l, 0:128, :], writes=["p1_wuq_f"])
            c.dma("sp", wuq_f[0:64, 1, :], self.w_uq[l, 128:192, :], writes=["p1_wuq_f"])
            c.dma("sp", wukv_f[:, :], self.w_ukv[l, :, :], writes=["p1_wukv_f"])
            c.dma("sp", gq[:, 0:1], self.mla_q_norm[l, 0:128].rearrange("(p o) -> p o", o=1), writes=["p1_gq"])
            c.dma("sp", gq[0:64, 1:2], self.mla_q_norm[l, 128:192].rearrange("(p o) -> p o", o=1), writes=["p1_gq"])
            c.dma("sp", gkv[:, 0:1], self.mla_kv_norm[l, :].rearrange("(p o) -> p o", o=1), writes=["p1_gkv"])
            self.ts("dve", wuq[:, 0, :], wuq_f[:, 0, :], gq[:, 0:1], ALU.mult, ["p1_wuq_f", "p1_gq"], ["p1_wuq"])
            self.ts("dve", wuq[0:64, 1, :], wuq_f[0:64, 1, :], gq[0:64, 1:2], ALU.mult, ["p1_wuq_f", "p1_gq"], ["p1_wuq"])
            self.ts("dve", wukv[:, :], wukv_f[:, :], gkv[:, 0:1], ALU.mult, ["p1_wukv_f", "p1_gkv"], ["p1_wukv"])

            xT = [self.sb("p1_xT%d" % i, [128, 8, 512], BF16, st) for i in range(2)]
            c0 = [self.sb("p1_c0_%d" % i, [128, 352], BF16, st) for i in range(2)]
            ssq = [self.sb("p1_ssq%d" % i, [128, 4], F32, st) for i in range(2)]
            junk = self.sb("p1_junk", [128, 512], F32, st)
            qkS = [self.sb("p1_qkS%d" % i, [128, 512], BF16, st) for i in range(2)]
            qkM = [self.sb("p1_qkM%d" % i, [128, 512], BF16, st) for i in range(2)]
            qkD = [self.sb("p1_qkD%d" % i, [128, 512], BF16, st) for i in range(2)]
            qA = [self.sb("p1_qA%d" % i, [128, 4, 96], BF16, st) for i in range(2)]
            kA = [self.sb("p1_kA%d" % i, [128, 4, 96], BF16, st) for i in range(2)]
            vall = [self.sb("p1_vall%d" % i, [128, 4, 4, 65], BF16, st) for i in range(2)]
            vst = [[vall[i][:, m, :, :] for i in range(2)] for m in range(4)]
            latT = [self.sb("p1_latT%d" % i, [128, 3, 128], BF16, st) for i in range(2)]
            rt1 = self.sb("p1_rt1", [128, 128], F32, st)
            rt2 = self.sb("p1_rt2", [128, 128], F32, st)
            rt5 = self.sb("p1_rt5", [128, 16], F32, st)
            rt6 = self.sb("p1_rt6", [128, 16], F32, st)
            rt3 = self.sb("p1_rt3", [128, 128], F32, st)
            rt4 = self.sb("p1_rt4", [128, 128], F32, st)
            hf = [self.sb("p1_hf%d" % i, [128, 512], F32, st) for i in range(2)]
            kpe = [self.sb("p1_kpe%d" % i, [128, 32], F32, st) for i in range(2)]
            TA = [self.sb("p1_TA%d" % bp, [96, 4, 2, 512], BF16, st) for bp in range(2)]
            TP = [{nm: self.sb("p1_TP%d%s" % (bp, nm), [128, 2, 2, 512], BF16, st) for nm in ("S", "M", "D")} for bp in range(2)]
            mbT = [self.sb("p1_mbT%d" % bp, [64, 512], BF16, st) for bp in range(2)]
            kmT = self.sb("p1_kmT", [128, 2, 16], BF16, st)
            kpart = self.sb("p1_kpart", [128, 2, 2], F32, st)
            ksum = self.sb("p1_ksum", [128, 2], F32, st)
            gate = self.sb("p1_gate", [128, 4, 16], F32, st)
            top8 = self.sb("p1_top8", [128, 4, 8], F32, st)
            mb = [self.sb("p1_mb%d" % i, [128, 4, 16], BF16, st) for i in range(2)]
            for m in range(4):
                for i in range(2):
                    self.ms("pool", vst[m][i][:, :, 64:65], 1.0, ["p1_v%d_%d" % (m, i)])
            self.ms("pool", kmT[:, :, :], 0.0, ["p1_kmT"])

            def load_x(b):
                c.dma("sp", xT[b % 2][:, :, :], self.xT_s[:, :, b * 512:(b + 1) * 512], reads=["xT_s"], writes=["p1_xT%d" % (b % 2)])

            psi = [0]

            def next_ps():
                i = psi[0] % 3
                psi[0] += 1
                return self.ps[i], "ps%d" % i

            tbank = [0]

            def next_tbank():
                tbank[0] += 1
                return 3 if tbank[0] % 2 == 0 else 7

            def tk(nm, bp, role, j):
                return "p1_T%d%s%d_%d" % (bp, nm, role, j)

            def mm_chunk(t, ci):
                b, tl = t // 4, t % 4
                c0_, cw = CH[ci]
                ps, pk = next_ps()
                for k in range(8):
                    self.mm(ps[:, 0:cw], xT[b % 2][:, k, tl * 128:(tl + 1) * 128], win[:, k, c0_:c0_ + cw],
                            k == 0, k == 7, ["p1_xT%d" % (b % 2), "p1_win%d" % k], [pk])
                return ps, pk

            def chunk0(t):
                i = t % 2
                ps, pk = mm_chunk(t, 0)
                cosA, sinA = self.cs[:, t, 0:16], self.sn[:, t, 0:16]
                ck = "p1_c0_%d" % i
                sk = "p1_ssq%d" % i
                self.cp("act", c0[i][:, 0:352], ps[:, 0:352], [pk], [ck])
                self.act(junk[:, 0:192], ps[:, 0:192], AF.Square, [pk], ["p1_junk", sk], accum_out=ssq[i][:, 0:1])
                self.act(junk[:, 192:320], ps[:, 192:320], AF.Square, [pk], ["p1_junk", sk], accum_out=ssq[i][:, 1:2])
                self.cp("pool", hf[i][:, 0:32], c0[i][:, 320:352], [ck], ["p1_hfa%d" % i])
                self.rope("pool", kpe[i][:, 0:16], kpe[i][:, 16:32], hf[i][:, 0:16], hf[i][:, 16:32], cosA, sinA,
                          rt5[:, 0:16], rt6[:, 0:16], ["p1_hfa%d" % i], ["p1_kpe%d" % i])
                self.ts("dve", ssq[i][:, 2:3], ssq[i][:, 0:1], 1.0 / 192, ALU.mult, [sk], [sk], s2=RMS_EPS, op1=ALU.add)
                self.ts("dve", ssq[i][:, 3:4], ssq[i][:, 1:2], 1.0 / 128, ALU.mult, [sk], [sk], s2=RMS_EPS, op1=ALU.add)
                self.act(ssq[i][:, 2:4], ssq[i][:, 2:4], AF.Ln, [sk], [sk])
                self.act(ssq[i][:, 2:4], ssq[i][:, 2:4], AF.Exp, [sk], [sk], scale=-0.5)

            def mla_up(t):
                i = t % 2
                cosA, sinA = self.cs[:, t, 0:16], self.sn[:, t, 0:16]
                ck, sk, lk = "p1_c0_%d" % i, "p1_ssq%d" % i, "p1_latT%d" % i
                pt, ptk = self.ps[4], "ps4"
                ptb = pt[:, :].bitcast(BF16)
                self.tr(ptb[:, 0:128], c0[i][:, 0:128], [ck], [ptk])
                self.tr(ptb[0:64, 128:256], c0[i][:, 128:192], [ck], [ptk])
                self.tr(ptb[:, 256:384], c0[i][:, 192:320], [ck], [ptk])
                self.cp("dve", latT[i][:, 0:3:2, :], ptb[:, 0:384].rearrange("p (a m) -> p a m", a=3)[:, 0:3:2, :], [ptk], [lk])
                self.cp("dve", latT[i][0:64, 1, :], ptb[0:64, 128:256], [ptk], [lk])
                pq, pqk = self.ps[5], "ps5"
                self.mm(pq[:, 0:384], latT[i][:, 0, :], wuq[:, 0, :], True, False, [lk, "p1_wuq"], [pqk])
                self.mm(pq[:, 0:384], latT[i][0:64, 1, :], wuq[0:64, 1, :], False, True, [lk, "p1_wuq"], [pqk])
                pkv, pkvk = self.ps[6], "ps6"
                self.mm(pkv[:, :], latT[i][:, 2, :], wukv[:, :], True, True, [lk, "p1_wukv"], [pkvk])
                pq3 = pq[:, 0:384].rearrange("p (h d) -> p h d", h=4)
                qk_ = "p1_qA%d" % i
                self.ts("dve", qA[i][:, :, 0:64], pq3[:, :, 0:64], ssq[i][:, 2:3], ALU.mult, [pqk, sk], [qk_])
                hk = "p1_hfb%d" % i
                h3 = hf[i][:, 64:192].rearrange("p (h d) -> p h d", h=4)
                self.ts("dve", h3, pq3[:, :, 64:96], ssq[i][:, 2:3], ALU.mult, [pqk, sk], [hk])
                cb = cosA.unsqueeze(1).to_broadcast([128, 4, 16])
                sb_ = sinA.unsqueeze(1).to_broadcast([128, 4, 16])
                self.rope("dve", qA[i][:, :, 64:80], qA[i][:, :, 80:96], h3[:, :, 0:16], h3[:, :, 16:32], cb, sb_,
                          rt1[:, 0:64].rearrange("p (h d) -> p h d", h=4), rt2[:, 0:64].rearrange("p (h d) -> p h d", h=4),
                          [hk], [qk_])
                pkv3 = pkv[:, :].rearrange("p (h d) -> p h d", h=4)
                kk_ = "p1_kA%d" % i
                self.ts("dve", kA[i][:, :, 0:64], pkv3[:, :, 0:64], ssq[i][:, 3:4], ALU.mult, [pkvk, sk], [kk_])
                self.ts("dve", vst[0][i][:, :, 0:64], pkv3[:, :, 64:128], ssq[i][:, 3:4], ALU.mult, [pkvk, sk], ["p1_v0_%d" % i])
                self.cp("pool", kA[i][:, :, 64:96], kpe[i][:, :].unsqueeze(1).to_broadcast([128, 4, 32]), ["p1_kpe%d" % i], [kk_])

            def chunk_plain(t, ci):
                i = t % 2
                ps, pk = mm_chunk(t, ci)
                if ci == 1:
                    self.cp("act", qkS[i][:, :], ps[:, :], [pk], ["p1_qkS%d" % i])
                elif ci == 2:
                    self.cp("act", vst[1][i][:, :, 0:64], ps[:, 0:256].rearrange("p (h d) -> p h d", h=4), [pk], ["p1_v1_%d" % i])
                    self.cp("act", vst[2][i][:, :, 0:64], ps[:, 256:512].rearrange("p (h d) -> p h d", h=4), [pk], ["p1_v2_%d" % i])
                else:
                    self.cp("act", vst[3][i][:, :, 0:64], ps[:, 0:256].rearrange("p (h d) -> p h d", h=4), [pk], ["p1_v3_%d" % i])

            def chunk_rope(t, ci):
                i = t % 2
                ps, pk = mm_chunk(t, ci)
                dst = (qkM if ci == 3 else qkD)[i]
                dk = ("p1_qkM%d" if ci == 3 else "p1_qkD%d") % i
                hk = "p1_hfc%d_%d" % (ci, i)
                self.cp("dve", dst[:, :], ps[:, :], [pk], [dk])
                if ci == 3:
                    g, nr = 8, 8
                    cb, sb_ = self.cs[:, t, 16:24], self.sn[:, t, 16:24]
                    hbase = 192
                else:
                    g, nr = 16, 4
                    cb, sb_ = self.cs[:, t, 24:28], self.sn[:, t, 24:28]
                    hbase = 320
                p3 = ps[:, :].rearrange("p (g d) -> p g d", g=g)
                d3 = dst[:, :].rearrange("p (g d) -> p g d", g=g)
                cbb = cb.unsqueeze(1).to_broadcast([128, g, nr])
                sbb = sb_.unsqueeze(1).to_broadcast([128, g, nr])
                h3 = hf[i][:, hbase:hbase + g * 2 * nr].rearrange("p (g d) -> p g d", g=g)
                self.cp("dve", h3, p3[:, :, 0:2 * nr], [pk], [hk])
                self.rope("pool", d3[:, :, 0:nr], d3[:, :, nr:2 * nr], h3[:, :, 0:nr], h3[:, :, nr:2 * nr], cbb, sbb,
                          rt3[:, 0:g * nr].rearrange("p (g d) -> p g d", g=g), rt4[:, 0:g * nr].rearrange("p (g d) -> p g d", g=g),
                          [hk], [dk])

            def v_store(t):
                i = t % 2
                c.dma("pool", self.v_s[t * 128:(t + 1) * 128, :], vall[i][:, :, :, :].rearrange("p m h d -> p (m h d)"),
                      reads=["p1_v%d_%d" % (m, i) for m in range(4)], writes=["v_s"])

            def tr_mla(t):
                i, bp = t % 2, (t // 4) % 2
                cols = slice((t % 4) * 128, (t % 4 + 1) * 128)
                for role, src, sk in ((0, qA[i], "p1_qA%d" % i), (1, kA[i], "p1_kA%d" % i)):
                    tb_ = next_tbank()
                    ptb = self.ps[tb_][:, :].bitcast(BF16)
                    for h in range(4):
                        self.tr(ptb[0:96, h * 128:(h + 1) * 128], src[:, h, :], [sk], ["ps%d" % tb_])
                    for h in range(4):
                        self.cp("act" if role == 0 else "dve", TA[bp][:, h, role, cols], ptb[0:96, h * 128:(h + 1) * 128], ["ps%d" % tb_],
                                ["p1_TA%d" % bp])

            def tr_pairs(t, nm):
                i, bp = t % 2, (t // 4) % 2
                cols = slice((t % 4) * 128, (t % 4 + 1) * 128)
                src, sk = {"S": (qkS[i], "p1_qkS%d" % i), "M": (qkM[i], "p1_qkM%d" % i), "D": (qkD[i], "p1_qkD%d" % i)}[nm]
                tb_ = next_tbank()
                ptb = self.ps[tb_][:, :].bitcast(BF16)
                for j in range(4):
                    self.tr(ptb[:, j * 128:(j + 1) * 128], src[:, j * 128:(j + 1) * 128], [sk], ["ps%d" % tb_])
                for j in range(4):
                    role, pr = j // 2, j % 2
                    self.cp("act" if nm != "D" else "dve", TP[bp][nm][:, role, pr, cols], ptb[:, j * 128:(j + 1) * 128], ["ps%d" % tb_],
                            ["p1_TP%d%s" % (bp, nm)])

            def gating(t):
                i, bp = t % 2, (t // 4) % 2
                cols = slice((t % 4) * 128, (t % 4 + 1) * 128)
                blk = t // 2
                pg, pgk = self.ps[6], "ps6"
                for pr in range(2):
                    self.mm(pg[:, pr:pr + 1], qkM[i][:, 256 + pr * 128:256 + (pr + 1) * 128], self.ones_b[:, 0:1], True, True,
                            ["p1_qkM%d" % i, "const"], [pgk])
                self.cp("dve", kpart[:, t % 2, :], pg[:, 0:2], [pgk], ["p1_kpart"])
                if t % 2 == 1:
                    self.tt("dve", ksum[:, :], kpart[:, 0, :], kpart[:, 1, :], ALU.add, ["p1_kpart"], ["p1_ksum"])
                    self.ts("dve", kmT[:, :, blk], ksum[:, :], 1.0 / 256, ALU.mult, ["p1_ksum"], ["p1_kmT"])
                own = blk
                mk = "p1_mb%d" % i
                if own >= 4:
                    g3 = gate[:, :, :].rearrange("p (a b) j -> p a b j", b=2)
                    self.ms("pool", gate[:, :, :], -BIGV, ["p1_gate"])
                    for hh, (pg, pgk) in enumerate(((self.ps[5], "ps5"), (self.ps[6], "ps6"))):
                        p0 = hh * 64
                        for pr in range(2):
                            qT = TP[bp]["M"][p0:p0 + 64, 0, pr, cols]
                            self.mm(pg[:, pr * 16:(pr + 1) * 16], qT, kmT[p0:p0 + 64, pr, :],
                                    True, True, ["p1_TP%dM" % bp, "p1_kmT"], [pgk])
                        self.cp("dve", g3[:, :, hh, 0:own], pg[:, 0:32].rearrange("p (a j) -> p a j", a=2)[:, :, 0:own], [pgk], ["p1_gate"])
                    for h in range(4):
                        nc_ = self.nc
                        gh = gate[:, h, :]
                        th = top8[:, h, :]
                        c.op("dve", (lambda gh=gh, th=th: nc_.vector.max(out=th, in_=gh)), ["p1_gate"], ["p1_top8"])
                    for h in range(4):
                        self.ts("dve", mb[i][:, h, :], gate[:, h, :], top8[:, h, 2:3], ALU.is_ge, ["p1_gate", "p1_top8"], [mk],
                                s2=-1.0, op1=ALU.add)
                    self.ms("pool", mb[i][:, :, own:own + 1], 0.0, [mk])
                else:
                    self.ms("pool", mb[i][:, :, :], 0.0, [mk])
                pm, pmk = self.ps[4], "ps4"
                pmb = pm[:, :].bitcast(BF16)
                self.tr(pmb[0:64, 0:128], mb[i][:, :, :].rearrange("p h j -> p (h j)"), [mk], [pmk])
                self.cp("dve", mbT[bp][:, cols], pmb[0:64, 0:128], [pmk], ["p1_mbT%d" % bp])

            def block_store(b):
                bp = b % 2
                bc = slice(b * 512, (b + 1) * 512)
                c.dma("sp", self.qk_s[0, :, :, 0:96, bc].rearrange("h r p c -> p h r c"), TA[bp][:, :, :, :], reads=["p1_TA%d" % bp], writes=["qk_s"])
                for mi, nm in ((0, "S"), (1, "M"), (2, "D")):
                    c.dma("sp", self.qkp_s[mi, :, :, :, bc].rearrange("r q p c -> p r q c"), TP[bp][nm][:, :, :, :],
                          reads=["p1_TP%d%s" % (bp, nm)], writes=["qk_s"])
                c.dma("sp", self.mb_s[:, bc], mbT[bp][:, :], reads=["p1_mbT%d" % bp], writes=["qk_s"])

            load_x(0)
            for t in range(NT + 1):
                cur = t if t < NT else None
                prv = t - 1 if t >= 1 else None
                if cur is not None and cur % 4 == 0 and cur // 4 + 1 < NB:
                    load_x(cur // 4 + 1)
                if cur is not None:
                    chunk0(cur)
                    chunk_plain(cur, 1)
                if prv is not None:
                    tr_mla(prv)
                if cur is not None:
                    chunk_plain(cur, 2)
                    mla_up(cur)
                    chunk_rope(cur, 3)
                if prv is not None:
                    tr_pairs(prv, "S")
                    tr_pairs(prv, "M")
                if cur is not None:
                    chunk_rope(cur, 4)
                if prv is not None:
                    tr_pairs(prv, "D")
                if cur is not None:
                    chunk_plain(cur, 5)
                    v_store(cur)
                if prv is not None:
                    gating(prv)
                    if prv % 4 == 3:
                        block_store(prv // 4)
            for h in range(4):
                c.dma("sp", self.qk_s[2, h, 1, 64:80, :], self.c_blk, writes=["qk_s"])
            c.emit()


    def phase2(self, l):
        nc, c = self.nc, self.c
        lambda_init = 0.8 - 0.6 * math.exp(-0.3 * l)
        with ExitStack() as st:
            KT = [self.sb("p2_KT%d" % i, [128, S], BF16, st) for i in range(2)]
            QT = [self.sb("p2_QT%d" % i, [128, S], BF16, st) for i in range(2)]
            Vm = [self.sb("p2_Vm%d" % i, [128, NT, 260], BF16, st) for i in range(2)]
            om = [self.sb("p2_om%d" % i, [128, NT, 256], BF16, st) for i in range(2)]
            od = [self.sb("p2_od%d" % i, [128, NT, 64], F32, st) for i in range(2)]
            PT = [self.sb("p2_PT%d" % i, [128, 512], BF16, st) for i in range(4)]
            SBANK = [0, 1, 2, 7]
            E32 = [self.sb("p2_E%d" % i, [128, 512], F32, st) for i in range(3)]
            Psp = [self.sb("p2_P%d" % i, [128, 512], BF16, st) for i in range(3)]
            T = [self.sb("p2_T%d" % i, [128, 512], F32, st) for i in range(3)]
            Aex = [self.sb("p2_A%d" % i, [128, 512], BF16, st) for i in range(3)]
            Run = self.sb("p2_Run", [128, 512], BF16, st)
            rc = self.sb("p2_rc", [128, 4], F32, st)
            hcount = [0]
            vcount = [0]

            def load_head(m, h, rows, r0=0):
                j = hcount[0] % 2
                hcount[0] += 1
                for buf, bk in ((KT[j], "p2_KT%d" % j), (QT[j], "p2_QT%d" % j)):
                    if rows <= 32:
                        self.ms("pool", buf[32:64, :], 0.0, [bk])
                    self.ms("pool", buf[64:128, :], 0.0, [bk])
                for role, buf, bk in ((1, KT[j], "p2_KT%d" % j), (0, QT[j], "p2_QT%d" % j)):
                    if m == 0:
                        src = self.qk_s[0, h, role, 0:rows, :]
                    else:
                        rb = (h % 2) * 64 + r0
                        src = self.qkp_s[m - 1, role, h // 2, rb:rb + min(rows, 64), :]
                    c.dma("sp", buf[0:min(rows, 96 if m == 0 else 64), :], src, reads=["qk_s"], writes=[bk])
                if m == 2:
                    c.dma("sp", QT[j][64:80, :], self.mb_s[h * 16:(h + 1) * 16, :], reads=["qk_s"], writes=["p2_QT%d" % j])
                    c.dma("sp", KT[j][64:80, :], self.c_blk, writes=["p2_KT%d" % j])
                return KT[j], QT[j], ["p2_KT%d" % j, "p2_QT%d" % j]

            def load_v(m):
                j = vcount[0] % 2
                vcount[0] += 1
                c.dma("sp", Vm[j][:, :, :], self.v_s[:, m * 260:(m + 1) * 260].rearrange("(t p) c -> p t c", p=128), reads=["v_s"], writes=["p2_Vm%d" % j])
                return Vm[j], "p2_Vm%d" % j

            sslot = [0]

            def softmax_head(kt, qt, kqk, R, scale, vm, vkey, h, out_fn):
                for b in range(NB):
                    n = 4 * b + 4

                    def S_stage(i):
                        j = i - 4 * b
                        c0 = max(0, j) * 128
                        slot = sslot[0]
                        sslot[0] = (slot + 1) % 4
                        ps, pk = self.ps[SBANK[slot]], "ps%d" % SBANK[slot]
                        self.mm(ps[:, c0:512], kt[:, i * 128:(i + 1) * 128], qt[:, b * 512 + c0:(b + 1) * 512], True, True, kqk, [pk])
                        pt, ptk = PT[slot], "p2_PT%d" % slot
                        self.act(pt[:, c0:512], ps[:, c0:512], AF.Exp, [pk], [ptk], scale=scale)
                        if j >= 0:
                            self.tt("pool", pt[:, c0:c0 + 128], pt[:, c0:c0 + 128], self.mask_le[:, :], ALU.mult, [ptk, "const"], [ptk])
                        return slot

                    def PV_stage(i, slot):
                        for s_ in range(4):
                            if 4 * b + s_ >= i:
                                self.mm(self.ps[3 + s_][:, 0:65], PT[slot][:, s_ * 128:(s_ + 1) * 128], vm[:, i, h * 65:(h + 1) * 65],
                                        i == 0, i == 4 * b + s_, ["p2_PT%d" % slot, vkey], ["ps%d" % (3 + s_)])

                    slots = {}
                    for i in range(min(2, n)):
                        slots[i] = S_stage(i)
                    for i in range(n):
                        if i + 2 < n:
                            slots[i + 2] = S_stage(i + 2)
                        PV_stage(i, slots[i])
                        if i >= 4 * b:
                            s_ = i - 4 * b
                            out_fn(b, s_, self.ps[3 + s_], "ps%d" % (3 + s_))

            def sb_head(kt, qt, kqk, vm, vkey, h, omt, omk):
                scale = 0.125
                for b in range(NB):
                    order = list(range(4 * b + 3, -1, -1))
                    n = len(order)
                    self.ms("pool", Run[:, :], 0.0, ["p2_Run"])

                    def geo(idx):
                        i = order[idx]
                        j = i - 4 * b
                        return i, j, max(0, j) * 128

                    def A_stage(idx):
                        i, j, c0 = geo(idx)
                        z, q3 = idx % 2, idx % 3
                        ps, pk = self.ps[z], "ps%d" % z
                        self.mm(ps[:, c0:512], kt[:, i * 128:(i + 1) * 128], qt[:, b * 512 + c0:(b + 1) * 512], True, True, kqk, [pk])
                        e, ek = E32[q3], "p2_E%d" % q3
                        self.act(e[:, c0:512], ps[:, c0:512], AF.Exp, [pk], [ek], scale=scale)
                        if j >= 0:
                            self.tt("pool", e[:, c0:c0 + 128], e[:, c0:c0 + 128], self.mask_lt[:, :], ALU.mult, [ek, "const"], [ek])
                        self.act(Psp[q3][:, c0:512], e[:, c0:512], AF.Ln, [ek], ["p2_P%d" % q3], bias=1.0)

                    def B_stage(idx):
                        i, j, c0 = geo(idx)
                        z, q3 = idx % 2, idx % 3
                        af, afk = (self.ps[2], "ps2") if z == 0 else (self.ps[7], "ps7")
                        pk2 = "p2_P%d" % q3
                        first = idx == 0
                        self.mm(af[:, c0:512], self.tri[:, :], Psp[q3][:, c0:512], True, first, ["const", pk2], [afk])
                        if not first:
                            self.mm(af[:, c0:512], self.ones_b[:, :], Run[:, c0:512], False, True, ["const", "p2_Run"], [afk])
                        self.tt("pool", Run[:, c0:512], Run[:, c0:512], Psp[q3][:, c0:512], ALU.add, ["p2_Run", pk2], ["p2_Run"])
                        t, tk = T[q3], "p2_T%d" % q3
                        self.stt(t[:, c0:512], self.ps[z][:, c0:512], scale, Psp[q3][:, c0:512], ALU.mult, ALU.subtract, ["ps%d" % z, pk2], [tk])
                        self.tt("dve", t[:, c0:512], t[:, c0:512], af[:, c0:512], ALU.subtract, [tk, afk], [tk])
                        a, ak = Aex[q3], "p2_A%d" % q3
                        self.act(a[:, c0:512], t[:, c0:512], AF.Exp, [tk], [ak])
                        if j >= 0:
                            self.tt("pool", a[:, c0:c0 + 128], a[:, c0:c0 + 128], self.mask_lt[:, :], ALU.mult, [ak, "const"], [ak])

                    def C_stage(idx):
                        i, j, c0 = geo(idx)
                        q3 = idx % 3
                        for s_ in range(4):
                            if 4 * b + s_ >= i:
                                self.mm(self.ps[3 + s_][:, 0:64], Aex[q3][:, s_ * 128:(s_ + 1) * 128], vm[:, i, h * 65:h * 65 + 64],
                                        i == 4 * b + s_, i == 0, ["p2_A%d" % q3, vkey], ["ps%d" % (3 + s_)])

                    for step in range(n + 2):
                        if step < n:
                            A_stage(step)
                        if 0 <= step - 1 < n:
                            B_stage(step - 1)
                        if 0 <= step - 2 < n:
                            C_stage(step - 2)
                    for s_ in range(4):
                        self.cp("dve", omt[:, 4 * b + s_, h * 64:(h + 1) * 64], self.ps[3 + s_][:, 0:64], ["ps%d" % (3 + s_)], [omk])

            def norm_out(dst, dkey, col0, f32=False):
                def fn(b, s_, O, ok):
                    self.recip(rc[:, s_:s_ + 1], O[:, 64:65], [ok], ["p2_rc"])
                    self.ts("dve", dst[:, 4 * b + s_, col0:col0 + 64], O[:, 0:64], rc[:, s_:s_ + 1], ALU.mult, [ok, "p2_rc"], [dkey])
                return fn

            def store_om(m, omt, omk):
                c.dma("sp", self.o_s[:, m * 256:(m + 1) * 256].rearrange("(t p) c -> p t c", p=128), omt[:, :, :], reads=[omk], writes=["o_s"])

            oi = 0
            for m, R, scale in ((0, 96, 96 ** -0.5), (2, 80, 0.125)):
                vm, vkey = load_v(m)
                omt, omk = om[oi % 2], "p2_om%d" % (oi % 2)
                oi += 1
                for h in range(4):
                    kt, qt, kqk = load_head(m, h, R)
                    softmax_head(kt, qt, kqk, R, scale, vm, vkey, h, norm_out(omt, omk, h * 64))
                store_om(m, omt, omk)
            vm, vkey = load_v(1)
            omt, omk = om[oi % 2], "p2_om%d" % (oi % 2)
            oi += 1
            for h in range(4):
                kt, qt, kqk = load_head(1, h, 64)
                sb_head(kt, qt, kqk, vm, vkey, h, omt, omk)
            store_om(1, omt, omk)
            vm, vkey = load_v(3)
            dcount = 0
            for comp in range(2):
                for h in range(4):
                    kt, qt, kqk = load_head(3, h, 32, comp * 32)
                    odt, odk = od[dcount % 2], "p2_od%d" % (dcount % 2)
                    dcount += 1
                    softmax_head(kt, qt, kqk, 32, 32 ** -0.5, vm, vkey, h, norm_out(odt, odk, 0))
                    c.dma("sp", self.od_s[comp, :, h * 64:(h + 1) * 64].rearrange("(t p) c -> p t c", p=128), odt[:, :, :],
                          reads=[odk], writes=["od_s"])
            dl = self.sb("p2_dl", [128, 128], F32, st)
            lam = self.sb("p2_lam", [128, 4], F32, st)
            gsub = self.sb("p2_gsub", [128, 64], F32, st)
            junk = self.sb("p2_junk", [128, 64], F32, st)
            c.dma("sp", dl[:, :], self.diff_lambda[l, :].partition_broadcast(128), writes=["p2_dl"])
            c.dma("sp", gsub[:, :], self.diff_subln[l, :].partition_broadcast(128), writes=["p2_gsub"])
            self.tt("dve", junk[:, 0:32], dl[:, 0:32], dl[:, 32:64], ALU.mult, ["p2_dl"], ["p2_junk"])
            self.red(lam[:, 0:1], junk[:, 0:32], ALU.add, ["p2_junk"], ["p2_lam"])
            self.tt("dve", junk[:, 32:64], dl[:, 64:96], dl[:, 96:128], ALU.mult, ["p2_dl"], ["p2_junk"])
            self.red(lam[:, 1:2], junk[:, 32:64], ALU.add, ["p2_junk"], ["p2_lam"])
            self.act(lam[:, 0:2], lam[:, 0:2], AF.Exp, ["p2_lam"], ["p2_lam"])
            self.tt("dve", lam[:, 2:3], lam[:, 1:2], lam[:, 0:1], ALU.subtract, ["p2_lam"], ["p2_lam"])
            self.ts("dve", lam[:, 2:3], lam[:, 2:3], -lambda_init, ALU.add, ["p2_lam"], ["p2_lam"])
            self.ts("dve", gsub[:, :], gsub[:, :], 1.0 - lambda_init, ALU.mult, ["p2_gsub"], ["p2_gsub"])
            omt, omk = om[oi % 2], "p2_om%d" % (oi % 2)
            oi += 1
            d1 = [self.sb("p2_d1_%d" % i, [128, 256], F32, st) for i in range(2)]
            d2 = [self.sb("p2_d2_%d" % i, [128, 256], F32, st) for i in range(2)]
            sq = self.sb("p2_sq", [128, 256], F32, st)
            ss = self.sb("p2_ss", [128, 4], F32, st)
            for t in range(NT):
                i = t % 2
                c.dma("sp", d1[i][:, :], self.od_s[0, t * 128:(t + 1) * 128, :], reads=["od_s"], writes=["p2_d1_%d" % i])
                c.dma("sp", d2[i][:, :], self.od_s[1, t * 128:(t + 1) * 128, :], reads=["od_s"], writes=["p2_d2_%d" % i])
                k1 = "p2_d1_%d" % i
                self.stt(d1[i][:, :], d2[i][:, :], lam[:, 2:3], d1[i][:, :], ALU.mult, ALU.add, ["p2_d2_%d" % i, k1, "p2_lam"], [k1])
                self.tt("dve", sq[:, :], d1[i][:, :], d1[i][:, :], ALU.mult, [k1], ["p2_sq"])
                self.red(ss[:, :], sq[:, :].rearrange("p (h d) -> p h d", h=4), ALU.add, ["p2_sq"], ["p2_ss"])
                self.ts("dve", ss[:, :], ss[:, :], 1.0 / 64, ALU.mult, ["p2_ss"], ["p2_ss"], s2=RMS_EPS, op1=ALU.add)
                self.act(ss[:, :], ss[:, :], AF.Ln, ["p2_ss"], ["p2_ss"])
                self.act(ss[:, :], ss[:, :], AF.Exp, ["p2_ss"], ["p2_ss"], scale=-0.5)
                d3 = d1[i][:, :].rearrange("p (h d) -> p h d", h=4)
                self.tt("dve", d3, d3, ss[:, :].unsqueeze(2).to_broadcast([128, 4, 64]), ALU.mult, [k1, "p2_ss"], [k1])
                self.tt("dve", omt[:, t, :].rearrange("p (h d) -> p h d", h=4), d3, gsub[:, :].unsqueeze(1).to_broadcast([128, 4, 64]),
                        ALU.mult, [k1, "p2_gsub"], [omk])
            store_om(3, omt, omk)
            c.emit()

    def ln_part1(self, y, yk, res, resk, pa, pak, pb, pbk, tmp, tk):
        nc = self.nc
        st6, mv = tmp["st6"], tmp["mv"]
        self.stt(y[:, 0:512], res[:, 0:512], ALPHA, pa[:, 0:512], ALU.mult, ALU.add, [resk, pak], [yk])
        self.stt(y[:, 512:1024], res[:, 512:1024], ALPHA, pb[:, 0:512], ALU.mult, ALU.add, [resk, pbk], [yk])
        self.c.op("dve", lambda: nc.vector.bn_stats(out=st6[:, 0:6], in_=y[:, 0:512]), [yk], [tk + "s"])
        self.c.op("dve", lambda: nc.vector.bn_stats(out=st6[:, 6:12], in_=y[:, 512:1024]), [yk], [tk + "s"])
        self.c.op("dve", lambda: nc.vector.bn_aggr(out=mv[:, 0:2], in_=st6[:, 0:12]), [tk + "s"], [tk])
        self.ts("dve", mv[:, 2:3], mv[:, 1:2], LN_EPS, ALU.add, [tk], [tk])
        self.act(mv[:, 2:3], mv[:, 2:3], AF.Ln, [tk], [tk])
        self.act(mv[:, 2:3], mv[:, 2:3], AF.Exp, [tk], [tk], scale=-0.5)

    def ln_part2(self, y, yk, g_t, b_t, gbk, out, outk, tmp, tk):
        mv = tmp["mv"]
        self.stt(y[:, :], y[:, :], mv[:, 0:1], g_t[:, :], ALU.subtract, ALU.mult, [yk, tk, gbk], [yk])
        self.stt(out, y[:, :], mv[:, 2:3], b_t[:, :], ALU.mult, ALU.add, [yk, tk, gbk], [outk])

    def ln_tile(self, y, yk, res, resk, pa, pak, pb, pbk, g_t, b_t, gbk, out, outk, tmp, tk="ln_mv"):
        self.ln_part1(y, yk, res, resk, pa, pak, pb, pbk, tmp, tk)
        self.ln_part2(y, yk, g_t, b_t, gbk, out, outk, tmp, tk)

    def to_xT(self, src, srck, xb, xbk, xts, xtsk, cols, eng):
        self.cp("act", xb[:, :], src, [srck], [xbk])
        ptb = self.ps[7][:, :].bitcast(BF16)
        for k in range(8):
            self.tr(ptb[:, k * 128:(k + 1) * 128], xb[:, k * 128:(k + 1) * 128], [xbk], ["ps7"])
        self.cp(eng, xts[:, :, cols], ptb.rearrange("p (k m) -> p k m", k=8), ["ps7"], [xtsk])

    def phase34(self, l):
        nc, c = self.nc, self.c
        xres_src = self.x_in if l == 0 else self.xres_s
        with ExitStack() as st:
            KmT = self.sb("p3_KmT", [128, 8, N_MEM], BF16, st)
            Vmem = self.sb("p3_Vmem", [128, 2, 4 * 257], BF16, st)
            with ExitStack() as st2:
                wk = self.sb("p3_wk", [128, 8, D], BF16, st2)
                wv = self.sb("p3_wv", [128, 8, D], BF16, st2)
                for k in range(8):
                    c.dma("pool", wk[:, k, :], self.wk[l, k * 128:(k + 1) * 128, :], writes=["p3_wk%d" % k])
                    c.dma("pool", wv[:, k, :], self.wv[l, k * 128:(k + 1) * 128, :], writes=["p3_wv%d" % k])
                self.ms("pool", Vmem[:, :, :], 1.0, ["p3_Vmem"])
                for j in range(8):
                    ps, pk = self.ps[j % 2], "ps%d" % (j % 2)
                    for k in range(8):
                        self.mm(ps[:, 0:N_MEM], wk[:, k, j * 128:(j + 1) * 128], self.memT[:, k, :], k == 0, k == 7, ["p3_wk%d" % k, "memT"], [pk])
                    self.cp("act" if j % 2 == 0 else "dve", KmT[:, j, :], ps[:, 0:N_MEM], [pk], ["p3_KmT"])
                for mt in range(2):
                    for nh in range(2):
                        ps, pk = self.ps[2 + nh], "ps%d" % (2 + nh)
                        for k in range(8):
                            self.mm(ps[:, :], self.memT[:, k, mt * 128:(mt + 1) * 128], wv[:, k, nh * 512:(nh + 1) * 512], k == 0, k == 7,
                                    ["p3_wv%d" % k, "memT"], [pk])
                        dst = Vmem[:, mt, nh * 514:(nh + 1) * 514].rearrange("p (h d) -> p h d", h=2)[:, :, 0:256]
                        self.cp("act" if nh == 0 else "dve", dst, ps[:, :].rearrange("p (h d) -> p h d", h=2), [pk], ["p3_Vmem"])
                c.emit()
            wout = self.sb("p3_wout", [128, 8, D], BF16, st)
            wq = self.sb("p3_wq", [128, 8, D], BF16, st)
            wo = self.sb("p3_wo", [128, 8, D], BF16, st)
            for k in range(8):
                c.dma("pool", wout[:, k, :], self.w_out[l, k * 128:(k + 1) * 128, :], writes=["p3_wout%d" % k])
            for k in range(8):
                c.dma("pool", wq[:, k, :], self.wq[l, k * 128:(k + 1) * 128, :], writes=["p3_wq%d" % k])
            for k in range(8):
                c.dma("pool", wo[:, k, :], self.wo[l, k * 128:(k + 1) * 128, :], writes=["p3_wo%d" % k])
            gb = {}
            for nm, src in (("g1", self.ln_g[0]), ("b1", self.ln_b[0]), ("g2", self.ln_g[1]), ("b2", self.ln_b[1])):
                gb[nm] = self.sb("p3_" + nm, [128, D], F32, st)
                c.dma("sp", gb[nm][:, :], src[l, :].partition_broadcast(128), writes=["p3_gb"])
            tmpA = {"st6": self.sb("p3_st6a", [128, 12], F32, st), "mv": self.sb("p3_mva", [128, 4], F32, st)}
            tmpB = {"st6": self.sb("p3_st6b", [128, 12], F32, st), "mv": self.sb("p3_mvb", [128, 4], F32, st)}
            ob = [self.sb("p3_ob0", [128, 4, D], BF16, st)] * 2
            oT = [self.sb("p3_oT0", [128, 8, 512], BF16, st)] * 2
            xr = [self.sb("p3_xr%d" % i, [128, D], F32, st) for i in range(2)]
            xr2 = [self.sb("p3_xq%d" % i, [128, D], F32, st) for i in range(2)]
            yA = self.sb("p3_yA", [128, D], F32, st)
            yB = self.sb("p3_yB", [128, D], F32, st)
            x1t = [self.sb("p3_x1t%d" % i, [128, D], F32, st) for i in range(2)]
            xbA = self.sb("p3_xbA", [128, D], BF16, st)
            xbB = self.sb("p3_xbB", [128, D], BF16, st)
            x1T = [self.sb("p3_x1T%d" % i, [128, 8, 512], BF16, st) for i in range(2)]
            qT = self.sb("p3_qT", [128, 8, 512], BF16, st)
            PT2 = [self.sb("p3_PT%d" % i, [128, 512], BF16, st) for i in range(4)]
            xo = self.sb("p3_xo", [128, 4, D], BF16, st)
            xoT = self.sb("p3_xoT", [128, 8, 512], BF16, st)
            x2 = [self.sb("p3_x2_%d" % i, [128, D], F32, st) for i in range(2)]
            x2T = [self.sb("p3_x2T0", [128, 8, 512], BF16, st)] * 2
            rc = self.sb("p3_rc", [128, 2], F32, st)

            def to_xT2(src, srck, xb, xbk, xts, xtsk, cols, bank, eng):
                self.cp("act", xb[:, :], src, [srck], [xbk])
                ptb = self.ps[bank][:, :].bitcast(BF16)
                for k in range(8):
                    self.tr(ptb[:, k * 128:(k + 1) * 128], xb[:, k * 128:(k + 1) * 128], [xbk], ["ps%d" % bank])
                self.cp(eng, xts[:, :, cols], ptb.rearrange("p (k m) -> p k m", k=8), ["ps%d" % bank], [xtsk])

            def load_ob(b):
                c.dma("sp", ob[0][:, :, :], self.o_s[b * 512:(b + 1) * 512, :].rearrange("(t p) c -> p t c", p=128),
                      reads=["o_s"], writes=["p3_ob0"])

            def H1(b):
                steps = []
                p = b % 2
                obk, oTk, x1Tk = "p3_ob0", "p3_oT0", "p3_x1T%d" % p

                def s_oT(tl):
                    ptb = self.ps[2][:, :].bitcast(BF16)
                    for k in range(8):
                        self.tr(ptb[:, k * 128:(k + 1) * 128], ob[p][:, tl, k * 128:(k + 1) * 128], [obk], ["ps2"])
                    self.cp("act", oT[p][:, :, tl * 128:(tl + 1) * 128], ptb.rearrange("p (k m) -> p k m", k=8), ["ps2"], [oTk])

                def s_mix(tl):
                    t = b * 4 + tl
                    xi = t % 2
                    c.dma("sp", xr[xi][:, :], xres_src[t * 128:(t + 1) * 128, :], reads=["xres_s%d" % t], writes=["p3_xr%d" % xi])
                    for nh in range(2):
                        for k in range(8):
                            self.mm(self.ps[nh][:, :], oT[p][:, k, tl * 128:(tl + 1) * 128], wout[:, k, nh * 512:(nh + 1) * 512], k == 0, k == 7,
                                    [oTk, "p3_wout%d" % k], ["ps%d" % nh])
                    self.ln_part1(yA, "p3_yA", xr[xi], "p3_xr%d" % xi, self.ps[0], "ps0", self.ps[1], "ps1", tmpA, "lnA")

                def s_mix_b(tl):
                    t = b * 4 + tl
                    xi = t % 2
                    self.ln_part2(yA, "p3_yA", gb["g1"], gb["b1"], "p3_gb", x1t[xi][:, :], "p3_x1t%d" % xi, tmpA, "lnA")
                    c.dma("sp", self.x1_s[t * 128:(t + 1) * 128, :], x1t[xi][:, :], reads=["p3_x1t%d" % xi], writes=["x1_s%d" % t])

                def s_x1T(tl):
                    xi = (b * 4 + tl) % 2
                    to_xT2(x1t[xi][:, :], "p3_x1t%d" % xi, xbA, "p3_xbA", x1T[p], x1Tk, slice(tl * 128, (tl + 1) * 128), 2, "act")

                steps.append(lambda: load_ob(b))
                for tl in range(4):
                    steps.append(lambda tl=tl: s_oT(tl))
                for tl in range(4):
                    steps.append(lambda tl=tl: s_mix(tl))
                    if tl > 0:
                        steps.append(lambda tl=tl: s_x1T(tl - 1))
                    steps.append(lambda tl=tl: s_mix_b(tl))
                steps.append(lambda: s_x1T(3))
                return steps

            pcount = [0]

            def H2(b):
                steps = []
                p = b % 2
                x1Tk = "p3_x1T%d" % p
                x2Tb, x2Tk = x2T[0], "p3_x2T0"

                def s_q(j):
                    ps, pk = self.ps[3 + j % 2], "ps%d" % (3 + j % 2)
                    for k in range(8):
                        self.mm(ps[:, :], wq[:, k, j * 128:(j + 1) * 128], x1T[p][:, k, :], k == 0, k == 7, ["p3_wq%d" % k, x1Tk], [pk])
                    self.cp("act" if j % 2 == 0 else "dve", qT[:, j, :], ps[:, :], [pk], ["p3_qT"])

                def s_att(h):
                    pts = []
                    for mt in range(2):
                        ps, pk = self.ps[5 + mt], "ps%d" % (5 + mt)
                        for cc in range(2):
                            self.mm(ps[:, :], KmT[:, h * 2 + cc, mt * 128:(mt + 1) * 128], qT[:, h * 2 + cc, :], cc == 0, cc == 1,
                                    ["p3_KmT", "p3_qT"], [pk])
                        pi = pcount[0] % 4
                        pcount[0] += 1
                        self.act(PT2[pi][:, :], ps[:, :], AF.Exp, [pk], ["p3_PT%d" % pi], scale=256 ** -0.5)
                        pts.append(pi)
                    for tl in range(4):
                        ps, pk = self.ps[7], "ps7"
                        for mt in range(2):
                            self.mm(ps[:, 0:257], PT2[pts[mt]][:, tl * 128:(tl + 1) * 128], Vmem[:, mt, h * 257:(h + 1) * 257], mt == 0, mt == 1,
                                    ["p3_PT%d" % pts[mt], "p3_Vmem"], [pk])
                        self.recip(rc[:, tl % 2:tl % 2 + 1], ps[:, 256:257], [pk], ["p3_rc%d" % (tl % 2)])
                        self.ts("dve", xo[:, tl, h * 256:(h + 1) * 256], ps[:, 0:256], rc[:, tl % 2:tl % 2 + 1], ALU.mult, [pk, "p3_rc%d" % (tl % 2)], ["p3_xo"])

                def s_xoT(tl):
                    ptb = self.ps[5][:, :].bitcast(BF16)
                    for k in range(8):
                        self.tr(ptb[:, k * 128:(k + 1) * 128], xo[:, tl, k * 128:(k + 1) * 128], ["p3_xo"], ["ps5"])
                    self.cp("act", xoT[:, :, tl * 128:(tl + 1) * 128], ptb.rearrange("p (k m) -> p k m", k=8), ["ps5"], ["p3_xoT"])

                def s_wo(tl):
                    t = b * 4 + tl
                    for nh in range(2):
                        for k in range(8):
                            self.mm(self.ps[3 + nh][:, :], xoT[:, k, tl * 128:(tl + 1) * 128], wo[:, k, nh * 512:(nh + 1) * 512], k == 0, k == 7,
                                    ["p3_xoT", "p3_wo%d" % k], ["ps%d" % (3 + nh)])
                    x2t, x2k = x2[t % 2], "p3_x2_%d" % (t % 2)
                    c.dma("sp", xr2[t % 2][:, :], self.x1_s[t * 128:(t + 1) * 128, :], reads=["x1_s%d" % t], writes=["p3_xq%d" % (t % 2)])
                    self.ln_part1(yB, "p3_yB", xr2[t % 2], "p3_xq%d" % (t % 2), self.ps[3], "ps3", self.ps[4], "ps4", tmpB, "lnB")

                def s_wo_b(tl):
                    t = b * 4 + tl
                    x2t, x2k = x2[t % 2], "p3_x2_%d" % (t % 2)
                    self.ln_part2(yB, "p3_yB", gb["g2"], gb["b2"], "p3_gb", x2t[:, :], x2k, tmpB, "lnB")
                    c.dma("sp", self.xres_s[t * 128:(t + 1) * 128, :], x2t[:, :], reads=[x2k], writes=["xres_s%d" % t])

                def s_x2T(tl):
                    t = b * 4 + tl
                    to_xT2(x2[t % 2][:, :], "p3_x2_%d" % (t % 2), xbB, "p3_xbB", x2Tb, x2Tk, slice(tl * 128, (tl + 1) * 128), 6, "act")
                    if tl == 3:
                        c.dma("sp", self.xT_s[:, :, b * 512:(b + 1) * 512], x2Tb[:, :, :], reads=[x2Tk], writes=["xT_s"])

                for j in range(8):
                    steps.append(lambda j=j: s_q(j))
                for h in range(4):
                    steps.append(lambda h=h: s_att(h))
                for tl in range(4):
                    steps.append(lambda tl=tl: s_xoT(tl))
                for tl in range(4):
                    steps.append(lambda tl=tl: s_wo(tl))
                    if tl > 0:
                        steps.append(lambda tl=tl: s_x2T(tl - 1))
                    steps.append(lambda tl=tl: s_wo_b(tl))
                steps.append(lambda: s_x2T(3))
                return steps

            def merge(a, bsteps):
                na, nb = len(a), len(bsteps)
                ia = ib = 0
                while ia < na or ib < nb:
                    if ib >= nb or (ia < na and ia * nb <= ib * na):
                        a[ia]()
                        ia += 1
                    else:
                        bsteps[ib]()
                        ib += 1

            for f in H1(0):
                f()
            for b in range(NB):
                merge(H2(b), H1(b + 1) if b + 1 < NB else [])
            c.emit()

    def phase5(self, l, last):
        nc, c = self.nc, self.c
        with ExitStack() as st:
            wr = self.sb("p5_wr", [128, 8, 40], BF16, st)
            br = self.sb("p5_br", [128, 40], F32, st)
            c.dma("pool", wr[:, :, :], self.w_router[l].rearrange("(k p) n -> p k n", p=128), writes=["p5_wr"])
            c.dma("sp", br[:, :], self.b_router[l, :].partition_broadcast(128), writes=["p5_br"])
            xT = [self.sb("p5_xT%d" % i, [128, 8, 512], BF16, st) for i in range(2)]
            lg = self.sb("p5_lg", [128, 40], F32, st)
            sm = self.sb("p5_sm", [128, 16], F32, st)
            ohg = self.sb("p5_ohg", [128, 8], F32, st)
            j8 = self.sb("p5_j8", [128, 8], F32, st)
            t32 = self.sb("p5_t32", [128, 32], F32, st)
            el = self.sb("p5_el", [128, 4], F32, st)
            el2 = self.sb("p5_el2", [128, 4], F32, st)
            mk1 = self.sb("p5_mk1", [128, 4], F32, st)
            mk2 = self.sb("p5_mk2", [128, 4], F32, st)
            g4 = self.sb("p5_g4", [128, 4], F32, st)
            g32 = self.sb("p5_g32", [128, 32], F32, st)
            gTs = [self.sb("p5_gTs%d" % i, [32, 512], F32, st) for i in range(2)]

            def load_x(b):
                c.dma("sp", xT[b % 2][:, :, :], self.xT_s[:, :, b * 512:(b + 1) * 512], reads=["xT_s"], writes=["p5_xT%d" % (b % 2)])

            load_x(0)
            K = "p5_r"
            for b in range(NB):
                if b + 1 < NB:
                    load_x(b + 1)
                gT, gTk = gTs[b % 2], "p5_gTs%d" % (b % 2)
                for tl in range(4):
                    ps, pk = self.ps[tl % 2], "ps%d" % (tl % 2)
                    for k in range(8):
                        self.mm(ps[:, 0:40], xT[b % 2][:, k, tl * 128:(tl + 1) * 128], wr[:, k, :], k == 0, k == 7, ["p5_xT%d" % (b % 2), "p5_wr"], [pk])
                    self.tt("dve", lg[:, :], ps[:, 0:40], br[:, :], ALU.add, [pk, "p5_br"], [K])
                    self.red(sm[:, 0:1], lg[:, 0:8], ALU.max, [K], [K])
                    self.ts("dve", ohg[:, :], lg[:, 0:8], sm[:, 0:1], ALU.is_equal, [K], [K])
                    self.ts("dve", sm[:, 1:2], sm[:, 0:1], -1.0, ALU.mult, [K], [K])
                    self.act(j8[:, :], lg[:, 0:8], AF.Exp, [K], [K], bias=sm[:, 1:2], accum_out=sm[:, 2:3])
                    self.recip(sm[:, 3:4], sm[:, 2:3], [K], [K])
                    lge = lg[:, 8:40].rearrange("p (g e) -> p g e", g=8)
                    self.tt("dve", t32[:, :].rearrange("p (g e) -> p g e", g=8), lge, ohg[:, :].unsqueeze(2).to_broadcast([128, 8, 4]),
                            ALU.mult, [K], [K])
                    self.red(el[:, :], t32[:, :].rearrange("p (g e) -> p e g", g=8), ALU.add, [K], [K])
                    self.red(sm[:, 4:5], el[:, :], ALU.max, [K], [K])
                    self.ts("dve", mk1[:, :], el[:, :], sm[:, 4:5], ALU.is_equal, [K], [K])
                    self.stt(el2[:, :], mk1[:, :], -1e30, el[:, :], ALU.mult, ALU.add, [K], [K])
                    self.red(sm[:, 5:6], el2[:, :], ALU.max, [K], [K])
                    self.ts("dve", mk2[:, :], el2[:, :], sm[:, 5:6], ALU.is_equal, [K], [K])
                    self.tt("dve", sm[:, 6:7], sm[:, 5:6], sm[:, 4:5], ALU.subtract, [K], [K])
                    self.act(sm[:, 6:7], sm[:, 6:7], AF.Exp, [K], [K])
                    self.ts("dve", sm[:, 7:8], sm[:, 6:7], 1.0, ALU.add, [K], [K])
                    self.recip(sm[:, 7:8], sm[:, 7:8], [K], [K])
                    self.tt("dve", sm[:, 8:9], sm[:, 7:8], sm[:, 6:7], ALU.mult, [K], [K])
                    self.tt("dve", sm[:, 7:8], sm[:, 7:8], sm[:, 3:4], ALU.mult, [K], [K])
                    self.tt("dve", sm[:, 8:9], sm[:, 8:9], sm[:, 3:4], ALU.mult, [K], [K])
                    self.ts("dve", g4[:, :], mk1[:, :], sm[:, 7:8], ALU.mult, [K], [K])
                    self.stt(g4[:, :], mk2[:, :], sm[:, 8:9], g4[:, :], ALU.mult, ALU.add, [K], [K])
                    self.tt("dve", g32[:, :].rearrange("p (g e) -> p g e", g=8), ohg[:, :].unsqueeze(2).to_broadcast([128, 8, 4]),
                            g4[:, :].unsqueeze(1).to_broadcast([128, 8, 4]), ALU.mult, [K], [K])
                    pt, ptk = self.ps[2 + tl % 2], "ps%d" % (2 + tl % 2)
                    self.tr(pt[0:32, 0:128], g32[:, :], [K], [ptk], f32=True)
                    self.cp("dve", gT[:, tl * 128:(tl + 1) * 128], pt[0:32, 0:128], [ptk], [gTk])
                c.dma("sp", self.gT_s[:, b * 512:(b + 1) * 512], gT[:, :], reads=[gTk], writes=["gT_s"])
            c.emit()
        SBT = 2048
        NSB = S // SBT
        TPS = SBT // 128
        with ExitStack() as st:
            acc = self.sb("p5_acc", [128, TPS, D], F32, st)
            for sbi in range(NSB):
                t0 = sbi * TPS
                with ExitStack() as st2:
                    xTs = self.sb("p5_xTs", [128, 8, SBT], BF16, st2)
                    Wg = [self.sb("p5_Wg%d" % i, [128, 8, 256], BF16, st2) for i in range(3)]
                    Wu = [self.sb("p5_Wu%d" % i, [128, 8, 256], BF16, st2) for i in range(3)]
                    Wd = [self.sb("p5_Wd%d" % i, [128, 2, D], BF16, st2) for i in range(3)]
                    gbt = [self.sb("p5_gbt%d" % i, [128, 512], F32, st2) for i in range(3)]
                    sg = [self.sb("p5_sg%d" % i, [128, 512], F32, st2) for i in range(2)]
                    tu = [self.sb("p5_tu%d" % i, [128, 512], F32, st2) for i in range(2)]
                    hid = [self.sb("p5_hid%d" % i, [128, 2, 512], BF16, st2) for i in range(2)]
                    for q in range(SBT // 512):
                        c.dma("sp", xTs[:, :, q * 512:(q + 1) * 512], self.xT_s[:, :, sbi * SBT + q * 512: sbi * SBT + (q + 1) * 512],
                              reads=["xT_s"], writes=["p5_xTs%d" % q])

                    def load_w(e):
                        i = e % 3
                        c.dma("pool", Wg[i][:, :, :], self.e_gate[l, e].rearrange("(k p) f -> p k f", p=128), writes=["p5_Wg%d" % i])
                        c.dma("pool", Wu[i][:, :, :], self.e_up[l, e].rearrange("(k p) f -> p k f", p=128), writes=["p5_Wu%d" % i])
                        c.dma("pool", Wd[i][:, :, :], self.e_down[l, e].rearrange("(k p) d -> p k d", p=128), writes=["p5_Wd%d" % i])

                    load_w(0)
                    load_w(1)
                    load_w(2)
                    ocnt = [0]
                    NTB = SBT // 512

                    def gu_stage(n):
                        e, tb = n // NTB, n % NTB
                        wi, gi, hi = e % 3, n % 3, n % 2
                        tok0 = sbi * SBT + tb * 512
                        c.dma("sp", gbt[gi][:, :], self.gT_s[e, tok0:tok0 + 512].partition_broadcast(128), reads=["gT_s"], writes=["p5_gbt%d" % gi])
                        for fc in range(2):
                            pg, pgk = self.ps[fc], "ps%d" % fc
                            pu, puk = self.ps[2 + fc], "ps%d" % (2 + fc)
                            for k in range(8):
                                self.mm(pg[:, :], Wg[wi][:, k, fc * 128:(fc + 1) * 128], xTs[:, k, tb * 512:(tb + 1) * 512], k == 0, k == 7,
                                        ["p5_Wg%d" % wi, "p5_xTs%d" % tb], [pgk])
                            for k in range(8):
                                self.mm(pu[:, :], Wu[wi][:, k, fc * 128:(fc + 1) * 128], xTs[:, k, tb * 512:(tb + 1) * 512], k == 0, k == 7,
                                        ["p5_Wu%d" % wi, "p5_xTs%d" % tb], [puk])
                            self.act(sg[fc][:, :], pg[:, :], AF.Silu, [pgk], ["p5_sg%d" % fc])
                            self.tt("dve", tu[fc][:, :], sg[fc][:, :], pu[:, :], ALU.mult, ["p5_sg%d" % fc, puk], ["p5_tu%d" % fc])
                            self.tt("pool", hid[hi][:, fc, :], tu[fc][:, :], gbt[gi][:, :], ALU.mult, ["p5_tu%d" % fc, "p5_gbt%d" % gi], ["p5_hid%d" % hi])

                    def down_stage(n):
                        e, tb = n // NTB, n % NTB
                        wi, hi = e % 3, n % 2
                        for tl in range(4):
                            tile = tb * 4 + tl
                            for dh in range(2):
                                po, pok = self.ps[4 + ocnt[0] % 4], "ps%d" % (4 + ocnt[0] % 4)
                                ocnt[0] += 1
                                for fc in range(2):
                                    self.mm(po[:, :], hid[hi][:, fc, tl * 128:(tl + 1) * 128], Wd[wi][:, fc, dh * 512:(dh + 1) * 512], fc == 0, fc == 1,
                                            ["p5_hid%d" % hi, "p5_Wd%d" % wi], [pok])
                                a = acc[:, tile, dh * 512:(dh + 1) * 512]
                                ak = "p5_acc%d" % tile
                                if e == 0:
                                    self.cp("dve", a, po[:, :], [pok], [ak])
                                else:
                                    self.tt("dve", a, a, po[:, :], ALU.add, [pok, ak], [ak])

                    NN = 32 * NTB
                    gu_stage(0)
                    for n in range(NN):
                        if n + 1 < NN:
                            gu_stage(n + 1)
                        down_stage(n)
                        if n % NTB == NTB - 1 and n // NTB + 3 < 32:
                            load_w(n // NTB + 3)
                    c.emit()
                with ExitStack() as st2:
                    g_t = self.sb("p5_g", [128, D], F32, st2)
                    b_t = self.sb("p5_b", [128, D], F32, st2)
                    c.dma("sp", g_t[:, :], self.ln_g[2][l, :].partition_broadcast(128), writes=["p5_gb"])
                    c.dma("sp", b_t[:, :], self.ln_b[2][l, :].partition_broadcast(128), writes=["p5_gb"])
                    tmp = {"st6": self.sb("p5_st6", [128, 12], F32, st2), "mv": self.sb("p5_mv", [128, 4], F32, st2)}
                    xr = [self.sb("p5_xr%d" % i, [128, D], F32, st2) for i in range(2)]
                    y = self.sb("p5_y", [128, D], F32, st2)
                    x3 = [self.sb("p5_x3_%d" % i, [128, D], F32, st2) for i in range(2)]
                    xb = self.sb("p5_xb", [128, D], BF16, st2)
                    x3T = [self.sb("p5_x3T%d" % i, [128, 8, 512], BF16, st2) for i in range(2)]
                    for tile in range(TPS):
                        t = t0 + tile
                        i = tile % 2
                        c.dma("sp", xr[i][:, :], self.xres_s[t * 128:(t + 1) * 128, :], reads=["xres_s%d" % t], writes=["p5_xr%d" % i])
                        ak = "p5_acc%d" % tile
                        self.ln_tile(y, "p5_y", xr[i], "p5_xr%d" % i, acc[:, tile, 0:512], ak, acc[:, tile, 512:1024], ak, g_t, b_t, "p5_gb",
                                     x3[i][:, :], "p5_x3_%d" % i, tmp)
                        dst = self.out if last else self.xres_s
                        c.dma("sp", dst[t * 128:(t + 1) * 128, :], x3[i][:, :], reads=["p5_x3_%d" % i], writes=["xres_s%d" % t])
                        if not last:
                            bq = tile // 4
                            self.to_xT(x3[i][:, :], "p5_x3_%d" % i, xb, "p5_xb", x3T[bq % 2], "p5_x3T%d" % (bq % 2),
                                       slice((tile % 4) * 128, (tile % 4 + 1) * 128), "dve")
                            if tile % 4 == 3:
                                tok0 = (t0 + bq * 4) * 128
                                c.dma("sp", self.xT_s[:, :, tok0:tok0 + 512], x3T[bq % 2][:, :, :], reads=["p5_x3T%d" % (bq % 2)], writes=["xT_s"])
                    c.emit()


_CACHE = {}


def _consts():
    ident = np.eye(128, dtype=np.float32)
    p = np.arange(128)[:, None]
    f = np.arange(128)[None, :]
    invs = []
    for rot in (32, 16, 8):
        invs.append((np.float32(ROPE_THETA) ** (-np.arange(0, rot, 2, dtype=np.float32) / np.float32(rot))).astype(np.float32))
    invf = np.tile(np.concatenate(invs)[None, :], (128, 1)).astype(np.float32)
    blk = np.zeros((16, S), dtype=np.float32)
    for j in range(16):
        blk[j, j * 256:(j + 1) * 256] = BIGV
    bf = ml_dtypes.bfloat16
    return {
        "c_ident_b": ident.astype(bf), "c_ident_f": ident,
        "c_mask_le": (p <= f).astype(np.float32).astype(bf),
        "c_mask_lt": (p < f).astype(np.float32).astype(bf),
        "c_tri": (p > f).astype(np.float32).astype(bf),
        "c_invf": invf, "c_blk": blk.astype(bf),
    }


def make_in_maps(inputs, n_cores=8):
    f = lambda a: np.ascontiguousarray(np.asarray(a))
    shared = {
        "w_in": f(np.asarray(inputs["w_in"])[:, :, COL_PERM]),
        "mla_q_norm": f(inputs["mla_q_norm"]), "w_uq": f(inputs["w_uq"]),
        "mla_kv_norm": f(inputs["mla_kv_norm"]), "w_ukv": f(inputs["w_ukv"]),
        "diff_lambda": f(np.asarray(inputs["diff_lambda"]).reshape(DEPTH, 128)),
        "diff_subln": f(inputs["diff_subln"]), "w_out": f(inputs["w_out"]),
        "ln_mix_g": f(inputs["ln_mix_g"]), "ln_mix_b": f(inputs["ln_mix_b"]),
        "ln_mem_g": f(inputs["ln_mem_g"]), "ln_mem_b": f(inputs["ln_mem_b"]),
        "ln_ffn_g": f(inputs["ln_ffn_g"]), "ln_ffn_b": f(inputs["ln_ffn_b"]),
        "xattn_wq": f(inputs["xattn_wq"]), "xattn_wk": f(inputs["xattn_wk"]),
        "xattn_wv": f(inputs["xattn_wv"]), "xattn_wo": f(inputs["xattn_wo"]),
        "w_router": f(np.concatenate([np.asarray(inputs["router_group_w"]), np.asarray(inputs["router_expert_w"])], axis=2)),
        "b_router": f(np.concatenate([np.asarray(inputs["router_group_b"]), np.asarray(inputs["router_expert_b"])], axis=1)),
        "expert_w_gate": f(inputs["expert_w_gate"]), "expert_w_up": f(inputs["expert_w_up"]),
        "expert_w_down": f(inputs["expert_w_down"]),
    }
    shared.update(_consts())
    maps = []
    x = np.asarray(inputs["x"])
    mem = np.asarray(inputs["mem"])
    pos = np.asarray(inputs["positions"])
    for b in range(n_cores):
        m = dict(shared)
        m["x"] = f(x[b])
        m["mem"] = f(mem[b])
        m["pos"] = f(pos[b].reshape(NT, 128).T.astype(np.int32))
        maps.append(m)
    return maps


def kernel(**inputs):
    if "nc" not in _CACHE:
        _CACHE["nc"] = Builder().build()
    nc = _CACHE["nc"]
    maps = make_in_maps(inputs)
    res = run_bass_kernel_spmd(nc, maps, core_ids=list(range(8)))
    return np.stack([r["out"] for r in res.results], axis=0).astype(np.float32)
```

```python
import math
import os
LV = int(os.environ.get('P1LV', '9'))
SUB = int(os.environ.get('P1SUB', '9'))
from contextlib import ExitStack

import numpy as np
import ml_dtypes
import concourse.bass as bass
import concourse.mybir as mybir
from concourse.bass_utils import run_bass_kernel_spmd

F32 = mybir.dt.float32
BF16 = mybir.dt.bfloat16
I32 = mybir.dt.int32
AF = mybir.ActivationFunctionType
ALU = mybir.AluOpType
AX = mybir.AxisListType

D = 1024
S = 4096
NT = S // 128
NB = S // 512
DEPTH = 4
N_MEM = 256
IN_COLS = 2656
ALPHA = (2 * DEPTH) ** 0.25
LN_EPS = 1e-5
RMS_EPS = 1e-6
ROPE_THETA = 500000.0
BIGV = 30000.0
TWO_PI = 2.0 * math.pi

O_SB = 352
O_MB = 352 + 768
O_DF = 352 + 1536
COL_PERM = np.concatenate([
    np.arange(0, 352),
    np.arange(O_SB, O_SB + 512),
    np.arange(O_SB + 512, O_SB + 768), np.arange(O_MB + 512, O_MB + 768),
    np.arange(O_MB, O_MB + 512),
    np.arange(O_DF, O_DF + 512),
    np.arange(O_DF + 512, O_DF + 768),
])
CH = [(0, 352), (352, 512), (864, 512), (1376, 512), (1888, 512), (2400, 256)]


class Ctx:
    def __init__(self, nc, stack, n_dma_sems=24):
        self.nc = nc
        self.eng = {"pe": nc.tensor, "act": nc.scalar, "dve": nc.vector, "pool": nc.gpsimd, "sp": nc.sync}
        self.sem = {}
        self.cnt = {}
        for e in self.eng:
            self.sem[e] = stack.enter_context(nc.semaphore("s_" + e))
            self.cnt[e] = 0
        self.dring = {}
        for q in ("sp", "pool"):
            sems = [stack.enter_context(nc.semaphore("d_%s_%d" % (q, i))) for i in range(n_dma_sems)]
            self.dring[q] = {"sems": sems, "cnt": [0] * n_dma_sems, "next": 0}
        self.waited = {e: {} for e in self.eng}
        self.last_w = {}
        self.readers = {}
        self.prog = {e: [] for e in self.eng}
        self.n_ins = 0

    def _wait(self, e, tok):
        sem, val, src = tok
        if src == e and e == "pe":
            return
        w = self.waited[e]
        if w.get(sem.name, 0) >= val:
            return
        w[sem.name] = val
        self.prog[e].append(("w", sem, val))

    def _deps(self, e, reads, writes):
        for k in reads:
            t = self.last_w.get(k)
            if t is not None:
                self._wait(e, t)
            if k.startswith("ps"):
                for t in self.readers.get(k, ()):
                    if t[2] != e:
                        self._wait(e, t)
        for k in writes:
            t = self.last_w.get(k)
            if t is not None:
                self._wait(e, t)
            for t in self.readers.get(k, ()):
                self._wait(e, t)

    def _commit(self, tok, reads, writes):
        for k in reads:
            lst = self.readers.setdefault(k, [])
            lst.append(tok)
            if len(lst) > 16:
                best = {}
                for t in lst:
                    if t[0].name not in best or best[t[0].name][1] < t[1]:
                        best[t[0].name] = t
                self.readers[k] = list(best.values())
        for k in writes:
            self.last_w[k] = tok
            self.readers[k] = []

    def op(self, e, fn, reads=(), writes=()):
        self._deps(e, reads, writes)
        self.cnt[e] += 1
        self.n_ins += 1
        self.prog[e].append(("i", fn, self.sem[e], 1))
        tok = (self.sem[e], self.cnt[e], e)
        self._commit(tok, reads, writes)
        return tok

    def dma(self, q, out, in_, reads=(), writes=(), **kw):
        ring = self.dring[q]
        i = ring["next"]
        ring["next"] = (i + 1) % len(ring["sems"])
        sem = ring["sems"][i]
        if ring["cnt"][i] > 0:
            self._wait(q, (sem, ring["cnt"][i], None))
        self._deps(q, reads, writes)
        self.prog[q].append(("d", out, in_, kw, sem))
        self.n_ins += 1
        ring["cnt"][i] += 16
        tok = (sem, ring["cnt"][i], None)
        self._commit(tok, reads, writes)
        return tok

    def wait_all(self, e):
        for x in self.eng:
            if self.cnt[x] > 0 and x != e:
                self._wait(e, (self.sem[x], self.cnt[x], x))
        for q, ring in self.dring.items():
            for sem, c in zip(ring["sems"], ring["cnt"]):
                if c > 0:
                    self._wait(e, (sem, c, None))

    def emit(self):
        nc = self.nc
        self.wait_all("sp")
        prog = self.prog
        self.prog = {e: [] for e in self.eng}

        def run(e, engobj):
            for it in prog[e]:
                if it[0] == "w":
                    engobj.wait_ge(it[1], it[2])
                elif it[0] == "i":
                    it[1]().then_inc(it[2], it[3])
                else:
                    engobj.dma_start(out=it[1], in_=it[2], **it[3]).then_inc(it[4], 16)

        with nc.Block() as block:
            @block.sync
            def _(eng):
                run("sp", eng)

            @block.scalar
            def _(eng):
                run("act", eng)

            @block.tensor
            def _(eng):
                run("pe", eng)

            @block.vector
            def _(eng):
                run("dve", eng)

            @block.gpsimd
            def _(eng):
                run("pool", eng)


class Builder:
    def __init__(self, n_layers=DEPTH, debug=False, stop_after=None):
        self.n_layers = n_layers
        self.debug = debug
        self.stop_after = stop_after
        self.nc = bass.Bass("TRN2", target_bir_lowering=False)
        self.dbg_names = []

    def mm(self, out, lhsT, rhs, start, stop, r, w):
        nc = self.nc
        self.c.op("pe", lambda: nc.tensor.matmul(out, lhsT=lhsT, rhs=rhs, start=start, stop=stop), r, w)

    def tr(self, out, in_, r, w, f32=False):
        nc = self.nc
        n = in_.shape[0]
        ident = (self.ident_f if f32 else self.ident_b)[0:n, 0:n]
        self.c.op("pe", lambda: nc.tensor.transpose(out, in_, ident), list(r) + ["const"], w)

    def act(self, out, in_, func, r, w, scale=1.0, bias=0.0, accum_out=None):
        nc = self.nc
        kw = {}
        if accum_out is not None:
            kw["accum_out"] = accum_out
        self.c.op("act", lambda: nc.scalar.activation(out=out, in_=in_, func=func, scale=scale, bias=bias, **kw), r, w)

    def tt(self, e, out, in0, in1, op, r, w):
        eng = self.c.eng[e]
        self.c.op(e, lambda: eng.tensor_tensor(out=out, in0=in0, in1=in1, op=op), r, w)

    def ts(self, e, out, in0, s1, op0, r, w, s2=None, op1=ALU.bypass, accum_out=None):
        eng = self.c.eng[e]
        kw = {}
        if accum_out is not None:
            kw["accum_out"] = accum_out
        self.c.op(e, lambda: eng.tensor_scalar(out=out, in0=in0, scalar1=s1, scalar2=s2, op0=op0, op1=op1, **kw), r, w)

    def stt(self, out, in0, scalar, in1, op0, op1, r, w):
        nc = self.nc
        self.c.op("dve", lambda: nc.vector.scalar_tensor_tensor(out=out, in0=in0, scalar=scalar, in1=in1, op0=op0, op1=op1), r, w)

    def cp(self, e, out, in_, r, w):
        if e == "act":
            nc = self.nc
            self.c.op("act", lambda: nc.scalar.copy(out=out, in_=in_), r, w)
        else:
            eng = self.c.eng[e]
            self.c.op(e, lambda: eng.tensor_copy(out=out, in_=in_), r, w)

    def ms(self, e, ap, val, w):
        eng = self.c.eng[e]
        self.c.op(e, lambda: eng.memset(ap, val), (), w)

    def recip(self, out, in_, r, w):
        nc = self.nc
        self.c.op("dve", lambda: nc.vector.reciprocal(out=out, in_=in_), r, w)

    def red(self, out, in_, op, r, w, axis=AX.X):
        nc = self.nc
        self.c.op("dve", lambda: nc.vector.tensor_reduce(out=out, in_=in_, axis=axis, op=op), r, w)

    def sb(self, name, shape, dt, st=None):
        self.uid = getattr(self, "uid", 0) + 1
        return (st or self.st).enter_context(self.nc.sbuf_tensor("%s_u%d" % (name, self.uid), list(shape), dt))

    def scr(self, name, shape, dt):
        kind = "ExternalOutput" if self.debug else "Internal"
        if self.debug:
            self.dbg_names.append(name)
        return self.nc.dram_tensor(name, list(shape), dt, kind=kind).ap()

    def build(self):
        nc = self.nc
        L = DEPTH
        inp = lambda n, s, dt=F32: nc.dram_tensor(n, list(s), dt, kind="ExternalInput").ap()
        self.x_in = inp("x", [S, D])
        self.mem_in = inp("mem", [N_MEM, D])
        self.pos_in = inp("pos", [128, NT], I32)
        self.w_in = inp("w_in", [L, D, IN_COLS])
        self.mla_q_norm = inp("mla_q_norm", [L, 192])
        self.w_uq = inp("w_uq", [L, 192, 384])
        self.mla_kv_norm = inp("mla_kv_norm", [L, 128])
        self.w_ukv = inp("w_ukv", [L, 128, 512])
        self.diff_lambda = inp("diff_lambda", [L, 128])
        self.diff_subln = inp("diff_subln", [L, 64])
        self.w_out = inp("w_out", [L, D, D])
        self.ln_g = [inp(n, [L, D]) for n in ("ln_mix_g", "ln_mem_g", "ln_ffn_g")]
        self.ln_b = [inp(n, [L, D]) for n in ("ln_mix_b", "ln_mem_b", "ln_ffn_b")]
        self.wq = inp("xattn_wq", [L, D, D])
        self.wk = inp("xattn_wk", [L, D, D])
        self.wv = inp("xattn_wv", [L, D, D])
        self.wo = inp("xattn_wo", [L, D, D])
        self.w_router = inp("w_router", [L, D, 40])
        self.b_router = inp("b_router", [L, 40])
        self.e_gate = inp("expert_w_gate", [L, 32, D, 256])
        self.e_up = inp("expert_w_up", [L, 32, D, 256])
        self.e_down = inp("expert_w_down", [L, 32, 256, D])
        self.c_ident_b = inp("c_ident_b", [128, 128], BF16)
        self.c_ident_f = inp("c_ident_f", [128, 128], F32)
        self.c_mask_le = inp("c_mask_le", [128, 128], BF16)
        self.c_mask_lt = inp("c_mask_lt", [128, 128], BF16)
        self.c_tri = inp("c_tri", [128, 128], BF16)
        self.c_invf = inp("c_invf", [128, 28], F32)
        self.c_blk = inp("c_blk", [16, S], BF16)
        self.out = nc.dram_tensor("out", [S, D], F32, kind="ExternalOutput").ap()
        self.xT_s = self.scr("xT_s", [128, 8, S], BF16)
        self.xres_s = self.scr("xres_s", [S, D], F32)
        self.qk_s = self.scr("qk_s", [4, 4, 2, 96, S], BF16)
        self.v_s = self.scr("v_s", [S, 4 * 260], BF16)
        self.qkp_s = self.scr("qkp_s", [3, 2, 2, 128, S], BF16)
        self.mb_s = self.scr("mb_s", [64, S], BF16)
        self.o_s = self.scr("o_s", [S, D], BF16)
        self.od_s = self.scr("od_s", [2, S, 256], F32)
        self.gT_s = self.scr("gT_s", [32, S], F32)
        self.x1_s = self.scr("x1_s", [S, D], F32)

        with ExitStack() as st:
            self.st = st
            self.c = Ctx(nc, st)
            self.ps = [st.enter_context(nc.psum_tensor("ps%d" % i, [128, 512], F32)) for i in range(8)]
            self.ident_b = self.sb("ident_b", [128, 128], BF16)
            self.ident_f = self.sb("ident_f", [128, 128], F32)
            self.mask_le = self.sb("mask_le", [128, 128], BF16)
            self.mask_lt = self.sb("mask_lt", [128, 128], BF16)
            self.tri = self.sb("tri", [128, 128], BF16)
            self.ones_b = self.sb("ones_b", [128, 128], BF16)
            self.cs = self.sb("rope_cos", [128, NT, 28], F32)
            self.sn = self.sb("rope_sin", [128, NT, 28], F32)
            self.memT = self.sb("memT", [128, 8, N_MEM], BF16)
            self.phase0()
            for l in range(self.n_layers if self.stop_after != (0, 0) else 0):
                last = (l == DEPTH - 1)
                self.phase1(l)
                if self.stop_after == (l, 1):
                    break
                self.phase2(l)
                if self.stop_after == (l, 2):
                    break
                self.phase34(l)
                if self.stop_after == (l, 3):
                    break
                self.phase5(l, last)
                if self.stop_after == (l, 5):
                    break
        return nc

    def phase0(self):
        nc, c = self.nc, self.c
        with ExitStack() as st:
            for dst, src, k in ((self.ident_b, self.c_ident_b, "const"), (self.ident_f, self.c_ident_f, "const"),
                                (self.mask_le, self.c_mask_le, "const"), (self.mask_lt, self.c_mask_lt, "const"),
                                (self.tri, self.c_tri, "const")):
                c.dma("sp", dst[:, :], src, writes=[k])
            self.ms("pool", self.ones_b[:, :], 1.0, ["const"])
            pos_i = self.sb("pos_i", [128, NT], I32, st)
            pos_f = self.sb("pos_f", [128, NT], F32, st)
            invf = self.sb("invf", [128, 28], F32, st)
            ang = self.sb("ang", [128, NT, 28], F32, st)
            t1 = self.sb("rp_t1", [128, NT, 28], F32, st)
            ki = self.sb("rp_ki", [128, NT, 28], I32, st)
            kf = self.sb("rp_kf", [128, NT, 28], F32, st)
            r = self.sb("rp_r", [128, NT, 28], F32, st)
            m = self.sb("rp_m", [128, NT, 28], F32, st)
            rc = self.sb("rp_rc", [128, NT, 28], F32, st)
            c.dma("sp", pos_i[:, :], self.pos_in, writes=["pos_i"])
            c.dma("sp", invf[:, :], self.c_invf, writes=["invf"])
            self.cp("dve", pos_f[:, :], pos_i[:, :], ["pos_i"], ["pos_f"])
            self.tt("dve", ang[:, :, :], pos_f[:, :].unsqueeze(2).to_broadcast([128, NT, 28]),
                    invf[:, :].unsqueeze(1).to_broadcast([128, NT, 28]), ALU.mult, ["pos_f", "invf"], ["ang"])
            self.ts("dve", t1[:, :, :], ang[:, :, :], 1.0 / TWO_PI, ALU.mult, ["ang"], ["rp_t1"])
            self.cp("dve", ki[:, :, :], t1[:, :, :], ["rp_t1"], ["rp_ki"])
            self.cp("dve", kf[:, :, :], ki[:, :, :], ["rp_ki"], ["rp_kf"])
            HI = 6.28125
            LO = TWO_PI - HI
            self.stt(r[:, :, :], kf[:, :, :], -HI, ang[:, :, :], ALU.mult, ALU.add, ["rp_kf", "ang"], ["rp_r"])
            self.stt(r[:, :, :], kf[:, :, :], -LO, r[:, :, :], ALU.mult, ALU.add, ["rp_kf", "rp_r"], ["rp_r"])

            def wrap(buf, key):
                self.ts("dve", m[:, :, :], buf[:, :, :], math.pi, ALU.is_gt, [key], ["rp_m"])
                self.stt(buf[:, :, :], m[:, :, :], -TWO_PI, buf[:, :, :], ALU.mult, ALU.add, ["rp_m", key], [key])
                self.ts("dve", m[:, :, :], buf[:, :, :], -math.pi, ALU.is_lt, [key], ["rp_m"])
                self.stt(buf[:, :, :], m[:, :, :], TWO_PI, buf[:, :, :], ALU.mult, ALU.add, ["rp_m", key], [key])
                self.ts("dve", buf[:, :, :], buf[:, :, :], math.pi, ALU.min, [key], [key], s2=-math.pi, op1=ALU.max)

            wrap(r, "rp_r")
            self.ts("dve", rc[:, :, :], r[:, :, :], math.pi / 2, ALU.add, ["rp_r"], ["rp_rc"])
            wrap(rc, "rp_rc")
            self.act(self.sn[:, :, :], r[:, :, :], AF.Sin, ["rp_r"], ["rope"])
            self.act(self.cs[:, :, :], rc[:, :, :], AF.Sin, ["rp_rc"], ["rope"])
            memb = self.sb("memb", [128, 2, D], BF16, st)
            c.dma("pool", memb[:, :, :], self.mem_in.rearrange("(t p) d -> p t d", p=128), writes=["memb"])
            for t in range(2):
                pst = self.ps[t][:, :].bitcast(BF16)
                for k in range(8):
                    self.tr(pst[:, k * 128:(k + 1) * 128], memb[:, t, k * 128:(k + 1) * 128], ["memb"], ["ps%d" % t])
                self.cp("dve", self.memT[:, :, t * 128:(t + 1) * 128],
                        pst.rearrange("p (k m) -> p k m", k=8), ["ps%d" % t], ["memT"])
            xb = [self.sb("p0_xb%d" % i, [128, D], BF16, st) for i in range(2)]
            xt = [self.sb("p0_xt%d" % i, [128, 8, 128], BF16, st) for i in range(2)]
            for t in range(NT):
                i = t % 2
                c.dma("pool", xb[i][:, :], self.x_in[t * 128:(t + 1) * 128, :], writes=["p0_xb%d" % i])
                pst = self.ps[2 + i][:, :].bitcast(BF16)
                for k in range(8):
                    self.tr(pst[:, k * 128:(k + 1) * 128], xb[i][:, k * 128:(k + 1) * 128], ["p0_xb%d" % i], ["ps%d" % (2 + i)])
                self.cp("dve" if i == 0 else "act", xt[i][:, :, :], pst.rearrange("p (k m) -> p k m", k=8),
                        ["ps%d" % (2 + i)], ["p0_xt%d" % i])
                c.dma("sp", self.xT_s[:, :, t * 128:(t + 1) * 128], xt[i][:, :, :], reads=["p0_xt%d" % i], writes=["xT_s"])
            c.emit()

    def rope(self, e, out1, out2, a, b, cos, sin, tmp1, tmp2, rk, wk_):
        k1, k2 = "rtmp1" + e, "rtmp2" + e
        self.tt(e, tmp1, a, cos, ALU.mult, rk + ["rope"], [k1])
        self.tt(e, tmp2, b, sin, ALU.mult, rk + ["rope"], [k2])
        self.tt(e, out1, tmp1, tmp2, ALU.subtract, [k1, k2], wk_)
        self.tt(e, tmp1, a, sin, ALU.mult, rk + ["rope"], [k1])
        self.tt(e, tmp2, b, cos, ALU.mult, rk + ["rope"], [k2])
        self.tt(e, out2, tmp1, tmp2, ALU.add, [k1, k2], wk_)

    def phase1(self, l):
        nc, c = self.nc, self.c
        with ExitStack() as st:
            win = self.sb("p1_win", [128, 8, IN_COLS], BF16, st)
            wuq = self.sb("p1_wuq", [128, 2, 384], BF16, st)
            wukv = self.sb("p1_wukv", [128, 512], BF16, st)
            gq = self.sb("p1_gq", [128, 2], F32, st)
            gkv = self.sb("p1_gkv", [128, 1], F32, st)
            for k in range(8):
                c.dma("pool", win[:, k, :], self.w_in[l, k * 128:(k + 1) * 128, :], writes=["p1_win%d" % k])
            wuq_f = self.sb("p1_wuq_f", [128, 2, 384], F32, st)
            wukv_f = self.sb("p1_wukv_f", [128, 512], F32, st)
            c.dma("sp", wuq_f[:, 0, :], self.w_uq[l, 0:128, :], writes=["p1_wuq_f"])
            c.dma("sp", wuq_f[0:64, 1, :], self.w_uq[l, 128:192, :], writes=["p1_wuq_f"])
            c.dma("sp", wukv_f[:, :], self.w_ukv[l, :, :], writes=["p1_wukv_f"])
            c.dma("sp", gq[:, 0:1], self.mla_q_norm[l, 0:128].rearrange("(p o) -> p o", o=1), writes=["p1_gq"])
            c.dma("sp", gq[0:64, 1:2], self.mla_q_norm[l, 128:192].rearrange("(p o) -> p o", o=1), writes=["p1_gq"])
            c.dma("sp", gkv[:, 0:1], self.mla_kv_norm[l, :].rearrange("(p o) -> p o", o=1), writes=["p1_gkv"])
            self.ts("dve", wuq[:, 0, :], wuq_f[:, 0, :], gq[:, 0:1], ALU.mult, ["p1_wuq_f", "p1_gq"], ["p1_wuq"])
            self.ts("dve", wuq[0:64, 1, :], wuq_f[0:64, 1, :], gq[0:64, 1:2], ALU.mult, ["p1_wuq_f", "p1_gq"], ["p1_wuq"])
            self.ts("dve", wukv[:, :], wukv_f[:, :], gkv[:, 0:1], ALU.mult, ["p1_wukv_f", "p1_gkv"], ["p1_wukv"])

            xT = [self.sb("p1_xT%d" % i, [128, 8, 512], BF16, st) for i in range(2)]
            c0 = [self.sb("p1_c0_%d" % i, [128, 352], BF16, st) for i in range(2)]
            ssq = [self.sb("p1_ssq%d" % i, [128, 4], F32, st) for i in range(2)]
            junk = self.sb("p1_junk", [128, 512], F32, st)
            qkS = [self.sb("p1_qkS%d" % i, [128, 512], BF16, st) for i in range(2)]
            qkM = [self.sb("p1_qkM%d" % i, [128, 512], BF16, st) for i in range(2)]
            qkD = [self.sb("p1_qkD%d" % i, [128, 512], BF16, st) for i in range(2)]
            qA = [self.sb("p1_qA%d" % i, [128, 4, 96], BF16, st) for i in range(2)]
            kA = [self.sb("p1_kA%d" % i, [128, 4, 96], BF16, st) for i in range(2)]
            vall = [self.sb("p1_vall%d" % i, [128, 4, 4, 65], BF16, st) for i in range(2)]
            vst = [[vall[i][:, m, :, :] for i in range(2)] for m in range(4)]
            latT = [self.sb("p1_latT%d" % i, [128, 3, 128], BF16, st) for i in range(2)]
            rt1 = self.sb("p1_rt1", [128, 128], F32, st)
            rt2 = self.sb("p1_rt2", [128, 128], F32, st)
            rt5 = self.sb("p1_rt5", [128, 16], F32, st)
            rt6 = self.sb("p1_rt6", [128, 16], F32, st)
            rt3 = self.sb("p1_rt3", [128, 128], F32, st)
            rt4 = self.sb("p1_rt4", [128, 128], F32, st)
            hf = [self.sb("p1_hf%d" % i, [128, 512], F32, st) for i in range(2)]
            kpe = [self.sb("p1_kpe%d" % i, [128, 32], F32, st) for i in range(2)]
            TA = [self.sb("p1_TA%d" % bp, [96, 4, 2, 512], BF16, st) for bp in range(2)]
            TP = [{nm: self.sb("p1_TP%d%s" % (bp, nm), [128, 2, 2, 512], BF16, st) for nm in ("S", "M", "D")} for bp in range(2)]
            mbT = [self.sb("p1_mbT%d" % bp, [64, 512], BF16, st) for bp in range(2)]
            kmT = self.sb("p1_kmT", [128, 2, 16], BF16, st)
            kpart = self.sb("p1_kpart", [128, 2, 2], F32, st)
            ksum = self.sb("p1_ksum", [128, 2], F32, st)
            gate = self.sb("p1_gate", [128, 4, 16], F32, st)
            top8 = self.sb("p1_top8", [128, 4, 8], F32, st)
            mb = [self.sb("p1_mb%d" % i, [128, 4, 16], BF16, st) for i in range(2)]
            for m in range(4):
                for i in range(2):
                    self.ms("pool", vst[m][i][:, :, 64:65], 1.0, ["p1_v%d_%d" % (m, i)])
            self.ms("pool", kmT[:, :, :], 0.0, ["p1_kmT"])

            def load_x(b):
                c.dma("sp", xT[b % 2][:, :, :], self.xT_s[:, :, b * 512:(b + 1) * 512], reads=["xT_s"], writes=["p1_xT%d" % (b % 2)])

            psi = [0]

            def next_ps():
                i = psi[0] % 3
                psi[0] += 1
                return self.ps[i], "ps%d" % i

            tbank = [0]

            def next_tbank():
                tbank[0] += 1
                return 3 if tbank[0] % 2 == 0 else 7

            def tk(nm, bp, role, j):
                return "p1_T%d%s%d_%d" % (bp, nm, role, j)

            def mm_chunk(t, ci):
                b, tl = t // 4, t % 4
                c0_, cw = CH[ci]
                ps, pk = next_ps()
                for k in range(8):
                    self.mm(ps[:, 0:cw], xT[b % 2][:, k, tl * 128:(tl + 1) * 128], win[:, k, c0_:c0_ + cw],
                            k == 0, k == 7, ["p1_xT%d" % (b % 2), "p1_win%d" % k], [pk])
                return ps, pk

            def chunk0(t):
                i = t % 2
                ps, pk = mm_chunk(t, 0)
                cosA, sinA = self.cs[:, t, 0:16], self.sn[:, t, 0:16]
                ck = "p1_c0_%d" % i
                sk = "p1_ssq%d" % i
                self.cp("act", c0[i][:, 0:352], ps[:, 0:352], [pk], [ck])
                self.act(junk[:, 0:192], ps[:, 0:192], AF.Square, [pk], ["p1_junk", sk], accum_out=ssq[i][:, 0:1])
                self.act(junk[:, 192:320], ps[:, 192:320], AF.Square, [pk], ["p1_junk", sk], accum_out=ssq[i][:, 1:2])
                self.cp("pool", hf[i][:, 0:32], c0[i][:, 320:352], [ck], ["p1_hfa%d" % i])
                self.rope("pool", kpe[i][:, 0:16], kpe[i][:, 16:32], hf[i][:, 0:16], hf[i][:, 16:32], cosA, sinA,
                          rt5[:, 0:16], rt6[:, 0:16], ["p1_hfa%d" % i], ["p1_kpe%d" % i])
                self.ts("dve", ssq[i][:, 2:3], ssq[i][:, 0:1], 1.0 / 192, ALU.mult, [sk], [sk], s2=RMS_EPS, op1=ALU.add)
                self.ts("dve", ssq[i][:, 3:4], ssq[i][:, 1:2], 1.0 / 128, ALU.mult, [sk], [sk], s2=RMS_EPS, op1=ALU.add)
                self.act(ssq[i][:, 2:4], ssq[i][:, 2:4], AF.Ln, [sk], [sk])
                self.act(ssq[i][:, 2:4], ssq[i][:, 2:4], AF.Exp, [sk], [sk], scale=-0.5)

            def mla_up(t):
                i = t % 2
                cosA, sinA = self.cs[:, t, 0:16], self.sn[:, t, 0:16]
                ck, sk, lk = "p1_c0_%d" % i, "p1_ssq%d" % i, "p1_latT%d" % i
                pt, ptk = self.ps[4], "ps4"
                ptb = pt[:, :].bitcast(BF16)
                self.tr(ptb[:, 0:128], c0[i][:, 0:128], [ck], [ptk])
                self.tr(ptb[0:64, 128:256], c0[i][:, 128:192], [ck], [ptk])
                self.tr(ptb[:, 256:384], c0[i][:, 192:320], [ck], [ptk])
                self.cp("dve", latT[i][:, 0:3:2, :], ptb[:, 0:384].rearrange("p (a m) -> p a m", a=3)[:, 0:3:2, :], [ptk], [lk])
                self.cp("dve", latT[i][0:64, 1, :], ptb[0:64, 128:256], [ptk], [lk])
                pq, pqk = self.ps[5], "ps5"
                self.mm(pq[:, 0:384], latT[i][:, 0, :], wuq[:, 0, :], True, False, [lk, "p1_wuq"], [pqk])
                self.mm(pq[:, 0:384], latT[i][0:64, 1, :], wuq[0:64, 1, :], False, True, [lk, "p1_wuq"], [pqk])
                pkv, pkvk = self.ps[6], "ps6"
                self.mm(pkv[:, :], latT[i][:, 2, :], wukv[:, :], True, True, [lk, "p1_wukv"], [pkvk])
                pq3 = pq[:, 0:384].rearrange("p (h d) -> p h d", h=4)
                qk_ = "p1_qA%d" % i
                self.ts("dve", qA[i][:, :, 0:64], pq3[:, :, 0:64], ssq[i][:, 2:3], ALU.mult, [pqk, sk], [qk_])
                hk = "p1_hfb%d" % i
                h3 = hf[i][:, 64:192].rearrange("p (h d) -> p h d", h=4)
                self.ts("dve", h3, pq3[:, :, 64:96], ssq[i][:, 2:3], ALU.mult, [pqk, sk], [hk])
                cb = cosA.unsqueeze(1).to_broadcast([128, 4, 16])
                sb_ = sinA.unsqueeze(1).to_broadcast([128, 4, 16])
                self.rope("dve", qA[i][:, :, 64:80], qA[i][:, :, 80:96], h3[:, :, 0:16], h3[:, :, 16:32], cb, sb_,
                          rt1[:, 0:64].rearrange("p (h d) -> p h d", h=4), rt2[:, 0:64].rearrange("p (h d) -> p h d", h=4),
                          [hk], [qk_])
                pkv3 = pkv[:, :].rearrange("p (h d) -> p h d", h=4)
                kk_ = "p1_kA%d" % i
                self.ts("dve", kA[i][:, :, 0:64], pkv3[:, :, 0:64], ssq[i][:, 3:4], ALU.mult, [pkvk, sk], [kk_])
                self.ts("dve", vst[0][i][:, :, 0:64], pkv3[:, :, 64:128], ssq[i][:, 3:4], ALU.mult, [pkvk, sk], ["p1_v0_%d" % i])
                self.cp("pool", kA[i][:, :, 64:96], kpe[i][:, :].unsqueeze(1).to_broadcast([128, 4, 32]), ["p1_kpe%d" % i], [kk_])

            def chunk_plain(t, ci):
                i = t % 2
                ps, pk = mm_chunk(t, ci)
                if ci == 1:
                    self.cp("act", qkS[i][:, :], ps[:, :], [pk], ["p1_qkS%d" % i])
                elif ci == 2:
                    self.cp("act", vst[1][i][:, :, 0:64], ps[:, 0:256].rearrange("p (h d) -> p h d", h=4), [pk], ["p1_v1_%d" % i])
                    self.cp("act", vst[2][i][:, :, 0:64], ps[:, 256:512].rearrange("p (h d) -> p h d", h=4), [pk], ["p1_v2_%d" % i])
                else:
                    self.cp("act", vst[3][i][:, :, 0:64], ps[:, 0:256].rearrange("p (h d) -> p h d", h=4), [pk], ["p1_v3_%d" % i])

            def chunk_rope(t, ci):
                i = t % 2
                ps, pk = mm_chunk(t, ci)
                dst = (qkM if ci == 3 else qkD)[i]
                dk = ("p1_qkM%d" if ci == 3 else "p1_qkD%d") % i
                hk = "p1_hfc%d_%d" % (ci, i)
                self.cp("dve", dst[:, :], ps[:, :], [pk], [dk])
                if ci == 3:
                    g, nr = 8, 8
                    cb, sb_ = self.cs[:, t, 16:24], self.sn[:, t, 16:24]
                    hbase = 192
                else:
                    g, nr = 16, 4
                    cb, sb_ = self.cs[:, t, 24:28], self.sn[:, t, 24:28]
                    hbase = 320
                p3 = ps[:, :].rearrange("p (g d) -> p g d", g=g)
                d3 = dst[:, :].rearrange("p (g d) -> p g d", g=g)
                cbb = cb.unsqueeze(1).to_broadcast([128, g, nr])
                sbb = sb_.unsqueeze(1).to_broadcast([128, g, nr])
                h3 = hf[i][:, hbase:hbase + g * 2 * nr].rearrange("p (g d) -> p g d", g=g)
                self.cp("dve", h3, p3[:, :, 0:2 * nr], [pk], [hk])
                self.rope("pool", d3[:, :, 0:nr], d3[:, :, nr:2 * nr], h3[:, :, 0:nr], h3[:, :, nr:2 * nr], cbb, sbb,
                          rt3[:, 0:g * nr].rearrange("p (g d) -> p g d", g=g), rt4[:, 0:g * nr].rearrange("p (g d) -> p g d", g=g),
                          [hk], [dk])

            def v_store(t):
                i = t % 2
                c.dma("pool", self.v_s[t * 128:(t + 1) * 128, :], vall[i][:, :, :, :].rearrange("p m h d -> p (m h d)"),
                      reads=["p1_v%d_%d" % (m, i) for m in range(4)], writes=["v_s"])

            def tr_mla(t):
                i, bp = t % 2, (t // 4) % 2
                cols = slice((t % 4) * 128, (t % 4 + 1) * 128)
                for role, src, sk in ((0, qA[i], "p1_qA%d" % i), (1, kA[i], "p1_kA%d" % i)):
                    tb_ = next_tbank()
                    ptb = self.ps[tb_][:, :].bitcast(BF16)
                    for h in range(4):
                        self.tr(ptb[0:96, h * 128:(h + 1) * 128], src[:, h, :], [sk], ["ps%d" % tb_])
                    for h in range(4):
                        self.cp("act" if role == 0 else "dve", TA[bp][:, h, role, cols], ptb[0:96, h * 128:(h + 1) * 128], ["ps%d" % tb_],
                                ["p1_TA%d" % bp])

            def tr_pairs(t, nm):
                i, bp = t % 2, (t // 4) % 2
                cols = slice((t % 4) * 128, (t % 4 + 1) * 128)
                src, sk = {"S": (qkS[i], "p1_qkS%d" % i), "M": (qkM[i], "p1_qkM%d" % i), "D": (qkD[i], "p1_qkD%d" % i)}[nm]
                tb_ = next_tbank()
                ptb = self.ps[tb_][:, :].bitcast(BF16)
                for j in range(4):
                    self.tr(ptb[:, j * 128:(j + 1) * 128], src[:, j * 128:(j + 1) * 128], [sk], ["ps%d" % tb_])
                for j in range(4):
                    role, pr = j // 2, j % 2
                    self.cp("act" if nm != "D" else "dve", TP[bp][nm][:, role, pr, cols], ptb[:, j * 128:(j + 1) * 128], ["ps%d" % tb_],
                            ["p1_TP%d%s" % (bp, nm)])

            def gating(t):
                i, bp = t % 2, (t // 4) % 2
                cols = slice((t % 4) * 128, (t % 4 + 1) * 128)
                blk = t // 2
                pg, pgk = self.ps[6], "ps6"
                for pr in range(2):
                    self.mm(pg[:, pr:pr + 1], qkM[i][:, 256 + pr * 128:256 + (pr + 1) * 128], self.ones_b[:, 0:1], True, True,
                            ["p1_qkM%d" % i, "const"], [pgk])
                self.cp("dve", kpart[:, t % 2, :], pg[:, 0:2], [pgk], ["p1_kpart"])
                if t % 2 == 1:
                    self.tt("dve", ksum[:, :], kpart[:, 0, :], kpart[:, 1, :], ALU.add, ["p1_kpart"], ["p1_ksum"])
                    self.ts("dve", kmT[:, :, blk], ksum[:, :], 1.0 / 256, ALU.mult, ["p1_ksum"], ["p1_kmT"])
                own = blk
                mk = "p1_mb%d" % i
                if own >= 4:
                    g3 = gate[:, :, :].rearrange("p (a b) j -> p a b j", b=2)
                    self.ms("pool", gate[:, :, :], -BIGV, ["p1_gate"])
                    for hh, (pg, pgk) in enumerate(((self.ps[5], "ps5"), (self.ps[6], "ps6"))):
                        p0 = hh * 64
                        for pr in range(2):
                            qT = TP[bp]["M"][p0:p0 + 64, 0, pr, cols]
                            self.mm(pg[:, pr * 16:(pr + 1) * 16], qT, kmT[p0:p0 + 64, pr, :],
                                    True, True, ["p1_TP%dM" % bp, "p1_kmT"], [pgk])
                        self.cp("dve", g3[:, :, hh, 0:own], pg[:, 0:32].rearrange("p (a j) -> p a j", a=2)[:, :, 0:own], [pgk], ["p1_gate"])
                    for h in range(4):
                        nc_ = self.nc
                        gh = gate[:, h, :]
                        th = top8[:, h, :]
                        c.op("dve", (lambda gh=gh, th=th: nc_.vector.max(out=th, in_=gh)), ["p1_gate"], ["p1_top8"])
                    for h in range(4):
                        self.ts("dve", mb[i][:, h, :], gate[:, h, :], top8[:, h, 2:3], ALU.is_ge, ["p1_gate", "p1_top8"], [mk],
                                s2=-1.0, op1=ALU.add)
                    self.ms("pool", mb[i][:, :, own:own + 1], 0.0, [mk])
                else:
                    self.ms("pool", mb[i][:, :, :], 0.0, [mk])
                pm, pmk = self.ps[4], "ps4"
                pmb = pm[:, :].bitcast(BF16)
                self.tr(pmb[0:64, 0:128], mb[i][:, :, :].rearrange("p h j -> p (h j)"), [mk], [pmk])
                self.cp("dve", mbT[bp][:, cols], pmb[0:64, 0:128], [pmk], ["p1_mbT%d" % bp])

            def block_store(b):
                bp = b % 2
                bc = slice(b * 512, (b + 1) * 512)
                c.dma("sp", self.qk_s[0, :, :, 0:96, bc].rearrange("h r p c -> p h r c"), TA[bp][:, :, :, :], reads=["p1_TA%d" % bp], writes=["qk_s"])
                for mi, nm in ((0, "S"), (1, "M"), (2, "D")):
                    c.dma("sp", self.qkp_s[mi, :, :, :, bc].rearrange("r q p c -> p r q c"), TP[bp][nm][:, :, :, :],
                          reads=["p1_TP%d%s" % (bp, nm)], writes=["qk_s"])
                c.dma("sp", self.mb_s[:, bc], mbT[bp][:, :], reads=["p1_mbT%d" % bp], writes=["qk_s"])

            load_x(0)
            for t in range(NT + 1):
                cur = t if t < NT else None
                prv = t - 1 if t >= 1 else None
                if cur is not None and cur % 4 == 0 and cur // 4 + 1 < NB:
                    load_x(cur // 4 + 1)
                if cur is not None:
                    chunk0(cur)
                    chunk_plain(cur, 1)
                if prv is not None:
                    tr_mla(prv)
                if cur is not None:
                    chunk_plain(cur, 2)
                    mla_up(cur)
                    chunk_rope(cur, 3)
                if prv is not None:
                    tr_pairs(prv, "S")
                    tr_pairs(prv, "M")
                if cur is not None:
                    chunk_rope(cur, 4)
                if prv is not None:
                    tr_pairs(prv, "D")
                if cur is not None:
                    chunk_plain(cur, 5)
                    v_store(cur)
                if prv is not None:
                    gating(prv)
                    if prv % 4 == 3:
                        block_store(prv // 4)
            for h in range(4):
                c.dma("sp", self.qk_s[2, h, 1, 64:80, :], self.c_blk, writes=["qk_s"])
            c.emit()


    def phase2(self, l):
        nc, c = self.nc, self.c
        lambda_init = 0.8 - 0.6 * math.exp(-0.3 * l)
        with ExitStack() as st:
            KT = [self.sb("p2_KT%d" % i, [128, S], BF16, st) for i in range(2)]
            QT = [self.sb("p2_QT%d" % i, [128, S], BF16, st) for i in range(2)]
            Vm = [self.sb("p2_Vm%d" % i, [128, NT, 260], BF16, st) for i in range(2)]
            om = [self.sb("p2_om%d" % i, [128, NT, 256], BF16, st) for i in range(2)]
            od = [self.sb("p2_od%d" % i, [128, NT, 64], F32, st) for i in range(2)]
            PT = [self.sb("p2_PT%d" % i, [128, 512], BF16, st) for i in range(4)]
            SBANK = [0, 1, 2, 7]
            E32 = [self.sb("p2_E%d" % i, [128, 512], F32, st) for i in range(3)]
            Psp = [self.sb("p2_P%d" % i, [128, 512], BF16, st) for i in range(3)]
            T = [self.sb("p2_T%d" % i, [128, 512], F32, st) for i in range(3)]
            Aex = [self.sb("p2_A%d" % i, [128, 512], BF16, st) for i in range(3)]
            Run = self.sb("p2_Run", [128, 512], BF16, st)
            rc = self.sb("p2_rc", [128, 4], F32, st)
            hcount = [0]
            vcount = [0]

            def load_head(m, h, rows, r0=0):
                j = hcount[0] % 2
                hcount[0] += 1
                for buf, bk in ((KT[j], "p2_KT%d" % j), (QT[j], "p2_QT%d" % j)):
                    if rows <= 32:
                        self.ms("pool", buf[32:64, :], 0.0, [bk])
                    self.ms("pool", buf[64:128, :], 0.0, [bk])
                for role, buf, bk in ((1, KT[j], "p2_KT%d" % j), (0, QT[j], "p2_QT%d" % j)):
                    if m == 0:
                        src = self.qk_s[0, h, role, 0:rows, :]
                    else:
                        rb = (h % 2) * 64 + r0
                        src = self.qkp_s[m - 1, role, h // 2, rb:rb + min(rows, 64), :]
                    c.dma("sp", buf[0:min(rows, 96 if m == 0 else 64), :], src, reads=["qk_s"], writes=[bk])
                if m == 2:
                    c.dma("sp", QT[j][64:80, :], self.mb_s[h * 16:(h + 1) * 16, :], reads=["qk_s"], writes=["p2_QT%d" % j])
                    c.dma("sp", KT[j][64:80, :], self.c_blk, writes=["p2_KT%d" % j])
                return KT[j], QT[j], ["p2_KT%d" % j, "p2_QT%d" % j]

            def load_v(m):
                j = vcount[0] % 2
                vcount[0] += 1
                c.dma("sp", Vm[j][:, :, :], self.v_s[:, m * 260:(m + 1) * 260].rearrange("(t p) c -> p t c", p=128), reads=["v_s"], writes=["p2_Vm%d" % j])
                return Vm[j], "p2_Vm%d" % j

            sslot = [0]

            def softmax_head(kt, qt, kqk, R, scale, vm, vkey, h, out_fn):
                for b in range(NB):
                    n = 4 * b + 4

                    def S_stage(i):
                        j = i - 4 * b
                        c0 = max(0, j) * 128
                        slot = sslot[0]
                        sslot[0] = (slot + 1) % 4
                        ps, pk = self.ps[SBANK[slot]], "ps%d" % SBANK[slot]
                        self.mm(ps[:, c0:512], kt[:, i * 128:(i + 1) * 128], qt[:, b * 512 + c0:(b + 1) * 512], True, True, kqk, [pk])
                        pt, ptk = PT[slot], "p2_PT%d" % slot
                        self.act(pt[:, c0:512], ps[:, c0:512], AF.Exp, [pk], [ptk], scale=scale)
                        if j >= 0:
                            self.tt("pool", pt[:, c0:c0 + 128], pt[:, c0:c0 + 128], self.mask_le[:, :], ALU.mult, [ptk, "const"], [ptk])
                        return slot

                    def PV_stage(i, slot):
                        for s_ in range(4):
                            if 4 * b + s_ >= i:
                                self.mm(self.ps[3 + s_][:, 0:65], PT[slot][:, s_ * 128:(s_ + 1) * 128], vm[:, i, h * 65:(h + 1) * 65],
                                        i == 0, i == 4 * b + s_, ["p2_PT%d" % slot, vkey], ["ps%d" % (3 + s_)])

                    slots = {}
                    for i in range(min(2, n)):
                        slots[i] = S_stage(i)
                    for i in range(n):
                        if i + 2 < n:
                            slots[i + 2] = S_stage(i + 2)
                        PV_stage(i, slots[i])
                        if i >= 4 * b:
                            s_ = i - 4 * b
                            out_fn(b, s_, self.ps[3 + s_], "ps%d" % (3 + s_))

            def sb_head(kt, qt, kqk, vm, vkey, h, omt, omk):
                scale = 0.125
                for b in range(NB):
                    order = list(range(4 * b + 3, -1, -1))
                    n = len(order)
                    self.ms("pool", Run[:, :], 0.0, ["p2_Run"])

                    def geo(idx):
                        i = order[idx]
                        j = i - 4 * b
                        return i, j, max(0, j) * 128

                    def A_stage(idx):
                        i, j, c0 = geo(idx)
                        z, q3 = idx % 2, idx % 3
                        ps, pk = self.ps[z], "ps%d" % z
                        self.mm(ps[:, c0:512], kt[:, i * 128:(i + 1) * 128], qt[:, b * 512 + c0:(b + 1) * 512], True, True, kqk, [pk])
                        e, ek = E32[q3], "p2_E%d" % q3
                        self.act(e[:, c0:512], ps[:, c0:512], AF.Exp, [pk], [ek], scale=scale)
                        if j >= 0:
                            self.tt("pool", e[:, c0:c0 + 128], e[:, c0:c0 + 128], self.mask_lt[:, :], ALU.mult, [ek, "const"], [ek])
                        self.act(Psp[q3][:, c0:512], e[:, c0:512], AF.Ln, [ek], ["p2_P%d" % q3], bias=1.0)

                    def B_stage(idx):
                        i, j, c0 = geo(idx)
                        z, q3 = idx % 2, idx % 3
                        af, afk = (self.ps[2], "ps2") if z == 0 else (self.ps[7], "ps7")
                        pk2 = "p2_P%d" % q3
                        first = idx == 0
                        self.mm(af[:, c0:512], self.tri[:, :], Psp[q3][:, c0:512], True, first, ["const", pk2], [afk])
                        if not first:
                            self.mm(af[:, c0:512], self.ones_b[:, :], Run[:, c0:512], False, True, ["const", "p2_Run"], [afk])
                        self.tt("pool", Run[:, c0:512], Run[:, c0:512], Psp[q3][:, c0:512], ALU.add, ["p2_Run", pk2], ["p2_Run"])
                        t, tk = T[q3], "p2_T%d" % q3
                        self.stt(t[:, c0:512], self.ps[z][:, c0:512], scale, Psp[q3][:, c0:512], ALU.mult, ALU.subtract, ["ps%d" % z, pk2], [tk])
                        self.tt("dve", t[:, c0:512], t[:, c0:512], af[:, c0:512], ALU.subtract, [tk, afk], [tk])
                        a, ak = Aex[q3], "p2_A%d" % q3
                        self.act(a[:, c0:512], t[:, c0:512], AF.Exp, [tk], [ak])
                        if j >= 0:
                            self.tt("pool", a[:, c0:c0 + 128], a[:, c0:c0 + 128], self.mask_lt[:, :], ALU.mult, [ak, "const"], [ak])

                    def C_stage(idx):
                        i, j, c0 = geo(idx)
                        q3 = idx % 3
                        for s_ in range(4):
                            if 4 * b + s_ >= i:
                                self.mm(self.ps[3 + s_][:, 0:64], Aex[q3][:, s_ * 128:(s_ + 1) * 128], vm[:, i, h * 65:h * 65 + 64],
                                        i == 4 * b + s_, i == 0, ["p2_A%d" % q3, vkey], ["ps%d" % (3 + s_)])

                    for step in range(n + 2):
                        if step < n:
                            A_stage(step)
                        if 0 <= step - 1 < n:
                            B_stage(step - 1)
                        if 0 <= step - 2 < n:
                            C_stage(step - 2)
                    for s_ in range(4):
                        self.cp("dve", omt[:, 4 * b + s_, h * 64:(h + 1) * 64], self.ps[3 + s_][:, 0:64], ["ps%d" % (3 + s_)], [omk])

            def norm_out(dst, dkey, col0, f32=False):
                def fn(b, s_, O, ok):
                    self.recip(rc[:, s_:s_ + 1], O[:, 64:65], [ok], ["p2_rc"])
                    self.ts("dve", dst[:, 4 * b + s_, col0:col0 + 64], O[:, 0:64], rc[:, s_:s_ + 1], ALU.mult, [ok, "p2_rc"], [dkey])
                return fn

            def store_om(m, omt, omk):
                c.dma("sp", self.o_s[:, m * 256:(m + 1) * 256].rearrange("(t p) c -> p t c", p=128), omt[:, :, :], reads=[omk], writes=["o_s"])

            oi = 0
            for m, R, scale in ((0, 96, 96 ** -0.5), (2, 80, 0.125)):
                vm, vkey = load_v(m)
                omt, omk = om[oi % 2], "p2_om%d" % (oi % 2)
                oi += 1
                for h in range(4):
                    kt, qt, kqk = load_head(m, h, R)
                    softmax_head(kt, qt, kqk, R, scale, vm, vkey, h, norm_out(omt, omk, h * 64))
                store_om(m, omt, omk)
            vm, vkey = load_v(1)
            omt, omk = om[oi % 2], "p2_om%d" % (oi % 2)
            oi += 1
            for h in range(4):
                kt, qt, kqk = load_head(1, h, 64)
                sb_head(kt, qt, kqk, vm, vkey, h, omt, omk)
            store_om(1, omt, omk)
            vm, vkey = load_v(3)
            dcount = 0
            for comp in range(2):
                for h in range(4):
                    kt, qt, kqk = load_head(3, h, 32, comp * 32)
                    odt, odk = od[dcount % 2], "p2_od%d" % (dcount % 2)
                    dcount += 1
                    softmax_head(kt, qt, kqk, 32, 32 ** -0.5, vm, vkey, h, norm_out(odt, odk, 0))
                    c.dma("sp", self.od_s[comp, :, h * 64:(h + 1) * 64].rearrange("(t p) c -> p t c", p=128), odt[:, :, :],
                          reads=[odk], writes=["od_s"])
            dl = self.sb("p2_dl", [128, 128], F32, st)
            lam = self.sb("p2_lam", [128, 4], F32, st)
            gsub = self.sb("p2_gsub", [128, 64], F32, st)
            junk = self.sb("p2_junk", [128, 64], F32, st)
            c.dma("sp", dl[:, :], self.diff_lambda[l, :].partition_broadcast(128), writes=["p2_dl"])
            c.dma("sp", gsub[:, :], self.diff_subln[l, :].partition_broadcast(128), writes=["p2_gsub"])
            self.tt("dve", junk[:, 0:32], dl[:, 0:32], dl[:, 32:64], ALU.mult, ["p2_dl"], ["p2_junk"])
            self.red(lam[:, 0:1], junk[:, 0:32], ALU.add, ["p2_junk"], ["p2_lam"])
            self.tt("dve", junk[:, 32:64], dl[:, 64:96], dl[:, 96:128], ALU.mult, ["p2_dl"], ["p2_junk"])
            self.red(lam[:, 1:2], junk[:, 32:64], ALU.add, ["p2_junk"], ["p2_lam"])
            self.act(lam[:, 0:2], lam[:, 0:2], AF.Exp, ["p2_lam"], ["p2_lam"])
            self.tt("dve", lam[:, 2:3], lam[:, 1:2], lam[:, 0:1], ALU.subtract, ["p2_lam"], ["p2_lam"])
            self.ts("dve", lam[:, 2:3], lam[:, 2:3], -lambda_init, ALU.add, ["p2_lam"], ["p2_lam"])
            self.ts("dve", gsub[:, :], gsub[:, :], 1.0 - lambda_init, ALU.mult, ["p2_gsub"], ["p2_gsub"])
            omt, omk = om[oi % 2], "p2_om%d" % (oi % 2)
            oi += 1
            d1 = [self.sb("p2_d1_%d" % i, [128, 256], F32, st) for i in range(2)]
            d2 = [self.sb("p2_d2_%d" % i, [128, 256], F32, st) for i in range(2)]
            sq = self.sb("p2_sq", [128, 256], F32, st)
            ss = self.sb("p2_ss", [128, 4], F32, st)
            for t in range(NT):
                i = t % 2
                c.dma("sp", d1[i][:, :], self.od_s[0, t * 128:(t + 1) * 128, :], reads=["od_s"], writes=["p2_d1_%d" % i])
                c.dma("sp", d2[i][:, :], self.od_s[1, t * 128:(t + 1) * 128, :], reads=["od_s"], writes=["p2_d2_%d" % i])
                k1 = "p2_d1_%d" % i
                self.stt(d1[i][:, :], d2[i][:, :], lam[:, 2:3], d1[i][:, :], ALU.mult, ALU.add, ["p2_d2_%d" % i, k1, "p2_lam"], [k1])
                self.tt("dve", sq[:, :], d1[i][:, :], d1[i][:, :], ALU.mult, [k1], ["p2_sq"])
                self.red(ss[:, :], sq[:, :].rearrange("p (h d) -> p h d", h=4), ALU.add, ["p2_sq"], ["p2_ss"])
                self.ts("dve", ss[:, :], ss[:, :], 1.0 / 64, ALU.mult, ["p2_ss"], ["p2_ss"], s2=RMS_EPS, op1=ALU.add)
                self.act(ss[:, :], ss[:, :], AF.Ln, ["p2_ss"], ["p2_ss"])
                self.act(ss[:, :], ss[:, :], AF.Exp, ["p2_ss"], ["p2_ss"], scale=-0.5)
                d3 = d1[i][:, :].rearrange("p (h d) -> p h d", h=4)
                self.tt("dve", d3, d3, ss[:, :].unsqueeze(2).to_broadcast([128, 4, 64]), ALU.mult, [k1, "p2_ss"], [k1])
                self.tt("dve", omt[:, t, :].rearrange("p (h d) -> p h d", h=4), d3, gsub[:, :].unsqueeze(1).to_broadcast([128, 4, 64]),
                        ALU.mult, [k1, "p2_gsub"], [omk])
            store_om(3, omt, omk)
            c.emit()

    def ln_part1(self, y, yk, res, resk, pa, pak, pb, pbk, tmp, tk):
        nc = self.nc
        st6, mv = tmp["st6"], tmp["mv"]
        self.stt(y[:, 0:512], res[:, 0:512], ALPHA, pa[:, 0:512], ALU.mult, ALU.add, [resk, pak], [yk])
        self.stt(y[:, 512:1024], res[:, 512:1024], ALPHA, pb[:, 0:512], ALU.mult, ALU.add, [resk, pbk], [yk])
        self.c.op("dve", lambda: nc.vector.bn_stats(out=st6[:, 0:6], in_=y[:, 0:512]), [yk], [tk + "s"])
        self.c.op("dve", lambda: nc.vector.bn_stats(out=st6[:, 6:12], in_=y[:, 512:1024]), [yk], [tk + "s"])
        self.c.op("dve", lambda: nc.vector.bn_aggr(out=mv[:, 0:2], in_=st6[:, 0:12]), [tk + "s"], [tk])
        self.ts("dve", mv[:, 2:3], mv[:, 1:2], LN_EPS, ALU.add, [tk], [tk])
        self.act(mv[:, 2:3], mv[:, 2:3], AF.Ln, [tk], [tk])
        self.act(mv[:, 2:3], mv[:, 2:3], AF.Exp, [tk], [tk], scale=-0.5)

    def ln_part2(self, y, yk, g_t, b_t, gbk, out, outk, tmp, tk):
        mv = tmp["mv"]
        self.stt(y[:, :], y[:, :], mv[:, 0:1], g_t[:, :], ALU.subtract, ALU.mult, [yk, tk, gbk], [yk])
        self.stt(out, y[:, :], mv[:, 2:3], b_t[:, :], ALU.mult, ALU.add, [yk, tk, gbk], [outk])

    def ln_tile(self, y, yk, res, resk, pa, pak, pb, pbk, g_t, b_t, gbk, out, outk, tmp, tk="ln_mv"):
        self.ln_part1(y, yk, res, resk, pa, pak, pb, pbk, tmp, tk)
        self.ln_part2(y, yk, g_t, b_t, gbk, out, outk, tmp, tk)

    def to_xT(self, src, srck, xb, xbk, xts, xtsk, cols, eng):
        self.cp("act", xb[:, :], src, [srck], [xbk])
        ptb = self.ps[7][:, :].bitcast(BF16)
        for k in range(8):
            self.tr(ptb[:, k * 128:(k + 1) * 128], xb[:, k * 128:(k + 1) * 128], [xbk], ["ps7"])
        self.cp(eng, xts[:, :, cols], ptb.rearrange("p (k m) -> p k m", k=8), ["ps7"], [xtsk])

    def phase34(self, l):
        nc, c = self.nc, self.c
        xres_src = self.x_in if l == 0 else self.xres_s
        with ExitStack() as st:
            KmT = self.sb("p3_KmT", [128, 8, N_MEM], BF16, st)
            Vmem = self.sb("p3_Vmem", [128, 2, 4 * 257], BF16, st)
            with ExitStack() as st2:
                wk = self.sb("p3_wk", [128, 8, D], BF16, st2)
                wv = self.sb("p3_wv", [128, 8, D], BF16, st2)
                for k in range(8):
                    c.dma("pool", wk[:, k, :], self.wk[l, k * 128:(k + 1) * 128, :], writes=["p3_wk%d" % k])
                    c.dma("pool", wv[:, k, :], self.wv[l, k * 128:(k + 1) * 128, :], writes=["p3_wv%d" % k])
                self.ms("pool", Vmem[:, :, :], 1.0, ["p3_Vmem"])
                for j in range(8):
                    ps, pk = self.ps[j % 2], "ps%d" % (j % 2)
                    for k in range(8):
                        self.mm(ps[:, 0:N_MEM], wk[:, k, j * 128:(j + 1) * 128], self.memT[:, k, :], k == 0, k == 7, ["p3_wk%d" % k, "memT"], [pk])
                    self.cp("act" if j % 2 == 0 else "dve", KmT[:, j, :], ps[:, 0:N_MEM], [pk], ["p3_KmT"])
                for mt in range(2):
                    for nh in range(2):
                        ps, pk = self.ps[2 + nh], "ps%d" % (2 + nh)
                        for k in range(8):
                            self.mm(ps[:, :], self.memT[:, k, mt * 128:(mt + 1) * 128], wv[:, k, nh * 512:(nh + 1) * 512], k == 0, k == 7,
                                    ["p3_wv%d" % k, "memT"], [pk])
                        dst = Vmem[:, mt, nh * 514:(nh + 1) * 514].rearrange("p (h d) -> p h d", h=2)[:, :, 0:256]
                        self.cp("act" if nh == 0 else "dve", dst, ps[:, :].rearrange("p (h d) -> p h d", h=2), [pk], ["p3_Vmem"])
                c.emit()
            wout = self.sb("p3_wout", [128, 8, D], BF16, st)
            wq = self.sb("p3_wq", [128, 8, D], BF16, st)
            wo = self.sb("p3_wo", [128, 8, D], BF16, st)
            for k in range(8):
                c.dma("pool", wout[:, k, :], self.w_out[l, k * 128:(k + 1) * 128, :], writes=["p3_wout%d" % k])
            for k in range(8):
                c.dma("pool", wq[:, k, :], self.wq[l, k * 128:(k + 1) * 128, :], writes=["p3_wq%d" % k])
            for k in range(8):
                c.dma("pool", wo[:, k, :], self.wo[l, k * 128:(k + 1) * 128, :], writes=["p3_wo%d" % k])
            gb = {}
            for nm, src in (("g1", self.ln_g[0]), ("b1", self.ln_b[0]), ("g2", self.ln_g[1]), ("b2", self.ln_b[1])):
                gb[nm] = self.sb("p3_" + nm, [128, D], F32, st)
                c.dma("sp", gb[nm][:, :], src[l, :].partition_broadcast(128), writes=["p3_gb"])
            tmpA = {"st6": self.sb("p3_st6a", [128, 12], F32, st), "mv": self.sb("p3_mva", [128, 4], F32, st)}
            tmpB = {"st6": self.sb("p3_st6b", [128, 12], F32, st), "mv": self.sb("p3_mvb", [128, 4], F32, st)}
            ob = [self.sb("p3_ob0", [128, 4, D], BF16, st)] * 2
            oT = [self.sb("p3_oT0", [128, 8, 512], BF16, st)] * 2
            xr = [self.sb("p3_xr%d" % i, [128, D], F32, st) for i in range(2)]
            xr2 = [self.sb("p3_xq%d" % i, [128, D], F32, st) for i in range(2)]
            yA = self.sb("p3_yA", [128, D], F32, st)
            yB = self.sb("p3_yB", [128, D], F32, st)
            x1t = [self.sb("p3_x1t%d" % i, [128, D], F32, st) for i in range(2)]
            xbA = self.sb("p3_xbA", [128, D], BF16, st)
            xbB = self.sb("p3_xbB", [128, D], BF16, st)
            x1T = [self.sb("p3_x1T%d" % i, [128, 8, 512], BF16, st) for i in range(2)]
            qT = self.sb("p3_qT", [128, 8, 512], BF16, st)
            PT2 = [self.sb("p3_PT%d" % i, [128, 512], BF16, st) for i in range(4)]
            xo = self.sb("p3_xo", [128, 4, D], BF16, st)
            xoT = self.sb("p3_xoT", [128, 8, 512], BF16, st)
            x2 = [self.sb("p3_x2_%d" % i, [128, D], F32, st) for i in range(2)]
            x2T = [self.sb("p3_x2T0", [128, 8, 512], BF16, st)] * 2
            rc = self.sb("p3_rc", [128, 2], F32, st)

            def to_xT2(src, srck, xb, xbk, xts, xtsk, cols, bank, eng):
                self.cp("act", xb[:, :], src, [srck], [xbk])
                ptb = self.ps[bank][:, :].bitcast(BF16)
                for k in range(8):
                    self.tr(ptb[:, k * 128:(k + 1) * 128], xb[:, k * 128:(k + 1) * 128], [xbk], ["ps%d" % bank])
                self.cp(eng, xts[:, :, cols], ptb.rearrange("p (k m) -> p k m", k=8), ["ps%d" % bank], [xtsk])

            def load_ob(b):
                c.dma("sp", ob[0][:, :, :], self.o_s[b * 512:(b + 1) * 512, :].rearrange("(t p) c -> p t c", p=128),
                      reads=["o_s"], writes=["p3_ob0"])

            def H1(b):
                steps = []
                p = b % 2
                obk, oTk, x1Tk = "p3_ob0", "p3_oT0", "p3_x1T%d" % p

                def s_oT(tl):
                    ptb = self.ps[2][:, :].bitcast(BF16)
                    for k in range(8):
                        self.tr(ptb[:, k * 128:(k + 1) * 128], ob[p][:, tl, k * 128:(k + 1) * 128], [obk], ["ps2"])
                    self.cp("act", oT[p][:, :, tl * 128:(tl + 1) * 128], ptb.rearrange("p (k m) -> p k m", k=8), ["ps2"], [oTk])

                def s_mix(tl):
                    t = b * 4 + tl
                    xi = t % 2
                    c.dma("sp", xr[xi][:, :], xres_src[t * 128:(t + 1) * 128, :], reads=["xres_s%d" % t], writes=["p3_xr%d" % xi])
                    for nh in range(2):
                        for k in range(8):
                            self.mm(self.ps[nh][:, :], oT[p][:, k, tl * 128:(tl + 1) * 128], wout[:, k, nh * 512:(nh + 1) * 512], k == 0, k == 7,
                                    [oTk, "p3_wout%d" % k], ["ps%d" % nh])
                    self.ln_part1(yA, "p3_yA", xr[xi], "p3_xr%d" % xi, self.ps[0], "ps0", self.ps[1], "ps1", tmpA, "lnA")

                def s_mix_b(tl):
                    t = b * 4 + tl
                    xi = t % 2
                    self.ln_part2(yA, "p3_yA", gb["g1"], gb["b1"], "p3_gb", x1t[xi][:, :], "p3_x1t%d" % xi, tmpA, "lnA")
                    c.dma("sp", self.x1_s[t * 128:(t + 1) * 128, :], x1t[xi][:, :], reads=["p3_x1t%d" % xi], writes=["x1_s%d" % t])

                def s_x1T(tl):
                    xi = (b * 4 + tl) % 2
                    to_xT2(x1t[xi][:, :], "p3_x1t%d" % xi, xbA, "p3_xbA", x1T[p], x1Tk, slice(tl * 128, (tl + 1) * 128), 2, "act")

                steps.append(lambda: load_ob(b))
                for tl in range(4):
                    steps.append(lambda tl=tl: s_oT(tl))
                for tl in range(4):
                    steps.append(lambda tl=tl: s_mix(tl))
                    if tl > 0:
                        steps.append(lambda tl=tl: s_x1T(tl - 1))
                    steps.append(lambda tl=tl: s_mix_b(tl))
                steps.append(lambda: s_x1T(3))
                return steps

            pcount = [0]

            def H2(b):
                steps = []
                p = b % 2
                x1Tk = "p3_x1T%d" % p
                x2Tb, x2Tk = x2T[0], "p3_x2T0"

                def s_q(j):
                    ps, pk = self.ps[3 + j % 2], "ps%d" % (3 + j % 2)
                    for k in range(8):
                        self.mm(ps[:, :], wq[:, k, j * 128:(j + 1) * 128], x1T[p][:, k, :], k == 0, k == 7, ["p3_wq%d" % k, x1Tk], [pk])
                    self.cp("act" if j % 2 == 0 else "dve", qT[:, j, :], ps[:, :], [pk], ["p3_qT"])

                def s_att(h):
                    pts = []
                    for mt in range(2):
                        ps, pk = self.ps[5 + mt], "ps%d" % (5 + mt)
                        for cc in range(2):
                            self.mm(ps[:, :], KmT[:, h * 2 + cc, mt * 128:(mt + 1) * 128], qT[:, h * 2 + cc, :], cc == 0, cc == 1,
                                    ["p3_KmT", "p3_qT"], [pk])
                        pi = pcount[0] % 4
                        pcount[0] += 1
                        self.act(PT2[pi][:, :], ps[:, :], AF.Exp, [pk], ["p3_PT%d" % pi], scale=256 ** -0.5)
                        pts.append(pi)
                    for tl in range(4):
                        ps, pk = self.ps[7], "ps7"
                        for mt in range(2):
                            self.mm(ps[:, 0:257], PT2[pts[mt]][:, tl * 128:(tl + 1) * 128], Vmem[:, mt, h * 257:(h + 1) * 257], mt == 0, mt == 1,
                                    ["p3_PT%d" % pts[mt], "p3_Vmem"], [pk])
                        self.recip(rc[:, tl % 2:tl % 2 + 1], ps[:, 256:257], [pk], ["p3_rc%d" % (tl % 2)])
                        self.ts("dve", xo[:, tl, h * 256:(h + 1) * 256], ps[:, 0:256], rc[:, tl % 2:tl % 2 + 1], ALU.mult, [pk, "p3_rc%d" % (tl % 2)], ["p3_xo"])

                def s_xoT(tl):
                    ptb = self.ps[5][:, :].bitcast(BF16)
                    for k in range(8):
                        self.tr(ptb[:, k * 128:(k + 1) * 128], xo[:, tl, k * 128:(k + 1) * 128], ["p3_xo"], ["ps5"])
                    self.cp("act", xoT[:, :, tl * 128:(tl + 1) * 128], ptb.rearrange("p (k m) -> p k m", k=8), ["ps5"], ["p3_xoT"])

                def s_wo(tl):
                    t = b * 4 + tl
                    for nh in range(2):
                        for k in range(8):
                            self.mm(self.ps[3 + nh][:, :], xoT[:, k, tl * 128:(tl + 1) * 128], wo[:, k, nh * 512:(nh + 1) * 512], k == 0, k == 7,
                                    ["p3_xoT", "p3_wo%d" % k], ["ps%d" % (3 + nh)])
                    x2t, x2k = x2[t % 2], "p3_x2_%d" % (t % 2)
                    c.dma("sp", xr2[t % 2][:, :], self.x1_s[t * 128:(t + 1) * 128, :], reads=["x1_s%d" % t], writes=["p3_xq%d" % (t % 2)])
                    self.ln_part1(yB, "p3_yB", xr2[t % 2], "p3_xq%d" % (t % 2), self.ps[3], "ps3", self.ps[4], "ps4", tmpB, "lnB")

                def s_wo_b(tl):
                    t = b * 4 + tl
                    x2t, x2k = x2[t % 2], "p3_x2_%d" % (t % 2)
                    self.ln_part2(yB, "p3_yB", gb["g2"], gb["b2"], "p3_gb", x2t[:, :], x2k, tmpB, "lnB")
                    c.dma("sp", self.xres_s[t * 128:(t + 1) * 128, :], x2t[:, :], reads=[x2k], writes=["xres_s%d" % t])

                def s_x2T(tl):
                    t = b * 4 + tl
                    to_xT2(x2[t % 2][:, :], "p3_x2_%d" % (t % 2), xbB, "p3_xbB", x2Tb, x2Tk, slice(tl * 128, (tl + 1) * 128), 6, "act")
                    if tl == 3:
                        c.dma("sp", self.xT_s[:, :, b * 512:(b + 1) * 512], x2Tb[:, :, :], reads=[x2Tk], writes=["xT_s"])

                for j in range(8):
                    steps.append(lambda j=j: s_q(j))
                for h in range(4):
                    steps.append(lambda h=h: s_att(h))
                for tl in range(4):
                    steps.append(lambda tl=tl: s_xoT(tl))
                for tl in range(4):
                    steps.append(lambda tl=tl: s_wo(tl))
                    if tl > 0:
                        steps.append(lambda tl=tl: s_x2T(tl - 1))
                    steps.append(lambda tl=tl: s_wo_b(tl))
                steps.append(lambda: s_x2T(3))
                return steps

            def merge(a, bsteps):
                na, nb = len(a), len(bsteps)
                ia = ib = 0
                while ia < na or ib < nb:
                    if ib >= nb or (ia < na and ia * nb <= ib * na):
                        a[ia]()
                        ia += 1
                    else:
                        bsteps[ib]()
                        ib += 1

            for f in H1(0):
                f()
            for b in range(NB):
                merge(H2(b), H1(b + 1) if b + 1 < NB else [])
            c.emit()

    def phase5(self, l, last):
        nc, c = self.nc, self.c
        with ExitStack() as st:
            wr = self.sb("p5_wr", [128, 8, 40], BF16, st)
            br = self.sb("p5_br", [128, 40], F32, st)
            c.dma("pool", wr[:, :, :], self.w_router[l].rearrange("(k p) n -> p k n", p=128), writes=["p5_wr"])
            c.dma("sp", br[:, :], self.b_router[l, :].partition_broadcast(128), writes=["p5_br"])
            xT = [self.sb("p5_xT%d" % i, [128, 8, 512], BF16, st) for i in range(2)]
            lg = self.sb("p5_lg", [128, 4, 40], F32, st)
            gmax = self.sb("p5_gmax", [128, 4], F32, st)
            ohg = self.sb("p5_ohg", [128, 4, 8], F32, st)
            z8 = self.sb("p5_z8", [128, 4, 8], F32, st)
            se = self.sb("p5_se", [128, 4], F32, st)
            gw = self.sb("p5_gw", [128, 4], F32, st)
            t32 = self.sb("p5_t32", [128, 4, 32], F32, st)
            el = self.sb("p5_el", [128, 4, 4], F32, st)
            el2 = self.sb("p5_el2", [128, 4, 4], F32, st)
            mk1 = self.sb("p5_mk1", [128, 4, 4], F32, st)
            mk2 = self.sb("p5_mk2", [128, 4, 4], F32, st)
            m1 = self.sb("p5_m1", [128, 4], F32, st)
            m2 = self.sb("p5_m2", [128, 4], F32, st)
            e21 = self.sb("p5_e21", [128, 4], F32, st)
            w1 = self.sb("p5_w1", [128, 4], F32, st)
            w2 = self.sb("p5_w2", [128, 4], F32, st)
            g4 = self.sb("p5_g4", [128, 4, 4], F32, st)
            g4b = self.sb("p5_g4b", [128, 4, 4], F32, st)
            g32 = self.sb("p5_g32", [128, 4, 32], F32, st)
            gTs = [self.sb("p5_gTs%d" % i, [32, 512], F32, st) for i in range(2)]

            def load_x(b):
                c.dma("sp", xT[b % 2][:, :, :], self.xT_s[:, :, b * 512:(b + 1) * 512], reads=["xT_s"], writes=["p5_xT%d" % (b % 2)])

            load_x(0)
            K = "p5_r"
            bc3 = lambda ap, n: ap.unsqueeze(2).to_broadcast([128, 4, n])
            for b in range(NB):
                if b + 1 < NB:
                    load_x(b + 1)
                gT, gTk = gTs[b % 2], "p5_gTs%d" % (b % 2)
                ps, pk = self.ps[b % 2], "ps%d" % (b % 2)
                for tl in range(4):
                    for k in range(8):
                        self.mm(ps[:, tl * 40:(tl + 1) * 40], xT[b % 2][:, k, tl * 128:(tl + 1) * 128], wr[:, k, :], k == 0, k == 7,
                                ["p5_xT%d" % (b % 2), "p5_wr"], [pk])
                self.tt("dve", lg[:, :, :], ps[:, 0:160].rearrange("p (t n) -> p t n", t=4), br[:, :].unsqueeze(1).to_broadcast([128, 4, 40]),
                        ALU.add, [pk, "p5_br"], [K])
                lgg = lg[:, :, 0:8]
                self.red(gmax[:, :], lgg, ALU.max, [K], [K])
                self.tt("dve", ohg[:, :, :], lgg, bc3(gmax[:, :], 8), ALU.is_equal, [K], [K])
                self.tt("dve", z8[:, :, :], lgg, bc3(gmax[:, :], 8), ALU.subtract, [K], [K])
                self.act(z8[:, :, :], z8[:, :, :], AF.Exp, [K], [K])
                self.red(se[:, :], z8[:, :, :], ALU.add, [K], [K])
                self.recip(gw[:, :], se[:, :], [K], [K])
                lge = lg[:, :, 8:40].rearrange("p t (g e) -> p t g e", g=8)
                self.tt("dve", t32[:, :, :].rearrange("p t (g e) -> p t g e", g=8), lge, ohg[:, :, :].unsqueeze(3).to_broadcast([128, 4, 8, 4]),
                        ALU.mult, [K], [K])
                self.red(el[:, :, :], t32[:, :, :].rearrange("p t (g e) -> p t e g", g=8), ALU.add, [K], [K])
                self.red(m1[:, :], el[:, :, :], ALU.max, [K], [K])
                self.tt("dve", mk1[:, :, :], el[:, :, :], bc3(m1[:, :], 4), ALU.is_equal, [K], [K])
                self.stt(el2[:, :, :], mk1[:, :, :], -1e30, el[:, :, :], ALU.mult, ALU.add, [K], [K])
                self.red(m2[:, :], el2[:, :, :], ALU.max, [K], [K])
                self.tt("dve", mk2[:, :, :], el2[:, :, :], bc3(m2[:, :], 4), ALU.is_equal, [K], [K])
                self.tt("dve", e21[:, :], m2[:, :], m1[:, :], ALU.subtract, [K], [K])
                self.act(e21[:, :], e21[:, :], AF.Exp, [K], [K])
                self.ts("dve", w1[:, :], e21[:, :], 1.0, ALU.add, [K], [K])
                self.recip(w1[:, :], w1[:, :], [K], [K])
                self.tt("dve", w2[:, :], w1[:, :], e21[:, :], ALU.mult, [K], [K])
                self.tt("dve", w1[:, :], w1[:, :], gw[:, :], ALU.mult, [K], [K])
                self.tt("dve", w2[:, :], w2[:, :], gw[:, :], ALU.mult, [K], [K])
                self.tt("dve", g4[:, :, :], mk1[:, :, :], bc3(w1[:, :], 4), ALU.mult, [K], [K])
                self.tt("dve", g4b[:, :, :], mk2[:, :, :], bc3(w2[:, :], 4), ALU.mult, [K], [K])
                self.tt("dve", g4[:, :, :], g4[:, :, :], g4b[:, :, :], ALU.add, [K], [K])
                self.tt("dve", g32[:, :, :].rearrange("p t (g e) -> p t g e", g=8), ohg[:, :, :].unsqueeze(3).to_broadcast([128, 4, 8, 4]),
                        g4[:, :, :].unsqueeze(2).to_broadcast([128, 4, 8, 4]), ALU.mult, [K], [K])
                pt, ptk = self.ps[2 + b % 2], "ps%d" % (2 + b % 2)
                for tl in range(4):
                    self.tr(pt[0:32, tl * 128:(tl + 1) * 128], g32[:, tl, :], [K], [ptk], f32=True)
                self.cp("act", gT[:, :], pt[0:32, :], [ptk], [gTk])
                c.dma("sp", self.gT_s[:, b * 512:(b + 1) * 512], gT[:, :], reads=[gTk], writes=["gT_s"])
            c.emit()
        SBT = 2048
        NSB = S // SBT
        TPS = SBT // 128
        with ExitStack() as st:
            acc = self.sb("p5_acc", [128, TPS, D], F32, st)
            for sbi in range(NSB):
                t0 = sbi * TPS
                with ExitStack() as st2:
                    xTs = self.sb("p5_xTs", [128, 8, SBT], BF16, st2)
                    Wg = [self.sb("p5_Wg%d" % i, [128, 8, 256], BF16, st2) for i in range(3)]
                    Wu = [self.sb("p5_Wu%d" % i, [128, 8, 256], BF16, st2) for i in range(3)]
                    Wd = [self.sb("p5_Wd%d" % i, [128, 2, D], BF16, st2) for i in range(3)]
                    gbt = [self.sb("p5_gbt%d" % i, [128, 512], F32, st2) for i in range(3)]
                    sg = [self.sb("p5_sg%d" % i, [128, 512], F32, st2) for i in range(2)]
                    tu = [self.sb("p5_tu%d" % i, [128, 512], F32, st2) for i in range(2)]
                    hid = [self.sb("p5_hid%d" % i, [128, 2, 512], BF16, st2) for i in range(2)]
                    for q in range(SBT // 512):
                        c.dma("sp", xTs[:, :, q * 512:(q + 1) * 512], self.xT_s[:, :, sbi * SBT + q * 512: sbi * SBT + (q + 1) * 512],
                              reads=["xT_s"], writes=["p5_xTs%d" % q])

                    def load_w(e):
                        i = e % 3
                        c.dma("pool", Wg[i][:, :, :], self.e_gate[l, e].rearrange("(k p) f -> p k f", p=128), writes=["p5_Wg%d" % i])
                        c.dma("pool", Wu[i][:, :, :], self.e_up[l, e].rearrange("(k p) f -> p k f", p=128), writes=["p5_Wu%d" % i])
                        c.dma("pool", Wd[i][:, :, :], self.e_down[l, e].rearrange("(k p) d -> p k d", p=128), writes=["p5_Wd%d" % i])

                    load_w(0)
                    load_w(1)
                    load_w(2)
                    ocnt = [0]
                    NTB = SBT // 512

                    def gu_stage(n):
                        e, tb = n // NTB, n % NTB
                        wi, gi, hi = e % 3, n % 3, n % 2
                        tok0 = sbi * SBT + tb * 512
                        c.dma("sp", gbt[gi][:, :], self.gT_s[e, tok0:tok0 + 512].partition_broadcast(128), reads=["gT_s"], writes=["p5_gbt%d" % gi])
                        for fc in range(2):
                            pg, pgk = self.ps[fc], "ps%d" % fc
                            pu, puk = self.ps[2 + fc], "ps%d" % (2 + fc)
                            for k in range(8):
                                self.mm(pg[:, :], Wg[wi][:, k, fc * 128:(fc + 1) * 128], xTs[:, k, tb * 512:(tb + 1) * 512], k == 0, k == 7,
                                        ["p5_Wg%d" % wi, "p5_xTs%d" % tb], [pgk])
                            for k in range(8):
                                self.mm(pu[:, :], Wu[wi][:, k, fc * 128:(fc + 1) * 128], xTs[:, k, tb * 512:(tb + 1) * 512], k == 0, k == 7,
                                        ["p5_Wu%d" % wi, "p5_xTs%d" % tb], [puk])
                            self.act(sg[fc][:, :], pg[:, :], AF.Silu, [pgk], ["p5_sg%d" % fc])
                            self.tt("dve", tu[fc][:, :], sg[fc][:, :], pu[:, :], ALU.mult, ["p5_sg%d" % fc, puk], ["p5_tu%d" % fc])
                            self.tt("pool", hid[hi][:, fc, :], tu[fc][:, :], gbt[gi][:, :], ALU.mult, ["p5_tu%d" % fc, "p5_gbt%d" % gi], ["p5_hid%d" % hi])

                    def down_stage(n):
                        e, tb = n // NTB, n % NTB
                        wi, hi = e % 3, n % 2
                        for tl in range(4):
                            tile = tb * 4 + tl
                            for dh in range(2):
                                po, pok = self.ps[4 + ocnt[0] % 4], "ps%d" % (4 + ocnt[0] % 4)
                                ocnt[0] += 1
                                for fc in range(2):
                                    self.mm(po[:, :], hid[hi][:, fc, tl * 128:(tl + 1) * 128], Wd[wi][:, fc, dh * 512:(dh + 1) * 512], fc == 0, fc == 1,
                                            ["p5_hid%d" % hi, "p5_Wd%d" % wi], [pok])
                                a = acc[:, tile, dh * 512:(dh + 1) * 512]
                                ak = "p5_acc%d" % tile
                                if e == 0:
                                    self.cp("dve", a, po[:, :], [pok], [ak])
                                else:
                                    self.tt("dve", a, a, po[:, :], ALU.add, [pok, ak], [ak])

                    NN = 32 * NTB
                    gu_stage(0)
                    for n in range(NN):
                        if n + 1 < NN:
                            gu_stage(n + 1)
                        down_stage(n)
                        if n % NTB == NTB - 1 and n // NTB + 3 < 32:
                            load_w(n // NTB + 3)
                    c.emit()
                with ExitStack() as st2:
                    g_t = self.sb("p5_g", [128, D], F32, st2)
                    b_t = self.sb("p5_b", [128, D], F32, st2)
                    c.dma("sp", g_t[:, :], self.ln_g[2][l, :].partition_broadcast(128), writes=["p5_gb"])
                    c.dma("sp", b_t[:, :], self.ln_b[2][l, :].partition_broadcast(128), writes=["p5_gb"])
                    tmps = [{"st6": self.sb("p5_st6%d" % i, [128, 12], F32, st2), "mv": self.sb("p5_mv%d" % i, [128, 4], F32, st2)} for i in range(2)]
                    xr = [self.sb("p5_xr%d" % i, [128, D], F32, st2) for i in range(2)]
                    ys = [self.sb("p5_y%d" % i, [128, D], F32, st2) for i in range(2)]
                    x3 = [self.sb("p5_x3_%d" % i, [128, D], F32, st2) for i in range(2)]
                    xb = self.sb("p5_xb", [128, D], BF16, st2)
                    x3T = [self.sb("p5_x3T%d" % i, [128, 8, 512], BF16, st2) for i in range(2)]

                    def ln1(tile):
                        t = t0 + tile
                        i = tile % 2
                        c.dma("sp", xr[i][:, :], self.xres_s[t * 128:(t + 1) * 128, :], reads=["xres_s%d" % t], writes=["p5_xr%d" % i])
                        ak = "p5_acc%d" % tile
                        self.ln_part1(ys[i], "p5_y%d" % i, xr[i], "p5_xr%d" % i, acc[:, tile, 0:512], ak, acc[:, tile, 512:1024], ak, tmps[i], "p5_ln%d" % i)

                    def ln2(tile):
                        t = t0 + tile
                        i = tile % 2
                        self.ln_part2(ys[i], "p5_y%d" % i, g_t, b_t, "p5_gb", x3[i][:, :], "p5_x3_%d" % i, tmps[i], "p5_ln%d" % i)
                        dst = self.out if last else self.xres_s
                        c.dma("sp", dst[t * 128:(t + 1) * 128, :], x3[i][:, :], reads=["p5_x3_%d" % i], writes=["xres_s%d" % t])
                        if not last:
                            bq = tile // 4
                            self.to_xT(x3[i][:, :], "p5_x3_%d" % i, xb, "p5_xb", x3T[bq % 2], "p5_x3T%d" % (bq % 2),
                                       slice((tile % 4) * 128, (tile % 4 + 1) * 128), "act")
                            if tile % 4 == 3:
                                tok0 = (t0 + bq * 4) * 128
                                c.dma("sp", self.xT_s[:, :, tok0:tok0 + 512], x3T[bq % 2][:, :, :], reads=["p5_x3T%d" % (bq % 2)], writes=["xT_s"])

                    for tile in range(TPS + 1):
                        if tile < TPS:
                            ln1(tile)
                        if tile >= 1:
                            ln2(tile - 1)
                    c.emit()


_CACHE = {}


def _consts():
    ident = np.eye(128, dtype=np.float32)
    p = np.arange(128)[:, None]
    f = np.arange(128)[None, :]
    invs = []
    for rot in (32, 16, 8):
        invs.append((np.float32(ROPE_THETA) ** (-np.arange(0, rot, 2, dtype=np.float32) / np.float32(rot))).astype(np.float32))
    invf = np.tile(np.concatenate(invs)[None, :], (128, 1)).astype(np.float32)
    blk = np.zeros((16, S), dtype=np.float32)
    for j in range(16):
        blk[j, j * 256:(j + 1) * 256] = BIGV
    bf = ml_dtypes.bfloat16
    return {
        "c_ident_b": ident.astype(bf), "c_ident_f": ident,
        "c_mask_le": (p <= f).astype(np.float32).astype(bf),
        "c_mask_lt": (p < f).astype(np.float32).astype(bf),
        "c_tri": (p > f).astype(np.float32).astype(bf),
        "c_invf": invf, "c_blk": blk.astype(bf),
    }


def make_in_maps(inputs, n_cores=8):
    f = lambda a: np.ascontiguousarray(np.asarray(a))
    shared = {
        "w_in": f(np.asarray(inputs["w_in"])[:, :, COL_PERM]),
        "mla_q_norm": f(inputs["mla_q_norm"]), "w_uq": f(inputs["w_uq"]),
        "mla_kv_norm": f(inputs["mla_kv_norm"]), "w_ukv": f(inputs["w_ukv"]),
        "diff_lambda": f(np.asarray(inputs["diff_lambda"]).reshape(DEPTH, 128)),
        "diff_subln": f(inputs["diff_subln"]), "w_out": f(inputs["w_out"]),
        "ln_mix_g": f(inputs["ln_mix_g"]), "ln_mix_b": f(inputs["ln_mix_b"]),
        "ln_mem_g": f(inputs["ln_mem_g"]), "ln_mem_b": f(inputs["ln_mem_b"]),
        "ln_ffn_g": f(inputs["ln_ffn_g"]), "ln_ffn_b": f(inputs["ln_ffn_b"]),
        "xattn_wq": f(inputs["xattn_wq"]), "xattn_wk": f(inputs["xattn_wk"]),
        "xattn_wv": f(inputs["xattn_wv"]), "xattn_wo": f(inputs["xattn_wo"]),
        "w_router": f(np.concatenate([np.asarray(inputs["router_group_w"]), np.asarray(inputs["router_expert_w"])], axis=2)),
        "b_router": f(np.concatenate([np.asarray(inputs["router_group_b"]), np.asarray(inputs["router_expert_b"])], axis=1)),
        "expert_w_gate": f(inputs["expert_w_gate"]), "expert_w_up": f(inputs["expert_w_up"]),
        "expert_w_down": f(inputs["expert_w_down"]),
    }
    shared.update(_consts())
    maps = []
    x = np.asarray(inputs["x"])
    mem = np.asarray(inputs["mem"])
    pos = np.asarray(inputs["positions"])
    for b in range(n_cores):
        m = dict(shared)
        m["x"] = f(x[b])
        m["mem"] = f(mem[b])
        m["pos"] = f(pos[b].reshape(NT, 128).T.astype(np.int32))
        maps.append(m)
    return maps


def kernel(**inputs):
    if "nc" not in _CACHE:
        _CACHE["nc"] = Builder().build()
    nc = _CACHE["nc"]
    maps = make_in_maps(inputs)
    res = run_bass_kernel_spmd(nc, maps, core_ids=list(range(8)))
    return np.stack([r["out"] for r in res.results], axis=0).astype(np.float32)
```

```python
import math
import os
LV = int(os.environ.get('P1LV', '9'))
SUB = int(os.environ.get('P1SUB', '9'))
from contextlib import ExitStack

import numpy as np
import ml_dtypes
import concourse.bass as bass
import concourse.mybir as mybir
from concourse.bass_utils import run_bass_kernel_spmd

F32 = mybir.dt.float32
BF16 = mybir.dt.bfloat16
I32 = mybir.dt.int32
AF = mybir.ActivationFunctionType
ALU = mybir.AluOpType
AX = mybir.AxisListType

D = 1024
S = 4096
NT = S // 128
NB = S // 512
DEPTH = 4
N_MEM = 256
IN_COLS = 2656
ALPHA = (2 * DEPTH) ** 0.25
LN_EPS = 1e-5
RMS_EPS = 1e-6
ROPE_THETA = 500000.0
BIGV = 30000.0
TWO_PI = 2.0 * math.pi

O_SB = 352
O_MB = 352 + 768
O_DF = 352 + 1536
COL_PERM = np.concatenate([
    np.arange(0, 352),
    np.arange(O_SB, O_SB + 512),
    np.arange(O_SB + 512, O_SB + 768), np.arange(O_MB + 512, O_MB + 768),
    np.arange(O_MB, O_MB + 512),
    np.arange(O_DF, O_DF + 512),
    np.arange(O_DF + 512, O_DF + 768),
])
CH = [(0, 352), (352, 512), (864, 512), (1376, 512), (1888, 512), (2400, 256)]


class Ctx:
    def __init__(self, nc, stack, n_dma_sems=24):
        self.nc = nc
        self.eng = {"pe": nc.tensor, "act": nc.scalar, "dve": nc.vector, "pool": nc.gpsimd, "sp": nc.sync}
        self.sem = {}
        self.cnt = {}
        for e in self.eng:
            self.sem[e] = stack.enter_context(nc.semaphore("s_" + e))
            self.cnt[e] = 0
        self.dring = {}
        for q in ("sp", "pool"):
            sems = [stack.enter_context(nc.semaphore("d_%s_%d" % (q, i))) for i in range(n_dma_sems)]
            self.dring[q] = {"sems": sems, "cnt": [0] * n_dma_sems, "next": 0}
        self.waited = {e: {} for e in self.eng}
        self.last_w = {}
        self.readers = {}
        self.prog = {e: [] for e in self.eng}
        self.n_ins = 0

    def _wait(self, e, tok):
        sem, val, src = tok
        if src == e and e == "pe":
            return
        w = self.waited[e]
        if w.get(sem.name, 0) >= val:
            return
        w[sem.name] = val
        self.prog[e].append(("w", sem, val))

    def _deps(self, e, reads, writes):
        for k in reads:
            t = self.last_w.get(k)
            if t is not None:
                self._wait(e, t)
            if k.startswith("ps"):
                for t in self.readers.get(k, ()):
                    if t[2] != e:
                        self._wait(e, t)
        for k in writes:
            t = self.last_w.get(k)
            if t is not None:
                self._wait(e, t)
            for t in self.readers.get(k, ()):
                self._wait(e, t)

    def _commit(self, tok, reads, writes):
        for k in reads:
            lst = self.readers.setdefault(k, [])
            lst.append(tok)
            if len(lst) > 16:
                best = {}
                for t in lst:
                    if t[0].name not in best or best[t[0].name][1] < t[1]:
                        best[t[0].name] = t
                self.readers[k] = list(best.values())
        for k in writes:
            self.last_w[k] = tok
            self.readers[k] = []

    def op(self, e, fn, reads=(), writes=()):
        self._deps(e, reads, writes)
        self.cnt[e] += 1
        self.n_ins += 1
        self.prog[e].append(("i", fn, self.sem[e], 1))
        tok = (self.sem[e], self.cnt[e], e)
        self._commit(tok, reads, writes)
        return tok

    def dma(self, q, out, in_, reads=(), writes=(), **kw):
        ring = self.dring[q]
        i = ring["next"]
        ring["next"] = (i + 1) % len(ring["sems"])
        sem = ring["sems"][i]
        if ring["cnt"][i] > 0:
            self._wait(q, (sem, ring["cnt"][i], None))
        self._deps(q, reads, writes)
        self.prog[q].append(("d", out, in_, kw, sem))
        self.n_ins += 1
        ring["cnt"][i] += 16
        tok = (sem, ring["cnt"][i], None)
        self._commit(tok, reads, writes)
        return tok

    def wait_all(self, e):
        for x in self.eng:
            if self.cnt[x] > 0 and x != e:
                self._wait(e, (self.sem[x], self.cnt[x], x))
        for q, ring in self.dring.items():
            for sem, c in zip(ring["sems"], ring["cnt"]):
                if c > 0:
                    self._wait(e, (sem, c, None))

    def emit(self):
        nc = self.nc
        self.wait_all("sp")
        prog = self.prog
        self.prog = {e: [] for e in self.eng}

        def run(e, engobj):
            for it in prog[e]:
                if it[0] == "w":
                    engobj.wait_ge(it[1], it[2])
                elif it[0] == "i":
                    it[1]().then_inc(it[2], it[3])
                else:
                    engobj.dma_start(out=it[1], in_=it[2], **it[3]).then_inc(it[4], 16)

        with nc.Block() as block:
            @block.sync
            def _(eng):
                run("sp", eng)

            @block.scalar
            def _(eng):
                run("act", eng)

            @block.tensor
            def _(eng):
                run("pe", eng)

            @block.vector
            def _(eng):
                run("dve", eng)

            @block.gpsimd
            def _(eng):
                run("pool", eng)


class Builder:
    def __init__(self, n_layers=DEPTH, debug=False, stop_after=None):
        self.n_layers = n_layers
        self.debug = debug
        self.stop_after = stop_after
        self.nc = bass.Bass("TRN2", target_bir_lowering=False)
        self.dbg_names = []

    def mm(self, out, lhsT, rhs, start, stop, r, w):
        nc = self.nc
        self.c.op("pe", lambda: nc.tensor.matmul(out, lhsT=lhsT, rhs=rhs, start=start, stop=stop), r, w)

    def tr(self, out, in_, r, w, f32=False):
        nc = self.nc
        n = in_.shape[0]
        ident = (self.ident_f if f32 else self.ident_b)[0:n, 0:n]
        self.c.op("pe", lambda: nc.tensor.transpose(out, in_, ident), list(r) + ["const"], w)

    def act(self, out, in_, func, r, w, scale=1.0, bias=0.0, accum_out=None):
        nc = self.nc
        kw = {}
        if accum_out is not None:
            kw["accum_out"] = accum_out
        self.c.op("act", lambda: nc.scalar.activation(out=out, in_=in_, func=func, scale=scale, bias=bias, **kw), r, w)

    def tt(self, e, out, in0, in1, op, r, w):
        eng = self.c.eng[e]
        self.c.op(e, lambda: eng.tensor_tensor(out=out, in0=in0, in1=in1, op=op), r, w)

    def ts(self, e, out, in0, s1, op0, r, w, s2=None, op1=ALU.bypass, accum_out=None):
        eng = self.c.eng[e]
        kw = {}
        if accum_out is not None:
            kw["accum_out"] = accum_out
        self.c.op(e, lambda: eng.tensor_scalar(out=out, in0=in0, scalar1=s1, scalar2=s2, op0=op0, op1=op1, **kw), r, w)

    def stt(self, out, in0, scalar, in1, op0, op1, r, w):
        nc = self.nc
        self.c.op("dve", lambda: nc.vector.scalar_tensor_tensor(out=out, in0=in0, scalar=scalar, in1=in1, op0=op0, op1=op1), r, w)

    def cp(self, e, out, in_, r, w):
        if e == "act":
            nc = self.nc
            self.c.op("act", lambda: nc.scalar.copy(out=out, in_=in_), r, w)
        else:
            eng = self.c.eng[e]
            self.c.op(e, lambda: eng.tensor_copy(out=out, in_=in_), r, w)

    def ms(self, e, ap, val, w):
        eng = self.c.eng[e]
        self.c.op(e, lambda: eng.memset(ap, val), (), w)

    def recip(self, out, in_, r, w):
        nc = self.nc
        self.c.op("dve", lambda: nc.vector.reciprocal(out=out, in_=in_), r, w)

    def red(self, out, in_, op, r, w, axis=AX.X):
        nc = self.nc
        self.c.op("dve", lambda: nc.vector.tensor_reduce(out=out, in_=in_, axis=axis, op=op), r, w)

    def sb(self, name, shape, dt, st=None):
        self.uid = getattr(self, "uid", 0) + 1
        return (st or self.st).enter_context(self.nc.sbuf_tensor("%s_u%d" % (name, self.uid), list(shape), dt))

    def scr(self, name, shape, dt):
        kind = "ExternalOutput" if self.debug else "Internal"
        if self.debug:
            self.dbg_names.append(name)
        return self.nc.dram_tensor(name, list(shape), dt, kind=kind).ap()

    def build(self):
        nc = self.nc
        L = DEPTH
        inp = lambda n, s, dt=F32: nc.dram_tensor(n, list(s), dt, kind="ExternalInput").ap()
        self.x_in = inp("x", [S, D])
        self.mem_in = inp("mem", [N_MEM, D])
        self.pos_in = inp("pos", [128, NT], I32)
        self.w_in = inp("w_in", [L, D, IN_COLS])
        self.mla_q_norm = inp("mla_q_norm", [L, 192])
        self.w_uq = inp("w_uq", [L, 192, 384])
        self.mla_kv_norm = inp("mla_kv_norm", [L, 128])
        self.w_ukv = inp("w_ukv", [L, 128, 512])
        self.diff_lambda = inp("diff_lambda", [L, 128])
        self.diff_subln = inp("diff_subln", [L, 64])
        self.w_out = inp("w_out", [L, D, D])
        self.ln_g = [inp(n, [L, D]) for n in ("ln_mix_g", "ln_mem_g", "ln_ffn_g")]
        self.ln_b = [inp(n, [L, D]) for n in ("ln_mix_b", "ln_mem_b", "ln_ffn_b")]
        self.wq = inp("xattn_wq", [L, D, D])
        self.wk = inp("xattn_wk", [L, D, D])
        self.wv = inp("xattn_wv", [L, D, D])
        self.wo = inp("xattn_wo", [L, D, D])
        self.w_router = inp("w_router", [L, D, 40])
        self.b_router = inp("b_router", [L, 40])
        self.e_gate = inp("expert_w_gate", [L, 32, D, 256])
        self.e_up = inp("expert_w_up", [L, 32, D, 256])
        self.e_down = inp("expert_w_down", [L, 32, 256, D])
        self.c_ident_b = inp("c_ident_b", [128, 128], BF16)
        self.c_ident_f = inp("c_ident_f", [128, 128], F32)
        self.c_mask_le = inp("c_mask_le", [128, 128], BF16)
        self.c_mask_lt = inp("c_mask_lt", [128, 128], BF16)
        self.c_tri = inp("c_tri", [128, 128], BF16)
        self.c_invf = inp("c_invf", [128, 28], F32)
        self.c_blk = inp("c_blk", [16, S], BF16)
        self.out = nc.dram_tensor("out", [S, D], F32, kind="ExternalOutput").ap()
        self.xT_s = self.scr("xT_s", [128, 8, S], BF16)
        self.xres_s = self.scr("xres_s", [S, D], F32)
        self.qk_s = self.scr("qk_s", [4, 4, 2, 96, S], BF16)
        self.v_s = self.scr("v_s", [S, 4 * 260], BF16)
        self.qkp_s = self.scr("qkp_s", [3, 2, 2, 128, S], BF16)
        self.mb_s = self.scr("mb_s", [64, S], BF16)
        self.o_s = self.scr("o_s", [S, D], BF16)
        self.od_s = self.scr("od_s", [2, S, 256], F32)
        self.gT_s = self.scr("gT_s", [32, S], F32)
        self.x1_s = self.scr("x1_s", [S, D], F32)

        with ExitStack() as st:
            self.st = st
            self.c = Ctx(nc, st)
            self.ps = [st.enter_context(nc.psum_tensor("ps%d" % i, [128, 512], F32)) for i in range(8)]
            self.ident_b = self.sb("ident_b", [128, 128], BF16)
            self.ident_f = self.sb("ident_f", [128, 128], F32)
            self.mask_le = self.sb("mask_le", [128, 128], BF16)
            self.mask_lt = self.sb("mask_lt", [128, 128], BF16)
            self.tri = self.sb("tri", [128, 128], BF16)
            self.ones_b = self.sb("ones_b", [128, 128], BF16)
            self.cs = self.sb("rope_cos", [128, NT, 28], F32)
            self.sn = self.sb("rope_sin", [128, NT, 28], F32)
            self.memT = self.sb("memT", [128, 8, N_MEM], BF16)
            self.phase0()
            for l in range(self.n_layers if self.stop_after != (0, 0) else 0):
                last = (l == DEPTH - 1)
                self.phase1(l)
                if self.stop_after == (l, 1):
                    break
                self.phase2(l)
                if self.stop_after == (l, 2):
                    break
                self.phase34(l)
                if self.stop_after == (l, 3):
                    break
                self.phase5(l, last)
                if self.stop_after == (l, 5):
                    break
        return nc

    def phase0(self):
        nc, c = self.nc, self.c
        with ExitStack() as st:
            for dst, src, k in ((self.ident_b, self.c_ident_b, "const"), (self.ident_f, self.c_ident_f, "const"),
                                (self.mask_le, self.c_mask_le, "const"), (self.mask_lt, self.c_mask_lt, "const"),
                                (self.tri, self.c_tri, "const")):
                c.dma("sp", dst[:, :], src, writes=[k])
            self.ms("pool", self.ones_b[:, :], 1.0, ["const"])
            pos_i = self.sb("pos_i", [128, NT], I32, st)
            pos_f = self.sb("pos_f", [128, NT], F32, st)
            invf = self.sb("invf", [128, 28], F32, st)
            ang = self.sb("ang", [128, NT, 28], F32, st)
            t1 = self.sb("rp_t1", [128, NT, 28], F32, st)
            ki = self.sb("rp_ki", [128, NT, 28], I32, st)
            kf = self.sb("rp_kf", [128, NT, 28], F32, st)
            r = self.sb("rp_r", [128, NT, 28], F32, st)
            m = self.sb("rp_m", [128, NT, 28], F32, st)
            rc = self.sb("rp_rc", [128, NT, 28], F32, st)
            c.dma("sp", pos_i[:, :], self.pos_in, writes=["pos_i"])
            c.dma("sp", invf[:, :], self.c_invf, writes=["invf"])
            self.cp("dve", pos_f[:, :], pos_i[:, :], ["pos_i"], ["pos_f"])
            self.tt("dve", ang[:, :, :], pos_f[:, :].unsqueeze(2).to_broadcast([128, NT, 28]),
                    invf[:, :].unsqueeze(1).to_broadcast([128, NT, 28]), ALU.mult, ["pos_f", "invf"], ["ang"])
            self.ts("dve", t1[:, :, :], ang[:, :, :], 1.0 / TWO_PI, ALU.mult, ["ang"], ["rp_t1"])
            self.cp("dve", ki[:, :, :], t1[:, :, :], ["rp_t1"], ["rp_ki"])
            self.cp("dve", kf[:, :, :], ki[:, :, :], ["rp_ki"], ["rp_kf"])
            HI = 6.28125
            LO = TWO_PI - HI
            self.stt(r[:, :, :], kf[:, :, :], -HI, ang[:, :, :], ALU.mult, ALU.add, ["rp_kf", "ang"], ["rp_r"])
            self.stt(r[:, :, :], kf[:, :, :], -LO, r[:, :, :], ALU.mult, ALU.add, ["rp_kf", "rp_r"], ["rp_r"])

            def wrap(buf, key):
                self.ts("dve", m[:, :, :], buf[:, :, :], math.pi, ALU.is_gt, [key], ["rp_m"])
                self.stt(buf[:, :, :], m[:, :, :], -TWO_PI, buf[:, :, :], ALU.mult, ALU.add, ["rp_m", key], [key])
                self.ts("dve", m[:, :, :], buf[:, :, :], -math.pi, ALU.is_lt, [key], ["rp_m"])
                self.stt(buf[:, :, :], m[:, :, :], TWO_PI, buf[:, :, :], ALU.mult, ALU.add, ["rp_m", key], [key])
                self.ts("dve", buf[:, :, :], buf[:, :, :], math.pi, ALU.min, [key], [key], s2=-math.pi, op1=ALU.max)

            wrap(r, "rp_r")
            self.ts("dve", rc[:, :, :], r[:, :, :], math.pi / 2, ALU.add, ["rp_r"], ["rp_rc"])
            wrap(rc, "rp_rc")
            self.act(self.sn[:, :, :], r[:, :, :], AF.Sin, ["rp_r"], ["rope"])
            self.act(self.cs[:, :, :], rc[:, :, :], AF.Sin, ["rp_rc"], ["rope"])
            memb = self.sb("memb", [128, 2, D], BF16, st)
            c.dma("pool", memb[:, :, :], self.mem_in.rearrange("(t p) d -> p t d", p=128), writes=["memb"])
            for t in range(2):
                pst = self.ps[t][:, :].bitcast(BF16)
                for k in range(8):
                    self.tr(pst[:, k * 128:(k + 1) * 128], memb[:, t, k * 128:(k + 1) * 128], ["memb"], ["ps%d" % t])
                self.cp("dve", self.memT[:, :, t * 128:(t + 1) * 128],
                        pst.rearrange("p (k m) -> p k m", k=8), ["ps%d" % t], ["memT"])
            xb = [self.sb("p0_xb%d" % i, [128, D], BF16, st) for i in range(2)]
            xt = [self.sb("p0_xt%d" % i, [128, 8, 128], BF16, st) for i in range(2)]
            for t in range(NT):
                i = t % 2
                c.dma("pool", xb[i][:, :], self.x_in[t * 128:(t + 1) * 128, :], writes=["p0_xb%d" % i])
                pst = self.ps[2 + i][:, :].bitcast(BF16)
                for k in range(8):
                    self.tr(pst[:, k * 128:(k + 1) * 128], xb[i][:, k * 128:(k + 1) * 128], ["p0_xb%d" % i], ["ps%d" % (2 + i)])
                self.cp("dve" if i == 0 else "act", xt[i][:, :, :], pst.rearrange("p (k m) -> p k m", k=8),
                        ["ps%d" % (2 + i)], ["p0_xt%d" % i])
                c.dma("sp", self.xT_s[:, :, t * 128:(t + 1) * 128], xt[i][:, :, :], reads=["p0_xt%d" % i], writes=["xT_s"])
            c.emit()

    def rope(self, e, out1, out2, a, b, cos, sin, tmp1, tmp2, rk, wk_):
        k1, k2 = "rtmp1" + e, "rtmp2" + e
        self.tt(e, tmp1, a, cos, ALU.mult, rk + ["rope"], [k1])
        self.tt(e, tmp2, b, sin, ALU.mult, rk + ["rope"], [k2])
        self.tt(e, out1, tmp1, tmp2, ALU.subtract, [k1, k2], wk_)
        self.tt(e, tmp1, a, sin, ALU.mult, rk + ["rope"], [k1])
        self.tt(e, tmp2, b, cos, ALU.mult, rk + ["rope"], [k2])
        self.tt(e, out2, tmp1, tmp2, ALU.add, [k1, k2], wk_)

    def phase1(self, l):
        nc, c = self.nc, self.c
        with ExitStack() as st:
            win = self.sb("p1_win", [128, 8, IN_COLS], BF16, st)
            wuq = self.sb("p1_wuq", [128, 2, 384], BF16, st)
            wukv = self.sb("p1_wukv", [128, 512], BF16, st)
            gq = self.sb("p1_gq", [128, 2], F32, st)
            gkv = self.sb("p1_gkv", [128, 1], F32, st)
            for k in range(8):
                c.dma("pool", win[:, k, :], self.w_in[l, k * 128:(k + 1) * 128, :], writes=["p1_win%d" % k])
            wuq_f = self.sb("p1_wuq_f", [128, 2, 384], F32, st)
            wukv_f = self.sb("p1_wukv_f", [128, 512], F32, st)
            c.dma("sp", wuq_f[:, 0, :], self.w_uq[l, 0:128, :], writes=["p1_wuq_f"])
            c.dma("sp", wuq_f[0:64, 1, :], self.w_uq[l, 128:192, :], writes=["p1_wuq_f"])
            c.dma("sp", wukv_f[:, :], self.w_ukv[l, :, :], writes=["p1_wukv_f"])
            c.dma("sp", gq[:, 0:1], self.mla_q_norm[l, 0:128].rearrange("(p o) -> p o", o=1), writes=["p1_gq"])
            c.dma("sp", gq[0:64, 1:2], self.mla_q_norm[l, 128:192].rearrange("(p o) -> p o", o=1), writes=["p1_gq"])
            c.dma("sp", gkv[:, 0:1], self.mla_kv_norm[l, :].rearrange("(p o) -> p o", o=1), writes=["p1_gkv"])
            self.ts("dve", wuq[:, 0, :], wuq_f[:, 0, :], gq[:, 0:1], ALU.mult, ["p1_wuq_f", "p1_gq"], ["p1_wuq"])
            self.ts("dve", wuq[0:64, 1, :], wuq_f[0:64, 1, :], gq[0:64, 1:2], ALU.mult, ["p1_wuq_f", "p1_gq"], ["p1_wuq"])
            self.ts("dve", wukv[:, :], wukv_f[:, :], gkv[:, 0:1], ALU.mult, ["p1_wukv_f", "p1_gkv"], ["p1_wukv"])

            xT = [self.sb("p1_xT%d" % i, [128, 8, 512], BF16, st) for i in range(2)]
            c0 = [self.sb("p1_c0_%d" % i, [128, 352], BF16, st) for i in range(2)]
            ssq = [self.sb("p1_ssq%d" % i, [128, 4], F32, st) for i in range(2)]
            junk = self.sb("p1_junk", [128, 512], F32, st)
            qkS = [self.sb("p1_qkS%d" % i, [128, 512], BF16, st) for i in range(2)]
            qkM = [self.sb("p1_qkM%d" % i, [128, 512], BF16, st) for i in range(2)]
            qkD = [self.sb("p1_qkD%d" % i, [128, 512], BF16, st) for i in range(2)]
            qA = [self.sb("p1_qA%d" % i, [128, 4, 96], BF16, st) for i in range(2)]
            kA = [self.sb("p1_kA%d" % i, [128, 4, 96], BF16, st) for i in range(2)]
            vall = [self.sb("p1_vall%d" % i, [128, 4, 4, 65], BF16, st) for i in range(2)]
            vst = [[vall[i][:, m, :, :] for i in range(2)] for m in range(4)]
            latT = [self.sb("p1_latT%d" % i, [128, 3, 128], BF16, st) for i in range(2)]
            rt1 = self.sb("p1_rt1", [128, 128], F32, st)
            rt2 = self.sb("p1_rt2", [128, 128], F32, st)
            rt5 = self.sb("p1_rt5", [128, 16], F32, st)
            rt6 = self.sb("p1_rt6", [128, 16], F32, st)
            rt3 = self.sb("p1_rt3", [128, 128], F32, st)
            rt4 = self.sb("p1_rt4", [128, 128], F32, st)
            hf = [self.sb("p1_hf%d" % i, [128, 512], F32, st) for i in range(2)]
            kpe = [self.sb("p1_kpe%d" % i, [128, 32], F32, st) for i in range(2)]
            TA = [self.sb("p1_TA%d" % bp, [96, 4, 2, 512], BF16, st) for bp in range(2)]
            TP = [{nm: self.sb("p1_TP%d%s" % (bp, nm), [128, 2, 2, 512], BF16, st) for nm in ("S", "M", "D")} for bp in range(2)]
            mbT = [self.sb("p1_mbT%d" % bp, [64, 512], BF16, st) for bp in range(2)]
            kmT = self.sb("p1_kmT", [128, 2, 16], BF16, st)
            kpart = self.sb("p1_kpart", [128, 2, 2], F32, st)
            ksum = self.sb("p1_ksum", [128, 2], F32, st)
            gate = self.sb("p1_gate", [128, 4, 16], F32, st)
            top8 = self.sb("p1_top8", [128, 4, 8], F32, st)
            mb = [self.sb("p1_mb%d" % i, [128, 4, 16], BF16, st) for i in range(2)]
            for m in range(4):
                for i in range(2):
                    self.ms("pool", vst[m][i][:, :, 64:65], 1.0, ["p1_v%d_%d" % (m, i)])
            self.ms("pool", kmT[:, :, :], 0.0, ["p1_kmT"])

            def load_x(b):
                c.dma("sp", xT[b % 2][:, :, :], self.xT_s[:, :, b * 512:(b + 1) * 512], reads=["xT_s"], writes=["p1_xT%d" % (b % 2)])

            psi = [0]

            def next_ps():
                i = psi[0] % 3
                psi[0] += 1
                return self.ps[i], "ps%d" % i

            tbank = [0]

            def next_tbank():
                tbank[0] += 1
                return 3 if tbank[0] % 2 == 0 else 7

            def tk(nm, bp, role, j):
                return "p1_T%d%s%d_%d" % (bp, nm, role, j)

            def mm_chunk(t, ci):
                b, tl = t // 4, t % 4
                c0_, cw = CH[ci]
                ps, pk = next_ps()
                for k in range(8):
                    self.mm(ps[:, 0:cw], xT[b % 2][:, k, tl * 128:(tl + 1) * 128], win[:, k, c0_:c0_ + cw],
                            k == 0, k == 7, ["p1_xT%d" % (b % 2), "p1_win%d" % k], [pk])
                return ps, pk

            def chunk0(t):
                i = t % 2
                ps, pk = mm_chunk(t, 0)
                cosA, sinA = self.cs[:, t, 0:16], self.sn[:, t, 0:16]
                ck = "p1_c0_%d" % i
                sk = "p1_ssq%d" % i
                self.cp("act", c0[i][:, 0:352], ps[:, 0:352], [pk], [ck])
                self.act(junk[:, 0:192], ps[:, 0:192], AF.Square, [pk], ["p1_junk", sk], accum_out=ssq[i][:, 0:1])
                self.act(junk[:, 192:320], ps[:, 192:320], AF.Square, [pk], ["p1_junk", sk], accum_out=ssq[i][:, 1:2])
                self.cp("pool", hf[i][:, 0:32], c0[i][:, 320:352], [ck], ["p1_hfa%d" % i])
                self.rope("pool", kpe[i][:, 0:16], kpe[i][:, 16:32], hf[i][:, 0:16], hf[i][:, 16:32], cosA, sinA,
                          rt5[:, 0:16], rt6[:, 0:16], ["p1_hfa%d" % i], ["p1_kpe%d" % i])
                self.ts("dve", ssq[i][:, 2:3], ssq[i][:, 0:1], 1.0 / 192, ALU.mult, [sk], [sk], s2=RMS_EPS, op1=ALU.add)
                self.ts("dve", ssq[i][:, 3:4], ssq[i][:, 1:2], 1.0 / 128, ALU.mult, [sk], [sk], s2=RMS_EPS, op1=ALU.add)
                self.act(ssq[i][:, 2:4], ssq[i][:, 2:4], AF.Ln, [sk], [sk])
                self.act(ssq[i][:, 2:4], ssq[i][:, 2:4], AF.Exp, [sk], [sk], scale=-0.5)

            def mla_up(t):
                i = t % 2
                cosA, sinA = self.cs[:, t, 0:16], self.sn[:, t, 0:16]
                ck, sk, lk = "p1_c0_%d" % i, "p1_ssq%d" % i, "p1_latT%d" % i
                pt, ptk = self.ps[4], "ps4"
                ptb = pt[:, :].bitcast(BF16)
                self.tr(ptb[:, 0:128], c0[i][:, 0:128], [ck], [ptk])
                self.tr(ptb[0:64, 128:256], c0[i][:, 128:192], [ck], [ptk])
                self.tr(ptb[:, 256:384], c0[i][:, 192:320], [ck], [ptk])
                self.cp("dve", latT[i][:, 0:3:2, :], ptb[:, 0:384].rearrange("p (a m) -> p a m", a=3)[:, 0:3:2, :], [ptk], [lk])
                self.cp("dve", latT[i][0:64, 1, :], ptb[0:64, 128:256], [ptk], [lk])
                pq, pqk = self.ps[5], "ps5"
                self.mm(pq[:, 0:384], latT[i][:, 0, :], wuq[:, 0, :], True, False, [lk, "p1_wuq"], [pqk])
                self.mm(pq[:, 0:384], latT[i][0:64, 1, :], wuq[0:64, 1, :], False, True, [lk, "p1_wuq"], [pqk])
                pkv, pkvk = self.ps[6], "ps6"
                self.mm(pkv[:, :], latT[i][:, 2, :], wukv[:, :], True, True, [lk, "p1_wukv"], [pkvk])
                pq3 = pq[:, 0:384].rearrange("p (h d) -> p h d", h=4)
                qk_ = "p1_qA%d" % i
                self.ts("dve", qA[i][:, :, 0:64], pq3[:, :, 0:64], ssq[i][:, 2:3], ALU.mult, [pqk, sk], [qk_])
                hk = "p1_hfb%d" % i
                h3 = hf[i][:, 64:192].rearrange("p (h d) -> p h d", h=4)
                self.ts("dve", h3, pq3[:, :, 64:96], ssq[i][:, 2:3], ALU.mult, [pqk, sk], [hk])
                cb = cosA.unsqueeze(1).to_broadcast([128, 4, 16])
                sb_ = sinA.unsqueeze(1).to_broadcast([128, 4, 16])
                self.rope("dve", qA[i][:, :, 64:80], qA[i][:, :, 80:96], h3[:, :, 0:16], h3[:, :, 16:32], cb, sb_,
                          rt1[:, 0:64].rearrange("p (h d) -> p h d", h=4), rt2[:, 0:64].rearrange("p (h d) -> p h d", h=4),
                          [hk], [qk_])
                pkv3 = pkv[:, :].rearrange("p (h d) -> p h d", h=4)
                kk_ = "p1_kA%d" % i
                self.ts("dve", kA[i][:, :, 0:64], pkv3[:, :, 0:64], ssq[i][:, 3:4], ALU.mult, [pkvk, sk], [kk_])
                self.ts("dve", vst[0][i][:, :, 0:64], pkv3[:, :, 64:128], ssq[i][:, 3:4], ALU.mult, [pkvk, sk], ["p1_v0_%d" % i])
                self.cp("pool", kA[i][:, :, 64:96], kpe[i][:, :].unsqueeze(1).to_broadcast([128, 4, 32]), ["p1_kpe%d" % i], [kk_])

            def chunk_plain(t, ci):
                i = t % 2
                ps, pk = mm_chunk(t, ci)
                if ci == 1:
                    self.cp("act", qkS[i][:, :], ps[:, :], [pk], ["p1_qkS%d" % i])
                elif ci == 2:
                    self.cp("act", vst[1][i][:, :, 0:64], ps[:, 0:256].rearrange("p (h d) -> p h d", h=4), [pk], ["p1_v1_%d" % i])
                    self.cp("act", vst[2][i][:, :, 0:64], ps[:, 256:512].rearrange("p (h d) -> p h d", h=4), [pk], ["p1_v2_%d" % i])
                else:
                    self.cp("act", vst[3][i][:, :, 0:64], ps[:, 0:256].rearrange("p (h d) -> p h d", h=4), [pk], ["p1_v3_%d" % i])

            def chunk_rope(t, ci):
                i = t % 2
                ps, pk = mm_chunk(t, ci)
                dst = (qkM if ci == 3 else qkD)[i]
                dk = ("p1_qkM%d" if ci == 3 else "p1_qkD%d") % i
                hk = "p1_hfc%d_%d" % (ci, i)
                self.cp("dve", dst[:, :], ps[:, :], [pk], [dk])
                if ci == 3:
                    g, nr = 8, 8
                    cb, sb_ = self.cs[:, t, 16:24], self.sn[:, t, 16:24]
                    hbase = 192
                else:
                    g, nr = 16, 4
                    cb, sb_ = self.cs[:, t, 24:28], self.sn[:, t, 24:28]
                    hbase = 320
                p3 = ps[:, :].rearrange("p (g d) -> p g d", g=g)
                d3 = dst[:, :].rearrange("p (g d) -> p g d", g=g)
                cbb = cb.unsqueeze(1).to_broadcast([128, g, nr])
                sbb = sb_.unsqueeze(1).to_broadcast([128, g, nr])
                h3 = hf[i][:, hbase:hbase + g * 2 * nr].rearrange("p (g d) -> p g d", g=g)
                self.cp("dve", h3, p3[:, :, 0:2 * nr], [pk], [hk])
                self.rope("pool", d3[:, :, 0:nr], d3[:, :, nr:2 * nr], h3[:, :, 0:nr], h3[:, :, nr:2 * nr], cbb, sbb,
                          rt3[:, 0:g * nr].rearrange("p (g d) -> p g d", g=g), rt4[:, 0:g * nr].rearrange("p (g d) -> p g d", g=g),
                          [hk], [dk])

            def v_store(t):
                i = t % 2
                c.dma("pool", self.v_s[t * 128:(t + 1) * 128, :], vall[i][:, :, :, :].rearrange("p m h d -> p (m h d)"),
                      reads=["p1_v%d_%d" % (m, i) for m in range(4)], writes=["v_s"])

            def tr_mla(t):
                i, bp = t % 2, (t // 4) % 2
                cols = slice((t % 4) * 128, (t % 4 + 1) * 128)
                for role, src, sk in ((0, qA[i], "p1_qA%d" % i), (1, kA[i], "p1_kA%d" % i)):
                    tb_ = next_tbank()
                    ptb = self.ps[tb_][:, :].bitcast(BF16)
                    for h in range(4):
                        self.tr(ptb[0:96, h * 128:(h + 1) * 128], src[:, h, :], [sk], ["ps%d" % tb_])
                    for h in range(4):
                        self.cp("act" if role == 0 else "dve", TA[bp][:, h, role, cols], ptb[0:96, h * 128:(h + 1) * 128], ["ps%d" % tb_],
                                ["p1_TA%d" % bp])

            def tr_pairs(t, nm):
                i, bp = t % 2, (t // 4) % 2
                cols = slice((t % 4) * 128, (t % 4 + 1) * 128)
                src, sk = {"S": (qkS[i], "p1_qkS%d" % i), "M": (qkM[i], "p1_qkM%d" % i), "D": (qkD[i], "p1_qkD%d" % i)}[nm]
                tb_ = next_tbank()
                ptb = self.ps[tb_][:, :].bitcast(BF16)
                for j in range(4):
                    self.tr(ptb[:, j * 128:(j + 1) * 128], src[:, j * 128:(j + 1) * 128], [sk], ["ps%d" % tb_])
                for j in range(4):
                    role, pr = j // 2, j % 2
                    self.cp("act" if nm != "D" else "dve", TP[bp][nm][:, role, pr, cols], ptb[:, j * 128:(j + 1) * 128], ["ps%d" % tb_],
                            ["p1_TP%d%s" % (bp, nm)])

            def gating(t):
                i, bp = t % 2, (t // 4) % 2
                cols = slice((t % 4) * 128, (t % 4 + 1) * 128)
                blk = t // 2
                pg, pgk = self.ps[6], "ps6"
                for pr in range(2):
                    self.mm(pg[:, pr:pr + 1], qkM[i][:, 256 + pr * 128:256 + (pr + 1) * 128], self.ones_b[:, 0:1], True, True,
                            ["p1_qkM%d" % i, "const"], [pgk])
                self.cp("dve", kpart[:, t % 2, :], pg[:, 0:2], [pgk], ["p1_kpart"])
                if t % 2 == 1:
                    self.tt("dve", ksum[:, :], kpart[:, 0, :], kpart[:, 1, :], ALU.add, ["p1_kpart"], ["p1_ksum"])
                    self.ts("dve", kmT[:, :, blk], ksum[:, :], 1.0 / 256, ALU.mult, ["p1_ksum"], ["p1_kmT"])
                own = blk
                mk = "p1_mb%d" % i
                if own >= 4:
                    g3 = gate[:, :, :].rearrange("p (a b) j -> p a b j", b=2)
                    self.ms("pool", gate[:, :, :], -BIGV, ["p1_gate"])
                    for hh, (pg, pgk) in enumerate(((self.ps[5], "ps5"), (self.ps[6], "ps6"))):
                        p0 = hh * 64
                        for pr in range(2):
                            qT = TP[bp]["M"][p0:p0 + 64, 0, pr, cols]
                            self.mm(pg[:, pr * 16:(pr + 1) * 16], qT, kmT[p0:p0 + 64, pr, :],
                                    True, True, ["p1_TP%dM" % bp, "p1_kmT"], [pgk])
                        self.cp("dve", g3[:, :, hh, 0:own], pg[:, 0:32].rearrange("p (a j) -> p a j", a=2)[:, :, 0:own], [pgk], ["p1_gate"])
                    for h in range(4):
                        nc_ = self.nc
                        gh = gate[:, h, :]
                        th = top8[:, h, :]
                        c.op("dve", (lambda gh=gh, th=th: nc_.vector.max(out=th, in_=gh)), ["p1_gate"], ["p1_top8"])
                    for h in range(4):
                        self.ts("dve", mb[i][:, h, :], gate[:, h, :], top8[:, h, 2:3], ALU.is_ge, ["p1_gate", "p1_top8"], [mk],
                                s2=-1.0, op1=ALU.add)
                    self.ms("pool", mb[i][:, :, own:own + 1], 0.0, [mk])
                else:
                    self.ms("pool", mb[i][:, :, :], 0.0, [mk])
                pm, pmk = self.ps[4], "ps4"
                pmb = pm[:, :].bitcast(BF16)
                self.tr(pmb[0:64, 0:128], mb[i][:, :, :].rearrange("p h j -> p (h j)"), [mk], [pmk])
                self.cp("dve", mbT[bp][:, cols], pmb[0:64, 0:128], [pmk], ["p1_mbT%d" % bp])

            def block_store(b):
                bp = b % 2
                bc = slice(b * 512, (b + 1) * 512)
                c.dma("sp", self.qk_s[0, :, :, 0:96, bc].rearrange("h r p c -> p h r c"), TA[bp][:, :, :, :], reads=["p1_TA%d" % bp], writes=["qk_s"])
                for mi, nm in ((0, "S"), (1, "M"), (2, "D")):
                    c.dma("sp", self.qkp_s[mi, :, :, :, bc].rearrange("r q p c -> p r q c"), TP[bp][nm][:, :, :, :],
                          reads=["p1_TP%d%s" % (bp, nm)], writes=["qk_s"])
                c.dma("sp", self.mb_s[:, bc], mbT[bp][:, :], reads=["p1_mbT%d" % bp], writes=["qk_s"])

            load_x(0)
            for t in range(NT + 1):
                cur = t if t < NT else None
                prv = t - 1 if t >= 1 else None
                if cur is not None and cur % 4 == 0 and cur // 4 + 1 < NB:
                    load_x(cur // 4 + 1)
                if cur is not None:
                    chunk0(cur)
                    chunk_plain(cur, 1)
                if prv is not None:
                    tr_mla(prv)
                if cur is not None:
                    chunk_plain(cur, 2)
                    mla_up(cur)
                    chunk_rope(cur, 3)
                if prv is not None:
                    tr_pairs(prv, "S")
                    tr_pairs(prv, "M")
                if cur is not None:
                    chunk_rope(cur, 4)
                if prv is not None:
                    tr_pairs(prv, "D")
                if cur is not None:
                    chunk_plain(cur, 5)
                    v_store(cur)
                if prv is not None:
                    gating(prv)
                    if prv % 4 == 3:
                        block_store(prv // 4)
            for h in range(4):
                c.dma("sp", self.qk_s[2, h, 1, 64:80, :], self.c_blk, writes=["qk_s"])
            c.emit()


    def phase2(self, l):
        nc, c = self.nc, self.c
        lambda_init = 0.8 - 0.6 * math.exp(-0.3 * l)
        with ExitStack() as st:
            KT = [self.sb("p2_KT%d" % i, [128, S], BF16, st) for i in range(2)]
            QT = [self.sb("p2_QT%d" % i, [128, S], BF16, st) for i in range(2)]
            Vm = [self.sb("p2_Vm%d" % i, [128, NT, 260], BF16, st) for i in range(2)]
            om = [self.sb("p2_om%d" % i, [128, NT, 256], BF16, st) for i in range(2)]
            od = [self.sb("p2_od%d" % i, [128, NT, 64], F32, st) for i in range(2)]
            PT = [self.sb("p2_PT%d" % i, [128, 512], BF16, st) for i in range(4)]
            SBANK = [0, 1, 2, 7]
            E32 = [self.sb("p2_E%d" % i, [128, 512], F32, st) for i in range(3)]
            Psp = [self.sb("p2_P%d" % i, [128, 512], BF16, st) for i in range(3)]
            T = [self.sb("p2_T%d" % i, [128, 512], F32, st) for i in range(3)]
            Aex = [self.sb("p2_A%d" % i, [128, 512], BF16, st) for i in range(3)]
            Run = self.sb("p2_Run", [128, 512], BF16, st)
            rc = self.sb("p2_rc", [128, 4], F32, st)
            hcount = [0]
            vcount = [0]

            def load_head(m, h, rows, r0=0):
                j = hcount[0] % 2
                hcount[0] += 1
                for buf, bk in ((KT[j], "p2_KT%d" % j), (QT[j], "p2_QT%d" % j)):
                    if rows <= 32:
                        self.ms("pool", buf[32:64, :], 0.0, [bk])
                    self.ms("pool", buf[64:128, :], 0.0, [bk])
                for role, buf, bk in ((1, KT[j], "p2_KT%d" % j), (0, QT[j], "p2_QT%d" % j)):
                    if m == 0:
                        src = self.qk_s[0, h, role, 0:rows, :]
                    else:
                        rb = (h % 2) * 64 + r0
                        src = self.qkp_s[m - 1, role, h // 2, rb:rb + min(rows, 64), :]
                    c.dma("sp", buf[0:min(rows, 96 if m == 0 else 64), :], src, reads=["qk_s"], writes=[bk])
                if m == 2:
                    c.dma("sp", QT[j][64:80, :], self.mb_s[h * 16:(h + 1) * 16, :], reads=["qk_s"], writes=["p2_QT%d" % j])
                    c.dma("sp", KT[j][64:80, :], self.c_blk, writes=["p2_KT%d" % j])
                return KT[j], QT[j], ["p2_KT%d" % j, "p2_QT%d" % j]

            def load_v(m):
                j = vcount[0] % 2
                vcount[0] += 1
                c.dma("sp", Vm[j][:, :, :], self.v_s[:, m * 260:(m + 1) * 260].rearrange("(t p) c -> p t c", p=128), reads=["v_s"], writes=["p2_Vm%d" % j])
                return Vm[j], "p2_Vm%d" % j

            sslot = [0]

            def softmax_head(kt, qt, kqk, R, scale, vm, vkey, h, out_fn):
                for b in range(NB):
                    n = 4 * b + 4

                    def S_stage(i):
                        j = i - 4 * b
                        c0 = max(0, j) * 128
                        slot = sslot[0]
                        sslot[0] = (slot + 1) % 4
                        ps, pk = self.ps[SBANK[slot]], "ps%d" % SBANK[slot]
                        self.mm(ps[:, c0:512], kt[:, i * 128:(i + 1) * 128], qt[:, b * 512 + c0:(b + 1) * 512], True, True, kqk, [pk])
                        pt, ptk = PT[slot], "p2_PT%d" % slot
                        self.act(pt[:, c0:512], ps[:, c0:512], AF.Exp, [pk], [ptk], scale=scale)
                        if j >= 0:
                            self.tt("pool", pt[:, c0:c0 + 128], pt[:, c0:c0 + 128], self.mask_le[:, :], ALU.mult, [ptk, "const"], [ptk])
                        return slot

                    def PV_stage(i, slot):
                        for s_ in range(4):
                            if 4 * b + s_ >= i:
                                self.mm(self.ps[3 + s_][:, 0:65], PT[slot][:, s_ * 128:(s_ + 1) * 128], vm[:, i, h * 65:(h + 1) * 65],
                                        i == 0, i == 4 * b + s_, ["p2_PT%d" % slot, vkey], ["ps%d" % (3 + s_)])

                    slots = {}
                    for i in range(min(2, n)):
                        slots[i] = S_stage(i)
                    for i in range(n):
                        if i + 2 < n:
                            slots[i + 2] = S_stage(i + 2)
                        PV_stage(i, slots[i])
                        if i >= 4 * b:
                            s_ = i - 4 * b
                            out_fn(b, s_, self.ps[3 + s_], "ps%d" % (3 + s_))

            def sb_head(kt, qt, kqk, vm, vkey, h, omt, omk):
                scale = 0.125
                for b in range(NB):
                    order = list(range(4 * b + 3, -1, -1))
                    n = len(order)
                    self.ms("pool", Run[:, :], 0.0, ["p2_Run"])

                    def geo(idx):
                        i = order[idx]
                        j = i - 4 * b
                        return i, j, max(0, j) * 128

                    def A_stage(idx):
                        i, j, c0 = geo(idx)
                        z, q3 = idx % 2, idx % 3
                        ps, pk = self.ps[z], "ps%d" % z
                        self.mm(ps[:, c0:512], kt[:, i * 128:(i + 1) * 128], qt[:, b * 512 + c0:(b + 1) * 512], True, True, kqk, [pk])
                        e, ek = E32[q3], "p2_E%d" % q3
                        self.act(e[:, c0:512], ps[:, c0:512], AF.Exp, [pk], [ek], scale=scale)
                        if j >= 0:
                            self.tt("pool", e[:, c0:c0 + 128], e[:, c0:c0 + 128], self.mask_lt[:, :], ALU.mult, [ek, "const"], [ek])
                        self.act(Psp[q3][:, c0:512], e[:, c0:512], AF.Ln, [ek], ["p2_P%d" % q3], bias=1.0)

                    def B_stage(idx):
                        i, j, c0 = geo(idx)
                        z, q3 = idx % 2, idx % 3
                        af, afk = (self.ps[2], "ps2") if z == 0 else (self.ps[7], "ps7")
                        pk2 = "p2_P%d" % q3
                        first = idx == 0
                        self.mm(af[:, c0:512], self.tri[:, :], Psp[q3][:, c0:512], True, first, ["const", pk2], [afk])
                        if not first:
                            self.mm(af[:, c0:512], self.ones_b[:, :], Run[:, c0:512], False, True, ["const", "p2_Run"], [afk])
                        self.tt("pool", Run[:, c0:512], Run[:, c0:512], Psp[q3][:, c0:512], ALU.add, ["p2_Run", pk2], ["p2_Run"])
                        t, tk = T[q3], "p2_T%d" % q3
                        self.stt(t[:, c0:512], self.ps[z][:, c0:512], scale, Psp[q3][:, c0:512], ALU.mult, ALU.subtract, ["ps%d" % z, pk2], [tk])
                        self.tt("dve", t[:, c0:512], t[:, c0:512], af[:, c0:512], ALU.subtract, [tk, afk], [tk])
                        a, ak = Aex[q3], "p2_A%d" % q3
                        self.act(a[:, c0:512], t[:, c0:512], AF.Exp, [tk], [ak])
                        if j >= 0:
                            self.tt("pool", a[:, c0:c0 + 128], a[:, c0:c0 + 128], self.mask_lt[:, :], ALU.mult, [ak, "const"], [ak])

                    def C_stage(idx):
                        i, j, c0 = geo(idx)
                        q3 = idx % 3
                        for s_ in range(4):
                            if 4 * b + s_ >= i:
                                self.mm(self.ps[3 + s_][:, 0:64], Aex[q3][:, s_ * 128:(s_ + 1) * 128], vm[:, i, h * 65:h * 65 + 64],
                                        i == 4 * b + s_, i == 0, ["p2_A%d" % q3, vkey], ["ps%d" % (3 + s_)])

                    for step in range(n + 2):
                        if step < n:
                            A_stage(step)
                        if 0 <= step - 1 < n:
                            B_stage(step - 1)
                        if 0 <= step - 2 < n:
                            C_stage(step - 2)
                    for s_ in range(4):
                        self.cp("dve", omt[:, 4 * b + s_, h * 64:(h + 1) * 64], self.ps[3 + s_][:, 0:64], ["ps%d" % (3 + s_)], [omk])

            def norm_out(dst, dkey, col0, f32=False):
                def fn(b, s_, O, ok):
                    self.recip(rc[:, s_:s_ + 1], O[:, 64:65], [ok], ["p2_rc"])
                    self.ts("dve", dst[:, 4 * b + s_, col0:col0 + 64], O[:, 0:64], rc[:, s_:s_ + 1], ALU.mult, [ok, "p2_rc"], [dkey])
                return fn

            def store_om(m, omt, omk):
                c.dma("sp", self.o_s[:, m * 256:(m + 1) * 256].rearrange("(t p) c -> p t c", p=128), omt[:, :, :], reads=[omk], writes=["o_s"])

            oi = 0
            for m, R, scale in ((0, 96, 96 ** -0.5), (2, 80, 0.125)):
                vm, vkey = load_v(m)
                omt, omk = om[oi % 2], "p2_om%d" % (oi % 2)
                oi += 1
                for h in range(4):
                    kt, qt, kqk = load_head(m, h, R)
                    softmax_head(kt, qt, kqk, R, scale, vm, vkey, h, norm_out(omt, omk, h * 64))
                store_om(m, omt, omk)
            vm, vkey = load_v(1)
            omt, omk = om[oi % 2], "p2_om%d" % (oi % 2)
            oi += 1
            for h in range(4):
                kt, qt, kqk = load_head(1, h, 64)
                sb_head(kt, qt, kqk, vm, vkey, h, omt, omk)
            store_om(1, omt, omk)
            vm, vkey = load_v(3)
            dcount = 0
            for comp in range(2):
                for h in range(4):
                    kt, qt, kqk = load_head(3, h, 32, comp * 32)
                    odt, odk = od[dcount % 2], "p2_od%d" % (dcount % 2)
                    dcount += 1
                    softmax_head(kt, qt, kqk, 32, 32 ** -0.5, vm, vkey, h, norm_out(odt, odk, 0))
                    c.dma("sp", self.od_s[comp, :, h * 64:(h + 1) * 64].rearrange("(t p) c -> p t c", p=128), odt[:, :, :],
                          reads=[odk], writes=["od_s"])
            dl = self.sb("p2_dl", [128, 128], F32, st)
            lam = self.sb("p2_lam", [128, 4], F32, st)
            gsub = self.sb("p2_gsub", [128, 64], F32, st)
            junk = self.sb("p2_junk", [128, 64], F32, st)
            c.dma("sp", dl[:, :], self.diff_lambda[l, :].partition_broadcast(128), writes=["p2_dl"])
            c.dma("sp", gsub[:, :], self.diff_subln[l, :].partition_broadcast(128), writes=["p2_gsub"])
            self.tt("dve", junk[:, 0:32], dl[:, 0:32], dl[:, 32:64], ALU.mult, ["p2_dl"], ["p2_junk"])
            self.red(lam[:, 0:1], junk[:, 0:32], ALU.add, ["p2_junk"], ["p2_lam"])
            self.tt("dve", junk[:, 32:64], dl[:, 64:96], dl[:, 96:128], ALU.mult, ["p2_dl"], ["p2_junk"])
            self.red(lam[:, 1:2], junk[:, 32:64], ALU.add, ["p2_junk"], ["p2_lam"])
            self.act(lam[:, 0:2], lam[:, 0:2], AF.Exp, ["p2_lam"], ["p2_lam"])
            self.tt("dve", lam[:, 2:3], lam[:, 1:2], lam[:, 0:1], ALU.subtract, ["p2_lam"], ["p2_lam"])
            self.ts("dve", lam[:, 2:3], lam[:, 2:3], -lambda_init, ALU.add, ["p2_lam"], ["p2_lam"])
            self.ts("dve", gsub[:, :], gsub[:, :], 1.0 - lambda_init, ALU.mult, ["p2_gsub"], ["p2_gsub"])
            omt, omk = om[oi % 2], "p2_om%d" % (oi % 2)
            oi += 1
            d1 = [self.sb("p2_d1_%d" % i, [128, 256], F32, st) for i in range(2)]
            d2 = [self.sb("p2_d2_%d" % i, [128, 256], F32, st) for i in range(2)]
            sq = self.sb("p2_sq", [128, 256], F32, st)
            ss = self.sb("p2_ss", [128, 4], F32, st)
            for t in range(NT):
                i = t % 2
                c.dma("sp", d1[i][:, :], self.od_s[0, t * 128:(t + 1) * 128, :], reads=["od_s"], writes=["p2_d1_%d" % i])
                c.dma("sp", d2[i][:, :], self.od_s[1, t * 128:(t + 1) * 128, :], reads=["od_s"], writes=["p2_d2_%d" % i])
                k1 = "p2_d1_%d" % i
                self.stt(d1[i][:, :], d2[i][:, :], lam[:, 2:3], d1[i][:, :], ALU.mult, ALU.add, ["p2_d2_%d" % i, k1, "p2_lam"], [k1])
                self.tt("dve", sq[:, :], d1[i][:, :], d1[i][:, :], ALU.mult, [k1], ["p2_sq"])
                self.red(ss[:, :], sq[:, :].rearrange("p (h d) -> p h d", h=4), ALU.add, ["p2_sq"], ["p2_ss"])
                self.ts("dve", ss[:, :], ss[:, :], 1.0 / 64, ALU.mult, ["p2_ss"], ["p2_ss"], s2=RMS_EPS, op1=ALU.add)
                self.act(ss[:, :], ss[:, :], AF.Ln, ["p2_ss"], ["p2_ss"])
                self.act(ss[:, :], ss[:, :], AF.Exp, ["p2_ss"], ["p2_ss"], scale=-0.5)
                d3 = d1[i][:, :].rearrange("p (h d) -> p h d", h=4)
                self.tt("dve", d3, d3, ss[:, :].unsqueeze(2).to_broadcast([128, 4, 64]), ALU.mult, [k1, "p2_ss"], [k1])
                self.tt("dve", omt[:, t, :].rearrange("p (h d) -> p h d", h=4), d3, gsub[:, :].unsqueeze(1).to_broadcast([128, 4, 64]),
                        ALU.mult, [k1, "p2_gsub"], [omk])
            store_om(3, omt, omk)
            c.emit()

    def ln_part1(self, y, yk, res, resk, pa, pak, pb, pbk, tmp, tk):
        nc = self.nc
        st6, mv = tmp["st6"], tmp["mv"]
        self.stt(y[:, 0:512], res[:, 0:512], ALPHA, pa[:, 0:512], ALU.mult, ALU.add, [resk, pak], [yk])
        self.stt(y[:, 512:1024], res[:, 512:1024], ALPHA, pb[:, 0:512], ALU.mult, ALU.add, [resk, pbk], [yk])
        self.c.op("dve", lambda: nc.vector.bn_stats(out=st6[:, 0:6], in_=y[:, 0:512]), [yk], [tk + "s"])
        self.c.op("dve", lambda: nc.vector.bn_stats(out=st6[:, 6:12], in_=y[:, 512:1024]), [yk], [tk + "s"])
        self.c.op("dve", lambda: nc.vector.bn_aggr(out=mv[:, 0:2], in_=st6[:, 0:12]), [tk + "s"], [tk])
        self.ts("dve", mv[:, 2:3], mv[:, 1:2], LN_EPS, ALU.add, [tk], [tk])
        self.act(mv[:, 2:3], mv[:, 2:3], AF.Ln, [tk], [tk])
        self.act(mv[:, 2:3], mv[:, 2:3], AF.Exp, [tk], [tk], scale=-0.5)

    def ln_part2(self, y, yk, g_t, b_t, gbk, out, outk, tmp, tk):
        mv = tmp["mv"]
        self.stt(y[:, :], y[:, :], mv[:, 0:1], g_t[:, :], ALU.subtract, ALU.mult, [yk, tk, gbk], [yk])
        self.stt(out, y[:, :], mv[:, 2:3], b_t[:, :], ALU.mult, ALU.add, [yk, tk, gbk], [outk])

    def ln_tile(self, y, yk, res, resk, pa, pak, pb, pbk, g_t, b_t, gbk, out, outk, tmp, tk="ln_mv"):
        self.ln_part1(y, yk, res, resk, pa, pak, pb, pbk, tmp, tk)
        self.ln_part2(y, yk, g_t, b_t, gbk, out, outk, tmp, tk)

    def to_xT(self, src, srck, xb, xbk, xts, xtsk, cols, eng):
        self.cp("act", xb[:, :], src, [srck], [xbk])
        ptb = self.ps[7][:, :].bitcast(BF16)
        for k in range(8):
            self.tr(ptb[:, k * 128:(k + 1) * 128], xb[:, k * 128:(k + 1) * 128], [xbk], ["ps7"])
        self.cp(eng, xts[:, :, cols], ptb.rearrange("p (k m) -> p k m", k=8), ["ps7"], [xtsk])

    def phase34(self, l):
        nc, c = self.nc, self.c
        xres_src = self.x_in if l == 0 else self.xres_s
        with ExitStack() as st:
            KmT = self.sb("p3_KmT", [128, 8, N_MEM], BF16, st)
            Vmem = self.sb("p3_Vmem", [128, 2, 4 * 257], BF16, st)
            wout = self.sb("p3_wout", [128, 8, D], BF16, st)
            wq = self.sb("p3_wq", [128, 8, D], BF16, st)
            wo = self.sb("p3_wo", [128, 8, D], BF16, st)
            with ExitStack() as st2:
                wk = self.sb("p3_wk", [128, 8, D], BF16, st2)
                wv = self.sb("p3_wv", [128, 8, D], BF16, st2)
                for k in range(8):
                    c.dma("pool", wk[:, k, :], self.wk[l, k * 128:(k + 1) * 128, :], writes=["p3_wk%d" % k])
                    c.dma("pool", wv[:, k, :], self.wv[l, k * 128:(k + 1) * 128, :], writes=["p3_wv%d" % k])
                self.ms("pool", Vmem[:, :, :], 1.0, ["p3_Vmem"])
                for k in range(8):
                    c.dma("pool", wout[:, k, :], self.w_out[l, k * 128:(k + 1) * 128, :], writes=["p3_wout%d" % k])
                for k in range(8):
                    c.dma("pool", wq[:, k, :], self.wq[l, k * 128:(k + 1) * 128, :], writes=["p3_wq%d" % k])
                for k in range(8):
                    c.dma("pool", wo[:, k, :], self.wo[l, k * 128:(k + 1) * 128, :], writes=["p3_wo%d" % k])
                for j in range(8):
                    ps, pk = self.ps[j % 2], "ps%d" % (j % 2)
                    for k in range(8):
                        self.mm(ps[:, 0:N_MEM], wk[:, k, j * 128:(j + 1) * 128], self.memT[:, k, :], k == 0, k == 7, ["p3_wk%d" % k, "memT"], [pk])
                    self.cp("act" if j % 2 == 0 else "dve", KmT[:, j, :], ps[:, 0:N_MEM], [pk], ["p3_KmT"])
                for mt in range(2):
                    for nh in range(2):
                        ps, pk = self.ps[2 + nh], "ps%d" % (2 + nh)
                        for k in range(8):
                            self.mm(ps[:, :], self.memT[:, k, mt * 128:(mt + 1) * 128], wv[:, k, nh * 512:(nh + 1) * 512], k == 0, k == 7,
                                    ["p3_wv%d" % k, "memT"], [pk])
                        dst = Vmem[:, mt, nh * 514:(nh + 1) * 514].rearrange("p (h d) -> p h d", h=2)[:, :, 0:256]
                        self.cp("act" if nh == 0 else "dve", dst, ps[:, :].rearrange("p (h d) -> p h d", h=2), [pk], ["p3_Vmem"])
                c.emit()
            gb = {}
            for nm, src in (("g1", self.ln_g[0]), ("b1", self.ln_b[0]), ("g2", self.ln_g[1]), ("b2", self.ln_b[1])):
                gb[nm] = self.sb("p3_" + nm, [128, D], F32, st)
                c.dma("sp", gb[nm][:, :], src[l, :].partition_broadcast(128), writes=["p3_gb"])
            tmpA = {"st6": self.sb("p3_st6a", [128, 12], F32, st), "mv": self.sb("p3_mva", [128, 4], F32, st)}
            tmpB = {"st6": self.sb("p3_st6b", [128, 12], F32, st), "mv": self.sb("p3_mvb", [128, 4], F32, st)}
            ob = [self.sb("p3_ob0", [128, 4, D], BF16, st)] * 2
            oT = [self.sb("p3_oT0", [128, 8, 512], BF16, st)] * 2
            xr = [self.sb("p3_xr%d" % i, [128, D], F32, st) for i in range(2)]
            xr2 = [self.sb("p3_xq%d" % i, [128, D], F32, st) for i in range(2)]
            yA = self.sb("p3_yA", [128, D], F32, st)
            yB = self.sb("p3_yB", [128, D], F32, st)
            x1t = [self.sb("p3_x1t%d" % i, [128, D], F32, st) for i in range(2)]
            xbA = self.sb("p3_xbA", [128, D], BF16, st)
            xbB = self.sb("p3_xbB", [128, D], BF16, st)
            x1T = [self.sb("p3_x1T%d" % i, [128, 8, 512], BF16, st) for i in range(2)]
            qT = self.sb("p3_qT", [128, 8, 512], BF16, st)
            PT2 = [self.sb("p3_PT%d" % i, [128, 512], BF16, st) for i in range(4)]
            xo = self.sb("p3_xo", [128, 4, D], BF16, st)
            xoT = self.sb("p3_xoT", [128, 8, 512], BF16, st)
            x2 = [self.sb("p3_x2_%d" % i, [128, D], F32, st) for i in range(2)]
            x2T = [self.sb("p3_x2T0", [128, 8, 512], BF16, st)] * 2
            rc = self.sb("p3_rc", [128, 2], F32, st)

            def to_xT2(src, srck, xb, xbk, xts, xtsk, cols, bank, eng):
                self.cp("act", xb[:, :], src, [srck], [xbk])
                ptb = self.ps[bank][:, :].bitcast(BF16)
                for k in range(8):
                    self.tr(ptb[:, k * 128:(k + 1) * 128], xb[:, k * 128:(k + 1) * 128], [xbk], ["ps%d" % bank])
                self.cp(eng, xts[:, :, cols], ptb.rearrange("p (k m) -> p k m", k=8), ["ps%d" % bank], [xtsk])

            def load_ob(b):
                c.dma("sp", ob[0][:, :, :], self.o_s[b * 512:(b + 1) * 512, :].rearrange("(t p) c -> p t c", p=128),
                      reads=["o_s"], writes=["p3_ob0"])

            def H1(b):
                steps = []
                p = b % 2
                obk, oTk, x1Tk = "p3_ob0", "p3_oT0", "p3_x1T%d" % p

                def s_oT(tl):
                    ptb = self.ps[2][:, :].bitcast(BF16)
                    for k in range(8):
                        self.tr(ptb[:, k * 128:(k + 1) * 128], ob[p][:, tl, k * 128:(k + 1) * 128], [obk], ["ps2"])
                    self.cp("act", oT[p][:, :, tl * 128:(tl + 1) * 128], ptb.rearrange("p (k m) -> p k m", k=8), ["ps2"], [oTk])

                def s_mix(tl):
                    t = b * 4 + tl
                    xi = t % 2
                    c.dma("sp", xr[xi][:, :], xres_src[t * 128:(t + 1) * 128, :], reads=["xres_s%d" % t], writes=["p3_xr%d" % xi])
                    for nh in range(2):
                        for k in range(8):
                            self.mm(self.ps[nh][:, :], oT[p][:, k, tl * 128:(tl + 1) * 128], wout[:, k, nh * 512:(nh + 1) * 512], k == 0, k == 7,
                                    [oTk, "p3_wout%d" % k], ["ps%d" % nh])
                    self.ln_part1(yA, "p3_yA", xr[xi], "p3_xr%d" % xi, self.ps[0], "ps0", self.ps[1], "ps1", tmpA, "lnA")

                def s_mix_b(tl):
                    t = b * 4 + tl
                    xi = t % 2
                    self.ln_part2(yA, "p3_yA", gb["g1"], gb["b1"], "p3_gb", x1t[xi][:, :], "p3_x1t%d" % xi, tmpA, "lnA")
                    c.dma("pool", self.x1_s[t * 128:(t + 1) * 128, :], x1t[xi][:, :], reads=["p3_x1t%d" % xi], writes=["x1_s%d" % t])

                def s_x1T(tl):
                    xi = (b * 4 + tl) % 2
                    to_xT2(x1t[xi][:, :], "p3_x1t%d" % xi, xbA, "p3_xbA", x1T[p], x1Tk, slice(tl * 128, (tl + 1) * 128), 2, "act")

                steps.append(lambda: load_ob(b))
                for tl in range(4):
                    steps.append(lambda tl=tl: s_oT(tl))
                for tl in range(4):
                    steps.append(lambda tl=tl: s_mix(tl))
                    if tl > 0:
                        steps.append(lambda tl=tl: s_x1T(tl - 1))
                    steps.append(lambda tl=tl: s_mix_b(tl))
                steps.append(lambda: s_x1T(3))
                return steps

            pcount = [0]

            def H2(b):
                steps = []
                p = b % 2
                x1Tk = "p3_x1T%d" % p
                x2Tb, x2Tk = x2T[0], "p3_x2T0"

                def s_q(j):
                    ps, pk = self.ps[3 + j % 2], "ps%d" % (3 + j % 2)
                    for k in range(8):
                        self.mm(ps[:, :], wq[:, k, j * 128:(j + 1) * 128], x1T[p][:, k, :], k == 0, k == 7, ["p3_wq%d" % k, x1Tk], [pk])
                    self.cp("act" if j % 2 == 0 else "dve", qT[:, j, :], ps[:, :], [pk], ["p3_qT"])

                def s_att(h):
                    pts = []
                    for mt in range(2):
                        ps, pk = self.ps[5 + mt], "ps%d" % (5 + mt)
                        for cc in range(2):
                            self.mm(ps[:, :], KmT[:, h * 2 + cc, mt * 128:(mt + 1) * 128], qT[:, h * 2 + cc, :], cc == 0, cc == 1,
                                    ["p3_KmT", "p3_qT"], [pk])
                        pi = pcount[0] % 4
                        pcount[0] += 1
                        self.act(PT2[pi][:, :], ps[:, :], AF.Exp, [pk], ["p3_PT%d" % pi], scale=256 ** -0.5)
                        pts.append(pi)
                    for tl in range(4):
                        ps, pk = self.ps[7], "ps7"
                        for mt in range(2):
                            self.mm(ps[:, 0:257], PT2[pts[mt]][:, tl * 128:(tl + 1) * 128], Vmem[:, mt, h * 257:(h + 1) * 257], mt == 0, mt == 1,
                                    ["p3_PT%d" % pts[mt], "p3_Vmem"], [pk])
                        self.recip(rc[:, tl % 2:tl % 2 + 1], ps[:, 256:257], [pk], ["p3_rc%d" % (tl % 2)])
                        self.ts("dve", xo[:, tl, h * 256:(h + 1) * 256], ps[:, 0:256], rc[:, tl % 2:tl % 2 + 1], ALU.mult, [pk, "p3_rc%d" % (tl % 2)], ["p3_xo"])

                def s_xoT(tl):
                    ptb = self.ps[5][:, :].bitcast(BF16)
                    for k in range(8):
                        self.tr(ptb[:, k * 128:(k + 1) * 128], xo[:, tl, k * 128:(k + 1) * 128], ["p3_xo"], ["ps5"])
                    self.cp("act", xoT[:, :, tl * 128:(tl + 1) * 128], ptb.rearrange("p (k m) -> p k m", k=8), ["ps5"], ["p3_xoT"])

                def s_wo(tl):
                    t = b * 4 + tl
                    for nh in range(2):
                        for k in range(8):
                            self.mm(self.ps[3 + nh][:, :], xoT[:, k, tl * 128:(tl + 1) * 128], wo[:, k, nh * 512:(nh + 1) * 512], k == 0, k == 7,
                                    ["p3_xoT", "p3_wo%d" % k], ["ps%d" % (3 + nh)])
                    x2t, x2k = x2[t % 2], "p3_x2_%d" % (t % 2)
                    c.dma("sp", xr2[t % 2][:, :], self.x1_s[t * 128:(t + 1) * 128, :], reads=["x1_s%d" % t], writes=["p3_xq%d" % (t % 2)])
                    self.ln_part1(yB, "p3_yB", xr2[t % 2], "p3_xq%d" % (t % 2), self.ps[3], "ps3", self.ps[4], "ps4", tmpB, "lnB")

                def s_wo_b(tl):
                    t = b * 4 + tl
                    x2t, x2k = x2[t % 2], "p3_x2_%d" % (t % 2)
                    self.ln_part2(yB, "p3_yB", gb["g2"], gb["b2"], "p3_gb", x2t[:, :], x2k, tmpB, "lnB")
                    c.dma("pool", self.xres_s[t * 128:(t + 1) * 128, :], x2t[:, :], reads=[x2k], writes=["xres_s%d" % t])

                def s_x2T(tl):
                    t = b * 4 + tl
                    to_xT2(x2[t % 2][:, :], "p3_x2_%d" % (t % 2), xbB, "p3_xbB", x2Tb, x2Tk, slice(tl * 128, (tl + 1) * 128), 6, "act")
                    if tl == 3:
                        c.dma("pool", self.xT_s[:, :, b * 512:(b + 1) * 512], x2Tb[:, :, :], reads=[x2Tk], writes=["xT_s"])

                for j in range(8):
                    steps.append(lambda j=j: s_q(j))
                for h in range(4):
                    steps.append(lambda h=h: s_att(h))
                for tl in range(4):
                    steps.append(lambda tl=tl: s_xoT(tl))
                for tl in range(4):
                    steps.append(lambda tl=tl: s_wo(tl))
                    if tl > 0:
                        steps.append(lambda tl=tl: s_x2T(tl - 1))
                    steps.append(lambda tl=tl: s_wo_b(tl))
                steps.append(lambda: s_x2T(3))
                return steps

            def merge(a, bsteps):
                na, nb = len(a), len(bsteps)
                ia = ib = 0
                while ia < na or ib < nb:
                    if ib >= nb or (ia < na and ia * nb <= ib * na):
                        a[ia]()
                        ia += 1
                    else:
                        bsteps[ib]()
                        ib += 1

            for f in H1(0):
                f()
            for b in range(NB):
                merge(H2(b), H1(b + 1) if b + 1 < NB else [])
            c.emit()

    def phase5(self, l, last):
        nc, c = self.nc, self.c
        with ExitStack() as st:
            wr = self.sb("p5_wr", [128, 8, 40], BF16, st)
            br = self.sb("p5_br", [128, 40], F32, st)
            c.dma("pool", wr[:, :, :], self.w_router[l].rearrange("(k p) n -> p k n", p=128), writes=["p5_wr"])
            c.dma("sp", br[:, :], self.b_router[l, :].partition_broadcast(128), writes=["p5_br"])
            xT = [self.sb("p5_xT%d" % i, [128, 8, 512], BF16, st) for i in range(2)]
            lg = self.sb("p5_lg", [128, 4, 40], F32, st)
            gmax = self.sb("p5_gmax", [128, 4], F32, st)
            ohg = self.sb("p5_ohg", [128, 4, 8], F32, st)
            z8 = self.sb("p5_z8", [128, 4, 8], F32, st)
            se = self.sb("p5_se", [128, 4], F32, st)
            gw = self.sb("p5_gw", [128, 4], F32, st)
            t32 = self.sb("p5_t32", [128, 4, 32], F32, st)
            el = self.sb("p5_el", [128, 4, 4], F32, st)
            el2 = self.sb("p5_el2", [128, 4, 4], F32, st)
            mk1 = self.sb("p5_mk1", [128, 4, 4], F32, st)
            mk2 = self.sb("p5_mk2", [128, 4, 4], F32, st)
            m1 = self.sb("p5_m1", [128, 4], F32, st)
            m2 = self.sb("p5_m2", [128, 4], F32, st)
            e21 = self.sb("p5_e21", [128, 4], F32, st)
            w1 = self.sb("p5_w1", [128, 4], F32, st)
            w2 = self.sb("p5_w2", [128, 4], F32, st)
            g4 = self.sb("p5_g4", [128, 4, 4], F32, st)
            g4b = self.sb("p5_g4b", [128, 4, 4], F32, st)
            g32 = self.sb("p5_g32", [128, 4, 32], F32, st)
            gTs = [self.sb("p5_gTs%d" % i, [32, 512], F32, st) for i in range(2)]

            def load_x(b):
                c.dma("sp", xT[b % 2][:, :, :], self.xT_s[:, :, b * 512:(b + 1) * 512], reads=["xT_s"], writes=["p5_xT%d" % (b % 2)])

            load_x(0)
            K = "p5_r"
            bc3 = lambda ap, n: ap.unsqueeze(2).to_broadcast([128, 4, n])
            for b in range(NB):
                if b + 1 < NB:
                    load_x(b + 1)
                gT, gTk = gTs[b % 2], "p5_gTs%d" % (b % 2)
                ps, pk = self.ps[b % 2], "ps%d" % (b % 2)
                for tl in range(4):
                    for k in range(8):
                        self.mm(ps[:, tl * 40:(tl + 1) * 40], xT[b % 2][:, k, tl * 128:(tl + 1) * 128], wr[:, k, :], k == 0, k == 7,
                                ["p5_xT%d" % (b % 2), "p5_wr"], [pk])
                self.tt("dve", lg[:, :, :], ps[:, 0:160].rearrange("p (t n) -> p t n", t=4), br[:, :].unsqueeze(1).to_broadcast([128, 4, 40]),
                        ALU.add, [pk, "p5_br"], [K])
                lgg = lg[:, :, 0:8]
                self.red(gmax[:, :], lgg, ALU.max, [K], [K])
                self.tt("dve", ohg[:, :, :], lgg, bc3(gmax[:, :], 8), ALU.is_equal, [K], [K])
                self.tt("dve", z8[:, :, :], lgg, bc3(gmax[:, :], 8), ALU.subtract, [K], [K])
                self.act(z8[:, :, :], z8[:, :, :], AF.Exp, [K], [K])
                self.red(se[:, :], z8[:, :, :], ALU.add, [K], [K])
                self.recip(gw[:, :], se[:, :], [K], [K])
                lge = lg[:, :, 8:40].rearrange("p t (g e) -> p t g e", g=8)
                self.tt("dve", t32[:, :, :].rearrange("p t (g e) -> p t g e", g=8), lge, ohg[:, :, :].unsqueeze(3).to_broadcast([128, 4, 8, 4]),
                        ALU.mult, [K], [K])
                self.red(el[:, :, :], t32[:, :, :].rearrange("p t (g e) -> p t e g", g=8), ALU.add, [K], [K])
                self.red(m1[:, :], el[:, :, :], ALU.max, [K], [K])
                self.tt("dve", mk1[:, :, :], el[:, :, :], bc3(m1[:, :], 4), ALU.is_equal, [K], [K])
                self.stt(el2[:, :, :], mk1[:, :, :], -1e30, el[:, :, :], ALU.mult, ALU.add, [K], [K])
                self.red(m2[:, :], el2[:, :, :], ALU.max, [K], [K])
                self.tt("dve", mk2[:, :, :], el2[:, :, :], bc3(m2[:, :], 4), ALU.is_equal, [K], [K])
                self.tt("dve", e21[:, :], m2[:, :], m1[:, :], ALU.subtract, [K], [K])
                self.act(e21[:, :], e21[:, :], AF.Exp, [K], [K])
                self.ts("dve", w1[:, :], e21[:, :], 1.0, ALU.add, [K], [K])
                self.recip(w1[:, :], w1[:, :], [K], [K])
                self.tt("dve", w2[:, :], w1[:, :], e21[:, :], ALU.mult, [K], [K])
                self.tt("dve", w1[:, :], w1[:, :], gw[:, :], ALU.mult, [K], [K])
                self.tt("dve", w2[:, :], w2[:, :], gw[:, :], ALU.mult, [K], [K])
                self.tt("dve", g4[:, :, :], mk1[:, :, :], bc3(w1[:, :], 4), ALU.mult, [K], [K])
                self.tt("dve", g4b[:, :, :], mk2[:, :, :], bc3(w2[:, :], 4), ALU.mult, [K], [K])
                self.tt("dve", g4[:, :, :], g4[:, :, :], g4b[:, :, :], ALU.add, [K], [K])
                self.tt("dve", g32[:, :, :].rearrange("p t (g e) -> p t g e", g=8), ohg[:, :, :].unsqueeze(3).to_broadcast([128, 4, 8, 4]),
                        g4[:, :, :].unsqueeze(2).to_broadcast([128, 4, 8, 4]), ALU.mult, [K], [K])
                pt, ptk = self.ps[2 + b % 2], "ps%d" % (2 + b % 2)
                for tl in range(4):
                    self.tr(pt[0:32, tl * 128:(tl + 1) * 128], g32[:, tl, :], [K], [ptk], f32=True)
                self.cp("act", gT[:, :], pt[0:32, :], [ptk], [gTk])
                c.dma("sp", self.gT_s[:, b * 512:(b + 1) * 512], gT[:, :], reads=[gTk], writes=["gT_s"])
            c.emit()
        SBT = 2048
        NSB = S // SBT
        TPS = SBT // 128
        with ExitStack() as st:
            acc = self.sb("p5_acc", [128, TPS, D], F32, st)
            for sbi in range(NSB):
                t0 = sbi * TPS
                with ExitStack() as st2:
                    xTs = self.sb("p5_xTs", [128, 8, SBT], BF16, st2)
                    Wg = [self.sb("p5_Wg%d" % i, [128, 8, 256], BF16, st2) for i in range(3)]
                    Wu = [self.sb("p5_Wu%d" % i, [128, 8, 256], BF16, st2) for i in range(3)]
                    Wd = [self.sb("p5_Wd%d" % i, [128, 2, D], BF16, st2) for i in range(3)]
                    gbt = [self.sb("p5_gbt%d" % i, [128, 512], F32, st2) for i in range(3)]
                    sg = [self.sb("p5_sg%d" % i, [128, 512], F32, st2) for i in range(2)]
                    tu = [self.sb("p5_tu%d" % i, [128, 512], F32, st2) for i in range(2)]
                    hid = [self.sb("p5_hid%d" % i, [128, 2, 512], BF16, st2) for i in range(2)]
                    for q in range(SBT // 512):
                        c.dma("sp", xTs[:, :, q * 512:(q + 1) * 512], self.xT_s[:, :, sbi * SBT + q * 512: sbi * SBT + (q + 1) * 512],
                              reads=["xT_s"], writes=["p5_xTs%d" % q])

                    def load_w(e):
                        i = e % 3
                        c.dma("pool", Wg[i][:, :, :], self.e_gate[l, e].rearrange("(k p) f -> p k f", p=128), writes=["p5_Wg%d" % i])
                        c.dma("pool", Wu[i][:, :, :], self.e_up[l, e].rearrange("(k p) f -> p k f", p=128), writes=["p5_Wu%d" % i])
                        c.dma("pool", Wd[i][:, :, :], self.e_down[l, e].rearrange("(k p) d -> p k d", p=128), writes=["p5_Wd%d" % i])

                    load_w(0)
                    load_w(1)
                    load_w(2)
                    ocnt = [0]
                    NTB = SBT // 512

                    def gu_stage(n):
                        e, tb = n // NTB, n % NTB
                        wi, gi, hi = e % 3, n % 3, n % 2
                        tok0 = sbi * SBT + tb * 512
                        c.dma("sp", gbt[gi][:, :], self.gT_s[e, tok0:tok0 + 512].partition_broadcast(128), reads=["gT_s"], writes=["p5_gbt%d" % gi])
                        for fc in range(2):
                            pg, pgk = self.ps[fc], "ps%d" % fc
                            pu, puk = self.ps[2 + fc], "ps%d" % (2 + fc)
                            for k in range(8):
                                self.mm(pg[:, :], Wg[wi][:, k, fc * 128:(fc + 1) * 128], xTs[:, k, tb * 512:(tb + 1) * 512], k == 0, k == 7,
                                        ["p5_Wg%d" % wi, "p5_xTs%d" % tb], [pgk])
                            for k in range(8):
                                self.mm(pu[:, :], Wu[wi][:, k, fc * 128:(fc + 1) * 128], xTs[:, k, tb * 512:(tb + 1) * 512], k == 0, k == 7,
                                        ["p5_Wu%d" % wi, "p5_xTs%d" % tb], [puk])
                            self.act(sg[fc][:, :], pg[:, :], AF.Silu, [pgk], ["p5_sg%d" % fc])
                            self.tt("dve", tu[fc][:, :], sg[fc][:, :], pu[:, :], ALU.mult, ["p5_sg%d" % fc, puk], ["p5_tu%d" % fc])
                            self.tt("pool", hid[hi][:, fc, :], tu[fc][:, :], gbt[gi][:, :], ALU.mult, ["p5_tu%d" % fc, "p5_gbt%d" % gi], ["p5_hid%d" % hi])

                    def down_stage(n):
                        e, tb = n // NTB, n % NTB
                        wi, hi = e % 3, n % 2
                        for tl in range(4):
                            tile = tb * 4 + tl
                            for dh in range(2):
                                po, pok = self.ps[4 + ocnt[0] % 4], "ps%d" % (4 + ocnt[0] % 4)
                                ocnt[0] += 1
                                for fc in range(2):
                                    self.mm(po[:, :], hid[hi][:, fc, tl * 128:(tl + 1) * 128], Wd[wi][:, fc, dh * 512:(dh + 1) * 512], fc == 0, fc == 1,
                                            ["p5_hid%d" % hi, "p5_Wd%d" % wi], [pok])
                                a = acc[:, tile, dh * 512:(dh + 1) * 512]
                                ak = "p5_acc%d" % tile
                                if e == 0:
                                    self.cp("dve", a, po[:, :], [pok], [ak])
                                else:
                                    self.tt("dve", a, a, po[:, :], ALU.add, [pok, ak], [ak])

                    NN = 32 * NTB
                    gu_stage(0)
                    for n in range(NN):
                        if n + 1 < NN:
                            gu_stage(n + 1)
                        down_stage(n)
                        if n % NTB == NTB - 1 and n // NTB + 3 < 32:
                            load_w(n // NTB + 3)
                    c.emit()
                with ExitStack() as st2:
                    g_t = self.sb("p5_g", [128, D], F32, st2)
                    b_t = self.sb("p5_b", [128, D], F32, st2)
                    c.dma("sp", g_t[:, :], self.ln_g[2][l, :].partition_broadcast(128), writes=["p5_gb"])
                    c.dma("sp", b_t[:, :], self.ln_b[2][l, :].partition_broadcast(128), writes=["p5_gb"])
                    tmps = [{"st6": self.sb("p5_st6%d" % i, [128, 12], F32, st2), "mv": self.sb("p5_mv%d" % i, [128, 4], F32, st2)} for i in range(2)]
                    xr = [self.sb("p5_xr%d" % i, [128, D], F32, st2) for i in range(2)]
                    ys = [self.sb("p5_y%d" % i, [128, D], F32, st2) for i in range(2)]
                    x3 = [self.sb("p5_x3_%d" % i, [128, D], F32, st2) for i in range(2)]
                    xb = self.sb("p5_xb", [128, D], BF16, st2)
                    x3T = [self.sb("p5_x3T%d" % i, [128, 8, 512], BF16, st2) for i in range(2)]

                    def ln1(tile):
                        t = t0 + tile
                        i = tile % 2
                        c.dma("sp", xr[i][:, :], self.xres_s[t * 128:(t + 1) * 128, :], reads=["xres_s%d" % t], writes=["p5_xr%d" % i])
                        ak = "p5_acc%d" % tile
                        self.ln_part1(ys[i], "p5_y%d" % i, xr[i], "p5_xr%d" % i, acc[:, tile, 0:512], ak, acc[:, tile, 512:1024], ak, tmps[i], "p5_ln%d" % i)

                    def ln2(tile):
                        t = t0 + tile
                        i = tile % 2
                        self.ln_part2(ys[i], "p5_y%d" % i, g_t, b_t, "p5_gb", x3[i][:, :], "p5_x3_%d" % i, tmps[i], "p5_ln%d" % i)
                        dst = self.out if last else self.xres_s
                        c.dma("pool", dst[t * 128:(t + 1) * 128, :], x3[i][:, :], reads=["p5_x3_%d" % i], writes=["xres_s%d" % t])
                        if not last:
                            bq = tile // 4
                            self.to_xT(x3[i][:, :], "p5_x3_%d" % i, xb, "p5_xb", x3T[bq % 2], "p5_x3T%d" % (bq % 2),
                                       slice((tile % 4) * 128, (tile % 4 + 1) * 128), "act")
                            if tile % 4 == 3:
                                tok0 = (t0 + bq * 4) * 128
                                c.dma("pool", self.xT_s[:, :, tok0:tok0 + 512], x3T[bq % 2][:, :, :], reads=["p5_x3T%d" % (bq % 2)], writes=["xT_s"])

                    for tile in range(TPS + 1):
                        if tile < TPS:
                            ln1(tile)
                        if tile >= 1:
                            ln2(tile - 1)
                    c.emit()


_CACHE = {}


def _consts():
    ident = np.eye(128, dtype=np.float32)
    p = np.arange(128)[:, None]
    f = np.arange(128)[None, :]
    invs = []
    for rot in (32, 16, 8):
        invs.append((np.float32(ROPE_THETA) ** (-np.arange(0, rot, 2, dtype=np.float32) / np.float32(rot))).astype(np.float32))
    invf = np.tile(np.concatenate(invs)[None, :], (128, 1)).astype(np.float32)
    blk = np.zeros((16, S), dtype=np.float32)
    for j in range(16):
        blk[j, j * 256:(j + 1) * 256] = BIGV
    bf = ml_dtypes.bfloat16
    return {
        "c_ident_b": ident.astype(bf), "c_ident_f": ident,
        "c_mask_le": (p <= f).astype(np.float32).astype(bf),
        "c_mask_lt": (p < f).astype(np.float32).astype(bf),
        "c_tri": (p > f).astype(np.float32).astype(bf),
        "c_invf": invf, "c_blk": blk.astype(bf),
    }


def make_in_maps(inputs, n_cores=8):
    f = lambda a: np.ascontiguousarray(np.asarray(a))
    shared = {
        "w_in": f(np.asarray(inputs["w_in"])[:, :, COL_PERM]),
        "mla_q_norm": f(inputs["mla_q_norm"]), "w_uq": f(inputs["w_uq"]),
        "mla_kv_norm": f(inputs["mla_kv_norm"]), "w_ukv": f(inputs["w_ukv"]),
        "diff_lambda": f(np.asarray(inputs["diff_lambda"]).reshape(DEPTH, 128)),
        "diff_subln": f(inputs["diff_subln"]), "w_out": f(inputs["w_out"]),
        "ln_mix_g": f(inputs["ln_mix_g"]), "ln_mix_b": f(inputs["ln_mix_b"]),
        "ln_mem_g": f(inputs["ln_mem_g"]), "ln_mem_b": f(inputs["ln_mem_b"]),
        "ln_ffn_g": f(inputs["ln_ffn_g"]), "ln_ffn_b": f(inputs["ln_ffn_b"]),
        "xattn_wq": f(inputs["xattn_wq"]), "xattn_wk": f(inputs["xattn_wk"]),
        "xattn_wv": f(inputs["xattn_wv"]), "xattn_wo": f(inputs["xattn_wo"]),
        "w_router": f(np.concatenate([np.asarray(inputs["router_group_w"]), np.asarray(inputs["router_expert_w"])], axis=2)),
        "b_router": f(np.concatenate([np.asarray(inputs["router_group_b"]), np.asarray(inputs["router_expert_b"])], axis=1)),
        "expert_w_gate": f(inputs["expert_w_gate"]), "expert_w_up": f(inputs["expert_w_up"]),
        "expert_w_down": f(inputs["expert_w_down"]),
    }
    shared.update(_consts())
    maps = []
    x = np.asarray(inputs["x"])
    mem = np.asarray(inputs["mem"])
    pos = np.asarray(inputs["positions"])
    for b in range(n_cores):
        m = dict(shared)
        m["x"] = f(x[b])
        m["mem"] = f(mem[b])
        m["pos"] = f(pos[b].reshape(NT, 128).T.astype(np.int32))
        maps.append(m)
    return maps


def kernel(**inputs):
    if "nc" not in _CACHE:
        _CACHE["nc"] = Builder().build()
    nc = _CACHE["nc"]
    maps = make_in_maps(inputs)
    res = run_bass_kernel_spmd(nc, maps, core_ids=list(range(8)))
    return np.stack([r["out"] for r in res.results], axis=0).astype(np.float32)
```

```python
import math
import os
LV = int(os.environ.get('P1LV', '9'))
SUB = int(os.environ.get('P1SUB', '9'))
from contextlib import ExitStack

import numpy as np
import ml_dtypes
import concourse.bass as bass
import concourse.mybir as mybir
from concourse.bass_utils import run_bass_kernel_spmd

F32 = mybir.dt.float32
BF16 = mybir.dt.bfloat16
I32 = mybir.dt.int32
AF = mybir.ActivationFunctionType
ALU = mybir.AluOpType
AX = mybir.AxisListType

D = 1024
S = 4096
NT = S // 128
NB = S // 512
DEPTH = 4
N_MEM = 256
IN_COLS = 2656
ALPHA = (2 * DEPTH) ** 0.25
LN_EPS = 1e-5
RMS_EPS = 1e-6
ROPE_THETA = 500000.0
BIGV = 30000.0
TWO_PI = 2.0 * math.pi

O_SB = 352
O_MB = 352 + 768
O_DF = 352 + 1536
COL_PERM = np.concatenate([
    np.arange(0, 352),
    np.arange(O_SB, O_SB + 512),
    np.arange(O_SB + 512, O_SB + 768), np.arange(O_MB + 512, O_MB + 768),
    np.arange(O_MB, O_MB + 512),
    np.arange(O_DF, O_DF + 512),
    np.arange(O_DF + 512, O_DF + 768),
])
CH = [(0, 352), (352, 512), (864, 512), (1376, 512), (1888, 512), (2400, 256)]


class Ctx:
    def __init__(self, nc, stack, n_dma_sems=24):
        self.nc = nc
        self.eng = {"pe": nc.tensor, "act": nc.scalar, "dve": nc.vector, "pool": nc.gpsimd, "sp": nc.sync}
        self.sem = {}
        self.cnt = {}
        for e in self.eng:
            self.sem[e] = stack.enter_context(nc.semaphore("s_" + e))
            self.cnt[e] = 0
        self.dring = {}
        for q in ("sp", "pool"):
            sems = [stack.enter_context(nc.semaphore("d_%s_%d" % (q, i))) for i in range(n_dma_sems)]
            self.dring[q] = {"sems": sems, "cnt": [0] * n_dma_sems, "next": 0}
        self.waited = {e: {} for e in self.eng}
        self.last_w = {}
        self.readers = {}
        self.prog = {e: [] for e in self.eng}
        self.n_ins = 0

    def _wait(self, e, tok):
        sem, val, src = tok
        if src == e and e == "pe":
            return
        w = self.waited[e]
        if w.get(sem.name, 0) >= val:
            return
        w[sem.name] = val
        self.prog[e].append(("w", sem, val))

    def _deps(self, e, reads, writes):
        for k in reads:
            t = self.last_w.get(k)
            if t is not None:
                self._wait(e, t)
            if k.startswith("ps"):
                for t in self.readers.get(k, ()):
                    if t[2] != e:
                        self._wait(e, t)
        for k in writes:
            t = self.last_w.get(k)
            if t is not None:
                self._wait(e, t)
            for t in self.readers.get(k, ()):
                self._wait(e, t)

    def _commit(self, tok, reads, writes):
        for k in reads:
            lst = self.readers.setdefault(k, [])
            lst.append(tok)
            if len(lst) > 16:
                best = {}
                for t in lst:
                    if t[0].name not in best or best[t[0].name][1] < t[1]:
                        best[t[0].name] = t
                self.readers[k] = list(best.values())
        for k in writes:
            self.last_w[k] = tok
            self.readers[k] = []

    def op(self, e, fn, reads=(), writes=()):
        self._deps(e, reads, writes)
        self.cnt[e] += 1
        self.n_ins += 1
        self.prog[e].append(("i", fn, self.sem[e], 1))
        tok = (self.sem[e], self.cnt[e], e)
        self._commit(tok, reads, writes)
        return tok

    def dma(self, q, out, in_, reads=(), writes=(), **kw):
        ring = self.dring[q]
        i = ring["next"]
        ring["next"] = (i + 1) % len(ring["sems"])
        sem = ring["sems"][i]
        if ring["cnt"][i] > 0:
            self._wait(q, (sem, ring["cnt"][i], None))
        self._deps(q, reads, writes)
        self.prog[q].append(("d", out, in_, kw, sem))
        self.n_ins += 1
        ring["cnt"][i] += 16
        tok = (sem, ring["cnt"][i], None)
        self._commit(tok, reads, writes)
        return tok

    def wait_all(self, e):
        for x in self.eng:
            if self.cnt[x] > 0 and x != e:
                self._wait(e, (self.sem[x], self.cnt[x], x))
        for q, ring in self.dring.items():
            for sem, c in zip(ring["sems"], ring["cnt"]):
                if c > 0:
                    self._wait(e, (sem, c, None))

    def emit(self):
        nc = self.nc
        self.wait_all("sp")
        prog = self.prog
        self.prog = {e: [] for e in self.eng}

        def run(e, engobj):
            for it in prog[e]:
                if it[0] == "w":
                    engobj.wait_ge(it[1], it[2])
                elif it[0] == "i":
                    it[1]().then_inc(it[2], it[3])
                else:
                    engobj.dma_start(out=it[1], in_=it[2], **it[3]).then_inc(it[4], 16)

        with nc.Block() as block:
            @block.sync
            def _(eng):
                run("sp", eng)

            @block.scalar
            def _(eng):
                run("act", eng)

            @block.tensor
            def _(eng):
                run("pe", eng)

            @block.vector
            def _(eng):
                run("dve", eng)

            @block.gpsimd
            def _(eng):
                run("pool", eng)


class Builder:
    def __init__(self, n_layers=DEPTH, debug=False, stop_after=None):
        self.n_layers = n_layers
        self.debug = debug
        self.stop_after = stop_after
        self.nc = bass.Bass("TRN2", target_bir_lowering=False)
        self.dbg_names = []

    def mm(self, out, lhsT, rhs, start, stop, r, w):
        nc = self.nc
        self.c.op("pe", lambda: nc.tensor.matmul(out, lhsT=lhsT, rhs=rhs, start=start, stop=stop), r, w)

    def tr(self, out, in_, r, w, f32=False):
        nc = self.nc
        n = in_.shape[0]
        ident = (self.ident_f if f32 else self.ident_b)[0:n, 0:n]
        self.c.op("pe", lambda: nc.tensor.transpose(out, in_, ident), list(r) + ["const"], w)

    def act(self, out, in_, func, r, w, scale=1.0, bias=0.0, accum_out=None):
        nc = self.nc
        kw = {}
        if accum_out is not None:
            kw["accum_out"] = accum_out
        self.c.op("act", lambda: nc.scalar.activation(out=out, in_=in_, func=func, scale=scale, bias=bias, **kw), r, w)

    def tt(self, e, out, in0, in1, op, r, w):
        eng = self.c.eng[e]
        self.c.op(e, lambda: eng.tensor_tensor(out=out, in0=in0, in1=in1, op=op), r, w)

    def ts(self, e, out, in0, s1, op0, r, w, s2=None, op1=ALU.bypass, accum_out=None):
        eng = self.c.eng[e]
        kw = {}
        if accum_out is not None:
            kw["accum_out"] = accum_out
        self.c.op(e, lambda: eng.tensor_scalar(out=out, in0=in0, scalar1=s1, scalar2=s2, op0=op0, op1=op1, **kw), r, w)

    def stt(self, out, in0, scalar, in1, op0, op1, r, w):
        nc = self.nc
        self.c.op("dve", lambda: nc.vector.scalar_tensor_tensor(out=out, in0=in0, scalar=scalar, in1=in1, op0=op0, op1=op1), r, w)

    def cp(self, e, out, in_, r, w):
        if e == "act":
            nc = self.nc
            self.c.op("act", lambda: nc.scalar.copy(out=out, in_=in_), r, w)
        else:
            eng = self.c.eng[e]
            self.c.op(e, lambda: eng.tensor_copy(out=out, in_=in_), r, w)

    def ms(self, e, ap, val, w):
        eng = self.c.eng[e]
        self.c.op(e, lambda: eng.memset(ap, val), (), w)

    def recip(self, out, in_, r, w):
        nc = self.nc
        self.c.op("dve", lambda: nc.vector.reciprocal(out=out, in_=in_), r, w)

    def red(self, out, in_, op, r, w, axis=AX.X):
        nc = self.nc
        self.c.op("dve", lambda: nc.vector.tensor_reduce(out=out, in_=in_, axis=axis, op=op), r, w)

    def sb(self, name, shape, dt, st=None):
        self.uid = getattr(self, "uid", 0) + 1
        return (st or self.st).enter_context(self.nc.sbuf_tensor("%s_u%d" % (name, self.uid), list(shape), dt))

    def scr(self, name, shape, dt):
        kind = "ExternalOutput" if self.debug else "Internal"
        if self.debug:
            self.dbg_names.append(name)
        return self.nc.dram_tensor(name, list(shape), dt, kind=kind).ap()

    def build(self):
        nc = self.nc
        L = DEPTH
        inp = lambda n, s, dt=F32: nc.dram_tensor(n, list(s), dt, kind="ExternalInput").ap()
        self.x_in = inp("x", [S, D])
        self.mem_in = inp("mem", [N_MEM, D])
        self.pos_in = inp("pos", [128, NT], I32)
        self.w_in = inp("w_in", [L, D, IN_COLS])
        self.mla_q_norm = inp("mla_q_norm", [L, 192])
        self.w_uq = inp("w_uq", [L, 192, 384])
        self.mla_kv_norm = inp("mla_kv_norm", [L, 128])
        self.w_ukv = inp("w_ukv", [L, 128, 512])
        self.diff_lambda = inp("diff_lambda", [L, 128])
        self.diff_subln = inp("diff_subln", [L, 64])
        self.w_out = inp("w_out", [L, D, D])
        self.ln_g = [inp(n, [L, D]) for n in ("ln_mix_g", "ln_mem_g", "ln_ffn_g")]
        self.ln_b = [inp(n, [L, D]) for n in ("ln_mix_b", "ln_mem_b", "ln_ffn_b")]
        self.wq = inp("xattn_wq", [L, D, D])
        self.wk = inp("xattn_wk", [L, D, D])
        self.wv = inp("xattn_wv", [L, D, D])
        self.wo = inp("xattn_wo", [L, D, D])
        self.w_router = inp("w_router", [L, D, 40])
        self.b_router = inp("b_router", [L, 40])
        self.e_gate = inp("expert_w_gate", [L, 32, D, 256])
        self.e_up = inp("expert_w_up", [L, 32, D, 256])
        self.e_down = inp("expert_w_down", [L, 32, 256, D])
        self.c_ident_b = inp("c_ident_b", [128, 128], BF16)
        self.c_ident_f = inp("c_ident_f", [128, 128], F32)
        self.c_mask_le = inp("c_mask_le", [128, 128], BF16)
        self.c_mask_lt = inp("c_mask_lt", [128, 128], BF16)
        self.c_tri = inp("c_tri", [128, 128], BF16)
        self.c_invf = inp("c_invf", [128, 28], F32)
        self.c_blk = inp("c_blk", [16, S], BF16)
        self.out = nc.dram_tensor("out", [S, D], F32, kind="ExternalOutput").ap()
        self.xT_s = self.scr("xT_s", [128, 8, S], BF16)
        self.xres_s = self.scr("xres_s", [S, D], F32)
        self.qk_s = self.scr("qk_s", [4, 4, 2, 96, S], BF16)
        self.v_s = self.scr("v_s", [S, 4 * 260], BF16)
        self.qkp_s = self.scr("qkp_s", [3, 2, 2, 128, S], BF16)
        self.mb_s = self.scr("mb_s", [64, S], BF16)
        self.o_s = self.scr("o_s", [S, D], BF16)
        self.od_s = self.scr("od_s", [2, S, 256], F32)
        self.gT_s = self.scr("gT_s", [32, S], F32)
        self.x1_s = self.scr("x1_s", [S, D], F32)

        with ExitStack() as st:
            self.st = st
            self.c = Ctx(nc, st)
            self.ps = [st.enter_context(nc.psum_tensor("ps%d" % i, [128, 512], F32)) for i in range(8)]
            self.ident_b = self.sb("ident_b", [128, 128], BF16)
            self.ident_f = self.sb("ident_f", [128, 128], F32)
            self.mask_le = self.sb("mask_le", [128, 128], BF16)
            self.mask_lt = self.sb("mask_lt", [128, 128], BF16)
            self.tri = self.sb("tri", [128, 128], BF16)
            self.ones_b = self.sb("ones_b", [128, 128], BF16)
            self.cs = self.sb("rope_cos", [128, NT, 28], F32)
            self.sn = self.sb("rope_sin", [128, NT, 28], F32)
            self.memT = self.sb("memT", [128, 8, N_MEM], BF16)
            self.phase0()
            for l in range(self.n_layers if self.stop_after != (0, 0) else 0):
                last = (l == DEPTH - 1)
                self.phase1(l)
                if self.stop_after == (l, 1):
                    break
                self.phase2(l)
                if self.stop_after == (l, 2):
                    break
                self.phase34(l)
                if self.stop_after == (l, 3):
                    break
                self.phase5(l, last)
                if self.stop_after == (l, 5):
                    break
        return nc

    def phase0(self):
        nc, c = self.nc, self.c
        with ExitStack() as st:
            for dst, src, k in ((self.ident_b, self.c_ident_b, "const"), (self.ident_f, self.c_ident_f, "const"),
                                (self.mask_le, self.c_mask_le, "const"), (self.mask_lt, self.c_mask_lt, "const"),
                                (self.tri, self.c_tri, "const")):
                c.dma("sp", dst[:, :], src, writes=[k])
            self.ms("pool", self.ones_b[:, :], 1.0, ["const"])
            pos_i = self.sb("pos_i", [128, NT], I32, st)
            pos_f = self.sb("pos_f", [128, NT], F32, st)
            invf = self.sb("invf", [128, 28], F32, st)
            ang = self.sb("ang", [128, NT, 28], F32, st)
            t1 = self.sb("rp_t1", [128, NT, 28], F32, st)
            ki = self.sb("rp_ki", [128, NT, 28], I32, st)
            kf = self.sb("rp_kf", [128, NT, 28], F32, st)
            r = self.sb("rp_r", [128, NT, 28], F32, st)
            m = self.sb("rp_m", [128, NT, 28], F32, st)
            rc = self.sb("rp_rc", [128, NT, 28], F32, st)
            c.dma("sp", pos_i[:, :], self.pos_in, writes=["pos_i"])
            c.dma("sp", invf[:, :], self.c_invf, writes=["invf"])
            self.cp("dve", pos_f[:, :], pos_i[:, :], ["pos_i"], ["pos_f"])
            self.tt("dve", ang[:, :, :], pos_f[:, :].unsqueeze(2).to_broadcast([128, NT, 28]),
                    invf[:, :].unsqueeze(1).to_broadcast([128, NT, 28]), ALU.mult, ["pos_f", "invf"], ["ang"])
            self.ts("dve", t1[:, :, :], ang[:, :, :], 1.0 / TWO_PI, ALU.mult, ["ang"], ["rp_t1"])
            self.cp("dve", ki[:, :, :], t1[:, :, :], ["rp_t1"], ["rp_ki"])
            self.cp("dve", kf[:, :, :], ki[:, :, :], ["rp_ki"], ["rp_kf"])
            HI = 6.28125
            LO = TWO_PI - HI
            self.stt(r[:, :, :], kf[:, :, :], -HI, ang[:, :, :], ALU.mult, ALU.add, ["rp_kf", "ang"], ["rp_r"])
            self.stt(r[:, :, :], kf[:, :, :], -LO, r[:, :, :], ALU.mult, ALU.add, ["rp_kf", "rp_r"], ["rp_r"])

            def wrap(buf, key):
                self.ts("dve", m[:, :, :], buf[:, :, :], math.pi, ALU.is_gt, [key], ["rp_m"])
                self.stt(buf[:, :, :], m[:, :, :], -TWO_PI, buf[:, :, :], ALU.mult, ALU.add, ["rp_m", key], [key])
                self.ts("dve", m[:, :, :], buf[:, :, :], -math.pi, ALU.is_lt, [key], ["rp_m"])
                self.stt(buf[:, :, :], m[:, :, :], TWO_PI, buf[:, :, :], ALU.mult, ALU.add, ["rp_m", key], [key])
                self.ts("dve", buf[:, :, :], buf[:, :, :], math.pi, ALU.min, [key], [key], s2=-math.pi, op1=ALU.max)

            wrap(r, "rp_r")
            self.ts("dve", rc[:, :, :], r[:, :, :], math.pi / 2, ALU.add, ["rp_r"], ["rp_rc"])
            wrap(rc, "rp_rc")
            self.act(self.sn[:, :, :], r[:, :, :], AF.Sin, ["rp_r"], ["rope"])
            self.act(self.cs[:, :, :], rc[:, :, :], AF.Sin, ["rp_rc"], ["rope"])
            memb = self.sb("memb", [128, 2, D], BF16, st)
            c.dma("pool", memb[:, :, :], self.mem_in.rearrange("(t p) d -> p t d", p=128), writes=["memb"])
            for t in range(2):
                pst = self.ps[t][:, :].bitcast(BF16)
                for k in range(8):
                    self.tr(pst[:, k * 128:(k + 1) * 128], memb[:, t, k * 128:(k + 1) * 128], ["memb"], ["ps%d" % t])
                self.cp("dve", self.memT[:, :, t * 128:(t + 1) * 128],
                        pst.rearrange("p (k m) -> p k m", k=8), ["ps%d" % t], ["memT"])
            xb = [self.sb("p0_xb%d" % i, [128, D], BF16, st) for i in range(2)]
            xt = [self.sb("p0_xt%d" % i, [128, 8, 128], BF16, st) for i in range(2)]
            for t in range(NT):
                i = t % 2
                c.dma("pool", xb[i][:, :], self.x_in[t * 128:(t + 1) * 128, :], writes=["p0_xb%d" % i])
                pst = self.ps[2 + i][:, :].bitcast(BF16)
                for k in range(8):
                    self.tr(pst[:, k * 128:(k + 1) * 128], xb[i][:, k * 128:(k + 1) * 128], ["p0_xb%d" % i], ["ps%d" % (2 + i)])
                self.cp("dve" if i == 0 else "act", xt[i][:, :, :], pst.rearrange("p (k m) -> p k m", k=8),
                        ["ps%d" % (2 + i)], ["p0_xt%d" % i])
                c.dma("sp", self.xT_s[:, :, t * 128:(t + 1) * 128], xt[i][:, :, :], reads=["p0_xt%d" % i], writes=["xT_s"])
            c.emit()

    def rope(self, e, out1, out2, a, b, cos, sin, tmp1, tmp2, rk, wk_):
        k1, k2 = "rtmp1" + e, "rtmp2" + e
        self.tt(e, tmp1, a, cos, ALU.mult, rk + ["rope"], [k1])
        self.tt(e, tmp2, b, sin, ALU.mult, rk + ["rope"], [k2])
        self.tt(e, out1, tmp1, tmp2, ALU.subtract, [k1, k2], wk_)
        self.tt(e, tmp1, a, sin, ALU.mult, rk + ["rope"], [k1])
        self.tt(e, tmp2, b, cos, ALU.mult, rk + ["rope"], [k2])
        self.tt(e, out2, tmp1, tmp2, ALU.add, [k1, k2], wk_)

    def phase1(self, l):
        nc, c = self.nc, self.c
        with ExitStack() as st:
            win = self.sb("p1_win", [128, 8, IN_COLS], BF16, st)
            wuq = self.sb("p1_wuq", [128, 2, 384], BF16, st)
            wukv = self.sb("p1_wukv", [128, 512], BF16, st)
            gq = self.sb("p1_gq", [128, 2], F32, st)
            gkv = self.sb("p1_gkv", [128, 1], F32, st)
            for k in range(8):
                c.dma("pool", win[:, k, :], self.w_in[l, k * 128:(k + 1) * 128, :], writes=["p1_win%d" % k])
            wuq_f = self.sb("p1_wuq_f", [128, 2, 384], F32, st)
            wukv_f = self.sb("p1_wukv_f", [128, 512], F32, st)
            c.dma("sp", wuq_f[:, 0, :], self.w_uq[l, 0:128, :], writes=["p1_wuq_f"])
            c.dma("sp", wuq_f[0:64, 1, :], self.w_uq[l, 128:192, :], writes=["p1_wuq_f"])
            c.dma("sp", wukv_f[:, :], self.w_ukv[l, :, :], writes=["p1_wukv_f"])
            c.dma("sp", gq[:, 0:1], self.mla_q_norm[l, 0:128].rearrange("(p o) -> p o", o=1), writes=["p1_gq"])
            c.dma("sp", gq[0:64, 1:2], self.mla_q_norm[l, 128:192].rearrange("(p o) -> p o", o=1), writes=["p1_gq"])
            c.dma("sp", gkv[:, 0:1], self.mla_kv_norm[l, :].rearrange("(p o) -> p o", o=1), writes=["p1_gkv"])
            self.ts("dve", wuq[:, 0, :], wuq_f[:, 0, :], gq[:, 0:1], ALU.mult, ["p1_wuq_f", "p1_gq"], ["p1_wuq"])
            self.ts("dve", wuq[0:64, 1, :], wuq_f[0:64, 1, :], gq[0:64, 1:2], ALU.mult, ["p1_wuq_f", "p1_gq"], ["p1_wuq"])
            self.ts("dve", wukv[:, :], wukv_f[:, :], gkv[:, 0:1], ALU.mult, ["p1_wukv_f", "p1_gkv"], ["p1_wukv"])

            xT = [self.sb("p1_xT%d" % i, [128, 8, 512], BF16, st) for i in range(2)]
            c0 = [self.sb("p1_c0_%d" % i, [128, 352], BF16, st) for i in range(2)]
            ssq = [self.sb("p1_ssq%d" % i, [128, 4], F32, st) for i in range(2)]
            junk = self.sb("p1_junk", [128, 512], F32, st)
            qkS = [self.sb("p1_qkS%d" % i, [128, 512], BF16, st) for i in range(2)]
            qkM = [self.sb("p1_qkM%d" % i, [128, 512], BF16, st) for i in range(2)]
            qkD = [self.sb("p1_qkD%d" % i, [128, 512], BF16, st) for i in range(2)]
            qA = [self.sb("p1_qA%d" % i, [128, 4, 96], BF16, st) for i in range(2)]
            kA = [self.sb("p1_kA%d" % i, [128, 4, 96], BF16, st) for i in range(2)]
            vall = [self.sb("p1_vall%d" % i, [128, 4, 4, 65], BF16, st) for i in range(2)]
            vst = [[vall[i][:, m, :, :] for i in range(2)] for m in range(4)]
            latT = [self.sb("p1_latT%d" % i, [128, 3, 128], BF16, st) for i in range(2)]
            rt1 = self.sb("p1_rt1", [128, 128], F32, st)
            rt2 = self.sb("p1_rt2", [128, 128], F32, st)
            rt5 = self.sb("p1_rt5", [128, 16], F32, st)
            rt6 = self.sb("p1_rt6", [128, 16], F32, st)
            rt3 = self.sb("p1_rt3", [128, 128], F32, st)
            rt4 = self.sb("p1_rt4", [128, 128], F32, st)
            hf = [self.sb("p1_hf%d" % i, [128, 512], F32, st) for i in range(2)]
            kpe = [self.sb("p1_kpe%d" % i, [128, 32], F32, st) for i in range(2)]
            TA = [self.sb("p1_TA%d" % bp, [96, 4, 2, 512], BF16, st) for bp in range(2)]
            TP = [{nm: self.sb("p1_TP%d%s" % (bp, nm), [128, 2, 2, 512], BF16, st) for nm in ("S", "M", "D")} for bp in range(2)]
            mbT = [self.sb("p1_mbT%d" % bp, [64, 512], BF16, st) for bp in range(2)]
            kmT = self.sb("p1_kmT", [128, 2, 16], BF16, st)
            kpart = self.sb("p1_kpart", [128, 2, 2], F32, st)
            ksum = self.sb("p1_ksum", [128, 2], F32, st)
            gate = self.sb("p1_gate", [128, 4, 16], F32, st)
            top8 = self.sb("p1_top8", [128, 4, 8], F32, st)
            mb = [self.sb("p1_mb%d" % i, [128, 4, 16], BF16, st) for i in range(2)]
            for m in range(4):
                for i in range(2):
                    self.ms("pool", vst[m][i][:, :, 64:65], 1.0, ["p1_v%d_%d" % (m, i)])
            self.ms("pool", kmT[:, :, :], 0.0, ["p1_kmT"])

            def load_x(b):
                c.dma("sp", xT[b % 2][:, :, :], self.xT_s[:, :, b * 512:(b + 1) * 512], reads=["xT_s"], writes=["p1_xT%d" % (b % 2)])

            psi = [0]

            def next_ps():
                i = psi[0] % 3
                psi[0] += 1
                return self.ps[i], "ps%d" % i

            tbank = [0]

            def next_tbank():
                tbank[0] += 1
                return 3 if tbank[0] % 2 == 0 else 7

            def tk(nm, bp, role, j):
                return "p1_T%d%s%d_%d" % (bp, nm, role, j)

            def mm_chunk(t, ci):
                b, tl = t // 4, t % 4
                c0_, cw = CH[ci]
                ps, pk = next_ps()
                for k in range(8):
                    self.mm(ps[:, 0:cw], xT[b % 2][:, k, tl * 128:(tl + 1) * 128], win[:, k, c0_:c0_ + cw],
                            k == 0, k == 7, ["p1_xT%d" % (b % 2), "p1_win%d" % k], [pk])
                return ps, pk

            def chunk0(t):
                i = t % 2
                ps, pk = mm_chunk(t, 0)
                cosA, sinA = self.cs[:, t, 0:16], self.sn[:, t, 0:16]
                ck = "p1_c0_%d" % i
                sk = "p1_ssq%d" % i
                self.cp("act", c0[i][:, 0:352], ps[:, 0:352], [pk], [ck])
                self.act(junk[:, 0:192], ps[:, 0:192], AF.Square, [pk], ["p1_junk", sk], accum_out=ssq[i][:, 0:1])
                self.act(junk[:, 192:320], ps[:, 192:320], AF.Square, [pk], ["p1_junk", sk], accum_out=ssq[i][:, 1:2])
                self.cp("pool", hf[i][:, 0:32], c0[i][:, 320:352], [ck], ["p1_hfa%d" % i])
                self.rope("pool", kpe[i][:, 0:16], kpe[i][:, 16:32], hf[i][:, 0:16], hf[i][:, 16:32], cosA, sinA,
                          rt5[:, 0:16], rt6[:, 0:16], ["p1_hfa%d" % i], ["p1_kpe%d" % i])
                self.ts("dve", ssq[i][:, 2:3], ssq[i][:, 0:1], 1.0 / 192, ALU.mult, [sk], [sk], s2=RMS_EPS, op1=ALU.add)
                self.ts("dve", ssq[i][:, 3:4], ssq[i][:, 1:2], 1.0 / 128, ALU.mult, [sk], [sk], s2=RMS_EPS, op1=ALU.add)
                self.act(ssq[i][:, 2:4], ssq[i][:, 2:4], AF.Ln, [sk], [sk])
                self.act(ssq[i][:, 2:4], ssq[i][:, 2:4], AF.Exp, [sk], [sk], scale=-0.5)

            def mla_up(t):
                i = t % 2
                cosA, sinA = self.cs[:, t, 0:16], self.sn[:, t, 0:16]
                ck, sk, lk = "p1_c0_%d" % i, "p1_ssq%d" % i, "p1_latT%d" % i
                pt, ptk = self.ps[4], "ps4"
                ptb = pt[:, :].bitcast(BF16)
                self.tr(ptb[:, 0:128], c0[i][:, 0:128], [ck], [ptk])
                self.tr(ptb[0:64, 128:256], c0[i][:, 128:192], [ck], [ptk])
                self.tr(ptb[:, 256:384], c0[i][:, 192:320], [ck], [ptk])
                self.cp("dve", latT[i][:, 0:3:2, :], ptb[:, 0:384].rearrange("p (a m) -> p a m", a=3)[:, 0:3:2, :], [ptk], [lk])
                self.cp("dve", latT[i][0:64, 1, :], ptb[0:64, 128:256], [ptk], [lk])
                pq, pqk = self.ps[5], "ps5"
                self.mm(pq[:, 0:384], latT[i][:, 0, :], wuq[:, 0, :], True, False, [lk, "p1_wuq"], [pqk])
                self.mm(pq[:, 0:384], latT[i][0:64, 1, :], wuq[0:64, 1, :], False, True, [lk, "p1_wuq"], [pqk])
                pkv, pkvk = self.ps[6], "ps6"
                self.mm(pkv[:, :], latT[i][:, 2, :], wukv[:, :], True, True, [lk, "p1_wukv"], [pkvk])
                pq3 = pq[:, 0:384].rearrange("p (h d) -> p h d", h=4)
                qk_ = "p1_qA%d" % i
                self.ts("dve", qA[i][:, :, 0:64], pq3[:, :, 0:64], ssq[i][:, 2:3], ALU.mult, [pqk, sk], [qk_])
                hk = "p1_hfb%d" % i
                h3 = hf[i][:, 64:192].rearrange("p (h d) -> p h d", h=4)
                self.ts("dve", h3, pq3[:, :, 64:96], ssq[i][:, 2:3], ALU.mult, [pqk, sk], [hk])
                cb = cosA.unsqueeze(1).to_broadcast([128, 4, 16])
                sb_ = sinA.unsqueeze(1).to_broadcast([128, 4, 16])
                self.rope("dve", qA[i][:, :, 64:80], qA[i][:, :, 80:96], h3[:, :, 0:16], h3[:, :, 16:32], cb, sb_,
                          rt1[:, 0:64].rearrange("p (h d) -> p h d", h=4), rt2[:, 0:64].rearrange("p (h d) -> p h d", h=4),
                          [hk], [qk_])
                pkv3 = pkv[:, :].rearrange("p (h d) -> p h d", h=4)
                kk_ = "p1_kA%d" % i
                self.ts("dve", kA[i][:, :, 0:64], pkv3[:, :, 0:64], ssq[i][:, 3:4], ALU.mult, [pkvk, sk], [kk_])
                self.ts("dve", vst[0][i][:, :, 0:64], pkv3[:, :, 64:128], ssq[i][:, 3:4], ALU.mult, [pkvk, sk], ["p1_v0_%d" % i])
                self.cp("pool", kA[i][:, :, 64:96], kpe[i][:, :].unsqueeze(1).to_broadcast([128, 4, 32]), ["p1_kpe%d" % i], [kk_])

            def chunk_plain(t, ci):
                i = t % 2
                ps, pk = mm_chunk(t, ci)
                if ci == 1:
                    self.cp("act", qkS[i][:, :], ps[:, :], [pk], ["p1_qkS%d" % i])
                elif ci == 2:
                    self.cp("act", vst[1][i][:, :, 0:64], ps[:, 0:256].rearrange("p (h d) -> p h d", h=4), [pk], ["p1_v1_%d" % i])
                    self.cp("act", vst[2][i][:, :, 0:64], ps[:, 256:512].rearrange("p (h d) -> p h d", h=4), [pk], ["p1_v2_%d" % i])
                else:
                    self.cp("act", vst[3][i][:, :, 0:64], ps[:, 0:256].rearrange("p (h d) -> p h d", h=4), [pk], ["p1_v3_%d" % i])

            def chunk_rope(t, ci):
                i = t % 2
                ps, pk = mm_chunk(t, ci)
                dst = (qkM if ci == 3 else qkD)[i]
                dk = ("p1_qkM%d" if ci == 3 else "p1_qkD%d") % i
                hk = "p1_hfc%d_%d" % (ci, i)
                self.cp("dve", dst[:, :], ps[:, :], [pk], [dk])
                if ci == 3:
                    g, nr = 8, 8
                    cb, sb_ = self.cs[:, t, 16:24], self.sn[:, t, 16:24]
                    hbase = 192
                else:
                    g, nr = 16, 4
                    cb, sb_ = self.cs[:, t, 24:28], self.sn[:, t, 24:28]
                    hbase = 320
                p3 = ps[:, :].rearrange("p (g d) -> p g d", g=g)
                d3 = dst[:, :].rearrange("p (g d) -> p g d", g=g)
                cbb = cb.unsqueeze(1).to_broadcast([128, g, nr])
                sbb = sb_.unsqueeze(1).to_broadcast([128, g, nr])
                h3 = hf[i][:, hbase:hbase + g * 2 * nr].rearrange("p (g d) -> p g d", g=g)
                self.cp("dve", h3, p3[:, :, 0:2 * nr], [pk], [hk])
                self.rope("pool", d3[:, :, 0:nr], d3[:, :, nr:2 * nr], h3[:, :, 0:nr], h3[:, :, nr:2 * nr], cbb, sbb,
                          rt3[:, 0:g * nr].rearrange("p (g d) -> p g d", g=g), rt4[:, 0:g * nr].rearrange("p (g d) -> p g d", g=g),
                          [hk], [dk])

            def v_store(t):
                i = t % 2
                c.dma("pool", self.v_s[t * 128:(t + 1) * 128, :], vall[i][:, :, :, :].rearrange("p m h d -> p (m h d)"),
                      reads=["p1_v%d_%d" % (m, i) for m in range(4)], writes=["v_s"])

            def tr_mla(t):
                i, bp = t % 2, (t // 4) % 2
                cols = slice((t % 4) * 128, (t % 4 + 1) * 128)
                for role, src, sk in ((0, qA[i], "p1_qA%d" % i), (1, kA[i], "p1_kA%d" % i)):
                    tb_ = next_tbank()
                    ptb = self.ps[tb_][:, :].bitcast(BF16)
                    for h in range(4):
                        self.tr(ptb[0:96, h * 128:(h + 1) * 128], src[:, h, :], [sk], ["ps%d" % tb_])
                    for h in range(4):
                        self.cp("act" if role == 0 else "dve", TA[bp][:, h, role, cols], ptb[0:96, h * 128:(h + 1) * 128], ["ps%d" % tb_],
                                ["p1_TA%d" % bp])

            def tr_pairs(t, nm):
                i, bp = t % 2, (t // 4) % 2
                cols = slice((t % 4) * 128, (t % 4 + 1) * 128)
                src, sk = {"S": (qkS[i], "p1_qkS%d" % i), "M": (qkM[i], "p1_qkM%d" % i), "D": (qkD[i], "p1_qkD%d" % i)}[nm]
                tb_ = next_tbank()
                ptb = self.ps[tb_][:, :].bitcast(BF16)
                for j in range(4):
                    self.tr(ptb[:, j * 128:(j + 1) * 128], src[:, j * 128:(j + 1) * 128], [sk], ["ps%d" % tb_])
                for j in range(4):
                    role, pr = j // 2, j % 2
                    self.cp("act" if nm != "D" else "dve", TP[bp][nm][:, role, pr, cols], ptb[:, j * 128:(j + 1) * 128], ["ps%d" % tb_],
                            ["p1_TP%d%s" % (bp, nm)])

            def gating(t):
                i, bp = t % 2, (t // 4) % 2
                cols = slice((t % 4) * 128, (t % 4 + 1) * 128)
                blk = t // 2
                pg, pgk = self.ps[6], "ps6"
                for pr in range(2):
                    self.mm(pg[:, pr:pr + 1], qkM[i][:, 256 + pr * 128:256 + (pr + 1) * 128], self.ones_b[:, 0:1], True, True,
                            ["p1_qkM%d" % i, "const"], [pgk])
                self.cp("dve", kpart[:, t % 2, :], pg[:, 0:2], [pgk], ["p1_kpart"])
                if t % 2 == 1:
                    self.tt("dve", ksum[:, :], kpart[:, 0, :], kpart[:, 1, :], ALU.add, ["p1_kpart"], ["p1_ksum"])
                    self.ts("dve", kmT[:, :, blk], ksum[:, :], 1.0 / 256, ALU.mult, ["p1_ksum"], ["p1_kmT"])
                own = blk
                mk = "p1_mb%d" % i
                if own >= 4:
                    g3 = gate[:, :, :].rearrange("p (a b) j -> p a b j", b=2)
                    self.ms("pool", gate[:, :, :], -BIGV, ["p1_gate"])
                    for hh, (pg, pgk) in enumerate(((self.ps[5], "ps5"), (self.ps[6], "ps6"))):
                        p0 = hh * 64
                        for pr in range(2):
                            qT = TP[bp]["M"][p0:p0 + 64, 0, pr, cols]
                            self.mm(pg[:, pr * 16:(pr + 1) * 16], qT, kmT[p0:p0 + 64, pr, :],
                                    True, True, ["p1_TP%dM" % bp, "p1_kmT"], [pgk])
                        self.cp("dve", g3[:, :, hh, 0:own], pg[:, 0:32].rearrange("p (a j) -> p a j", a=2)[:, :, 0:own], [pgk], ["p1_gate"])
                    for h in range(4):
                        nc_ = self.nc
                        gh = gate[:, h, :]
                        th = top8[:, h, :]
                        c.op("dve", (lambda gh=gh, th=th: nc_.vector.max(out=th, in_=gh)), ["p1_gate"], ["p1_top8"])
                    for h in range(4):
                        self.ts("dve", mb[i][:, h, :], gate[:, h, :], top8[:, h, 2:3], ALU.is_ge, ["p1_gate", "p1_top8"], [mk],
                                s2=-1.0, op1=ALU.add)
                    self.ms("pool", mb[i][:, :, own:own + 1], 0.0, [mk])
                else:
                    self.ms("pool", mb[i][:, :, :], 0.0, [mk])
                pm, pmk = self.ps[4], "ps4"
                pmb = pm[:, :].bitcast(BF16)
                self.tr(pmb[0:64, 0:128], mb[i][:, :, :].rearrange("p h j -> p (h j)"), [mk], [pmk])
                self.cp("dve", mbT[bp][:, cols], pmb[0:64, 0:128], [pmk], ["p1_mbT%d" % bp])

            def block_store(b):
                bp = b % 2
                bc = slice(b * 512, (b + 1) * 512)
                c.dma("sp", self.qk_s[0, :, :, 0:96, bc].rearrange("h r p c -> p h r c"), TA[bp][:, :, :, :], reads=["p1_TA%d" % bp], writes=["qk_s"])
                for mi, nm in ((0, "S"), (1, "M"), (2, "D")):
                    c.dma("sp", self.qkp_s[mi, :, :, :, bc].rearrange("r q p c -> p r q c"), TP[bp][nm][:, :, :, :],
                          reads=["p1_TP%d%s" % (bp, nm)], writes=["qk_s"])
                c.dma("sp", self.mb_s[:, bc], mbT[bp][:, :], reads=["p1_mbT%d" % bp], writes=["qk_s"])

            load_x(0)
            for t in range(NT + 1):
                cur = t if t < NT else None
                prv = t - 1 if t >= 1 else None
                if cur is not None and cur % 4 == 0 and cur // 4 + 1 < NB:
                    load_x(cur // 4 + 1)
                if cur is not None:
                    chunk0(cur)
                    chunk_plain(cur, 1)
                if prv is not None:
                    tr_mla(prv)
                if cur is not None:
                    chunk_plain(cur, 2)
                    mla_up(cur)
                    chunk_rope(cur, 3)
                if prv is not None:
                    tr_pairs(prv, "S")
                    tr_pairs(prv, "M")
                if cur is not None:
                    chunk_rope(cur, 4)
                if prv is not None:
                    tr_pairs(prv, "D")
                if cur is not None:
                    chunk_plain(cur, 5)
                    v_store(cur)
                if prv is not None:
                    gating(prv)
                    if prv % 4 == 3:
                        block_store(prv // 4)
            for h in range(4):
                c.dma("sp", self.qk_s[2, h, 1, 64:80, :], self.c_blk, writes=["qk_s"])
            c.emit()


    def phase2(self, l):
        nc, c = self.nc, self.c
        lambda_init = 0.8 - 0.6 * math.exp(-0.3 * l)
        with ExitStack() as st:
            KT = [self.sb("p2_KT%d" % i, [128, S], BF16, st) for i in range(2)]
            QT = [self.sb("p2_QT%d" % i, [128, S], BF16, st) for i in range(2)]
            Vm = [self.sb("p2_Vm%d" % i, [128, NT, 260], BF16, st) for i in range(2)]
            om = [self.sb("p2_om%d" % i, [128, NT, 256], BF16, st) for i in range(2)]
            od = [self.sb("p2_od%d" % i, [128, NT, 64], F32, st) for i in range(2)]
            PT = [self.sb("p2_PT%d" % i, [128, 512], BF16, st) for i in range(4)]
            SBANK = [0, 1, 2, 7]
            E32 = [self.sb("p2_E%d" % i, [128, 512], F32, st) for i in range(3)]
            Psp = [self.sb("p2_P%d" % i, [128, 512], BF16, st) for i in range(3)]
            T = [self.sb("p2_T%d" % i, [128, 512], F32, st) for i in range(3)]
            Aex = [self.sb("p2_A%d" % i, [128, 512], BF16, st) for i in range(3)]
            Run = self.sb("p2_Run", [128, 512], BF16, st)
            rc = self.sb("p2_rc", [128, 4], F32, st)
            hcount = [0]
            vcount = [0]

            def load_head(m, h, rows, r0=0):
                j = hcount[0] % 2
                hcount[0] += 1
                for buf, bk in ((KT[j], "p2_KT%d" % j), (QT[j], "p2_QT%d" % j)):
                    if rows <= 32:
                        self.ms("pool", buf[32:64, :], 0.0, [bk])
                    self.ms("pool", buf[64:128, :], 0.0, [bk])
                for role, buf, bk in ((1, KT[j], "p2_KT%d" % j), (0, QT[j], "p2_QT%d" % j)):
                    if m == 0:
                        src = self.qk_s[0, h, role, 0:rows, :]
                    else:
                        rb = (h % 2) * 64 + r0
                        src = self.qkp_s[m - 1, role, h // 2, rb:rb + min(rows, 64), :]
                    c.dma("sp", buf[0:min(rows, 96 if m == 0 else 64), :], src, reads=["qk_s"], writes=[bk])
                if m == 2:
                    c.dma("sp", QT[j][64:80, :], self.mb_s[h * 16:(h + 1) * 16, :], reads=["qk_s"], writes=["p2_QT%d" % j])
                    c.dma("sp", KT[j][64:80, :], self.c_blk, writes=["p2_KT%d" % j])
                return KT[j], QT[j], ["p2_KT%d" % j, "p2_QT%d" % j]

            def load_v(m):
                j = vcount[0] % 2
                vcount[0] += 1
                c.dma("sp", Vm[j][:, :, :], self.v_s[:, m * 260:(m + 1) * 260].rearrange("(t p) c -> p t c", p=128), reads=["v_s"], writes=["p2_Vm%d" % j])
                return Vm[j], "p2_Vm%d" % j

            sslot = [0]

            def softmax_head(kt, qt, kqk, R, scale, vm, vkey, h, out_fn):
                for b in range(NB):
                    n = 4 * b + 4

                    def S_stage(i):
                        j = i - 4 * b
                        c0 = max(0, j) * 128
                        slot = sslot[0]
                        sslot[0] = (slot + 1) % 4
                        ps, pk = self.ps[SBANK[slot]], "ps%d" % SBANK[slot]
                        self.mm(ps[:, c0:512], kt[:, i * 128:(i + 1) * 128], qt[:, b * 512 + c0:(b + 1) * 512], True, True, kqk, [pk])
                        pt, ptk = PT[slot], "p2_PT%d" % slot
                        self.act(pt[:, c0:512], ps[:, c0:512], AF.Exp, [pk], [ptk], scale=scale)
                        if j >= 0:
                            self.tt("dve", pt[:, c0:c0 + 128], pt[:, c0:c0 + 128], self.mask_le[:, :], ALU.mult, [ptk, "const"], [ptk])
                        return slot

                    def PV_stage(i, slot):
                        for s_ in range(4):
                            if 4 * b + s_ >= i:
                                self.mm(self.ps[3 + s_][:, 0:65], PT[slot][:, s_ * 128:(s_ + 1) * 128], vm[:, i, h * 65:(h + 1) * 65],
                                        i == 0, i == 4 * b + s_, ["p2_PT%d" % slot, vkey], ["ps%d" % (3 + s_)])

                    slots = {}
                    for i in range(min(2, n)):
                        slots[i] = S_stage(i)
                    for i in range(n):
                        if i + 2 < n:
                            slots[i + 2] = S_stage(i + 2)
                        PV_stage(i, slots[i])
                        if i >= 4 * b:
                            s_ = i - 4 * b
                            out_fn(b, s_, self.ps[3 + s_], "ps%d" % (3 + s_))

            def sb_head(kt, qt, kqk, vm, vkey, h, omt, omk):
                scale = 0.125
                for b in range(NB):
                    order = list(range(4 * b + 3, -1, -1))
                    n = len(order)
                    self.ms("pool", Run[:, :], 0.0, ["p2_Run"])

                    def geo(idx):
                        i = order[idx]
                        j = i - 4 * b
                        return i, j, max(0, j) * 128

                    def A_stage(idx):
                        i, j, c0 = geo(idx)
                        z, q3 = idx % 2, idx % 3
                        ps, pk = self.ps[z], "ps%d" % z
                        self.mm(ps[:, c0:512], kt[:, i * 128:(i + 1) * 128], qt[:, b * 512 + c0:(b + 1) * 512], True, True, kqk, [pk])
                        e, ek = E32[q3], "p2_E%d" % q3
                        self.act(e[:, c0:512], ps[:, c0:512], AF.Exp, [pk], [ek], scale=scale)
                        if j >= 0:
                            self.tt("pool", e[:, c0:c0 + 128], e[:, c0:c0 + 128], self.mask_lt[:, :], ALU.mult, [ek, "const"], [ek])
                        self.act(Psp[q3][:, c0:512], e[:, c0:512], AF.Ln, [ek], ["p2_P%d" % q3], bias=1.0)

                    def B_stage(idx):
                        i, j, c0 = geo(idx)
                        z, q3 = idx % 2, idx % 3
                        af, afk = (self.ps[2], "ps2") if z == 0 else (self.ps[7], "ps7")
                        pk2 = "p2_P%d" % q3
                        first = idx == 0
                        self.mm(af[:, c0:512], self.tri[:, :], Psp[q3][:, c0:512], True, first, ["const", pk2], [afk])
                        if not first:
                            self.mm(af[:, c0:512], self.ones_b[:, :], Run[:, c0:512], False, True, ["const", "p2_Run"], [afk])
                        self.tt("pool", Run[:, c0:512], Run[:, c0:512], Psp[q3][:, c0:512], ALU.add, ["p2_Run", pk2], ["p2_Run"])
                        t, tk = T[q3], "p2_T%d" % q3
                        self.stt(t[:, c0:512], self.ps[z][:, c0:512], scale, Psp[q3][:, c0:512], ALU.mult, ALU.subtract, ["ps%d" % z, pk2], [tk])
                        self.tt("dve", t[:, c0:512], t[:, c0:512], af[:, c0:512], ALU.subtract, [tk, afk], [tk])
                        a, ak = Aex[q3], "p2_A%d" % q3
                        self.act(a[:, c0:512], t[:, c0:512], AF.Exp, [tk], [ak])
                        if j >= 0:
                            self.tt("pool", a[:, c0:c0 + 128], a[:, c0:c0 + 128], self.mask_lt[:, :], ALU.mult, [ak, "const"], [ak])

                    def C_stage(idx):
                        i, j, c0 = geo(idx)
                        q3 = idx % 3
                        for s_ in range(4):
                            if 4 * b + s_ >= i:
                                self.mm(self.ps[3 + s_][:, 0:64], Aex[q3][:, s_ * 128:(s_ + 1) * 128], vm[:, i, h * 65:h * 65 + 64],
                                        i == 4 * b + s_, i == 0, ["p2_A%d" % q3, vkey], ["ps%d" % (3 + s_)])

                    for step in range(n + 2):
                        if step < n:
                            A_stage(step)
                        if 0 <= step - 1 < n:
                            B_stage(step - 1)
                        if 0 <= step - 2 < n:
                            C_stage(step - 2)
                    for s_ in range(4):
                        self.cp("dve", omt[:, 4 * b + s_, h * 64:(h + 1) * 64], self.ps[3 + s_][:, 0:64], ["ps%d" % (3 + s_)], [omk])

            def norm_out(dst, dkey, col0, f32=False):
                def fn(b, s_, O, ok):
                    self.recip(rc[:, s_:s_ + 1], O[:, 64:65], [ok], ["p2_rc"])
                    self.ts("dve", dst[:, 4 * b + s_, col0:col0 + 64], O[:, 0:64], rc[:, s_:s_ + 1], ALU.mult, [ok, "p2_rc"], [dkey])
                return fn

            def store_om(m, omt, omk):
                c.dma("sp", self.o_s[:, m * 256:(m + 1) * 256].rearrange("(t p) c -> p t c", p=128), omt[:, :, :], reads=[omk], writes=["o_s"])

            oi = 0
            for m, R, scale in ((0, 96, 96 ** -0.5), (2, 80, 0.125)):
                vm, vkey = load_v(m)
                omt, omk = om[oi % 2], "p2_om%d" % (oi % 2)
                oi += 1
                for h in range(4):
                    kt, qt, kqk = load_head(m, h, R)
                    softmax_head(kt, qt, kqk, R, scale, vm, vkey, h, norm_out(omt, omk, h * 64))
                store_om(m, omt, omk)
            vm, vkey = load_v(1)
            omt, omk = om[oi % 2], "p2_om%d" % (oi % 2)
            oi += 1
            for h in range(4):
                kt, qt, kqk = load_head(1, h, 64)
                sb_head(kt, qt, kqk, vm, vkey, h, omt, omk)
            store_om(1, omt, omk)
            vm, vkey = load_v(3)
            dcount = 0
            for comp in range(2):
                for h in range(4):
                    kt, qt, kqk = load_head(3, h, 32, comp * 32)
                    odt, odk = od[dcount % 2], "p2_od%d" % (dcount % 2)
                    dcount += 1
                    softmax_head(kt, qt, kqk, 32, 32 ** -0.5, vm, vkey, h, norm_out(odt, odk, 0))
                    c.dma("sp", self.od_s[comp, :, h * 64:(h + 1) * 64].rearrange("(t p) c -> p t c", p=128), odt[:, :, :],
                          reads=[odk], writes=["od_s"])
            dl = self.sb("p2_dl", [128, 128], F32, st)
            lam = self.sb("p2_lam", [128, 4], F32, st)
            gsub = self.sb("p2_gsub", [128, 64], F32, st)
            junk = self.sb("p2_junk", [128, 64], F32, st)
            c.dma("sp", dl[:, :], self.diff_lambda[l, :].partition_broadcast(128), writes=["p2_dl"])
            c.dma("sp", gsub[:, :], self.diff_subln[l, :].partition_broadcast(128), writes=["p2_gsub"])
            self.tt("dve", junk[:, 0:32], dl[:, 0:32], dl[:, 32:64], ALU.mult, ["p2_dl"], ["p2_junk"])
            self.red(lam[:, 0:1], junk[:, 0:32], ALU.add, ["p2_junk"], ["p2_lam"])
            self.tt("dve", junk[:, 32:64], dl[:, 64:96], dl[:, 96:128], ALU.mult, ["p2_dl"], ["p2_junk"])
            self.red(lam[:, 1:2], junk[:, 32:64], ALU.add, ["p2_junk"], ["p2_lam"])
            self.act(lam[:, 0:2], lam[:, 0:2], AF.Exp, ["p2_lam"], ["p2_lam"])
            self.tt("dve", lam[:, 2:3], lam[:, 1:2], lam[:, 0:1], ALU.subtract, ["p2_lam"], ["p2_lam"])
            self.ts("dve", lam[:, 2:3], lam[:, 2:3], -lambda_init, ALU.add, ["p2_lam"], ["p2_lam"])
            self.ts("dve", gsub[:, :], gsub[:, :], 1.0 - lambda_init, ALU.mult, ["p2_gsub"], ["p2_gsub"])
            omt, omk = om[oi % 2], "p2_om%d" % (oi % 2)
            oi += 1
            d1 = [self.sb("p2_d1_%d" % i, [128, 256], F32, st) for i in range(2)]
            d2 = [self.sb("p2_d2_%d" % i, [128, 256], F32, st) for i in range(2)]
            sq = self.sb("p2_sq", [128, 256], F32, st)
            ss = self.sb("p2_ss", [128, 4], F32, st)
            for t in range(NT):
                i = t % 2
                c.dma("sp", d1[i][:, :], self.od_s[0, t * 128:(t + 1) * 128, :], reads=["od_s"], writes=["p2_d1_%d" % i])
                c.dma("sp", d2[i][:, :], self.od_s[1, t * 128:(t + 1) * 128, :], reads=["od_s"], writes=["p2_d2_%d" % i])
                k1 = "p2_d1_%d" % i
                self.stt(d1[i][:, :], d2[i][:, :], lam[:, 2:3], d1[i][:, :], ALU.mult, ALU.add, ["p2_d2_%d" % i, k1, "p2_lam"], [k1])
                self.tt("dve", sq[:, :], d1[i][:, :], d1[i][:, :], ALU.mult, [k1], ["p2_sq"])
                self.red(ss[:, :], sq[:, :].rearrange("p (h d) -> p h d", h=4), ALU.add, ["p2_sq"], ["p2_ss"])
                self.ts("dve", ss[:, :], ss[:, :], 1.0 / 64, ALU.mult, ["p2_ss"], ["p2_ss"], s2=RMS_EPS, op1=ALU.add)
                self.act(ss[:, :], ss[:, :], AF.Ln, ["p2_ss"], ["p2_ss"])
                self.act(ss[:, :], ss[:, :], AF.Exp, ["p2_ss"], ["p2_ss"], scale=-0.5)
                d3 = d1[i][:, :].rearrange("p (h d) -> p h d", h=4)
                self.tt("dve", d3, d3, ss[:, :].unsqueeze(2).to_broadcast([128, 4, 64]), ALU.mult, [k1, "p2_ss"], [k1])
                self.tt("dve", omt[:, t, :].rearrange("p (h d) -> p h d", h=4), d3, gsub[:, :].unsqueeze(1).to_broadcast([128, 4, 64]),
                        ALU.mult, [k1, "p2_gsub"], [omk])
            store_om(3, omt, omk)
            c.emit()

    def ln_part1(self, y, yk, res, resk, pa, pak, pb, pbk, tmp, tk):
        nc = self.nc
        st6, mv = tmp["st6"], tmp["mv"]
        self.stt(y[:, 0:512], res[:, 0:512], ALPHA, pa[:, 0:512], ALU.mult, ALU.add, [resk, pak], [yk])
        self.stt(y[:, 512:1024], res[:, 512:1024], ALPHA, pb[:, 0:512], ALU.mult, ALU.add, [resk, pbk], [yk])
        self.c.op("dve", lambda: nc.vector.bn_stats(out=st6[:, 0:6], in_=y[:, 0:512]), [yk], [tk + "s"])
        self.c.op("dve", lambda: nc.vector.bn_stats(out=st6[:, 6:12], in_=y[:, 512:1024]), [yk], [tk + "s"])
        self.c.op("dve", lambda: nc.vector.bn_aggr(out=mv[:, 0:2], in_=st6[:, 0:12]), [tk + "s"], [tk])
        self.ts("dve", mv[:, 2:3], mv[:, 1:2], LN_EPS, ALU.add, [tk], [tk])
        self.act(mv[:, 2:3], mv[:, 2:3], AF.Ln, [tk], [tk])
        self.act(mv[:, 2:3], mv[:, 2:3], AF.Exp, [tk], [tk], scale=-0.5)

    def ln_part2(self, y, yk, g_t, b_t, gbk, out, outk, tmp, tk):
        mv = tmp["mv"]
        self.stt(y[:, :], y[:, :], mv[:, 0:1], g_t[:, :], ALU.subtract, ALU.mult, [yk, tk, gbk], [yk])
        self.stt(out, y[:, :], mv[:, 2:3], b_t[:, :], ALU.mult, ALU.add, [yk, tk, gbk], [outk])

    def ln_tile(self, y, yk, res, resk, pa, pak, pb, pbk, g_t, b_t, gbk, out, outk, tmp, tk="ln_mv"):
        self.ln_part1(y, yk, res, resk, pa, pak, pb, pbk, tmp, tk)
        self.ln_part2(y, yk, g_t, b_t, gbk, out, outk, tmp, tk)

    def to_xT(self, src, srck, xb, xbk, xts, xtsk, cols, eng):
        self.cp("act", xb[:, :], src, [srck], [xbk])
        ptb = self.ps[7][:, :].bitcast(BF16)
        for k in range(8):
            self.tr(ptb[:, k * 128:(k + 1) * 128], xb[:, k * 128:(k + 1) * 128], [xbk], ["ps7"])
        self.cp(eng, xts[:, :, cols], ptb.rearrange("p (k m) -> p k m", k=8), ["ps7"], [xtsk])

    def phase34(self, l):
        nc, c = self.nc, self.c
        xres_src = self.x_in if l == 0 else self.xres_s
        with ExitStack() as st:
            KmT = self.sb("p3_KmT", [128, 8, N_MEM], BF16, st)
            Vmem = self.sb("p3_Vmem", [128, 2, 4 * 257], BF16, st)
            wout = self.sb("p3_wout", [128, 8, D], BF16, st)
            wq = self.sb("p3_wq", [128, 8, D], BF16, st)
            wo = self.sb("p3_wo", [128, 8, D], BF16, st)
            with ExitStack() as st2:
                wk = self.sb("p3_wk", [128, 8, D], BF16, st2)
                wv = self.sb("p3_wv", [128, 8, D], BF16, st2)
                for k in range(8):
                    c.dma("pool", wk[:, k, :], self.wk[l, k * 128:(k + 1) * 128, :], writes=["p3_wk%d" % k])
                    c.dma("pool", wv[:, k, :], self.wv[l, k * 128:(k + 1) * 128, :], writes=["p3_wv%d" % k])
                self.ms("pool", Vmem[:, :, :], 1.0, ["p3_Vmem"])
                for k in range(8):
                    c.dma("pool", wout[:, k, :], self.w_out[l, k * 128:(k + 1) * 128, :], writes=["p3_wout%d" % k])
                for k in range(8):
                    c.dma("pool", wq[:, k, :], self.wq[l, k * 128:(k + 1) * 128, :], writes=["p3_wq%d" % k])
                for k in range(8):
                    c.dma("pool", wo[:, k, :], self.wo[l, k * 128:(k + 1) * 128, :], writes=["p3_wo%d" % k])
                for j in range(8):
                    ps, pk = self.ps[j % 2], "ps%d" % (j % 2)
                    for k in range(8):
                        self.mm(ps[:, 0:N_MEM], wk[:, k, j * 128:(j + 1) * 128], self.memT[:, k, :], k == 0, k == 7, ["p3_wk%d" % k, "memT"], [pk])
                    self.cp("act" if j % 2 == 0 else "dve", KmT[:, j, :], ps[:, 0:N_MEM], [pk], ["p3_KmT"])
                for mt in range(2):
                    for nh in range(2):
                        ps, pk = self.ps[2 + nh], "ps%d" % (2 + nh)
                        for k in range(8):
                            self.mm(ps[:, :], self.memT[:, k, mt * 128:(mt + 1) * 128], wv[:, k, nh * 512:(nh + 1) * 512], k == 0, k == 7,
                                    ["p3_wv%d" % k, "memT"], [pk])
                        dst = Vmem[:, mt, nh * 514:(nh + 1) * 514].rearrange("p (h d) -> p h d", h=2)[:, :, 0:256]
                        self.cp("act" if nh == 0 else "dve", dst, ps[:, :].rearrange("p (h d) -> p h d", h=2), [pk], ["p3_Vmem"])
                c.emit()
            gb = {}
            for nm, src in (("g1", self.ln_g[0]), ("b1", self.ln_b[0]), ("g2", self.ln_g[1]), ("b2", self.ln_b[1])):
                gb[nm] = self.sb("p3_" + nm, [128, D], F32, st)
                c.dma("sp", gb[nm][:, :], src[l, :].partition_broadcast(128), writes=["p3_gb"])
            tmpA = {"st6": self.sb("p3_st6a", [128, 12], F32, st), "mv": self.sb("p3_mva", [128, 4], F32, st)}
            tmpB = {"st6": self.sb("p3_st6b", [128, 12], F32, st), "mv": self.sb("p3_mvb", [128, 4], F32, st)}
            ob = [self.sb("p3_ob0", [128, 4, D], BF16, st)] * 2
            oT = [self.sb("p3_oT0", [128, 8, 512], BF16, st)] * 2
            xr = [self.sb("p3_xr%d" % i, [128, D], F32, st) for i in range(2)]
            xr2 = [self.sb("p3_xq%d" % i, [128, D], F32, st) for i in range(2)]
            yA = self.sb("p3_yA", [128, D], F32, st)
            yB = self.sb("p3_yB", [128, D], F32, st)
            x1t = [self.sb("p3_x1t%d" % i, [128, D], F32, st) for i in range(2)]
            xbA = self.sb("p3_xbA", [128, D], BF16, st)
            xbB = self.sb("p3_xbB", [128, D], BF16, st)
            x1T = [self.sb("p3_x1T%d" % i, [128, 8, 512], BF16, st) for i in range(2)]
            qT = self.sb("p3_qT", [128, 8, 512], BF16, st)
            PT2 = [self.sb("p3_PT%d" % i, [128, 512], BF16, st) for i in range(4)]
            xo = self.sb("p3_xo", [128, 4, D], BF16, st)
            xoT = self.sb("p3_xoT", [128, 8, 512], BF16, st)
            x2 = [self.sb("p3_x2_%d" % i, [128, D], F32, st) for i in range(2)]
            x2T = [self.sb("p3_x2T0", [128, 8, 512], BF16, st)] * 2
            rc = self.sb("p3_rc", [128, 2], F32, st)

            def to_xT2(src, srck, xb, xbk, xts, xtsk, cols, bank, eng):
                self.cp("act", xb[:, :], src, [srck], [xbk])
                ptb = self.ps[bank][:, :].bitcast(BF16)
                for k in range(8):
                    self.tr(ptb[:, k * 128:(k + 1) * 128], xb[:, k * 128:(k + 1) * 128], [xbk], ["ps%d" % bank])
                self.cp(eng, xts[:, :, cols], ptb.rearrange("p (k m) -> p k m", k=8), ["ps%d" % bank], [xtsk])

            def load_ob(b):
                c.dma("sp", ob[0][:, :, :], self.o_s[b * 512:(b + 1) * 512, :].rearrange("(t p) c -> p t c", p=128),
                      reads=["o_s"], writes=["p3_ob0"])

            def H1(b):
                steps = []
                p = b % 2
                obk, oTk, x1Tk = "p3_ob0", "p3_oT0", "p3_x1T%d" % p

                def s_oT(tl):
                    ptb = self.ps[2][:, :].bitcast(BF16)
                    for k in range(8):
                        self.tr(ptb[:, k * 128:(k + 1) * 128], ob[p][:, tl, k * 128:(k + 1) * 128], [obk], ["ps2"])
                    self.cp("act", oT[p][:, :, tl * 128:(tl + 1) * 128], ptb.rearrange("p (k m) -> p k m", k=8), ["ps2"], [oTk])

                def s_mix(tl):
                    t = b * 4 + tl
                    xi = t % 2
                    c.dma("sp", xr[xi][:, :], xres_src[t * 128:(t + 1) * 128, :], reads=["xres_s%d" % t], writes=["p3_xr%d" % xi])
                    for nh in range(2):
                        for k in range(8):
                            self.mm(self.ps[nh][:, :], oT[p][:, k, tl * 128:(tl + 1) * 128], wout[:, k, nh * 512:(nh + 1) * 512], k == 0, k == 7,
                                    [oTk, "p3_wout%d" % k], ["ps%d" % nh])
                    self.ln_part1(yA, "p3_yA", xr[xi], "p3_xr%d" % xi, self.ps[0], "ps0", self.ps[1], "ps1", tmpA, "lnA")

                def s_mix_b(tl):
                    t = b * 4 + tl
                    xi = t % 2
                    self.ln_part2(yA, "p3_yA", gb["g1"], gb["b1"], "p3_gb", x1t[xi][:, :], "p3_x1t%d" % xi, tmpA, "lnA")
                    c.dma("pool", self.x1_s[t * 128:(t + 1) * 128, :], x1t[xi][:, :], reads=["p3_x1t%d" % xi], writes=["x1_s%d" % t])

                def s_x1T(tl):
                    xi = (b * 4 + tl) % 2
                    to_xT2(x1t[xi][:, :], "p3_x1t%d" % xi, xbA, "p3_xbA", x1T[p], x1Tk, slice(tl * 128, (tl + 1) * 128), 2, "act")

                steps.append(lambda: load_ob(b))
                for tl in range(4):
                    steps.append(lambda tl=tl: s_oT(tl))
                for tl in range(4):
                    steps.append(lambda tl=tl: s_mix(tl))
                    if tl > 0:
                        steps.append(lambda tl=tl: s_x1T(tl - 1))
                    steps.append(lambda tl=tl: s_mix_b(tl))
                steps.append(lambda: s_x1T(3))
                return steps

            pcount = [0]

            def H2(b):
                steps = []
                p = b % 2
                x1Tk = "p3_x1T%d" % p
                x2Tb, x2Tk = x2T[0], "p3_x2T0"

                def s_q(j):
                    ps, pk = self.ps[3 + j % 2], "ps%d" % (3 + j % 2)
                    for k in range(8):
                        self.mm(ps[:, :], wq[:, k, j * 128:(j + 1) * 128], x1T[p][:, k, :], k == 0, k == 7, ["p3_wq%d" % k, x1Tk], [pk])
                    self.cp("act" if j % 2 == 0 else "dve", qT[:, j, :], ps[:, :], [pk], ["p3_qT"])

                def s_att(h):
                    pts = []
                    for mt in range(2):
                        ps, pk = self.ps[5 + mt], "ps%d" % (5 + mt)
                        for cc in range(2):
                            self.mm(ps[:, :], KmT[:, h * 2 + cc, mt * 128:(mt + 1) * 128], qT[:, h * 2 + cc, :], cc == 0, cc == 1,
                                    ["p3_KmT", "p3_qT"], [pk])
                        pi = pcount[0] % 4
                        pcount[0] += 1
                        self.act(PT2[pi][:, :], ps[:, :], AF.Exp, [pk], ["p3_PT%d" % pi], scale=256 ** -0.5)
                        pts.append(pi)
                    for tl in range(4):
                        ps, pk = self.ps[7], "ps7"
                        for mt in range(2):
                            self.mm(ps[:, 0:257], PT2[pts[mt]][:, tl * 128:(tl + 1) * 128], Vmem[:, mt, h * 257:(h + 1) * 257], mt == 0, mt == 1,
                                    ["p3_PT%d" % pts[mt], "p3_Vmem"], [pk])
                        self.recip(rc[:, tl % 2:tl % 2 + 1], ps[:, 256:257], [pk], ["p3_rc%d" % (tl % 2)])
                        self.ts("dve", xo[:, tl, h * 256:(h + 1) * 256], ps[:, 0:256], rc[:, tl % 2:tl % 2 + 1], ALU.mult, [pk, "p3_rc%d" % (tl % 2)], ["p3_xo"])

                def s_xoT(tl):
                    ptb = self.ps[5][:, :].bitcast(BF16)
                    for k in range(8):
                        self.tr(ptb[:, k * 128:(k + 1) * 128], xo[:, tl, k * 128:(k + 1) * 128], ["p3_xo"], ["ps5"])
                    self.cp("act", xoT[:, :, tl * 128:(tl + 1) * 128], ptb.rearrange("p (k m) -> p k m", k=8), ["ps5"], ["p3_xoT"])

                def s_wo(tl):
                    t = b * 4 + tl
                    for nh in range(2):
                        for k in range(8):
                            self.mm(self.ps[3 + nh][:, :], xoT[:, k, tl * 128:(tl + 1) * 128], wo[:, k, nh * 512:(nh + 1) * 512], k == 0, k == 7,
                                    ["p3_xoT", "p3_wo%d" % k], ["ps%d" % (3 + nh)])
                    x2t, x2k = x2[t % 2], "p3_x2_%d" % (t % 2)
                    c.dma("sp", xr2[t % 2][:, :], self.x1_s[t * 128:(t + 1) * 128, :], reads=["x1_s%d" % t], writes=["p3_xq%d" % (t % 2)])
                    self.ln_part1(yB, "p3_yB", xr2[t % 2], "p3_xq%d" % (t % 2), self.ps[3], "ps3", self.ps[4], "ps4", tmpB, "lnB")

                def s_wo_b(tl):
                    t = b * 4 + tl
                    x2t, x2k = x2[t % 2], "p3_x2_%d" % (t % 2)
                    self.ln_part2(yB, "p3_yB", gb["g2"], gb["b2"], "p3_gb", x2t[:, :], x2k, tmpB, "lnB")
                    c.dma("pool", self.xres_s[t * 128:(t + 1) * 128, :], x2t[:, :], reads=[x2k], writes=["xres_s%d" % t])

                def s_x2T(tl):
                    t = b * 4 + tl
                    to_xT2(x2[t % 2][:, :], "p3_x2_%d" % (t % 2), xbB, "p3_xbB", x2Tb, x2Tk, slice(tl * 128, (tl + 1) * 128), 6, "act")
                    if tl == 3:
                        c.dma("pool", self.xT_s[:, :, b * 512:(b + 1) * 512], x2Tb[:, :, :], reads=[x2Tk], writes=["xT_s"])

                for j in range(8):
                    steps.append(lambda j=j: s_q(j))
                for h in range(4):
                    steps.append(lambda h=h: s_att(h))
                for tl in range(4):
                    steps.append(lambda tl=tl: s_xoT(tl))
                for tl in range(4):
                    steps.append(lambda tl=tl: s_wo(tl))
                    if tl > 0:
                        steps.append(lambda tl=tl: s_x2T(tl - 1))
                    steps.append(lambda tl=tl: s_wo_b(tl))
                steps.append(lambda: s_x2T(3))
                return steps

            def merge(a, bsteps):
                na, nb = len(a), len(bsteps)
                ia = ib = 0
                while ia < na or ib < nb:
                    if ib >= nb or (ia < na and ia * nb <= ib * na):
                        a[ia]()
                        ia += 1
                    else:
                        bsteps[ib]()
                        ib += 1

            for f in H1(0):
                f()
            for b in range(NB):
                merge(H2(b), H1(b + 1) if b + 1 < NB else [])
            c.emit()

    def phase5(self, l, last):
        nc, c = self.nc, self.c
        with ExitStack() as st:
            wr = self.sb("p5_wr", [128, 8, 40], BF16, st)
            br = self.sb("p5_br", [128, 40], F32, st)
            c.dma("pool", wr[:, :, :], self.w_router[l].rearrange("(k p) n -> p k n", p=128), writes=["p5_wr"])
            c.dma("sp", br[:, :], self.b_router[l, :].partition_broadcast(128), writes=["p5_br"])
            xT = [self.sb("p5_xT%d" % i, [128, 8, 512], BF16, st) for i in range(2)]
            lg = self.sb("p5_lg", [128, 4, 40], F32, st)
            gmax = self.sb("p5_gmax", [128, 4], F32, st)
            ohg = self.sb("p5_ohg", [128, 4, 8], F32, st)
            z8 = self.sb("p5_z8", [128, 4, 8], F32, st)
            se = self.sb("p5_se", [128, 4], F32, st)
            gw = self.sb("p5_gw", [128, 4], F32, st)
            t32 = self.sb("p5_t32", [128, 4, 32], F32, st)
            el = self.sb("p5_el", [128, 4, 4], F32, st)
            el2 = self.sb("p5_el2", [128, 4, 4], F32, st)
            mk1 = self.sb("p5_mk1", [128, 4, 4], F32, st)
            mk2 = self.sb("p5_mk2", [128, 4, 4], F32, st)
            m1 = self.sb("p5_m1", [128, 4], F32, st)
            m2 = self.sb("p5_m2", [128, 4], F32, st)
            e21 = self.sb("p5_e21", [128, 4], F32, st)
            w1 = self.sb("p5_w1", [128, 4], F32, st)
            w2 = self.sb("p5_w2", [128, 4], F32, st)
            g4 = self.sb("p5_g4", [128, 4, 4], F32, st)
            g4b = self.sb("p5_g4b", [128, 4, 4], F32, st)
            g32 = self.sb("p5_g32", [128, 4, 32], F32, st)
            gTs = [self.sb("p5_gTs%d" % i, [32, 512], F32, st) for i in range(2)]

            def load_x(b):
                c.dma("sp", xT[b % 2][:, :, :], self.xT_s[:, :, b * 512:(b + 1) * 512], reads=["xT_s"], writes=["p5_xT%d" % (b % 2)])

            load_x(0)
            K = "p5_r"
            bc3 = lambda ap, n: ap.unsqueeze(2).to_broadcast([128, 4, n])
            for b in range(NB):
                if b + 1 < NB:
                    load_x(b + 1)
                gT, gTk = gTs[b % 2], "p5_gTs%d" % (b % 2)
                ps, pk = self.ps[b % 2], "ps%d" % (b % 2)
                for tl in range(4):
                    for k in range(8):
                        self.mm(ps[:, tl * 40:(tl + 1) * 40], xT[b % 2][:, k, tl * 128:(tl + 1) * 128], wr[:, k, :], k == 0, k == 7,
                                ["p5_xT%d" % (b % 2), "p5_wr"], [pk])
                self.tt("dve", lg[:, :, :], ps[:, 0:160].rearrange("p (t n) -> p t n", t=4), br[:, :].unsqueeze(1).to_broadcast([128, 4, 40]),
                        ALU.add, [pk, "p5_br"], [K])
                lgg = lg[:, :, 0:8]
                self.red(gmax[:, :], lgg, ALU.max, [K], [K])
                self.tt("dve", ohg[:, :, :], lgg, bc3(gmax[:, :], 8), ALU.is_equal, [K], [K])
                self.tt("dve", z8[:, :, :], lgg, bc3(gmax[:, :], 8), ALU.subtract, [K], [K])
                self.act(z8[:, :, :], z8[:, :, :], AF.Exp, [K], [K])
                self.red(se[:, :], z8[:, :, :], ALU.add, [K], [K])
                self.recip(gw[:, :], se[:, :], [K], [K])
                lge = lg[:, :, 8:40].rearrange("p t (g e) -> p t g e", g=8)
                self.tt("dve", t32[:, :, :].rearrange("p t (g e) -> p t g e", g=8), lge, ohg[:, :, :].unsqueeze(3).to_broadcast([128, 4, 8, 4]),
                        ALU.mult, [K], [K])
                self.red(el[:, :, :], t32[:, :, :].rearrange("p t (g e) -> p t e g", g=8), ALU.add, [K], [K])
                self.red(m1[:, :], el[:, :, :], ALU.max, [K], [K])
                self.tt("dve", mk1[:, :, :], el[:, :, :], bc3(m1[:, :], 4), ALU.is_equal, [K], [K])
                self.stt(el2[:, :, :], mk1[:, :, :], -1e30, el[:, :, :], ALU.mult, ALU.add, [K], [K])
                self.red(m2[:, :], el2[:, :, :], ALU.max, [K], [K])
                self.tt("dve", mk2[:, :, :], el2[:, :, :], bc3(m2[:, :], 4), ALU.is_equal, [K], [K])
                self.tt("dve", e21[:, :], m2[:, :], m1[:, :], ALU.subtract, [K], [K])
                self.act(e21[:, :], e21[:, :], AF.Exp, [K], [K])
                self.ts("dve", w1[:, :], e21[:, :], 1.0, ALU.add, [K], [K])
                self.recip(w1[:, :], w1[:, :], [K], [K])
                self.tt("dve", w2[:, :], w1[:, :], e21[:, :], ALU.mult, [K], [K])
                self.tt("dve", w1[:, :], w1[:, :], gw[:, :], ALU.mult, [K], [K])
                self.tt("dve", w2[:, :], w2[:, :], gw[:, :], ALU.mult, [K], [K])
                self.tt("dve", g4[:, :, :], mk1[:, :, :], bc3(w1[:, :], 4), ALU.mult, [K], [K])
                self.tt("dve", g4b[:, :, :], mk2[:, :, :], bc3(w2[:, :], 4), ALU.mult, [K], [K])
                self.tt("dve", g4[:, :, :], g4[:, :, :], g4b[:, :, :], ALU.add, [K], [K])
                self.tt("dve", g32[:, :, :].rearrange("p t (g e) -> p t g e", g=8), ohg[:, :, :].unsqueeze(3).to_broadcast([128, 4, 8, 4]),
                        g4[:, :, :].unsqueeze(2).to_broadcast([128, 4, 8, 4]), ALU.mult, [K], [K])
                pt, ptk = self.ps[2 + b % 2], "ps%d" % (2 + b % 2)
                for tl in range(4):
                    self.tr(pt[0:32, tl * 128:(tl + 1) * 128], g32[:, tl, :], [K], [ptk], f32=True)
                self.cp("act", gT[:, :], pt[0:32, :], [ptk], [gTk])
                c.dma("sp", self.gT_s[:, b * 512:(b + 1) * 512], gT[:, :], reads=[gTk], writes=["gT_s"])
            c.emit()
        SBT = 2048
        NSB = S // SBT
        TPS = SBT // 128
        with ExitStack() as st:
            acc = self.sb("p5_acc", [128, TPS, D], F32, st)
            for sbi in range(NSB):
                t0 = sbi * TPS
                with ExitStack() as st2:
                    xTs = self.sb("p5_xTs", [128, 8, SBT], BF16, st2)
                    Wg = [self.sb("p5_Wg%d" % i, [128, 8, 256], BF16, st2) for i in range(3)]
                    Wu = [self.sb("p5_Wu%d" % i, [128, 8, 256], BF16, st2) for i in range(3)]
                    Wd = [self.sb("p5_Wd%d" % i, [128, 2, D], BF16, st2) for i in range(3)]
                    gbt = [self.sb("p5_gbt%d" % i, [128, 512], F32, st2) for i in range(3)]
                    sg = [self.sb("p5_sg%d" % i, [128, 512], F32, st2) for i in range(2)]
                    tu = [self.sb("p5_tu%d" % i, [128, 512], F32, st2) for i in range(2)]
                    hid = [self.sb("p5_hid%d" % i, [128, 2, 512], BF16, st2) for i in range(2)]
                    for q in range(SBT // 512):
                        c.dma("sp", xTs[:, :, q * 512:(q + 1) * 512], self.xT_s[:, :, sbi * SBT + q * 512: sbi * SBT + (q + 1) * 512],
                              reads=["xT_s"], writes=["p5_xTs%d" % q])

                    def load_w(e):
                        i = e % 3
                        c.dma("pool", Wg[i][:, :, :], self.e_gate[l, e].rearrange("(k p) f -> p k f", p=128), writes=["p5_Wg%d" % i])
                        c.dma("pool", Wu[i][:, :, :], self.e_up[l, e].rearrange("(k p) f -> p k f", p=128), writes=["p5_Wu%d" % i])
                        c.dma("pool", Wd[i][:, :, :], self.e_down[l, e].rearrange("(k p) d -> p k d", p=128), writes=["p5_Wd%d" % i])

                    load_w(0)
                    load_w(1)
                    load_w(2)
                    ocnt = [0]
                    NTB = SBT // 512

                    def gu_stage(n):
                        e, tb = n // NTB, n % NTB
                        wi, gi, hi = e % 3, n % 3, n % 2
                        tok0 = sbi * SBT + tb * 512
                        c.dma("sp", gbt[gi][:, :], self.gT_s[e, tok0:tok0 + 512].partition_broadcast(128), reads=["gT_s"], writes=["p5_gbt%d" % gi])
                        for fc in range(2):
                            pg, pgk = self.ps[fc], "ps%d" % fc
                            pu, puk = self.ps[2 + fc], "ps%d" % (2 + fc)
                            for k in range(8):
                                self.mm(pg[:, :], Wg[wi][:, k, fc * 128:(fc + 1) * 128], xTs[:, k, tb * 512:(tb + 1) * 512], k == 0, k == 7,
                                        ["p5_Wg%d" % wi, "p5_xTs%d" % tb], [pgk])
                            for k in range(8):
                                self.mm(pu[:, :], Wu[wi][:, k, fc * 128:(fc + 1) * 128], xTs[:, k, tb * 512:(tb + 1) * 512], k == 0, k == 7,
                                        ["p5_Wu%d" % wi, "p5_xTs%d" % tb], [puk])
                            self.act(sg[fc][:, :], pg[:, :], AF.Silu, [pgk], ["p5_sg%d" % fc])
                            self.tt("dve", tu[fc][:, :], sg[fc][:, :], pu[:, :], ALU.mult, ["p5_sg%d" % fc, puk], ["p5_tu%d" % fc])
                            self.tt("pool", hid[hi][:, fc, :], tu[fc][:, :], gbt[gi][:, :], ALU.mult, ["p5_tu%d" % fc, "p5_gbt%d" % gi], ["p5_hid%d" % hi])

                    def down_stage(n):
                        e, tb = n // NTB, n % NTB
                        wi, hi = e % 3, n % 2
                        for tl in range(4):
                            tile = tb * 4 + tl
                            for dh in range(2):
                                po, pok = self.ps[4 + ocnt[0] % 4], "ps%d" % (4 + ocnt[0] % 4)
                                ocnt[0] += 1
                                for fc in range(2):
                                    self.mm(po[:, :], hid[hi][:, fc, tl * 128:(tl + 1) * 128], Wd[wi][:, fc, dh * 512:(dh + 1) * 512], fc == 0, fc == 1,
                                            ["p5_hid%d" % hi, "p5_Wd%d" % wi], [pok])
                                a = acc[:, tile, dh * 512:(dh + 1) * 512]
                                ak = "p5_acc%d" % tile
                                if e == 0:
                                    self.cp("dve", a, po[:, :], [pok], [ak])
                                else:
                                    self.tt("dve", a, a, po[:, :], ALU.add, [pok, ak], [ak])

                    NN = 32 * NTB
                    gu_stage(0)
                    for n in range(NN):
                        if n + 1 < NN:
                            gu_stage(n + 1)
                        down_stage(n)
                        if n % NTB == NTB - 1 and n // NTB + 3 < 32:
                            load_w(n // NTB + 3)
                    c.emit()
                with ExitStack() as st2:
                    g_t = self.sb("p5_g", [128, D], F32, st2)
                    b_t = self.sb("p5_b", [128, D], F32, st2)
                    c.dma("sp", g_t[:, :], self.ln_g[2][l, :].partition_broadcast(128), writes=["p5_gb"])
                    c.dma("sp", b_t[:, :], self.ln_b[2][l, :].partition_broadcast(128), writes=["p5_gb"])
                    tmps = [{"st6": self.sb("p5_st6%d" % i, [128, 12], F32, st2), "mv": self.sb("p5_mv%d" % i, [128, 4], F32, st2)} for i in range(2)]
                    xr = [self.sb("p5_xr%d" % i, [128, D], F32, st2) for i in range(2)]
                    ys = [self.sb("p5_y%d" % i, [128, D], F32, st2) for i in range(2)]
                    x3 = [self.sb("p5_x3_%d" % i, [128, D], F32, st2) for i in range(2)]
                    xb = self.sb("p5_xb", [128, D], BF16, st2)
                    x3T = [self.sb("p5_x3T%d" % i, [128, 8, 512], BF16, st2) for i in range(2)]

                    def ln1(tile):
                        t = t0 + tile
                        i = tile % 2
                        c.dma("sp", xr[i][:, :], self.xres_s[t * 128:(t + 1) * 128, :], reads=["xres_s%d" % t], writes=["p5_xr%d" % i])
                        ak = "p5_acc%d" % tile
                        self.ln_part1(ys[i], "p5_y%d" % i, xr[i], "p5_xr%d" % i, acc[:, tile, 0:512], ak, acc[:, tile, 512:1024], ak, tmps[i], "p5_ln%d" % i)

                    def ln2(tile):
                        t = t0 + tile
                        i = tile % 2
                        self.ln_part2(ys[i], "p5_y%d" % i, g_t, b_t, "p5_gb", x3[i][:, :], "p5_x3_%d" % i, tmps[i], "p5_ln%d" % i)
                        dst = self.out if last else self.xres_s
                        c.dma("pool", dst[t * 128:(t + 1) * 128, :], x3[i][:, :], reads=["p5_x3_%d" % i], writes=["xres_s%d" % t])
                        if not last:
                            bq = tile // 4
                            self.to_xT(x3[i][:, :], "p5_x3_%d" % i, xb, "p5_xb", x3T[bq % 2], "p5_x3T%d" % (bq % 2),
                                       slice((tile % 4) * 128, (tile % 4 + 1) * 128), "act")
                            if tile % 4 == 3:
                                tok0 = (t0 + bq * 4) * 128
                                c.dma("pool", self.xT_s[:, :, tok0:tok0 + 512], x3T[bq % 2][:, :, :], reads=["p5_x3T%d" % (bq % 2)], writes=["xT_s"])

                    for tile in range(TPS + 1):
                        if tile < TPS:
                            ln1(tile)
                        if tile >= 1:
                            ln2(tile - 1)
                    c.emit()


_CACHE = {}


def _consts():
    ident = np.eye(128, dtype=np.float32)
    p = np.arange(128)[:, None]
    f = np.arange(128)[None, :]
    invs = []
    for rot in (32, 16, 8):
        invs.append((np.float32(ROPE_THETA) ** (-np.arange(0, rot, 2, dtype=np.float32) / np.float32(rot))).astype(np.float32))
    invf = np.tile(np.concatenate(invs)[None, :], (128, 1)).astype(np.float32)
    blk = np.zeros((16, S), dtype=np.float32)
    for j in range(16):
        blk[j, j * 256:(j + 1) * 256] = BIGV
    bf = ml_dtypes.bfloat16
    return {
        "c_ident_b": ident.astype(bf), "c_ident_f": ident,
        "c_mask_le": (p <= f).astype(np.float32).astype(bf),
        "c_mask_lt": (p < f).astype(np.float32).astype(bf),
        "c_tri": (p > f).astype(np.float32).astype(bf),
        "c_invf": invf, "c_blk": blk.astype(bf),
    }


def make_in_maps(inputs, n_cores=8):
    f = lambda a: np.ascontiguousarray(np.asarray(a))
    shared = {
        "w_in": f(np.asarray(inputs["w_in"])[:, :, COL_PERM]),
        "mla_q_norm": f(inputs["mla_q_norm"]), "w_uq": f(inputs["w_uq"]),
        "mla_kv_norm": f(inputs["mla_kv_norm"]), "w_ukv": f(inputs["w_ukv"]),
        "diff_lambda": f(np.asarray(inputs["diff_lambda"]).reshape(DEPTH, 128)),
        "diff_subln": f(inputs["diff_subln"]), "w_out": f(inputs["w_out"]),
        "ln_mix_g": f(inputs["ln_mix_g"]), "ln_mix_b": f(inputs["ln_mix_b"]),
        "ln_mem_g": f(inputs["ln_mem_g"]), "ln_mem_b": f(inputs["ln_mem_b"]),
        "ln_ffn_g": f(inputs["ln_ffn_g"]), "ln_ffn_b": f(inputs["ln_ffn_b"]),
        "xattn_wq": f(inputs["xattn_wq"]), "xattn_wk": f(inputs["xattn_wk"]),
        "xattn_wv": f(inputs["xattn_wv"]), "xattn_wo": f(inputs["xattn_wo"]),
        "w_router": f(np.concatenate([np.asarray(inputs["router_group_w"]), np.asarray(inputs["router_expert_w"])], axis=2)),
        "b_router": f(np.concatenate([np.asarray(inputs["router_group_b"]), np.asarray(inputs["router_expert_b"])], axis=1)),
        "expert_w_gate": f(inputs["expert_w_gate"]), "expert_w_up": f(inputs["expert_w_up"]),
        "expert_w_down": f(inputs["expert_w_down"]),
    }
    shared.update(_consts())
    maps = []
    x = np.asarray(inputs["x"])
    mem = np.asarray(inputs["mem"])
    pos = np.asarray(inputs["positions"])
    for b in range(n_cores):
        m = dict(shared)
        m["x"] = f(x[b])
        m["mem"] = f(mem[b])
        m["pos"] = f(pos[b].reshape(NT, 128).T.astype(np.int32))
        maps.append(m)
    return maps


def kernel(**inputs):
    if "nc" not in _CACHE:
        _CACHE["nc"] = Builder().build()
    nc = _CACHE["nc"]
    maps = make_in_maps(inputs)
    res = run_bass_kernel_spmd(nc, maps, core_ids=list(range(8)))
    return np.stack([r["out"] for r in res.results], axis=0).astype(np.float32)
```
